# Optimizing a Trainium2 kernel written in Bass

```python
import jax, jax.numpy as jnp
from jax import lax
import numpy as np

D_MODEL = 1024
BATCH = 8
SEQ = 2048
DEPTH = 4
DEC_BATCH = 128
DEC_SEQ = 8
PAST_LEN = 16384
PAGE_SIZE = 128

F32 = jnp.float32
EPS = 1e-6
N_EVEN = (DEPTH + 1) // 2
N_ODD = DEPTH // 2
D_FF = 2816
GLA_H = 4
GLA_DK = D_MODEL // 16
GLA_DV = D_MODEL // 8
GLA_LOWRANK = 16
GLA_TAU = 16.0
GLA_CHUNK = 64
GLA_QK_W = GLA_H * GLA_DK
GLA_V_W = GLA_H * GLA_DV
GMLP_G = 4
GMLP_DG = D_MODEL // 8
GMLP_W = GMLP_G * GMLP_DG
GMLP_CHUNK = 128
AB_SPLITS = (GLA_QK_W, 2 * GLA_QK_W, 2 * GLA_QK_W + GLA_V_W, 2 * GLA_QK_W + 2 * GLA_V_W,
             2 * GLA_QK_W + 2 * GLA_V_W + GLA_LOWRANK, 2 * GLA_QK_W + 2 * GLA_V_W + GLA_LOWRANK + GMLP_W)
AB_IN = 2 * GLA_QK_W + 2 * GLA_V_W + GLA_LOWRANK + 2 * GMLP_W
AB_MIX = GLA_V_W + GMLP_W
ML_INNER = 2 * D_MODEL
ML_H = 4
ML_DH = ML_INNER // ML_H
ML_CONV = 4
ML_BLOCK = 4
ML_NB = ML_INNER // ML_BLOCK
ML_CHUNK = 128

kernel_name = 'hybrid_gla_gmlp_mlstm_macaron_step'


def _rms_norm(x, g):
    xf = x.astype(F32)
    y = xf * lax.rsqrt(jnp.mean(xf * xf, axis=-1, keepdims=True) + EPS)
    return (y * g.astype(F32)).astype(x.dtype)


def _head_layer_norm(x, g):
    xf = x.astype(F32)
    mu = jnp.mean(xf, axis=-1, keepdims=True)
    xc = xf - mu
    var = jnp.mean(xc * xc, axis=-1, keepdims=True)
    return (xc * lax.rsqrt(var + EPS) * g.astype(F32)).astype(x.dtype)


def _modulate(x, shift, scale):
    return x * (1.0 + scale[:, None, :]) + shift[:, None, :]


def _swiglu(x, w_gate, w_up, w_down):
    return (jax.nn.silu(x @ w_gate) * (x @ w_up)) @ w_down


def _to_chunks(a, L):
    B, H, T = a.shape[:3]
    return jnp.moveaxis(a.reshape(B, H, T // L, L, *a.shape[3:]), 2, 0)


def _from_chunks(a):
    a = jnp.moveaxis(a, 0, 2)
    B, H, NC, L = a.shape[:4]
    return a.reshape(B, H, NC * L, *a.shape[4:])


def _gla_chunk(S, inp):
    q, k, v, log_a = inp
    L = q.shape[2]
    causal = jnp.tril(jnp.ones((L, L), dtype=bool))
    b = jnp.cumsum(log_a.astype(F32), axis=2)
    rel = jnp.where(causal[:, :, None], b[:, :, :, None, :] - b[:, :, None, :, :], -jnp.inf)
    scores = jnp.einsum('bhtd,bhsd,bhtsd->bhts', q, k, jnp.exp(rel))
    o = jnp.einsum('bhts,bhsv->bhtv', scores, v) + jnp.einsum('bhtd,bhdv->bhtv', q * jnp.exp(b), S)
    b_last = b[:, :, -1:, :]
    S_new = jnp.exp(b_last)[:, :, 0, :, None] * S + jnp.einsum('bhsd,bhsv->bhdv', k * jnp.exp(b_last - b), v)
    return S_new.astype(S.dtype), o


def _gla_scan(S, q, k, v, log_a):
    T = q.shape[2]
    L = GLA_CHUNK if T % GLA_CHUNK == 0 else T
    S, o = lax.scan(_gla_chunk, S, (_to_chunks(q, L), _to_chunks(k, L), _to_chunks(v, L), _to_chunks(log_a, L)))
    return S, _from_chunks(o)


def _spatial_gate(u, vb, ws, bs):
    B, T = vb.shape[:2]
    L = GMLP_CHUNK if T % GMLP_CHUNK == 0 else T
    w = jnp.tril(ws[:, :L, :L])
    vc = vb.reshape(B, T // L, L, GMLP_G, GMLP_DG)
    z = jnp.einsum('gts,bnsgc->bntgc', w, vc) + bs[:, :L].T[None, None, :, :, None]
    return u * z.reshape(B, T, GMLP_G, GMLP_DG)


def _ab_mixer(h, S, w_in, w_a2, b_a, g_norm, v_norm, ws, bs, w_out):
    B, T, _ = h.shape
    q, k, vg, g, a_lr, u, vb = jnp.split(h @ w_in, AB_SPLITS, axis=-1)
    log_a = jax.nn.log_sigmoid((a_lr @ w_a2 + b_a).astype(F32)) / GLA_TAU

    def heads(a, d):
        return a.reshape(B, T, GLA_H, d).transpose(0, 2, 1, 3)

    S_new, o = _gla_scan(S, heads(q, GLA_DK) * GLA_DK ** -0.5, heads(k, GLA_DK), heads(vg, GLA_DV), heads(log_a, GLA_DK))
    o = _rms_norm(o.transpose(0, 2, 1, 3).astype(h.dtype), g_norm).reshape(B, T, GLA_V_W) * jax.nn.silu(g)
    vb = _rms_norm(vb.reshape(B, T, GMLP_G, GMLP_DG), v_norm)
    yb = _spatial_gate(u.reshape(B, T, GMLP_G, GMLP_DG), vb, ws, bs).reshape(B, T, GMLP_W)
    out = jnp.concatenate([o, yb], axis=-1) @ w_out
    return out, S_new, vb.reshape(B, T, GMLP_W)


def _mlstm_chunk(carry, inp):
    C, n, m = carry
    q, k, v, ig, lf = inp
    L = q.shape[2]
    causal = jnp.tril(jnp.ones((L, L), dtype=bool))
    F = jnp.cumsum(lf.astype(F32), axis=-1)
    igf = ig.astype(F32)
    mf = m.astype(F32)
    log_d = jnp.where(causal, F[..., :, None] - F[..., None, :] + igf[..., None, :], -jnp.inf)
    log_inter = F + mf[..., None]
    m_t = jnp.maximum(log_inter, jnp.max(log_d, axis=-1))
    d = jnp.exp(log_d - m_t[..., None])
    w_inter = jnp.exp(log_inter - m_t)
    s = jnp.einsum('bhtd,bhsd->bhts', q, k) * d
    num = jnp.einsum('bhts,bhsv->bhtv', s, v) + w_inter[..., None] * jnp.einsum('bhvd,bhtd->bhtv', C, q)
    den = jnp.sum(s, axis=-1) + w_inter * jnp.einsum('bhd,bhtd->bht', n, q)
    h = num / jnp.maximum(jnp.abs(den), jnp.exp(-m_t))[..., None]
    m_new = m_t[..., -1]
    w_rows = jnp.exp(F[..., -1:] - F + igf - m_new[..., None])
    decay_prev = jnp.exp(F[..., -1] + mf - m_new)
    C_new = decay_prev[..., None, None] * C + jnp.einsum('bhsv,bhsd->bhvd', v * w_rows[..., None], k)
    n_new = decay_prev[..., None] * n + jnp.einsum('bhs,bhsd->bhd', w_rows, k)
    return (C_new.astype(C.dtype), n_new.astype(n.dtype), m_new.astype(m.dtype)), h


def _mlstm_scan(C, n, m, q, k, v, ig, lf):
    T = q.shape[2]
    L = ML_CHUNK if T % ML_CHUNK == 0 else T
    (C, n, m), h = lax.scan(_mlstm_chunk, (C, n, m),
                            (_to_chunks(q, L), _to_chunks(k, L), _to_chunks(v, L), _to_chunks(ig, L), _to_chunks(lf, L)))
    return C, n, m, _from_chunks(h)


def _blockdiag(x, w):
    B, T, _ = x.shape
    return jnp.einsum('btni,nio->btno', x.reshape(B, T, ML_NB, ML_BLOCK), w).reshape(B, T, ML_INNER)


def _mlstm_mixer(h, C, n, m, conv_buf, w_in, conv_w, conv_b, wq, wk, wv, w_gates, b_gates, g_norm, skip, w_out):
    B, T, _ = h.shape
    xm, z = jnp.split(h @ w_in, 2, axis=-1)
    x_ext = jnp.concatenate([conv_buf.astype(xm.dtype), xm], axis=1)
    new_buf = x_ext[:, T:]
    xc = jax.nn.silu(sum(x_ext[:, j:j + T] * conv_w[j] for j in range(ML_CONV)) + conv_b)
    q = _blockdiag(xc, wq)
    k = _blockdiag(xc, wk)
    v = _blockdiag(xm, wv)
    gates = (jnp.concatenate([q, k, v], axis=-1) @ w_gates + b_gates).astype(F32)
    ig = gates[..., :ML_H].transpose(0, 2, 1)
    lf = jax.nn.log_sigmoid(gates[..., ML_H:]).transpose(0, 2, 1)

    def heads(a):
        return a.reshape(B, T, ML_H, ML_DH).transpose(0, 2, 1, 3)

    C, n, m, hh = _mlstm_scan(C, n, m, heads(q), heads(k) * ML_DH ** -0.5, heads(v), ig, lf)
    hh = hh.transpose(0, 2, 1, 3).astype(h.dtype)
    hn = _head_layer_norm(hh, g_norm.reshape(ML_H, ML_DH)).reshape(B, T, ML_INNER)
    out = (hn + skip * xc) * jax.nn.silu(z)
    return out @ w_out, C, n, m, new_buf


def _trunk(x, c, gla_S, ml_C, ml_n, ml_m, ml_conv, W):
    s_out, v_out, c_out, n_out, m_out, conv_out = [], [], [], [], [], []
    cs = jax.nn.silu(c)
    for l in range(DEPTH):
        mod = cs @ W['ada_w'][l] + W['ada_b'][l]
        sh1, sc1, g1, sh2, sc2, g2, sh3, sc3, g3 = jnp.split(mod, 9, axis=-1)
        h = _modulate(_rms_norm(x, W['ffn_norm'][l, 0]), sh1, sc1)
        x = x + 0.5 * g1[:, None, :] * _swiglu(h, W['ffn_w_gate'][l, 0], W['ffn_w_up'][l, 0], W['ffn_w_down'][l, 0])
        h = _modulate(_rms_norm(x, W['mix_norm'][l]), sh2, sc2)
        if l % 2 == 0:
            e = l // 2
            mix, s_new, v_rows = _ab_mixer(h, gla_S[e], W['ab_w_in'][e], W['gla_w_a2'][e], W['gla_b_a'][e],
                                           W['gla_norm'][e], W['gmlp_norm'][e], W['gmlp_ws'][e], W['gmlp_bs'][e],
                                           W['ab_w_out'][e])
            s_out.append(s_new)
            v_out.append(v_rows)
        else:
            o = l // 2
            mix, C_new, n_new, m_new, buf_new = _mlstm_mixer(
                h, ml_C[o], ml_n[o], ml_m[o], ml_conv[o], W['ml_w_in'][o], W['ml_conv_w'][o], W['ml_conv_b'][o],
                W['ml_wq'][o], W['ml_wk'][o], W['ml_wv'][o], W['ml_w_gates'][o], W['ml_b_gates'][o],
                W['ml_norm'][o], W['ml_skip'][o], W['ml_w_out'][o])
            c_out.append(C_new)
            n_out.append(n_new)
            m_out.append(m_new)
            conv_out.append(buf_new)
        x = x + g2[:, None, :] * mix
        h = _modulate(_rms_norm(x, W['ffn_norm'][l, 1]), sh3, sc3)
        x = x + 0.5 * g3[:, None, :] * _swiglu(h, W['ffn_w_gate'][l, 1], W['ffn_w_up'][l, 1], W['ffn_w_down'][l, 1])
    shf, scf = jnp.split(cs @ W['final_ada_w'] + W['final_ada_b'], 2, axis=-1)
    y = _modulate(_rms_norm(x, W['final_norm']), shf, scf)
    return (y, jnp.stack(s_out), jnp.stack(v_out), jnp.stack(c_out), jnp.stack(n_out), jnp.stack(m_out),
            jnp.stack(conv_out))


def setup_inputs(seed: int = 0) -> dict:
    key = jax.random.key(seed)
    ks = iter(jax.random.split(key, 48))
    D = D_MODEL

    def nrm(shape, scale):
        return jax.random.normal(next(ks), shape, F32) * scale

    def gain(shape):
        return 1.0 + nrm(shape, 0.02)

    inp = {}
    inp['x_prompt'] = nrm((BATCH, SEQ, D), 1.0)
    inp['x_sample'] = nrm((DEC_BATCH, DEC_SEQ, D), 1.0)
    inp['c_prompt'] = nrm((BATCH, D), 1.0)
    inp['c_sample'] = nrm((DEC_BATCH, D), 1.0)
    inp['state_gla_S'] = nrm((N_EVEN, DEC_BATCH, GLA_H, GLA_DK, GLA_DV), GLA_DK ** -0.5)
    inp['state_mlstm_C'] = nrm((N_ODD, DEC_BATCH, ML_H, ML_DH, ML_DH), ML_DH ** -0.5)
    inp['state_mlstm_n'] = nrm((N_ODD, DEC_BATCH, ML_H, ML_DH), ML_DH ** -0.5)
    inp['state_mlstm_m'] = nrm((N_ODD, DEC_BATCH, ML_H), 1.0)
    inp['state_mlstm_conv'] = nrm((N_ODD, DEC_BATCH, ML_CONV - 1, ML_INNER), 1.0)
    inp['ada_w'] = nrm((DEPTH, D, 9 * D), 0.5 * D ** -0.5)
    inp['ada_b'] = nrm((DEPTH, 9 * D), 0.02)
    inp['ffn_norm'] = gain((DEPTH, 2, D))
    inp['ffn_w_gate'] = nrm((DEPTH, 2, D, D_FF), D ** -0.5)
    inp['ffn_w_up'] = nrm((DEPTH, 2, D, D_FF), D ** -0.5)
    inp['ffn_w_down'] = nrm((DEPTH, 2, D_FF, D), D_FF ** -0.5)
    inp['mix_norm'] = gain((DEPTH, D))
    inp['ab_w_in'] = nrm((N_EVEN, D, AB_IN), D ** -0.5)
    inp['gla_w_a2'] = nrm((N_EVEN, GLA_LOWRANK, GLA_QK_W), GLA_LOWRANK ** -0.5)
    inp['gla_b_a'] = nrm((N_EVEN, GLA_QK_W), 0.02)
    inp['gla_norm'] = gain((N_EVEN, GLA_DV))
    inp['gmlp_norm'] = gain((N_EVEN, GMLP_DG))
    inp['gmlp_ws'] = nrm((N_EVEN, GMLP_G, GMLP_CHUNK, GMLP_CHUNK), GMLP_CHUNK ** -0.5)
    inp['gmlp_bs'] = gain((N_EVEN, GMLP_G, GMLP_CHUNK))
    inp['ab_w_out'] = nrm((N_EVEN, AB_MIX, D), AB_MIX ** -0.5)
    inp['ml_w_in'] = nrm((N_ODD, D, 2 * ML_INNER), D ** -0.5)
    inp['ml_conv_w'] = nrm((N_ODD, ML_CONV, ML_INNER), ML_CONV ** -0.5)
    inp['ml_conv_b'] = nrm((N_ODD, ML_INNER), 0.02)
    inp['ml_wq'] = nrm((N_ODD, ML_NB, ML_BLOCK, ML_BLOCK), ML_BLOCK ** -0.5)
    inp['ml_wk'] = nrm((N_ODD, ML_NB, ML_BLOCK, ML_BLOCK), ML_BLOCK ** -0.5)
    inp['ml_wv'] = nrm((N_ODD, ML_NB, ML_BLOCK, ML_BLOCK), ML_BLOCK ** -0.5)
    inp['ml_w_gates'] = nrm((N_ODD, 3 * ML_INNER, 2 * ML_H), (3 * ML_INNER) ** -0.5)
    inp['ml_b_gates'] = jnp.concatenate(
        [nrm((N_ODD, ML_H), 0.1), jnp.linspace(3.0, 6.0, ML_H, dtype=F32)[None, :] + nrm((N_ODD, ML_H), 0.1)], axis=-1)
    inp['ml_norm'] = gain((N_ODD, ML_INNER))
    inp['ml_skip'] = gain((N_ODD, ML_INNER))
    inp['ml_w_out'] = nrm((N_ODD, ML_INNER, D), ML_INNER ** -0.5)
    inp['final_norm'] = gain((D,))
    inp['final_ada_w'] = nrm((D, 2 * D), 0.5 * D ** -0.5)
    inp['final_ada_b'] = nrm((2 * D,), 0.02)
    return inp


def reference(x_prompt, x_sample, c_prompt, c_sample, state_gla_S, state_mlstm_C, state_mlstm_n, state_mlstm_m,
              state_mlstm_conv, ada_w, ada_b, ffn_norm, ffn_w_gate, ffn_w_up, ffn_w_down, mix_norm, ab_w_in,
              gla_w_a2, gla_b_a, gla_norm, gmlp_norm, gmlp_ws, gmlp_bs, ab_w_out, ml_w_in, ml_conv_w, ml_conv_b,
              ml_wq, ml_wk, ml_wv, ml_w_gates, ml_b_gates, ml_norm, ml_skip, ml_w_out, final_norm, final_ada_w,
              final_ada_b):
    W = {'ada_w': ada_w, 'ada_b': ada_b, 'ffn_norm': ffn_norm, 'ffn_w_gate': ffn_w_gate, 'ffn_w_up': ffn_w_up,
         'ffn_w_down': ffn_w_down, 'mix_norm': mix_norm, 'ab_w_in': ab_w_in, 'gla_w_a2': gla_w_a2,
         'gla_b_a': gla_b_a, 'gla_norm': gla_norm, 'gmlp_norm': gmlp_norm, 'gmlp_ws': gmlp_ws,
         'gmlp_bs': gmlp_bs, 'ab_w_out': ab_w_out, 'ml_w_in': ml_w_in, 'ml_conv_w': ml_conv_w,
         'ml_conv_b': ml_conv_b, 'ml_wq': ml_wq, 'ml_wk': ml_wk, 'ml_wv': ml_wv, 'ml_w_gates': ml_w_gates,
         'ml_b_gates': ml_b_gates, 'ml_norm': ml_norm, 'ml_skip': ml_skip, 'ml_w_out': ml_w_out,
         'final_norm': final_norm, 'final_ada_w': final_ada_w, 'final_ada_b': final_ada_b}
    dt = x_prompt.dtype
    B = x_prompt.shape[0]
    z_gla = jnp.zeros((N_EVEN, B, GLA_H, GLA_DK, GLA_DV), dt)
    z_C = jnp.zeros((N_ODD, B, ML_H, ML_DH, ML_DH), dt)
    z_n = jnp.zeros((N_ODD, B, ML_H, ML_DH), dt)
    z_m = jnp.zeros((N_ODD, B, ML_H), dt)
    z_conv = jnp.zeros((N_ODD, B, ML_CONV - 1, ML_INNER), dt)
    y_p, s_p, _, c_p, n_p, m_p, cv_p = _trunk(x_prompt, c_prompt, z_gla, z_C, z_n, z_m, z_conv, W)
    y_s, s_s, v_s, c_s, n_s, m_s, cv_s = _trunk(x_sample, c_sample, state_gla_S, state_mlstm_C, state_mlstm_n,
                                                state_mlstm_m, state_mlstm_conv, W)
    return (y_p, y_s, s_p, s_s, v_s, c_p, c_s, n_p, n_s, m_p, m_s, cv_p, cv_s)
```

```python
import numpy as np
from contextlib import ExitStack
import concourse.bass as bass
import concourse.mybir as mybir
from concourse.bass_utils import run_bass_kernel_spmd

F32 = mybir.dt.float32
BF16 = mybir.dt.bfloat16
AF = mybir.ActivationFunctionType
ALU = mybir.AluOpType

D = 1024; KD = 8; TP = 2048; TS = 128; T = TP + TS; NSEQ = 17
DFF = 2816; NG = 11
NL = 4
EPS = 1e-6
NEG = -1.0e30
MLSTM_MODE = 3
DBG_ONLY_MLSTM = False
FRONT_STEPS = 9
SKIP_INTER = False


class V:
    def __init__(self, buf, ap):
        self.buf = buf; self.ap = ap
    def __getitem__(self, idx):
        return V(self.buf, self.ap[idx])
    def rr(self, pat, **kw):
        return V(self.buf, self.ap.rearrange(pat, **kw))
    def bc(self, shape):
        return V(self.buf, self.ap.broadcast_to(list(shape)))
    def un(self, axis):
        return V(self.buf, self.ap.unsqueeze(axis))


class Buf:
    def __init__(self, t, name):
        self.t = t; self.name = name; self.w = None; self.r = {}
    def __getitem__(self, idx):
        return V(self, self.t[idx])
    @property
    def v(self):
        return V(self, self.t[:])


class Kern:
    def __init__(self, nc, es):
        self.nc = nc; self.es = es
        self.engs = {"pe": nc.tensor, "act": nc.scalar, "dve": nc.vector, "sp": nc.sync, "pool": nc.gpsimd}
        self.sems = {}; self.cnt = {}
        for e in ["pe", "act", "dve"]:
            self.sems[e] = es.enter_context(nc.semaphore("s_" + e)); self.cnt[e] = 0
        self.rings = {}
        for q, n in (("sp", 10), ("pool", 10)):
            keys = []
            for i in range(n):
                k = "%s_d%d" % (q, i)
                self.sems[k] = es.enter_context(nc.semaphore("s_" + k)); self.cnt[k] = 0
                keys.append(k)
            self.rings[q] = [keys, 0]
        self.known = {e: {} for e in self.engs}
        self.psb = []
        for i in range(8):
            t = es.enter_context(nc.psum_tensor("psb%d" % i, [128, 512], F32))
            self.psb.append(Buf(t, "psb%d" % i))
        self.psi = 0
        self.ninstr = 0

    def sb(self, name, shape, dt, es=None):
        self.uid = getattr(self, "uid", 0) + 1
        t = (es or self.es).enter_context(self.nc.sbuf_tensor("sb%d_%s" % (self.uid, name), list(shape), dt))
        return Buf(t, name)

    def ps(self):
        b = self.psb[self.psi % 6]; self.psi += 1
        return b

    def ps_pin(self, i):
        return self.psb[6 + i]

    def _emit(self, eng, fn, reads, writes, dma=False):
        waits = {}
        known = self.known[eng]
        def need(tok):
            if tok is None: return
            k, v = tok
            if k == "pe" and eng == "pe": return
            if known.get(k, 0) >= v: return
            if waits.get(k, 0) < v: waits[k] = v
        rb = []; wb = []
        for x in reads:
            if x is None or isinstance(x, (int, float)): continue
            b = x.buf if isinstance(x, V) else x
            if b is not None and b not in rb: rb.append(b)
        for x in writes:
            b = x.buf if isinstance(x, V) else x
            if b is not None and b not in wb: wb.append(b)
        for b in rb: need(b.w)
        for b in wb:
            need(b.w)
            for k, v in b.r.items(): need((k, v))
        if dma:
            keys, pos = self.rings[eng]
            k = keys[pos % len(keys)]; self.rings[eng][1] = pos + 1
            need((k, self.cnt[k]))
            self.cnt[k] += 16; tok = (k, self.cnt[k]); inc = 16
        else:
            self.cnt[eng] += 1; tok = (eng, self.cnt[eng]); inc = 1
        e = self.engs[eng]
        for k, v in waits.items():
            e.wait_ge(self.sems[k], v); known[k] = v
        ins = fn(e)
        ins.then_inc(self.sems[tok[0]], inc)
        self.ninstr += 1
        for b in rb:
            if b in wb: continue
            if b.r.get(tok[0], 0) < tok[1]: b.r[tok[0]] = tok[1]
        for b in wb:
            b.w = tok; b.r = {}
        return tok

    def barrier(self):
        for eng in self.engs:
            e = self.engs[eng]; known = self.known[eng]
            for k, v in self.cnt.items():
                if v > 0 and known.get(k, 0) < v:
                    e.wait_ge(self.sems[k], v); known[k] = v

    @staticmethod
    def _a(x):
        return x.ap if isinstance(x, V) else x

    def mm(self, out, lhsT, rhs, start=True, stop=True):
        a = self._a
        return self._emit("pe", lambda e: e.matmul(a(out), lhsT=a(lhsT), rhs=a(rhs), start=start, stop=stop), [lhsT, rhs], [out])

    def tr(self, out, in_, ident):
        a = self._a
        return self._emit("pe", lambda e: e.transpose(a(out), a(in_), a(ident)), [in_, ident], [out])

    def act(self, out, in_, func, bias=None, scale=None, accum=None):
        a = self._a
        kw = {}
        if bias is not None: kw["bias"] = a(bias)
        if scale is not None: kw["scale"] = a(scale)
        if accum is not None: kw["accum_out"] = a(accum)
        w = [out] + ([accum] if accum is not None else [])
        return self._emit("act", lambda e: e.activation(out=a(out), in_=a(in_), func=func, **kw), [in_, bias, scale], w)

    def tt(self, out, in0, in1, op, eng="dve"):
        a = self._a
        return self._emit(eng, lambda e: e.tensor_tensor(out=a(out), in0=a(in0), in1=a(in1), op=op), [in0, in1], [out])

    def ts(self, out, in0, s1, op0, s2=None, op1=None, eng="dve"):
        a = self._a
        if op1 is None:
            return self._emit(eng, lambda e: e.tensor_scalar(out=a(out), in0=a(in0), scalar1=a(s1), scalar2=None, op0=op0), [in0, s1], [out])
        return self._emit(eng, lambda e: e.tensor_scalar(out=a(out), in0=a(in0), scalar1=a(s1), scalar2=a(s2), op0=op0, op1=op1), [in0, s1, s2], [out])

    def stt(self, out, in0, scalar, in1, op0, op1, eng="dve"):
        a = self._a
        return self._emit(eng, lambda e: e.scalar_tensor_tensor(out=a(out), in0=a(in0), scalar=a(scalar), in1=a(in1), op0=op0, op1=op1), [in0, scalar, in1], [out])

    def cp(self, out, in_, eng="dve"):
        a = self._a
        return self._emit(eng, lambda e: e.tensor_copy(out=a(out), in_=a(in_)), [in_], [out])

    def memset(self, out, val, eng="dve"):
        a = self._a
        return self._emit(eng, lambda e: e.memset(a(out), val), [], [out])

    def scan(self, out, d0, d1, init, op0, op1):
        a = self._a
        return self._emit("dve", lambda e: e.tensor_tensor_scan(out=a(out), data0=a(d0), data1=a(d1), initial=a(init), op0=op0, op1=op1), [d0, d1, init], [out])

    def recip(self, out, in_):
        a = self._a
        return self._emit("dve", lambda e: e.reciprocal(out=a(out), in_=a(in_)), [in_], [out])

    def dma(self, out, in_, q="sp"):
        a = self._a
        r = [in_] if isinstance(in_, V) else []
        w = [out] if isinstance(out, V) else []
        return self._emit(q, lambda e: e.dma_start(out=a(out), in_=a(in_)), r, w, dma=True)

    def finish(self):
        self.barrier()


IN_SPECS = {}
OUT_SPECS = {}


def _specs():
    I = {}
    I["xT"] = [128, KD, T]
    I["cT"] = [128, KD, NSEQ]
    I["ada_w"] = [NL, 9, 128, KD, 1024]
    I["ada_b"] = [128, NL, 72]
    I["fada_w"] = [2, 128, KD, 1024]
    I["fada_b"] = [128, 16]
    I["ffn_norm"] = [128, NL, 2, KD]
    I["mix_norm"] = [128, NL, KD]
    I["final_norm"] = [128, KD]
    I["ffn_wg"] = [NL, 2, NG, 128, KD, 256]
    I["ffn_wu"] = [NL, 2, NG, 128, KD, 256]
    I["ffn_wd"] = [NL, 2, NG, 128, 2, 1024]
    I["ab_w_in"] = [2, 128, KD, 2576]
    I["ab_w_out"] = [2, 128, KD, 1024]
    I["gla_wa2"] = [2, 17, 256]
    I["gla_norm"] = [2, 128, 1]
    I["gmlp_norm"] = [2, 128, 128]
    I["gmlp_wT_p"] = [2, 128, 4, 128]
    I["gmlp_wT_s"] = [2, 128, 4, 128]
    I["gmlp_bs_p"] = [2, 128, 4, 128]
    I["gmlp_bs_s"] = [2, 128, 4, 128]
    I["consts"] = [128, 6, 128]
    I["gla_S"] = [2, 64, 16, 4, 128]
    I["ml_w_in"] = [2, 128, KD, 4096]
    I["ml_w_out"] = [2, 128, 16, 1024]
    I["ml_conv_w"] = [2, 128, 16, 4]
    I["ml_conv_b"] = [2, 128, 16]
    I["ml_bdq"] = [2, 128, 16, 128]
    I["ml_bdk"] = [2, 128, 16, 128]
    I["ml_bdv"] = [2, 128, 16, 128]
    I["ml_w_gates"] = [2, 128, 48, 8]
    I["ml_b_gates"] = [2, 128, 8]
    I["ml_norm"] = [2, 128, 16]
    I["ml_skip"] = [2, 128, 16]
    I["ml_CT"] = [2, 16, 4, 128, 4, 512]
    I["ml_n"] = [2, 128, 16, 4, 4]
    I["ml_m"] = [2, 4, 16]
    I["ml_conv"] = [2, 128, 16, 16, 3]
    O = {}
    O["yT"] = [128, KD, T]
    O["gla_S_p"] = [2, 64, 4, 128]
    O["gla_S_s"] = [2, 64, 16, 4, 128]
    O["gmlp_v_s"] = [2, 128, 512]
    O["ml_CT_p"] = [2, 4, 128, 4, 512]
    O["ml_CT_s"] = [2, 16, 4, 128, 4, 512]
    O["ml_n_p"] = [2, 128, 4, 4]
    O["ml_n_s"] = [2, 128, 16, 4, 4]
    O["ml_m_p"] = [2, 4, 1]
    O["ml_m_s"] = [2, 4, 16]
    O["ml_conv_p"] = [2, 128, 16, 3]
    O["ml_conv_s"] = [2, 128, 16, 16, 3]
    return I, O


IN_SPECS, OUT_SPECS = _specs()
C_TRI, C_TRI8, C_ID, C_NEG, C_NEG8, C_MISC = range(6)


def build(nlayers=NL, dbg=None):
    nc = bass.Bass("TRN2", target_bir_lowering=False)
    din = {n: nc.dram_tensor(n, s, F32, kind="ExternalInput").ap() for n, s in IN_SPECS.items()}
    dout = {n: nc.dram_tensor(n, s, F32, kind="ExternalOutput").ap() for n, s in OUT_SPECS.items()}
    with ExitStack() as es:
        K = Kern(nc, es)
        _program(nc, K, es, din, dout, nlayers)
        K.finish()
    return nc


def _program(nc, K, es, din, dout, nlayers):
    TILES = [(i * 512, 512) for i in range(4)] + [(TP, TS)]
    xT = [K.sb("xT%d" % i, [128, KD, n], F32) for i, (o, n) in enumerate(TILES)]
    consts = K.sb("consts", [128, 6, 128], F32)
    ones_bf = K.sb("ones_bf", [128, 128], BF16)
    ones_f = K.sb("ones_f", [128, 128], F32)
    negm = K.sb("negm", [128, 2, 128], F32)
    modT = K.sb("modT", [128, 72, NSEQ], F32)
    csT = K.sb("csT", [128, KD, NSEQ], BF16)
    ffn_norm = K.sb("ffn_norm", [128, NL, 2, KD], F32)
    mix_norm = K.sb("mix_norm", [128, NL, KD], F32)
    final_norm = K.sb("final_norm", [128, KD], F32)
    epsb = K.sb("epsb", [128, 1], F32)
    Avec = K.sb("Avec", [128, KD, NSEQ], F32)
    Gvec = K.sb("Gvec", [128, KD, NSEQ], F32)

    K.dma(consts.v, din["consts"])
    for i, (o, n) in enumerate(TILES):
        K.dma(xT[i].v, din["xT"][:, :, o:o + n])
    K.dma(ffn_norm.v, din["ffn_norm"]); K.dma(mix_norm.v, din["mix_norm"]); K.dma(final_norm.v, din["final_norm"])
    K.memset(ones_bf.v, 1.0); K.memset(ones_f.v, 1.0); K.memset(epsb.v, EPS)
    tri = consts[:, C_TRI, :]; tri8 = consts[:, C_TRI8, :]; ident = consts[:, C_ID, :]
    seqmask = consts[:, C_MISC, 0:16]
    K.ts(negm[:, 0, :], tri, -1.0, ALU.add, -NEG, ALU.mult)
    K.ts(negm[:, 1, :], tri8, -1.0, ALU.add, -NEG, ALU.mult)

    cTf = K.sb("cTf", [128, KD, NSEQ], F32)
    K.dma(cTf.v, din["cT"])
    K.act(csT.v, cTf.v, AF.Silu)

    def ada(l):
        with ExitStack() as ph:
            adab = K.sb("adab", [128, 72], F32, ph)
            wbuf = [K.sb("adaw%d" % i, [128, KD, 1024], BF16, ph) for i in range(2)]
            if l < NL:
                K.dma(adab.v, din["ada_b"][:, l, :])
                pieces = [(din["ada_w"][l, v], v * 8) for v in range(9)]
            else:
                K.dma(adab[:, 0:16], din["fada_b"])
                pieces = [(din["fada_w"][v], v * 8) for v in range(2)]
            for i, (src, off) in enumerate(pieces):
                wb = wbuf[i % 2]
                K.dma(wb.v, src, q="pool")
                ps = K.ps()
                for j in range(8):
                    for k in range(KD):
                        K.mm(ps[:, j * NSEQ:(j + 1) * NSEQ], wb[:, k, j * 128:(j + 1) * 128], csT[:, k, :], start=(k == 0), stop=(k == KD - 1))
                K.tt(modT[:, off:off + 8, :], ps[:, 0:8 * NSEQ].rr("p (j s) -> p j s", s=NSEQ),
                     adab[:, off:off + 8].un(2).bc([128, 8, NSEQ]), ALU.add)
            K.barrier()

    def mod(l, v):
        return modT[:, v * 8:v * 8 + 8, :]

    def prep_AG(gamma, sc, gate, gmul):
        K.ts(Avec.v, sc, 1.0, ALU.add)
        K.tt(Avec.v, Avec.v, gamma.un(2).bc([128, KD, NSEQ]), ALU.mult)
        if gate is not None:
            K.ts(Gvec.v, gate, gmul, ALU.mult)

    def seq_affine(out, in_, A, B, ti, k):
        if ti < 4:
            K.act(out, in_, AF.Identity, bias=B[:, k, 0:1], scale=A[:, k, 0:1])
        else:
            o3 = out.rr("p (s t) -> p s t", t=8); i3 = in_.rr("p (s t) -> p s t", t=8)
            K.tt(o3, i3, A[:, k, 1:17].un(2).bc([128, 16, 8]), ALU.mult)
            K.tt(o3, o3, B[:, k, 1:17].un(2).bc([128, 16, 8]), ALU.add)

    def resid_add(ti, d, cols, ps_v, G):
        xv = xT[ti][:, d, cols]
        if ti < 4:
            K.stt(xv, ps_v, G[:, d, 0:1], xv, ALU.mult, ALU.add)
        else:
            tmp = rs_tmp[:, 0:TS]
            K.tt(tmp.rr("p (s t) -> p s t", t=8), ps_v.rr("p (s t) -> p s t", t=8), G[:, d, 1:17].un(2).bc([128, 16, 8]), ALU.mult)
            K.tt(xv, xv, tmp, ALU.add)

    rs_tmp = K.sb("rs_tmp", [128, 512], F32)
    sq = K.sb("nrm_sq", [128, KD, 512], BF16)
    rstd = K.sb("nrm_rstd", [128, 512], F32)
    ntmp = K.sb("nrm_tmp", [128, 512], F32)

    def rms_rstd(ps_ss, n, dim, out):
        K.act(out[:, 0:n], ps_ss[:, 0:n], AF.Ln, bias=epsb.v, scale=1.0 / dim)
        K.act(out[:, 0:n], out[:, 0:n], AF.Exp, scale=-0.5)

    def norm_tile(ti, hdst, B):
        o, n = TILES[ti]
        K.act(sq[:, :, 0:n], xT[ti].v, AF.Square)
        ps = K.ps()
        for k in range(KD):
            K.mm(ps[:, 0:n], ones_bf.v, sq[:, k, 0:n], start=(k == 0), stop=(k == KD - 1))
        rms_rstd(ps, n, D, rstd)
        for k in range(KD):
            K.tt(ntmp[:, 0:n], xT[ti][:, k, :], rstd[:, 0:n], ALU.mult)
            seq_affine(hdst[:, k, :], ntmp[:, 0:n], Avec, B, ti, k)

    def ffn(l, w):
        with ExitStack() as ph:
            hT = [K.sb("ffn_h%d" % i, [128, KD, n], BF16, ph) for i, (o, n) in enumerate(TILES)]
            wg = [K.sb("ffn_wg%d" % i, [128, KD, 256], BF16, ph) for i in range(2)]
            wu = [K.sb("ffn_wu%d" % i, [128, KD, 256], BF16, ph) for i in range(2)]
            wd = [K.sb("ffn_wd%d" % i, [128, 2, 1024], BF16, ph) for i in range(2)]
            hid = [K.sb("ffn_hid%d" % i, [128, 2, 512], BF16, ph) for i in range(2)]
            sg = K.sb("ffn_sg", [128, 512], F32, ph)
            prep_AG(ffn_norm[:, l, w, :], mod(l, 1 if w == 0 else 7), mod(l, 2 if w == 0 else 8), 0.5)
            B = mod(l, 0 if w == 0 else 6)
            for ti in range(5):
                norm_tile(ti, hT[ti].v, B)
            hi = 0
            for g in range(NG):
                b = g % 2
                K.dma(wg[b].v, din["ffn_wg"][l, w, g], q="pool")
                K.dma(wu[b].v, din["ffn_wu"][l, w, g], q="pool")
                K.dma(wd[b].v, din["ffn_wd"][l, w, g], q="pool")
                for ti, (o, n) in enumerate(TILES):
                    hb = hid[hi % 2]; hi += 1
                    for c in range(2):
                        pg = K.ps(); pu = K.ps()
                        for k in range(KD):
                            K.mm(pg[:, 0:n], wg[b][:, k, c * 128:(c + 1) * 128], hT[ti][:, k, :], start=(k == 0), stop=(k == KD - 1))
                        for k in range(KD):
                            K.mm(pu[:, 0:n], wu[b][:, k, c * 128:(c + 1) * 128], hT[ti][:, k, :], start=(k == 0), stop=(k == KD - 1))
                        K.act(sg[:, 0:n], pg[:, 0:n], AF.Silu)
                        K.tt(hb[:, c, 0:n], sg[:, 0:n], pu[:, 0:n], ALU.mult)
                    for d in range(KD):
                        py = K.ps()
                        for c in range(2):
                            K.mm(py[:, 0:n], wd[b][:, c, d * 128:(d + 1) * 128], hb[:, c, 0:n], start=(c == 0), stop=(c == 1))
                        resid_add(ti, d, slice(0, n), py[:, 0:n], Gvec)
            K.barrier()

    def ab_mixer(l):
        e = l // 2
        with ExitStack() as ph:
            w_in = K.sb("ab_win", [128, KD, 2576], BF16, ph)
            w_out = K.sb("ab_wout", [128, KD, 1024], BF16, ph)
            wa2 = K.sb("ab_wa2", [17, 256], F32, ph)
            gnorm = K.sb("ab_gn", [128, 1], F32, ph)
            vnB = K.sb("ab_vn", [128, 128], F32, ph)
            wTp = K.sb("ab_wTp", [128, 4, 128], F32, ph); wTs = K.sb("ab_wTs", [128, 4, 128], F32, ph)
            wTpb = K.sb("ab_wTpb", [128, 4, 128], BF16, ph); wTsb = K.sb("ab_wTsb", [128, 4, 128], BF16, ph)
            bsp = K.sb("ab_bsp", [128, 4, 128], F32, ph); bss = K.sb("ab_bss", [128, 4, 128], F32, ph)
            hT = K.sb("ab_h", [128, KD, 128], BF16, ph)
            qT = K.sb("ab_q", [64, 4, 128], F32, ph); kT = K.sb("ab_k", [64, 4, 128], F32, ph)
            qb = K.sb("ab_qb", [64, 4, 128], BF16, ph); kb = K.sb("ab_kb", [64, 4, 128], BF16, ph)
            kd = K.sb("ab_kd", [64, 4, 128], F32, ph); kdt = K.sb("ab_kdt", [128, 4, 64], BF16, ph)
            a_aug = K.sb("ab_aaug", [17, 128], F32, ph)
            la = K.sb("ab_la", [128, 256], F32, ph)
            eb = K.sb("ab_eb", [64, 4, 128], F32, ph); enb = K.sb("ab_enb", [64, 4, 128], F32, ph)
            vtok = K.sb("ab_vtok", [128, 512], BF16, ph)
            sgT = K.sb("ab_sg", [128, 4, 128], F32, ph)
            uT = K.sb("ab_u", [128, 4, 128], F32, ph)
            vbn = K.sb("ab_vbn", [128, 512], F32, ph); vbnb = K.sb("ab_vbnb", [128, 512], BF16, ph)
            ssq = K.sb("ab_ssq", [128, 4], F32, ph); vjunk = K.sb("ab_vj", [128, 128], F32, ph)
            sT = K.sb("ab_sT", [128, 128], BF16, ph)
            S = K.sb("ab_S", [64, 4, 128], F32, ph); Sb = K.sb("ab_Sb", [64, 4, 128], BF16, ph)
            Ss = K.sb("ab_Ss", [64, 4, 128], F32, ph); Ssn = K.sb("ab_Ssn", [64, 4, 128], F32, ph)
            Ssb = K.sb("ab_Ssb", [64, 4, 128], BF16, ph)
            R = K.sb("ab_R", [128, 4, 128], BF16, ph)
            ebl = K.sb("ab_ebl", [64, 4, 16], F32, ph)
            oT = K.sb("ab_o", [128, 4, 128], F32, ph); osq = K.sb("ab_osq", [128, 4, 128], BF16, ph)
            orstd = K.sb("ab_orstd", [128, 4, 128], F32, ph)
            mix = K.sb("ab_mix", [128, KD, 128], BF16, ph)
            ztmp = K.sb("ab_ztmp", [128, 128], F32, ph)
            K.dma(w_in.v, din["ab_w_in"][e], q="pool"); K.dma(w_out.v, din["ab_w_out"][e], q="pool")
            K.dma(wa2.v, din["gla_wa2"][e]); K.dma(gnorm.v, din["gla_norm"][e]); K.dma(vnB.v, din["gmlp_norm"][e])
            K.dma(wTp.v, din["gmlp_wT_p"][e]); K.dma(wTs.v, din["gmlp_wT_s"][e])
            K.dma(bsp.v, din["gmlp_bs_p"][e]); K.dma(bss.v, din["gmlp_bs_s"][e])
            K.tt(wTpb.v, wTp.v, tri.un(1).bc([128, 4, 128]), ALU.mult)
            K.tt(wTsb.v, wTs.v, tri8.un(1).bc([128, 4, 128]), ALU.mult)
            K.memset(a_aug.v, 1.0)
            K.memset(S.v, 0.0); K.memset(Sb.v, 0.0)
            prep_AG(mix_norm[:, l, :], mod(l, 4), mod(l, 5), 1.0)
            B = mod(l, 3)
            for ci in range(17):
                samp = (ci == 16)
                ti = ci // 4 if not samp else 4
                c0 = (ci % 4) * 128 if not samp else 0
                cols = slice(c0, c0 + 128)
                mask = tri8 if samp else tri
                K.act(sq[:, :, 0:128], xT[ti][:, :, cols], AF.Square)
                ps = K.ps()
                for k in range(KD):
                    K.mm(ps[:, 0:128], ones_bf.v, sq[:, k, 0:128], start=(k == 0), stop=(k == KD - 1))
                rms_rstd(ps, 128, D, rstd)
                for k in range(KD):
                    K.tt(ntmp[:, 0:128], xT[ti][:, k, cols], rstd[:, 0:128], ALU.mult)
                    seq_affine(hT[:, k, :], ntmp[:, 0:128], Avec, B, ti, k)
                def proj_f(col0, m, dst_ps):
                    for k in range(KD):
                        K.mm(dst_ps, w_in[:, k, col0:col0 + m], hT[:, k, :], start=(k == 0), stop=(k == KD - 1))
                ps = K.ps()
                for h in range(4):
                    proj_f(h * 64, 64, ps[0:64, h * 128:(h + 1) * 128])
                K.act(qT.v, ps[0:64, :].rr("p (h t) -> p h t", t=128), AF.Copy, scale=64 ** -0.5)
                ps = K.ps()
                for h in range(4):
                    proj_f(256 + h * 64, 64, ps[0:64, h * 128:(h + 1) * 128])
                K.cp(kT.v, ps[0:64, :].rr("p (h t) -> p h t", t=128))
                ps = K.ps()
                proj_f(1536, 16, ps[0:16, 0:128])
                K.cp(a_aug[0:16, :], ps[0:16, 0:128])
                ps = K.ps()
                for k in range(KD):
                    K.mm(ps[:, 0:512], hT[:, k, :], w_in[:, k, 512:1024], start=(k == 0), stop=(k == KD - 1))
                K.act(vtok.v, ps[:, 0:512], AF.Copy)
                ps = K.ps()
                for j in range(4):
                    proj_f(1024 + j * 128, 128, ps[:, j * 128:(j + 1) * 128])
                K.act(sgT.v, ps[:, :].rr("p (h t) -> p h t", t=128), AF.Silu)
                ps = K.ps()
                for j in range(4):
                    proj_f(1552 + j * 128, 128, ps[:, j * 128:(j + 1) * 128])
                K.cp(uT.v, ps[:, :].rr("p (h t) -> p h t", t=128))
                ps = K.ps()
                for k in range(KD):
                    K.mm(ps[:, 0:512], hT[:, k, :], w_in[:, k, 2576 - 512:2576], start=(k == 0), stop=(k == KD - 1))
                for g in range(4):
                    K.act(vjunk.v, ps[:, g * 128:(g + 1) * 128], AF.Square, accum=ssq[:, g:g + 1])
                K.act(ssq.v, ssq.v, AF.Ln, bias=epsb.v, scale=1.0 / 128)
                K.act(ssq.v, ssq.v, AF.Exp, scale=-0.5)
                for g in range(4):
                    K.stt(vbn[:, g * 128:(g + 1) * 128], ps[:, g * 128:(g + 1) * 128], ssq[:, g:g + 1], vnB.v, ALU.mult, ALU.mult)
                K.cp(vbnb.v, vbn.v)
                if samp:
                    K.dma(dout["gmlp_v_s"][e], vbn.v)
                ps = K.ps()
                K.mm(ps[:, 0:256], a_aug.v, wa2.v)
                K.act(la.v, ps[:, 0:256], AF.Exp, scale=-1.0)
                K.act(la.v, la.v, AF.Ln, bias=1.0)
                ps = K.ps()
                for h in range(4):
                    K.mm(ps[0:64, h * 128:(h + 1) * 128], la[:, h * 64:(h + 1) * 64], mask)
                bp = ps[0:64, :].rr("p (h t) -> p h t", t=128)
                K.act(eb.v, bp, AF.Exp, scale=-1.0 / 16)
                K.act(enb.v, bp, AF.Exp, scale=1.0 / 16)
                K.tt(qb.v, qT.v, eb.v, ALU.mult)
                K.tt(kb.v, kT.v, enb.v, ALU.mult)
                if not samp:
                    K.tt(kd.v, kT.v, enb.v, ALU.mult)
                    for h in range(4):
                        K.ts(kd[:, h, :], kd[:, h, :], eb[:, h, 127:128], ALU.mult)
                else:
                    K.cp(ebl.v, eb.v.rr("p h (s t) -> p h s t", t=8)[:, :, :, 7])
                    K.tt(kd.v, kT.v, enb.v, ALU.mult)
                    K.tt(kd.v.rr("p h (s t) -> p h s t", t=8), kd.v.rr("p h (s t) -> p h s t", t=8),
                         ebl.v.un(3).bc([64, 4, 16, 8]), ALU.mult)
                ps = K.ps()
                for h in range(4):
                    K.tr(ps[:, h * 64:(h + 1) * 64], kd[:, h, :], ident[0:64, 0:64])
                K.cp(kdt.v, ps[:, 0:256].rr("p (h d) -> p h d", d=64))
                for h in range(4):
                    vh = vtok[:, h * 128:(h + 1) * 128]
                    ps = K.ps()
                    K.mm(ps[:, 0:128], kb[:, h, :], qb[:, h, :])
                    K.tt(sT.v, ps[:, 0:128], mask, ALU.mult)
                    po = K.ps_pin(0)
                    if not samp:
                        K.mm(po[:, 0:128], vh, sT.v, start=True, stop=False)
                        K.mm(po[:, 0:128], Sb[:, h, :], qb[:, h, :], start=False, stop=True)
                        K.cp(oT[:, h, :], po[:, 0:128])
                        pk = K.ps()
                        K.mm(pk[0:64, 0:128], kdt[:, h, :], vh)
                        K.stt(S[:, h, :], S[:, h, :], eb[:, h, 127:128], pk[0:64, 0:128], ALU.mult, ALU.add)
                        K.act(Sb[:, h, :], S[:, h, :], AF.Copy)
                    else:
                        K.mm(po[:, 0:128], vh, sT.v, start=True, stop=False)
                        for q4 in range(4):
                            sl = slice(q4 * 4, q4 * 4 + 4)
                            K.dma(Ss.v, din["gla_S"][e][:, sl, h, :])
                            K.cp(Ssb.v, Ss.v)
                            for s4 in range(4):
                                s_ = q4 * 4 + s4
                                K.mm(po[:, s_ * 8:(s_ + 1) * 8], Ssb[:, s4, :], qb[:, h, s_ * 8:(s_ + 1) * 8], start=False, stop=(s_ == 15))
                            K.tt(R.v, vh.un(1).bc([128, 4, 128]), seqmask[:, sl].un(2).bc([128, 4, 128]), ALU.mult)
                            pk = K.ps()
                            K.mm(pk[0:64, 0:512], kdt[:, h, :], R.v.rr("p s v -> p (s v)"))
                            K.tt(Ssn.v, Ss.v, ebl[:, h, sl].un(2).bc([64, 4, 128]), ALU.mult)
                            K.tt(Ssn.v, Ssn.v, pk[0:64, 0:512].rr("p (s v) -> p s v", v=128), ALU.add)
                            K.dma(dout["gla_S_s"][e][:, sl, h, :], Ssn.v)
                        K.cp(oT[:, h, :], po[:, 0:128])
                if ci == 15:
                    K.dma(dout["gla_S_p"][e], S.v)
                K.act(osq.v, oT.v, AF.Square)
                ps = K.ps()
                for h in range(4):
                    K.mm(ps[:, h * 128:(h + 1) * 128], ones_bf.v, osq[:, h, :])
                K.act(orstd.v, ps[:, :].rr("p (h t) -> p h t", t=128), AF.Ln, bias=epsb.v, scale=1.0 / 128)
                K.act(orstd.v, orstd.v, AF.Exp, scale=-0.5)
                K.tt(oT.v, oT.v, orstd.v, ALU.mult)
                K.stt(mix[:, 0:4, :], oT.v, gnorm.v, sgT.v, ALU.mult, ALU.mult)
                wTb = wTsb if samp else wTpb
                bsB = bss if samp else bsp
                for g in range(4):
                    ps = K.ps()
                    K.mm(ps[:, 0:128], vbnb[:, g * 128:(g + 1) * 128], wTb[:, g, :])
                    K.tt(ztmp.v, ps[:, 0:128], bsB[:, g, :], ALU.add)
                    K.tt(mix[:, 4 + g, :], ztmp.v, uT[:, g, :], ALU.mult)
                for half in range(2):
                    ps = K.ps()
                    for dd in range(4):
                        d = half * 4 + dd
                        for k in range(KD):
                            K.mm(ps[:, dd * 128:(dd + 1) * 128], w_out[:, k, d * 128:(d + 1) * 128], mix[:, k, :], start=(k == 0), stop=(k == KD - 1))
                    for dd in range(4):
                        resid_add(ti, half * 4 + dd, cols, ps[:, dd * 128:(dd + 1) * 128], Gvec)
            K.barrier()

    def mlstm(l):
        o_ = l // 2
        with ExitStack() as ph:
            hT = K.sb("ml_h", [128, KD, T], BF16, ph)
            prep_AG(mix_norm[:, l, :], mod(l, 4), mod(l, 5), 1.0)
            B = mod(l, 3)
            for ti in range(5):
                norm_tile(ti, hT[:, :, TILES[ti][0]:TILES[ti][0] + TILES[ti][1]], B)
            cw = K.sb("ml_cw", [128, 16, 4], F32, ph); cb = K.sb("ml_cb", [128, 16], F32, ph)
            gn = K.sb("ml_gn", [128, 16], F32, ph); skp = K.sb("ml_skip", [128, 16], F32, ph)
            bd = {n: K.sb("ml_" + n, [128, 4, 128], BF16, ph) for n in ("bdq", "bdk", "bdv")}
            wgt = K.sb("ml_wgt", [128, 48, 8], BF16, ph); bg = K.sb("ml_bg", [128, 8], F32, ph)
            mst = K.sb("ml_mst", [4, 16], F32, ph)
            gacc = K.sb("ml_gacc", [128, 17, 8], F32, ph)
            colsAll = K.sb("ml_cols", [128, 17, 20], F32, ph)
            decB = K.sb("ml_decB", [128, 4, 32], F32, ph)
            sel = K.sb("ml_sel", [4, 4, 128], F32, ph)
            K.dma(cw.v, din["ml_conv_w"][o_]); K.dma(cb.v, din["ml_conv_b"][o_])
            K.dma(gn.v, din["ml_norm"][o_]); K.dma(skp.v, din["ml_skip"][o_])
            K.dma(wgt.v, din["ml_w_gates"][o_], q="pool"); K.dma(bg.v, din["ml_b_gates"][o_])
            K.dma(mst.v, din["ml_m"][o_])
            K.cp(sel.v, ident[0:4, 0:4].un(2).bc([4, 4, 128]))
            xme = K.sb("ml_xme", [128, 4, 136], F32, ph)
            xmes = K.sb("ml_xmes", [128, 4, 16, 16], F32, ph)
            xmb = K.sb("ml_xmb", [128, 4, 128], BF16, ph)
            acc = K.sb("ml_acc", [128, 4, 128], F32, ph)
            xc = K.sb("ml_xc", [128, 4, 128], BF16, ph)
            qT = K.sb("ml_q", [128, 4, 128], BF16, ph); kT = K.sb("ml_k", [128, 4, 128], BF16, ph); vT = K.sb("ml_v", [128, 4, 128], BF16, ph)
            w_x = K.sb("ml_wx", [128, KD, 512], BF16, ph)
            cvin = K.sb("ml_cvin", [128, 4, 16, 3], F32, ph); cvout = K.sb("ml_cvout", [128, 4, 16, 3], F32, ph)
            cvp = K.sb("ml_cvp", [128, 4, 3], F32, ph)

            def load_head(h):
                K.dma(w_x.v, din["ml_w_in"][o_][:, :, h * 512:(h + 1) * 512], q="pool")
                for n in bd: K.dma(bd[n].v, din["ml_" + n][o_][:, 4 * h:4 * h + 4, :], q="pool")
                K.dma(cvin.v, din["ml_conv"][o_][:, 4 * h:4 * h + 4, :, :])
                K.cp(xmes[:, :, :, 5:8], cvin.v)
                K.memset(xme[:, :, 0:8], 0.0)

            def front(ci, h, need_v_T):
                samp = (ci == 16)
                t0 = ci * 128
                ps = K.ps()
                for j in range(4):
                    for k in range(KD):
                        K.mm(ps[:, j * 128:(j + 1) * 128], w_x[:, k, j * 128:(j + 1) * 128], hT[:, k, t0:t0 + 128], start=(k == 0), stop=(k == KD - 1))
                if FRONT_STEPS == 0:
                    K.cp(xmb.v, ps[:, :].rr("p (f t) -> p f t", t=128)); return
                if not samp:
                    K.act(xme[:, :, 8:136], ps[:, :].rr("p (f t) -> p f t", t=128), AF.Copy)
                else:
                    K.act(xmes[:, :, :, 8:16], ps[:, :].rr("p (f s t) -> p f s t", t=8, s=16), AF.Copy)
                if not samp:
                    K.cp(xmb.v, xme[:, :, 8:136])
                else:
                    K.cp(xmb.v.rr("p f (s t) -> p f s t", t=8), xmes[:, :, :, 8:16])
                if FRONT_STEPS < 2: return
                for j in range(4):
                    fc = 4 * h + j
                    for tap in range(4):
                        if not samp:
                            src = xme[:, j, 5 + tap:5 + tap + 128]; a_ = acc[:, j, :]
                        else:
                            src = xmes[:, j, :, 5 + tap:5 + tap + 8]; a_ = acc[:, j, :].rr("p (s t) -> p s t", t=8)
                        if tap == 0:
                            K.ts(a_, src, cw[:, fc, 0:1], ALU.mult)
                        else:
                            K.stt(a_, src, cw[:, fc, tap:tap + 1], a_, ALU.mult, ALU.add)
                    if FRONT_STEPS >= 3:
                        K.act(xc[:, j, :], acc[:, j, :], AF.Silu, bias=cb[:, fc:fc + 1])
                if FRONT_STEPS < 4: return
                for nm, src, dst in (("bdq", xc, qT), ("bdk", xc, kT), ("bdv", xmb, vT)):
                    if nm == "bdv" and not need_v_T: continue
                    ps = K.ps()
                    for j in range(4):
                        K.mm(ps[:, j * 128:(j + 1) * 128], bd[nm][:, j, :], src[:, j, :])
                    K.act(dst.v, ps[:, :].rr("p (f t) -> p f t", t=128), AF.Copy)
                if FRONT_STEPS < 5: return
                if not samp:
                    if ci == 15 and need_v_T:
                        K.cp(cvp.v, xme[:, :, 133:136])
                        K.dma(dout["ml_conv_p"][o_][:, 4 * h:4 * h + 4, :], cvp.v)
                    K.cp(xme[:, :, 5:8], xme[:, :, 133:136])
                elif need_v_T:
                    K.cp(cvout.v, xmes[:, :, :, 13:16])
                    K.dma(dout["ml_conv_s"][o_][:, 4 * h:4 * h + 4, :, :], cvout.v)

            if MLSTM_MODE == 10:
                K.barrier(); return
            for h in range(4):
                load_head(h)
                if MLSTM_MODE == 12: continue
                for ci in (range(17) if MLSTM_MODE != 13 else range(16)):
                    front(ci, h, True)
                    if MLSTM_MODE in (11, 13): continue
                    ps = K.ps()
                    i = 0
                    for part, src in enumerate((qT, kT, vT)):
                        for j in range(4):
                            K.mm(ps[:, 0:8], src[:, j, :], wgt[:, part * 16 + 4 * h + j, :], start=(i == 0), stop=(i == 11)); i += 1
                    K.tt(gacc[:, ci, :], ps[:, 0:8], bg.v if h == 0 else gacc[:, ci, :], ALU.add)
            if MLSTM_MODE in (1, 11, 12, 13):
                K.barrier(); return
            K.act(gacc[:, :, 4:8], gacc[:, :, 4:8], AF.Exp, scale=-1.0)
            K.act(gacc[:, :, 4:8], gacc[:, :, 4:8], AF.Ln, bias=1.0)
            with ExitStack() as p1:
                R32 = {n: K.sb("ml_r_" + n, [32, T], F32, p1) for n in ("ig", "lf", "m", "F", "wr")}
                for n in R32: K.memset(R32[n].v, 0.0)
                R_ = {n: R32[n][0:4, :] for n in R32}
                mprev = K.sb("ml_mprev", [4, 32], F32, p1); rend = K.sb("ml_rend", [4, 32], F32, p1); fst = K.sb("ml_fst", [4, 16], F32, p1)
                dec = K.sb("ml_dec", [4, 32], F32, p1)
                for ci in range(17):
                    ps = K.ps()
                    K.tr(ps[0:4, 0:128], gacc[:, ci, 0:4], ident)
                    K.tr(ps[0:4, 128:256], gacc[:, ci, 4:8], ident)
                    K.cp(R_["ig"][:, ci * 128:(ci + 1) * 128], ps[0:4, 0:128])
                    K.ts(R_["lf"][:, ci * 128:(ci + 1) * 128], ps[0:4, 128:256], -1.0, ALU.mult)
                K.scan(R_["m"][:, 0:TP], R_["lf"][:, 0:TP], R_["ig"][:, 0:TP], 0.0, ALU.add, ALU.max)
                K.scan(R_["F"][:, 0:TP], R_["lf"][:, 0:TP], R_["lf"][:, 0:TP], 0.0, ALU.add, ALU.min)
                for s in range(16):
                    sl = slice(TP + s * 8, TP + s * 8 + 8)
                    K.scan(R_["m"][:, sl], R_["lf"][:, sl], R_["ig"][:, sl], mst[:, s:s + 1], ALU.add, ALU.max)
                    K.scan(R_["F"][:, sl], R_["lf"][:, sl], R_["lf"][:, sl], 0.0, ALU.add, ALU.min)
                seg = lambda n: (R_[n][:, 0:TP].rr("p (c t) -> p c t", t=128), R_[n][:, TP:T].rr("p (c t) -> p c t", t=8))
                Fp = seg("F")[0]; mp = seg("m")[0]
                K.memset(fst[:, 0:1], 0.0); K.memset(mprev[:, 0:1], 0.0)
                K.cp(fst[:, 1:16], Fp[:, 0:15, 127]); K.cp(mprev[:, 1:16], mp[:, 0:15, 127])
                K.cp(mprev[:, 16:32], mst.v)
                K.tt(Fp, Fp, fst.v.un(2).bc([4, 16, 128]), ALU.subtract)
                K.dma(dout["ml_m_p"][o_], R_["m"][:, TP - 1:TP])
                msout = K.sb("ml_msout", [4, 16], F32, p1)
                K.cp(msout.v, seg("m")[1][:, :, 7])
                K.dma(dout["ml_m_s"][o_], msout.v)
                K.tt(R_["ig"], R_["ig"], R_["F"], ALU.subtract)
                K.tt(R_["F"], R_["F"], R_["m"], ALU.subtract)
                K.act(R_["lf"], R_["m"], AF.Exp, scale=-1.0)
                R_["wi"] = R_["m"]; R32["wi"] = R32["m"]
                K.cp(rend[:, 0:16], seg("F")[0][:, :, 127]); K.cp(rend[:, 16:32], seg("F")[1][:, :, 7])
                for pi, (off, L) in enumerate(((0, 128), (16, 8))):
                    K.tt(seg("wi")[pi], seg("F")[pi], mprev[:, off:off + 16].un(2).bc([4, 16, L]), ALU.add)
                    K.tt(seg("wr")[pi], seg("ig")[pi], rend[:, off:off + 16].un(2).bc([4, 16, L]), ALU.add)
                K.act(R_["wi"], R_["wi"], AF.Exp)
                K.act(R_["wr"], R_["wr"], AF.Exp)
                K.ts(R_["wr"], R_["wr"], 512 ** -0.5, ALU.mult)
                K.cp(dec[:, 0:16], seg("wi")[0][:, :, 127]); K.cp(dec[:, 16:32], seg("wi")[1][:, :, 7])
                for ci in range(17):
                    ps = K.ps()
                    for i, n in enumerate(("ig", "wr", "F", "wi", "lf")):
                        K.tr(ps[:, i * 32:(i + 1) * 32], R32[n][:, ci * 128:(ci + 1) * 128], ident[0:32, 0:32])
                    K.cp(colsAll[:, ci, :].rr("p (i f) -> p i f", f=4), ps[:, 0:160].rr("p (i f) -> p i f", f=32)[:, :, 0:4])
                K.barrier()
            if MLSTM_MODE == 2:
                K.barrier(); return
            with ExitStack() as p2:
                w_z = K.sb("ml_wz", [128, KD, 512], BF16, p2)
                w_o = K.sb("ml_wo", [128, 4, 1024], BF16, p2)
                CT = K.sb("ml_CT", [128, 4, 512], F32, p2); CTb = K.sb("ml_CTb", [128, 4, 512], BF16, p2)
                nS = K.sb("ml_nS", [128, 16, 4, 4], F32, p2); nSn = K.sb("ml_nSn", [128, 16, 4, 4], F32, p2)
                nP = K.sb("ml_nP", [128, 4, 4], F32, p2)
                nB = K.sb("ml_nB", [128, 4, 128], BF16, p2)
                ktok = K.sb("ml_ktok", [128, 512], BF16, p2); vtok = K.sb("ml_vtok", [128, 512], BF16, p2)
                vw = K.sb("ml_vw", [128, 512], BF16, p2); km = K.sb("ml_km", [128, 512], BF16, p2)
                wrb = K.sb("ml_wrb", [128, 4], BF16, p2)
                diag = K.sb("ml_diag", [128, 128], F32, p2)
                bcs = K.sb("ml_bcs", [128, 3, 128], F32, p2)
                arg = K.sb("ml_arg", [128, 128], F32, p2); sT = K.sb("ml_sT", [128, 128], BF16, p2)
                qw = K.sb("ml_qw", [128, 4, 128], BF16, p2)
                hden = K.sb("ml_hden", [128, 128], F32, p2)
                hh = K.sb("ml_hh", [128, 4, 128], F32, p2); hsq = K.sb("ml_hsq", [128, 4, 128], F32, p2)
                mean = K.sb("ml_mean", [128, 128], F32, p2); var = K.sb("ml_var", [128, 128], F32, p2)
                sz = K.sb("ml_sz", [128, 4, 128], BF16, p2); pre = K.sb("ml_pre", [128, 4, 128], BF16, p2)
                skx = K.sb("ml_skx", [128, 128], F32, p2)
                interN = hsq; interD = mean
                K.dma(nS.v, din["ml_n"][o_])
                K.memset(nP.v, 0.0)
                for h in range(4):
                    load_head(h)
                    K.dma(w_z.v, din["ml_w_in"][o_][:, :, 2048 + h * 512:2048 + (h + 1) * 512], q="pool")
                    K.dma(w_o.v, din["ml_w_out"][o_][:, 4 * h:4 * h + 4, :], q="pool")
                    K.memset(CT.v, 0.0); K.memset(CTb.v, 0.0); K.memset(nB.v, 0.0)
                    for ci in range(17):
                        samp = (ci == 16)
                        ti = ci // 4 if not samp else 4
                        c0 = (ci % 4) * 128 if not samp else 0
                        cols = slice(c0, c0 + 128)
                        tsl = slice(ci * 128, ci * 128 + 128)
                        front(ci, h, False)
                        for nm, src, dst in (("bdk", xc, ktok), ("bdv", xmb, vtok)):
                            ps = K.ps()
                            for j in range(4):
                                K.mm(ps[:, j * 128:(j + 1) * 128], src[:, j, :], bd[nm][:, j, :])
                            K.act(dst.v, ps[:, :], AF.Copy)
                        gcol = colsAll[:, ci, h:h + 1]; wrcol = colsAll[:, ci, 4 + h:5 + h]
                        K.cp(wrb.v, colsAll[:, ci, 4:8])
                        ps = K.ps()
                        for i in range(3):
                            K.ts(diag.v, ident, colsAll[:, ci, 8 + 4 * i + h:9 + 4 * i + h], ALU.mult)
                            K.mm(ps[:, i * 128:(i + 1) * 128], ones_f.v, diag.v)
                        K.act(bcs.v, ps[:, 0:384].rr("p (i t) -> p i t", t=128), AF.Copy)
                        K.stt(arg.v, bcs[:, 0, :], gcol, negm[:, 1 if samp else 0, :], ALU.add, ALU.add)
                        K.act(arg.v, arg.v, AF.Exp)
                        ps = K.ps()
                        for j in range(4):
                            K.mm(ps[:, 0:128], kT[:, j, :], qT[:, j, :], start=(j == 0), stop=(j == 3))
                        K.stt(sT.v, ps[:, 0:128], 512 ** -0.5, arg.v, ALU.mult, ALU.mult)
                        K.tt(qw.v, qT.v, bcs[:, 1, :].un(1).bc([128, 4, 128]), ALU.mult)
                        pn = K.ps_pin(0); pd = K.ps_pin(1)
                        if not samp:
                            for vc in range(4):
                                K.mm(pn[:, vc * 128:(vc + 1) * 128], vtok[:, vc * 128:(vc + 1) * 128], sT.v, start=True, stop=False)
                                for dc in range(4):
                                    K.mm(pn[:, vc * 128:(vc + 1) * 128], CTb[:, dc, vc * 128:(vc + 1) * 128], qw[:, dc, :], start=False, stop=(dc == 3))
                            K.mm(pd[:, 0:128], ones_bf.v, sT.v, start=True, stop=False)
                            for dc in range(4):
                                K.mm(pd[:, 0:128], nB[:, dc, :], qw[:, dc, :], start=False, stop=(dc == 3))
                        else:
                            for vc in range(4):
                                K.mm(pn[:, vc * 128:(vc + 1) * 128], vtok[:, vc * 128:(vc + 1) * 128], sT.v, start=True, stop=True)
                            K.mm(pd[:, 0:128], ones_bf.v, sT.v, start=True, stop=True)
                            K.ts(vw.v, vtok.v, wrcol, ALU.mult)
                            for s in range(16):
                                K.dma(CT.v, din["ml_CT"][o_, s, h])
                                K.act(CTb.v, CT.v, AF.Copy)
                                K.cp(nB.v, nS[:, s, h, :].un(2).bc([128, 4, 128]))
                                ssl = slice(s * 8, s * 8 + 8)
                                pi = K.ps()
                                for vc in range(4):
                                    for dc in range(4):
                                        K.mm(pi[:, vc * 8:vc * 8 + 8], CTb[:, dc, vc * 128:(vc + 1) * 128], qw[:, dc, ssl], start=(dc == 0), stop=(dc == 3))
                                for dc in range(4):
                                    K.mm(pi[:, 32:40], nB[:, dc, :], qw[:, dc, ssl], start=(dc == 0), stop=(dc == 3))
                                K.cp(interN[:, :, ssl], pi[:, 0:32].rr("p (v t) -> p v t", t=8))
                                K.cp(interD[:, ssl], pi[:, 32:40])
                                K.ts(km.v, ktok.v, seqmask[:, s:s + 1], ALU.mult)
                                pnn = K.ps()
                                for dc in range(4):
                                    pk = K.ps()
                                    K.mm(pk[:, 0:512], km[:, dc * 128:(dc + 1) * 128], vw.v)
                                    K.stt(CT[:, dc, :], CT[:, dc, :], bcs[:, 1, s * 8 + 7:s * 8 + 8], pk[:, 0:512], ALU.mult, ALU.add)
                                    K.mm(pnn[:, dc * 4:(dc + 1) * 4], km[:, dc * 128:(dc + 1) * 128], wrb.v)
                                K.stt(nSn[:, s, h, :], nS[:, s, h, :], bcs[:, 1, s * 8 + 7:s * 8 + 8], pnn[:, 0:16].rr("p (d f) -> p d f", f=4)[:, :, h], ALU.mult, ALU.add)
                                K.dma(dout["ml_CT_s"][o_, s, h], CT.v)
                        if samp:
                            K.tt(interD.v, interD.v, pd[:, 0:128], ALU.add)
                            K.tt(interN.v, interN.v, pn[:, :].rr("p (v t) -> p v t", t=128), ALU.add)
                            den_v = interD.v; num_v = interN.v
                        else:
                            den_v = pd[:, 0:128]; num_v = pn[:, :].rr("p (v t) -> p v t", t=128)
                        K.ts(hden.v, den_v, -1.0, ALU.mult)
                        K.tt(hden.v, hden.v, den_v, ALU.max)
                        K.tt(hden.v, hden.v, bcs[:, 2, :], ALU.max)
                        K.recip(hden.v, hden.v)
                        K.tt(hh.v, num_v, hden.v.un(1).bc([128, 4, 128]), ALU.mult)
                        K.act(hsq.v, hh.v, AF.Square)
                        ps = K.ps()
                        for vc in range(4):
                            K.mm(ps[:, 0:128], ones_f.v, hh[:, vc, :], start=(vc == 0), stop=(vc == 3))
                        for vc in range(4):
                            K.mm(ps[:, 128:256], ones_f.v, hsq[:, vc, :], start=(vc == 0), stop=(vc == 3))
                        K.act(mean.v, ps[:, 0:128], AF.Copy, scale=1.0 / 512)
                        K.tt(var.v, mean.v, mean.v, ALU.mult)
                        K.stt(var.v, ps[:, 128:256], 1.0 / 512, var.v, ALU.mult, ALU.subtract)
                        K.act(var.v, var.v, AF.Ln, bias=epsb.v)
                        K.act(var.v, var.v, AF.Exp, scale=-0.5)
                        K.tt(hh.v, hh.v, mean.v.un(1).bc([128, 4, 128]), ALU.subtract)
                        K.tt(hh.v, hh.v, var.v.un(1).bc([128, 4, 128]), ALU.mult)
                        ps = K.ps()
                        for j in range(4):
                            for k in range(KD):
                                K.mm(ps[:, j * 128:(j + 1) * 128], w_z[:, k, j * 128:(j + 1) * 128], hT[:, k, tsl], start=(k == 0), stop=(k == KD - 1))
                        K.act(sz.v, ps[:, :].rr("p (f t) -> p f t", t=128), AF.Silu)
                        for j in range(4):
                            fc = 4 * h + j
                            K.ts(skx.v, xc[:, j, :], skp[:, fc:fc + 1], ALU.mult)
                            K.stt(hh[:, j, :], hh[:, j, :], gn[:, fc:fc + 1], skx.v, ALU.mult, ALU.add)
                        K.tt(pre.v, hh.v, sz.v, ALU.mult)
                        for half in range(2):
                            ps = K.ps()
                            for dd in range(4):
                                d = half * 4 + dd
                                for j in range(4):
                                    K.mm(ps[:, dd * 128:(dd + 1) * 128], w_o[:, j, d * 128:(d + 1) * 128], pre[:, j, :], start=(j == 0), stop=(j == 3))
                            for dd in range(4):
                                resid_add(ti, half * 4 + dd, cols, ps[:, dd * 128:(dd + 1) * 128], Gvec)
                        if not samp:
                            K.ts(vw.v, vtok.v, wrcol, ALU.mult)
                            pnn = K.ps()
                            for dc in range(4):
                                pk = K.ps()
                                K.mm(pk[:, 0:512], ktok[:, dc * 128:(dc + 1) * 128], vw.v)
                                K.stt(CT[:, dc, :], CT[:, dc, :], bcs[:, 1, 127:128], pk[:, 0:512], ALU.mult, ALU.add)
                                K.mm(pnn[:, dc * 4:(dc + 1) * 4], ktok[:, dc * 128:(dc + 1) * 128], wrb.v)
                            K.stt(nP[:, h, :], nP[:, h, :], bcs[:, 1, 127:128], pnn[:, 0:16].rr("p (d f) -> p d f", f=4)[:, :, h], ALU.mult, ALU.add)
                            K.act(CTb.v, CT.v, AF.Copy)
                            K.cp(nB.v, nP[:, h, :].un(2).bc([128, 4, 128]))
                            if ci == 15:
                                K.dma(dout["ml_CT_p"][o_, h], CT.v)
                K.dma(dout["ml_n_p"][o_], nP.v)
                K.dma(dout["ml_n_s"][o_], nSn.v)
                K.barrier()
        K.barrier()

    for l in range(nlayers):
        if DBG_ONLY_MLSTM:
            if l == 1: mlstm(l)
            continue
        ada(l)
        ffn(l, 0)
        if l % 2 == 0:
            ab_mixer(l)
        elif MLSTM_MODE:
            mlstm(l)
        ffn(l, 1)
    with ExitStack() as ph:
        ada(NL)
        yT = [K.sb("yT%d" % i, [128, KD, n], F32, ph) for i, (o, n) in enumerate(TILES)]
        K.ts(Avec.v, mod(NL, 1), 1.0, ALU.add)
        K.tt(Avec.v, Avec.v, final_norm.v.un(2).bc([128, KD, NSEQ]), ALU.mult)
        B = mod(NL, 0)
        for ti, (o, n) in enumerate(TILES):
            norm_tile(ti, yT[ti].v, B)
            K.dma(dout["yT"][:, :, o:o + n], yT[ti].v)
        K.barrier()


def _consts():
    c = np.zeros((128, 6, 128), np.float32)
    s = np.arange(128)[:, None]; t = np.arange(128)[None, :]
    c[:, C_TRI, :] = (s <= t)
    c[:, C_TRI8, :] = (s <= t) & (s // 8 == t // 8)
    c[:, C_ID, :] = (s == t)
    c[:, C_MISC, 0:16] = (np.arange(128)[:, None] // 8 == np.arange(16)[None, :])
    return c


def _bd(w):
    out = np.zeros((16, 128, 128), np.float32)
    wr = w.reshape(16, 32, 4, 4)
    for n in range(32):
        out[:, n * 4:(n + 1) * 4, n * 4:(n + 1) * 4] = wr[:, n]
    return np.ascontiguousarray(out.transpose(1, 0, 2))


def _kT(w, kc):
    return np.ascontiguousarray(w.reshape(kc, 128, -1).transpose(1, 0, 2))


def prep_shared(inp):
    f = lambda a: np.ascontiguousarray(np.asarray(a, dtype=np.float32))
    S = {}
    S["ada_w"] = f(inp["ada_w"].reshape(NL, KD, 128, 9, 1024).transpose(0, 3, 2, 1, 4))
    S["ada_b"] = f(inp["ada_b"].reshape(NL, 72, 128).transpose(2, 0, 1))
    S["fada_w"] = f(inp["final_ada_w"].reshape(KD, 128, 2, 1024).transpose(2, 1, 0, 3))
    S["fada_b"] = f(inp["final_ada_b"].reshape(16, 128).T)
    S["ffn_norm"] = f(inp["ffn_norm"].reshape(NL, 2, KD, 128).transpose(3, 0, 1, 2))
    S["mix_norm"] = f(inp["mix_norm"].reshape(NL, KD, 128).transpose(2, 0, 1))
    S["final_norm"] = f(inp["final_norm"].reshape(KD, 128).T)
    S["ffn_wg"] = f(inp["ffn_w_gate"].reshape(NL, 2, KD, 128, NG, 256).transpose(0, 1, 4, 3, 2, 5))
    S["ffn_wu"] = f(inp["ffn_w_up"].reshape(NL, 2, KD, 128, NG, 256).transpose(0, 1, 4, 3, 2, 5))
    S["ffn_wd"] = f(inp["ffn_w_down"].reshape(NL, 2, NG, 2, 128, 1024).transpose(0, 1, 2, 4, 3, 5))
    S["ab_w_in"] = f(inp["ab_w_in"].reshape(2, KD, 128, 2576).transpose(0, 2, 1, 3))
    S["ab_w_out"] = f(inp["ab_w_out"].reshape(2, KD, 128, 1024).transpose(0, 2, 1, 3))
    S["gla_wa2"] = f(np.concatenate([inp["gla_w_a2"], inp["gla_b_a"][:, None, :]], axis=1))
    S["gla_norm"] = f(inp["gla_norm"].reshape(2, 128, 1))
    S["gmlp_norm"] = f(np.broadcast_to(inp["gmlp_norm"][:, None, :], (2, 128, 128)))
    ws = np.asarray(inp["gmlp_ws"])
    S["gmlp_wT_p"] = f(ws.transpose(0, 3, 1, 2))
    w8 = ws[:, :, :8, :8]
    S["gmlp_wT_s"] = f(np.tile(w8.transpose(0, 3, 1, 2), (1, 16, 1, 16)))
    bs = np.asarray(inp["gmlp_bs"])
    S["gmlp_bs_p"] = f(np.broadcast_to(bs[:, None, :, :], (2, 128, 4, 128)))
    S["gmlp_bs_s"] = f(np.broadcast_to(np.tile(bs[:, :, :8], (1, 1, 16))[:, None, :, :], (2, 128, 4, 128)))
    S["consts"] = _consts()
    S["ml_w_in"] = f(inp["ml_w_in"].reshape(2, KD, 128, 4096).transpose(0, 2, 1, 3))
    S["ml_w_out"] = f(inp["ml_w_out"].reshape(2, 16, 128, 1024).transpose(0, 2, 1, 3))
    S["ml_conv_w"] = f(inp["ml_conv_w"].reshape(2, 4, 16, 128).transpose(0, 3, 2, 1))
    S["ml_conv_b"] = f(inp["ml_conv_b"].reshape(2, 16, 128).transpose(0, 2, 1))
    for n, k in (("ml_bdq", "ml_wq"), ("ml_bdk", "ml_wk"), ("ml_bdv", "ml_wv")):
        S[n] = np.stack([_bd(np.asarray(inp[k][o])) for o in range(2)])
    S["ml_w_gates"] = f(inp["ml_w_gates"].reshape(2, 48, 128, 8).transpose(0, 2, 1, 3))
    S["ml_b_gates"] = f(np.broadcast_to(inp["ml_b_gates"][:, None, :], (2, 128, 8)))
    S["ml_norm"] = f(inp["ml_norm"].reshape(2, 16, 128).transpose(0, 2, 1))
    S["ml_skip"] = f(inp["ml_skip"].reshape(2, 16, 128).transpose(0, 2, 1))
    return S


def prep_core(inp, i):
    f = lambda a: np.ascontiguousarray(np.asarray(a, dtype=np.float32))
    P = {}
    sl = slice(16 * i, 16 * i + 16)
    x = np.concatenate([inp["x_prompt"][i], inp["x_sample"][sl].reshape(TS, D)], axis=0)
    P["xT"] = f(x.T.reshape(KD, 128, T).transpose(1, 0, 2))
    c = np.concatenate([inp["c_prompt"][i:i + 1], inp["c_sample"][sl]], axis=0)
    P["cT"] = f(c.T.reshape(KD, 128, NSEQ).transpose(1, 0, 2))
    P["gla_S"] = f(inp["state_gla_S"][:, sl].transpose(0, 3, 1, 2, 4))
    C = inp["state_mlstm_C"][:, sl]
    P["ml_CT"] = f(C.transpose(0, 1, 2, 4, 3).reshape(2, 16, 4, 4, 128, 512).transpose(0, 1, 2, 4, 3, 5))
    P["ml_n"] = f(inp["state_mlstm_n"][:, sl].reshape(2, 16, 4, 4, 128).transpose(0, 4, 1, 2, 3))
    P["ml_m"] = f(inp["state_mlstm_m"][:, sl].transpose(0, 2, 1))
    P["ml_conv"] = f(inp["state_mlstm_conv"][:, sl].reshape(2, 16, 3, 16, 128).transpose(0, 4, 3, 1, 2))
    return P


def post_core(r):
    o = {}
    y = r["yT"].transpose(2, 1, 0).reshape(T, D)
    o["y_p"] = y[0:TP]; o["y_s"] = y[TP:].reshape(16, 8, D)
    o["s_p"] = r["gla_S_p"].transpose(0, 2, 1, 3)
    o["s_s"] = r["gla_S_s"].transpose(0, 2, 3, 1, 4)
    o["v_s"] = r["gmlp_v_s"].reshape(2, 16, 8, 512)
    o["c_p"] = r["ml_CT_p"].transpose(0, 1, 3, 2, 4).reshape(2, 4, 512, 512).transpose(0, 1, 3, 2)
    o["c_s"] = r["ml_CT_s"].transpose(0, 1, 2, 4, 3, 5).reshape(2, 16, 4, 512, 512).transpose(0, 1, 2, 4, 3)
    o["n_p"] = r["ml_n_p"].transpose(0, 2, 3, 1).reshape(2, 4, 512)
    o["n_s"] = r["ml_n_s"].transpose(0, 2, 3, 4, 1).reshape(2, 16, 4, 512)
    o["m_p"] = r["ml_m_p"].reshape(2, 4)
    o["m_s"] = r["ml_m_s"].transpose(0, 2, 1)
    o["cv_p"] = r["ml_conv_p"].transpose(0, 3, 2, 1).reshape(2, 3, 2048)
    o["cv_s"] = r["ml_conv_s"].transpose(0, 3, 4, 2, 1).reshape(2, 16, 3, 2048)
    return o


_NC_CACHE = {}


def kernel(**inputs):
    inp = {k: np.asarray(v) for k, v in inputs.items()}
    if "nc" not in _NC_CACHE:
        _NC_CACHE["nc"] = build()
    nc = _NC_CACHE["nc"]
    S = prep_shared(inp)
    in_maps = []
    for i in range(8):
        m = dict(S); m.update(prep_core(inp, i)); in_maps.append(m)
    res = run_bass_kernel_spmd(nc, in_maps, core_ids=list(range(8)))
    po = [post_core(r) for r in res.results]
    cat0 = lambda k: np.ascontiguousarray(np.stack([p[k] for p in po], axis=0)).astype(np.float32)
    cat1 = lambda k: np.ascontiguousarray(np.stack([p[k] for p in po], axis=1)).astype(np.float32)
    cat1s = lambda k: np.ascontiguousarray(np.concatenate([p[k] for p in po], axis=1)).astype(np.float32)
    y_p = cat0("y_p")
    y_s = np.ascontiguousarray(np.concatenate([p["y_s"] for p in po], axis=0)).astype(np.float32)
    return (y_p, y_s, cat1("s_p"), cat1s("s_s"), cat1s("v_s"), cat1("c_p"), cat1s("c_s"), cat1("n_p"), cat1s("n_s"),
            cat1("m_p"), cat1s("m_s"), cat1("cv_p"), cat1s("cv_s"))
```

```python
import numpy as np
from contextlib import ExitStack
import concourse.bass as bass
import concourse.mybir as mybir
from concourse.bass_utils import run_bass_kernel_spmd

F32 = mybir.dt.float32
BF16 = mybir.dt.bfloat16
AF = mybir.ActivationFunctionType
ALU = mybir.AluOpType

D = 1024; KD = 8; TP = 2048; TS = 128; T = TP + TS; NSEQ = 17
DFF = 2816; NG = 11
NL = 4
EPS = 1e-6
NEG = -1.0e30
MLSTM_MODE = 3
DBG_ONLY_MLSTM = False
FRONT_STEPS = 9
SKIP_INTER = False
SAME_ENG_SYNC = True


class V:
    def __init__(self, buf, ap):
        self.buf = buf; self.ap = ap
    def __getitem__(self, idx):
        return V(self.buf, self.ap[idx])
    def rr(self, pat, **kw):
        return V(self.buf, self.ap.rearrange(pat, **kw))
    def bc(self, shape):
        return V(self.buf, self.ap.broadcast_to(list(shape)))
    def un(self, axis):
        return V(self.buf, self.ap.unsqueeze(axis))


class Buf:
    def __init__(self, t, name):
        self.t = t; self.name = name; self.w = None; self.r = {}
    def __getitem__(self, idx):
        return V(self, self.t[idx])
    @property
    def v(self):
        return V(self, self.t[:])


class Kern:
    def __init__(self, nc, es):
        self.nc = nc; self.es = es
        self.engs = {"pe": nc.tensor, "act": nc.scalar, "dve": nc.vector, "sp": nc.sync, "pool": nc.gpsimd}
        self.sems = {}; self.cnt = {}
        for e in ["pe", "act", "dve"]:
            self.sems[e] = es.enter_context(nc.semaphore("s_" + e)); self.cnt[e] = 0
        self.rings = {}
        for q, n in (("sp", 10), ("pool", 10)):
            keys = []
            for i in range(n):
                k = "%s_d%d" % (q, i)
                self.sems[k] = es.enter_context(nc.semaphore("s_" + k)); self.cnt[k] = 0
                keys.append(k)
            self.rings[q] = [keys, 0]
        self.known = {e: {} for e in self.engs}
        self.psb = []
        for i in range(8):
            t = es.enter_context(nc.psum_tensor("psb%d" % i, [128, 512], F32))
            self.psb.append(Buf(t, "psb%d" % i))
        self.psi = 0
        self.ninstr = 0

    def sb(self, name, shape, dt, es=None):
        self.uid = getattr(self, "uid", 0) + 1
        t = (es or self.es).enter_context(self.nc.sbuf_tensor("sb%d_%s" % (self.uid, name), list(shape), dt))
        return Buf(t, name)

    def ps(self):
        b = self.psb[self.psi % 6]; self.psi += 1
        return b

    def ps_pin(self, i):
        return self.psb[6 + i]

    def _emit(self, eng, fn, reads, writes, dma=False):
        waits = {}
        known = self.known[eng]
        def need(tok):
            if tok is None: return
            k, v = tok
            if k == eng and (eng == "pe" or not SAME_ENG_SYNC): return
            if known.get(k, 0) >= v: return
            if waits.get(k, 0) < v: waits[k] = v
        rb = []; wb = []
        for x in reads:
            if x is None or isinstance(x, (int, float)): continue
            b = x.buf if isinstance(x, V) else x
            if b is not None and b not in rb: rb.append(b)
        for x in writes:
            b = x.buf if isinstance(x, V) else x
            if b is not None and b not in wb: wb.append(b)
        for b in rb: need(b.w)
        for b in wb:
            need(b.w)
            for k, v in b.r.items(): need((k, v))
        if dma:
            keys, pos = self.rings[eng]
            k = keys[pos % len(keys)]; self.rings[eng][1] = pos + 1
            need((k, self.cnt[k]))
            self.cnt[k] += 16; tok = (k, self.cnt[k]); inc = 16
        else:
            self.cnt[eng] += 1; tok = (eng, self.cnt[eng]); inc = 1
        e = self.engs[eng]
        for k, v in waits.items():
            e.wait_ge(self.sems[k], v); known[k] = v
        ins = fn(e)
        ins.then_inc(self.sems[tok[0]], inc)
        self.ninstr += 1
        for b in rb:
            if b in wb: continue
            if b.r.get(tok[0], 0) < tok[1]: b.r[tok[0]] = tok[1]
        for b in wb:
            b.w = tok; b.r = {}
        return tok

    def barrier(self):
        for eng in self.engs:
            e = self.engs[eng]; known = self.known[eng]
            for k, v in self.cnt.items():
                if v > 0 and known.get(k, 0) < v:
                    e.wait_ge(self.sems[k], v); known[k] = v

    @staticmethod
    def _a(x):
        return x.ap if isinstance(x, V) else x

    def mm(self, out, lhsT, rhs, start=True, stop=True):
        a = self._a
        return self._emit("pe", lambda e: e.matmul(a(out), lhsT=a(lhsT), rhs=a(rhs), start=start, stop=stop), [lhsT, rhs], [out])

    def tr(self, out, in_, ident):
        a = self._a
        return self._emit("pe", lambda e: e.transpose(a(out), a(in_), a(ident)), [in_, ident], [out])

    def act(self, out, in_, func, bias=None, scale=None, accum=None):
        a = self._a
        kw = {}
        if bias is not None: kw["bias"] = a(bias)
        if scale is not None: kw["scale"] = a(scale)
        if accum is not None: kw["accum_out"] = a(accum)
        w = [out] + ([accum] if accum is not None else [])
        return self._emit("act", lambda e: e.activation(out=a(out), in_=a(in_), func=func, **kw), [in_, bias, scale], w)

    def tt(self, out, in0, in1, op, eng="dve"):
        a = self._a
        return self._emit(eng, lambda e: e.tensor_tensor(out=a(out), in0=a(in0), in1=a(in1), op=op), [in0, in1], [out])

    def ts(self, out, in0, s1, op0, s2=None, op1=None, eng="dve"):
        a = self._a
        if op1 is None:
            return self._emit(eng, lambda e: e.tensor_scalar(out=a(out), in0=a(in0), scalar1=a(s1), scalar2=None, op0=op0), [in0, s1], [out])
        return self._emit(eng, lambda e: e.tensor_scalar(out=a(out), in0=a(in0), scalar1=a(s1), scalar2=a(s2), op0=op0, op1=op1), [in0, s1, s2], [out])

    def stt(self, out, in0, scalar, in1, op0, op1, eng="dve"):
        a = self._a
        return self._emit(eng, lambda e: e.scalar_tensor_tensor(out=a(out), in0=a(in0), scalar=a(scalar), in1=a(in1), op0=op0, op1=op1), [in0, scalar, in1], [out])

    def cp(self, out, in_, eng="dve"):
        a = self._a
        return self._emit(eng, lambda e: e.tensor_copy(out=a(out), in_=a(in_)), [in_], [out])

    def memset(self, out, val, eng="dve"):
        a = self._a
        return self._emit(eng, lambda e: e.memset(a(out), val), [], [out])

    def scan(self, out, d0, d1, init, op0, op1):
        a = self._a
        return self._emit("dve", lambda e: e.tensor_tensor_scan(out=a(out), data0=a(d0), data1=a(d1), initial=a(init), op0=op0, op1=op1), [d0, d1, init], [out])

    def recip(self, out, in_):
        a = self._a
        return self._emit("dve", lambda e: e.reciprocal(out=a(out), in_=a(in_)), [in_], [out])

    def dma(self, out, in_, q="sp"):
        a = self._a
        r = [in_] if isinstance(in_, V) else []
        w = [out] if isinstance(out, V) else []
        return self._emit(q, lambda e: e.dma_start(out=a(out), in_=a(in_)), r, w, dma=True)

    def finish(self):
        self.barrier()


IN_SPECS = {}
OUT_SPECS = {}


def _specs():
    I = {}
    I["xT"] = [128, KD, T]
    I["cT"] = [128, KD, NSEQ]
    I["ada_w"] = [NL, 9, 128, KD, 1024]
    I["ada_b"] = [128, NL, 72]
    I["fada_w"] = [2, 128, KD, 1024]
    I["fada_b"] = [128, 16]
    I["ffn_norm"] = [128, NL, 2, KD]
    I["mix_norm"] = [128, NL, KD]
    I["final_norm"] = [128, KD]
    I["ffn_wg"] = [NL, 2, NG, 128, KD, 256]
    I["ffn_wu"] = [NL, 2, NG, 128, KD, 256]
    I["ffn_wd"] = [NL, 2, NG, 128, 2, 1024]
    I["ab_w_in"] = [2, 128, KD, 2576]
    I["ab_w_out"] = [2, 128, KD, 1024]
    I["gla_wa2"] = [2, 17, 256]
    I["gla_norm"] = [2, 128, 1]
    I["gmlp_norm"] = [2, 128, 128]
    I["gmlp_wT_p"] = [2, 128, 4, 128]
    I["gmlp_wT_s"] = [2, 128, 4, 128]
    I["gmlp_bs_p"] = [2, 128, 4, 128]
    I["gmlp_bs_s"] = [2, 128, 4, 128]
    I["consts"] = [128, 6, 128]
    I["gla_S"] = [2, 64, 16, 4, 128]
    I["ml_w_in"] = [2, 128, KD, 4096]
    I["ml_w_out"] = [2, 128, 16, 1024]
    I["ml_conv_w"] = [2, 128, 16, 4]
    I["ml_conv_b"] = [2, 128, 16]
    I["ml_bdq"] = [2, 128, 16, 128]
    I["ml_bdk"] = [2, 128, 16, 128]
    I["ml_bdv"] = [2, 128, 16, 128]
    I["ml_w_gates"] = [2, 128, 48, 8]
    I["ml_b_gates"] = [2, 128, 8]
    I["ml_norm"] = [2, 128, 16]
    I["ml_skip"] = [2, 128, 16]
    I["ml_CT"] = [2, 16, 4, 128, 4, 512]
    I["ml_n"] = [2, 128, 16, 4, 4]
    I["ml_m"] = [2, 4, 16]
    I["ml_conv"] = [2, 128, 16, 16, 3]
    O = {}
    O["yT"] = [128, KD, T]
    O["gla_S_p"] = [2, 64, 4, 128]
    O["gla_S_s"] = [2, 64, 16, 4, 128]
    O["gmlp_v_s"] = [2, 128, 512]
    O["ml_CT_p"] = [2, 4, 128, 4, 512]
    O["ml_CT_s"] = [2, 16, 4, 128, 4, 512]
    O["ml_n_p"] = [2, 128, 4, 4]
    O["ml_n_s"] = [2, 128, 16, 4, 4]
    O["ml_m_p"] = [2, 4, 1]
    O["ml_m_s"] = [2, 4, 16]
    O["ml_conv_p"] = [2, 128, 16, 3]
    O["ml_conv_s"] = [2, 128, 16, 16, 3]
    return I, O


IN_SPECS, OUT_SPECS = _specs()
C_TRI, C_TRI8, C_ID, C_NEG, C_NEG8, C_MISC = range(6)


def build(nlayers=NL, dbg=None):
    nc = bass.Bass("TRN2", target_bir_lowering=False)
    din = {n: nc.dram_tensor(n, s, F32, kind="ExternalInput").ap() for n, s in IN_SPECS.items()}
    dout = {n: nc.dram_tensor(n, s, F32, kind="ExternalOutput").ap() for n, s in OUT_SPECS.items()}
    with ExitStack() as es:
        K = Kern(nc, es)
        _program(nc, K, es, din, dout, nlayers)
        K.finish()
    return nc


def _program(nc, K, es, din, dout, nlayers):
    TILES = [(i * 512, 512) for i in range(4)] + [(TP, TS)]
    xT = [K.sb("xT%d" % i, [128, KD, n], F32) for i, (o, n) in enumerate(TILES)]
    consts = K.sb("consts", [128, 6, 128], F32)
    ones_bf = K.sb("ones_bf", [128, 128], BF16)
    ones_f = K.sb("ones_f", [128, 128], F32)
    negm = K.sb("negm", [128, 2, 128], F32)
    modT = K.sb("modT", [128, 72, NSEQ], F32)
    csT = K.sb("csT", [128, KD, NSEQ], BF16)
    ffn_norm = K.sb("ffn_norm", [128, NL, 2, KD], F32)
    mix_norm = K.sb("mix_norm", [128, NL, KD], F32)
    final_norm = K.sb("final_norm", [128, KD], F32)
    epsb = K.sb("epsb", [128, 1], F32)
    Avec = K.sb("Avec", [128, KD, NSEQ], F32)
    Gvec = K.sb("Gvec", [128, KD, NSEQ], F32)

    K.dma(consts.v, din["consts"])
    for i, (o, n) in enumerate(TILES):
        K.dma(xT[i].v, din["xT"][:, :, o:o + n])
    K.dma(ffn_norm.v, din["ffn_norm"]); K.dma(mix_norm.v, din["mix_norm"]); K.dma(final_norm.v, din["final_norm"])
    K.memset(ones_bf.v, 1.0); K.memset(ones_f.v, 1.0); K.memset(epsb.v, EPS)
    tri = consts[:, C_TRI, :]; tri8 = consts[:, C_TRI8, :]; ident = consts[:, C_ID, :]
    seqmask = consts[:, C_MISC, 0:16]
    K.ts(negm[:, 0, :], tri, -1.0, ALU.add, -NEG, ALU.mult)
    K.ts(negm[:, 1, :], tri8, -1.0, ALU.add, -NEG, ALU.mult)

    cTf = K.sb("cTf", [128, KD, NSEQ], F32)
    K.dma(cTf.v, din["cT"])
    K.act(csT.v, cTf.v, AF.Silu)

    def ada(l):
        with ExitStack() as ph:
            adab = K.sb("adab", [128, 72], F32, ph)
            wbuf = [K.sb("adaw%d" % i, [128, KD, 1024], BF16, ph) for i in range(2)]
            if l < NL:
                K.dma(adab.v, din["ada_b"][:, l, :])
                pieces = [(din["ada_w"][l, v], v * 8) for v in range(9)]
            else:
                K.dma(adab[:, 0:16], din["fada_b"])
                pieces = [(din["fada_w"][v], v * 8) for v in range(2)]
            for i, (src, off) in enumerate(pieces):
                wb = wbuf[i % 2]
                K.dma(wb.v, src, q="pool")
                ps = K.ps()
                for j in range(8):
                    for k in range(KD):
                        K.mm(ps[:, j * NSEQ:(j + 1) * NSEQ], wb[:, k, j * 128:(j + 1) * 128], csT[:, k, :], start=(k == 0), stop=(k == KD - 1))
                K.tt(modT[:, off:off + 8, :], ps[:, 0:8 * NSEQ].rr("p (j s) -> p j s", s=NSEQ),
                     adab[:, off:off + 8].un(2).bc([128, 8, NSEQ]), ALU.add)
            K.barrier()

    def mod(l, v):
        return modT[:, v * 8:v * 8 + 8, :]

    def prep_AG(gamma, sc, gate, gmul):
        K.ts(Avec.v, sc, 1.0, ALU.add)
        K.tt(Avec.v, Avec.v, gamma.un(2).bc([128, KD, NSEQ]), ALU.mult)
        if gate is not None:
            K.ts(Gvec.v, gate, gmul, ALU.mult)

    def seq_affine(out, in_, A, B, ti, k):
        if ti < 4:
            K.act(out, in_, AF.Identity, bias=B[:, k, 0:1], scale=A[:, k, 0:1])
        else:
            o3 = out.rr("p (s t) -> p s t", t=8); i3 = in_.rr("p (s t) -> p s t", t=8)
            K.tt(o3, i3, A[:, k, 1:17].un(2).bc([128, 16, 8]), ALU.mult)
            K.tt(o3, o3, B[:, k, 1:17].un(2).bc([128, 16, 8]), ALU.add)

    def resid_add(ti, d, cols, ps_v, G):
        xv = xT[ti][:, d, cols]
        if ti < 4:
            K.stt(xv, ps_v, G[:, d, 0:1], xv, ALU.mult, ALU.add)
        else:
            tmp = rs_tmp.v
            K.tt(tmp.rr("p (s t) -> p s t", t=8), ps_v.rr("p (s t) -> p s t", t=8), G[:, d, 1:17].un(2).bc([128, 16, 8]), ALU.mult)
            K.tt(xv, xv, tmp, ALU.add)

    rs_tmp = K.sb("rs_tmp", [128, 128], F32)
    NB = {}

    def norm_bufs(es_, n):
        NB["sq"] = K.sb("nrm_sq", [128, KD, n], BF16, es_)
        NB["rstd"] = K.sb("nrm_rstd", [128, n], F32, es_)
        NB["ntmp"] = K.sb("nrm_tmp", [128, n], F32, es_)

    def rms_rstd(ps_ss, n, dim, out):
        K.act(out[:, 0:n], ps_ss[:, 0:n], AF.Ln, bias=epsb.v, scale=1.0 / dim)
        K.act(out[:, 0:n], out[:, 0:n], AF.Exp, scale=-0.5)

    def norm_tile(ti, hdst, B):
        o, n = TILES[ti]
        sq = NB["sq"]; rstd = NB["rstd"]; ntmp = NB["ntmp"]
        K.act(sq[:, :, 0:n], xT[ti].v, AF.Square)
        ps = K.ps()
        for k in range(KD):
            K.mm(ps[:, 0:n], ones_bf.v, sq[:, k, 0:n], start=(k == 0), stop=(k == KD - 1))
        rms_rstd(ps, n, D, rstd)
        for k in range(KD):
            K.tt(ntmp[:, 0:n], xT[ti][:, k, :], rstd[:, 0:n], ALU.mult)
            seq_affine(hdst[:, k, :], ntmp[:, 0:n], Avec, B, ti, k)

    def ffn(l, w):
        with ExitStack() as ph:
            hT = [K.sb("ffn_h%d" % i, [128, KD, n], BF16, ph) for i, (o, n) in enumerate(TILES)]
            wg = [K.sb("ffn_wg%d" % i, [128, KD, 256], BF16, ph) for i in range(2)]
            wu = [K.sb("ffn_wu%d" % i, [128, KD, 256], BF16, ph) for i in range(2)]
            wd = [K.sb("ffn_wd%d" % i, [128, 2, 1024], BF16, ph) for i in range(2)]
            hid = [K.sb("ffn_hid%d" % i, [128, 2, 512], BF16, ph) for i in range(2)]
            sg = K.sb("ffn_sg", [128, 512], F32, ph)
            prep_AG(ffn_norm[:, l, w, :], mod(l, 1 if w == 0 else 7), mod(l, 2 if w == 0 else 8), 0.5)
            B = mod(l, 0 if w == 0 else 6)
            with ExitStack() as nb:
                norm_bufs(nb, 512)
                for ti in range(5):
                    norm_tile(ti, hT[ti].v, B)
                K.barrier()
            hi = 0
            for g in range(NG):
                b = g % 2
                K.dma(wg[b].v, din["ffn_wg"][l, w, g], q="pool")
                K.dma(wu[b].v, din["ffn_wu"][l, w, g], q="pool")
                K.dma(wd[b].v, din["ffn_wd"][l, w, g], q="pool")
                for ti, (o, n) in enumerate(TILES):
                    hb = hid[hi % 2]; hi += 1
                    for c in range(2):
                        pg = K.ps(); pu = K.ps()
                        for k in range(KD):
                            K.mm(pg[:, 0:n], wg[b][:, k, c * 128:(c + 1) * 128], hT[ti][:, k, :], start=(k == 0), stop=(k == KD - 1))
                        for k in range(KD):
                            K.mm(pu[:, 0:n], wu[b][:, k, c * 128:(c + 1) * 128], hT[ti][:, k, :], start=(k == 0), stop=(k == KD - 1))
                        K.act(sg[:, 0:n], pg[:, 0:n], AF.Silu)
                        K.tt(hb[:, c, 0:n], sg[:, 0:n], pu[:, 0:n], ALU.mult)
                    for d in range(KD):
                        py = K.ps()
                        for c in range(2):
                            K.mm(py[:, 0:n], wd[b][:, c, d * 128:(d + 1) * 128], hb[:, c, 0:n], start=(c == 0), stop=(c == 1))
                        resid_add(ti, d, slice(0, n), py[:, 0:n], Gvec)
            K.barrier()

    def ab_mixer(l):
        e = l // 2
        with ExitStack() as ph:
            w_in = K.sb("ab_win", [128, KD, 2576], BF16, ph)
            w_out = K.sb("ab_wout", [128, KD, 1024], BF16, ph)
            wa2 = K.sb("ab_wa2", [17, 256], F32, ph)
            gnorm = K.sb("ab_gn", [128, 1], F32, ph)
            vnB = K.sb("ab_vn", [128, 128], F32, ph)
            wTp = K.sb("ab_wTp", [128, 4, 128], F32, ph); wTs = K.sb("ab_wTs", [128, 4, 128], F32, ph)
            wTpb = K.sb("ab_wTpb", [128, 4, 128], BF16, ph); wTsb = K.sb("ab_wTsb", [128, 4, 128], BF16, ph)
            bsp = K.sb("ab_bsp", [128, 4, 128], F32, ph); bss = K.sb("ab_bss", [128, 4, 128], F32, ph)
            hT = K.sb("ab_h", [128, KD, 128], BF16, ph)
            qT = K.sb("ab_q", [64, 4, 128], F32, ph); kT = K.sb("ab_k", [64, 4, 128], F32, ph)
            qb = K.sb("ab_qb", [64, 4, 128], BF16, ph); kb = K.sb("ab_kb", [64, 4, 128], BF16, ph)
            kd = K.sb("ab_kd", [64, 4, 128], F32, ph); kdt = K.sb("ab_kdt", [128, 4, 64], BF16, ph)
            a_aug = K.sb("ab_aaug", [17, 128], F32, ph)
            la = K.sb("ab_la", [128, 256], F32, ph)
            eb = K.sb("ab_eb", [64, 4, 128], F32, ph); enb = K.sb("ab_enb", [64, 4, 128], F32, ph)
            vtok = K.sb("ab_vtok", [128, 512], BF16, ph)
            sgT = K.sb("ab_sg", [128, 4, 128], F32, ph)
            uT = K.sb("ab_u", [128, 4, 128], F32, ph)
            vbn = K.sb("ab_vbn", [128, 512], F32, ph); vbnb = K.sb("ab_vbnb", [128, 512], BF16, ph)
            ssq = K.sb("ab_ssq", [128, 4], F32, ph); vjunk = K.sb("ab_vj", [128, 128], F32, ph)
            sT = K.sb("ab_sT", [128, 128], BF16, ph)
            S = K.sb("ab_S", [64, 4, 128], F32, ph); Sb = K.sb("ab_Sb", [64, 4, 128], BF16, ph)
            Ss = K.sb("ab_Ss", [64, 4, 128], F32, ph); Ssn = K.sb("ab_Ssn", [64, 4, 128], F32, ph)
            Ssb = K.sb("ab_Ssb", [64, 4, 128], BF16, ph)
            R = K.sb("ab_R", [128, 4, 128], BF16, ph)
            ebl = K.sb("ab_ebl", [64, 4, 16], F32, ph)
            oT = K.sb("ab_o", [128, 4, 128], F32, ph); osq = K.sb("ab_osq", [128, 4, 128], BF16, ph)
            orstd = K.sb("ab_orstd", [128, 4, 128], F32, ph)
            mix = K.sb("ab_mix", [128, KD, 128], BF16, ph)
            ztmp = K.sb("ab_ztmp", [128, 128], F32, ph)
            K.dma(w_in.v, din["ab_w_in"][e], q="pool"); K.dma(w_out.v, din["ab_w_out"][e], q="pool")
            K.dma(wa2.v, din["gla_wa2"][e]); K.dma(gnorm.v, din["gla_norm"][e]); K.dma(vnB.v, din["gmlp_norm"][e])
            K.dma(wTp.v, din["gmlp_wT_p"][e]); K.dma(wTs.v, din["gmlp_wT_s"][e])
            K.dma(bsp.v, din["gmlp_bs_p"][e]); K.dma(bss.v, din["gmlp_bs_s"][e])
            K.tt(wTpb.v, wTp.v, tri.un(1).bc([128, 4, 128]), ALU.mult)
            K.tt(wTsb.v, wTs.v, tri8.un(1).bc([128, 4, 128]), ALU.mult)
            K.memset(a_aug.v, 1.0)
            K.memset(S.v, 0.0); K.memset(Sb.v, 0.0)
            prep_AG(mix_norm[:, l, :], mod(l, 4), mod(l, 5), 1.0)
            B = mod(l, 3)
            norm_bufs(ph, 128)
            sq = NB["sq"]; rstd = NB["rstd"]; ntmp = NB["ntmp"]
            for ci in range(17):
                samp = (ci == 16)
                ti = ci // 4 if not samp else 4
                c0 = (ci % 4) * 128 if not samp else 0
                cols = slice(c0, c0 + 128)
                mask = tri8 if samp else tri
                K.act(sq[:, :, 0:128], xT[ti][:, :, cols], AF.Square)
                ps = K.ps()
                for k in range(KD):
                    K.mm(ps[:, 0:128], ones_bf.v, sq[:, k, 0:128], start=(k == 0), stop=(k == KD - 1))
                rms_rstd(ps, 128, D, rstd)
                for k in range(KD):
                    K.tt(ntmp[:, 0:128], xT[ti][:, k, cols], rstd[:, 0:128], ALU.mult)
                    seq_affine(hT[:, k, :], ntmp[:, 0:128], Avec, B, ti, k)
                def proj_f(col0, m, dst_ps):
                    for k in range(KD):
                        K.mm(dst_ps, w_in[:, k, col0:col0 + m], hT[:, k, :], start=(k == 0), stop=(k == KD - 1))
                ps = K.ps()
                for h in range(4):
                    proj_f(h * 64, 64, ps[0:64, h * 128:(h + 1) * 128])
                K.act(qT.v, ps[0:64, :].rr("p (h t) -> p h t", t=128), AF.Copy, scale=64 ** -0.5)
                ps = K.ps()
                for h in range(4):
                    proj_f(256 + h * 64, 64, ps[0:64, h * 128:(h + 1) * 128])
                K.cp(kT.v, ps[0:64, :].rr("p (h t) -> p h t", t=128))
                ps = K.ps()
                proj_f(1536, 16, ps[0:16, 0:128])
                K.cp(a_aug[0:16, :], ps[0:16, 0:128])
                ps = K.ps()
                for k in range(KD):
                    K.mm(ps[:, 0:512], hT[:, k, :], w_in[:, k, 512:1024], start=(k == 0), stop=(k == KD - 1))
                K.act(vtok.v, ps[:, 0:512], AF.Copy)
                ps = K.ps()
                for j in range(4):
                    proj_f(1024 + j * 128, 128, ps[:, j * 128:(j + 1) * 128])
                K.act(sgT.v, ps[:, :].rr("p (h t) -> p h t", t=128), AF.Silu)
                ps = K.ps()
                for j in range(4):
                    proj_f(1552 + j * 128, 128, ps[:, j * 128:(j + 1) * 128])
                K.cp(uT.v, ps[:, :].rr("p (h t) -> p h t", t=128))
                ps = K.ps()
                for k in range(KD):
                    K.mm(ps[:, 0:512], hT[:, k, :], w_in[:, k, 2576 - 512:2576], start=(k == 0), stop=(k == KD - 1))
                for g in range(4):
                    K.act(vjunk.v, ps[:, g * 128:(g + 1) * 128], AF.Square, accum=ssq[:, g:g + 1])
                K.act(ssq.v, ssq.v, AF.Ln, bias=epsb.v, scale=1.0 / 128)
                K.act(ssq.v, ssq.v, AF.Exp, scale=-0.5)
                for g in range(4):
                    K.stt(vbn[:, g * 128:(g + 1) * 128], ps[:, g * 128:(g + 1) * 128], ssq[:, g:g + 1], vnB.v, ALU.mult, ALU.mult)
                K.cp(vbnb.v, vbn.v)
                if samp:
                    K.dma(dout["gmlp_v_s"][e], vbn.v)
                ps = K.ps()
                K.mm(ps[:, 0:256], a_aug.v, wa2.v)
                K.act(la.v, ps[:, 0:256], AF.Exp, scale=-1.0)
                K.act(la.v, la.v, AF.Ln, bias=1.0)
                ps = K.ps()
                for h in range(4):
                    K.mm(ps[0:64, h * 128:(h + 1) * 128], la[:, h * 64:(h + 1) * 64], mask)
                bp = ps[0:64, :].rr("p (h t) -> p h t", t=128)
                K.act(eb.v, bp, AF.Exp, scale=-1.0 / 16)
                K.act(enb.v, bp, AF.Exp, scale=1.0 / 16)
                K.tt(qb.v, qT.v, eb.v, ALU.mult)
                K.tt(kb.v, kT.v, enb.v, ALU.mult)
                if not samp:
                    K.tt(kd.v, kT.v, enb.v, ALU.mult)
                    for h in range(4):
                        K.ts(kd[:, h, :], kd[:, h, :], eb[:, h, 127:128], ALU.mult)
                else:
                    K.cp(ebl.v, eb.v.rr("p h (s t) -> p h s t", t=8)[:, :, :, 7])
                    K.tt(kd.v, kT.v, enb.v, ALU.mult)
                    K.tt(kd.v.rr("p h (s t) -> p h s t", t=8), kd.v.rr("p h (s t) -> p h s t", t=8),
                         ebl.v.un(3).bc([64, 4, 16, 8]), ALU.mult)
                ps = K.ps()
                for h in range(4):
                    K.tr(ps[:, h * 64:(h + 1) * 64], kd[:, h, :], ident[0:64, 0:64])
                K.cp(kdt.v, ps[:, 0:256].rr("p (h d) -> p h d", d=64))
                for h in range(4):
                    vh = vtok[:, h * 128:(h + 1) * 128]
                    ps = K.ps()
                    K.mm(ps[:, 0:128], kb[:, h, :], qb[:, h, :])
                    K.tt(sT.v, ps[:, 0:128], mask, ALU.mult)
                    po = K.ps_pin(0)
                    if not samp:
                        K.mm(po[:, 0:128], vh, sT.v, start=True, stop=False)
                        K.mm(po[:, 0:128], Sb[:, h, :], qb[:, h, :], start=False, stop=True)
                        K.cp(oT[:, h, :], po[:, 0:128])
                        pk = K.ps()
                        K.mm(pk[0:64, 0:128], kdt[:, h, :], vh)
                        K.stt(S[:, h, :], S[:, h, :], eb[:, h, 127:128], pk[0:64, 0:128], ALU.mult, ALU.add)
                        K.act(Sb[:, h, :], S[:, h, :], AF.Copy)
                    else:
                        K.mm(po[:, 0:128], vh, sT.v, start=True, stop=False)
                        for q4 in range(4):
                            sl = slice(q4 * 4, q4 * 4 + 4)
                            K.dma(Ss.v, din["gla_S"][e][:, sl, h, :])
                            K.cp(Ssb.v, Ss.v)
                            for s4 in range(4):
                                s_ = q4 * 4 + s4
                                K.mm(po[:, s_ * 8:(s_ + 1) * 8], Ssb[:, s4, :], qb[:, h, s_ * 8:(s_ + 1) * 8], start=False, stop=(s_ == 15))
                            K.tt(R.v, vh.un(1).bc([128, 4, 128]), seqmask[:, sl].un(2).bc([128, 4, 128]), ALU.mult)
                            pk = K.ps()
                            K.mm(pk[0:64, 0:512], kdt[:, h, :], R.v.rr("p s v -> p (s v)"))
                            K.tt(Ssn.v, Ss.v, ebl[:, h, sl].un(2).bc([64, 4, 128]), ALU.mult)
                            K.tt(Ssn.v, Ssn.v, pk[0:64, 0:512].rr("p (s v) -> p s v", v=128), ALU.add)
                            K.dma(dout["gla_S_s"][e][:, sl, h, :], Ssn.v)
                        K.cp(oT[:, h, :], po[:, 0:128])
                if ci == 15:
                    K.dma(dout["gla_S_p"][e], S.v)
                K.act(osq.v, oT.v, AF.Square)
                ps = K.ps()
                for h in range(4):
                    K.mm(ps[:, h * 128:(h + 1) * 128], ones_bf.v, osq[:, h, :])
                K.act(orstd.v, ps[:, :].rr("p (h t) -> p h t", t=128), AF.Ln, bias=epsb.v, scale=1.0 / 128)
                K.act(orstd.v, orstd.v, AF.Exp, scale=-0.5)
                K.tt(oT.v, oT.v, orstd.v, ALU.mult)
                K.stt(mix[:, 0:4, :], oT.v, gnorm.v, sgT.v, ALU.mult, ALU.mult)
                wTb = wTsb if samp else wTpb
                bsB = bss if samp else bsp
                for g in range(4):
                    ps = K.ps()
                    K.mm(ps[:, 0:128], vbnb[:, g * 128:(g + 1) * 128], wTb[:, g, :])
                    K.tt(ztmp.v, ps[:, 0:128], bsB[:, g, :], ALU.add)
                    K.tt(mix[:, 4 + g, :], ztmp.v, uT[:, g, :], ALU.mult)
                for half in range(2):
                    ps = K.ps()
                    for dd in range(4):
                        d = half * 4 + dd
                        for k in range(KD):
                            K.mm(ps[:, dd * 128:(dd + 1) * 128], w_out[:, k, d * 128:(d + 1) * 128], mix[:, k, :], start=(k == 0), stop=(k == KD - 1))
                    for dd in range(4):
                        resid_add(ti, half * 4 + dd, cols, ps[:, dd * 128:(dd + 1) * 128], Gvec)
            K.barrier()

    def mlstm(l):
        o_ = l // 2
        with ExitStack() as ph:
            hT = K.sb("ml_h", [128, KD, T], BF16, ph)
            prep_AG(mix_norm[:, l, :], mod(l, 4), mod(l, 5), 1.0)
            B = mod(l, 3)
            with ExitStack() as nb:
                norm_bufs(nb, 512)
                for ti in range(5):
                    norm_tile(ti, hT[:, :, TILES[ti][0]:TILES[ti][0] + TILES[ti][1]], B)
                K.barrier()
            cw = K.sb("ml_cw", [128, 16, 4], F32, ph); cb = K.sb("ml_cb", [128, 16], F32, ph)
            gn = K.sb("ml_gn", [128, 16], F32, ph); skp = K.sb("ml_skip", [128, 16], F32, ph)
            bd = {n: K.sb("ml_" + n, [128, 4, 128], BF16, ph) for n in ("bdq", "bdk", "bdv")}
            wgt = K.sb("ml_wgt", [128, 48, 8], BF16, ph); bg = K.sb("ml_bg", [128, 8], F32, ph)
            mst = K.sb("ml_mst", [4, 16], F32, ph)
            gacc = K.sb("ml_gacc", [128, 17, 8], F32, ph)
            colsAll = K.sb("ml_cols", [128, 17, 20], F32, ph)
            decB = K.sb("ml_decB", [128, 4, 32], F32, ph)
            sel = K.sb("ml_sel", [4, 4, 128], F32, ph)
            K.dma(cw.v, din["ml_conv_w"][o_]); K.dma(cb.v, din["ml_conv_b"][o_])
            K.dma(gn.v, din["ml_norm"][o_]); K.dma(skp.v, din["ml_skip"][o_])
            K.dma(wgt.v, din["ml_w_gates"][o_], q="pool"); K.dma(bg.v, din["ml_b_gates"][o_])
            K.dma(mst.v, din["ml_m"][o_])
            K.cp(sel.v, ident[0:4, 0:4].un(2).bc([4, 4, 128]))
            xmes = K.sb("ml_xmes", [128, 4, 16, 16], F32, ph)

            def front_set(es_, tag):
                return {"xme": K.sb("ml_xme" + tag, [128, 4, 136], F32, es_), "xmb": K.sb("ml_xmb" + tag, [128, 4, 128], BF16, es_),
                        "acc": K.sb("ml_acc" + tag, [128, 4, 128], F32, es_), "xc": K.sb("ml_xc" + tag, [128, 4, 128], BF16, es_),
                        "qT": K.sb("ml_q" + tag, [128, 4, 128], BF16, es_), "kT": K.sb("ml_k" + tag, [128, 4, 128], BF16, es_),
                        "vT": K.sb("ml_v" + tag, [128, 4, 128], BF16, es_)}
            fb0 = front_set(ph, "0")
            w_x = K.sb("ml_wx", [128, KD, 512], BF16, ph)
            cvin = K.sb("ml_cvin", [128, 4, 16, 3], F32, ph); cvout = K.sb("ml_cvout", [128, 4, 16, 3], F32, ph)
            cvp = K.sb("ml_cvp", [128, 4, 3], F32, ph)

            def load_head(h):
                K.dma(w_x.v, din["ml_w_in"][o_][:, :, h * 512:(h + 1) * 512], q="pool")
                for n in bd: K.dma(bd[n].v, din["ml_" + n][o_][:, 4 * h:4 * h + 4, :], q="pool")
                K.dma(cvin.v, din["ml_conv"][o_][:, 4 * h:4 * h + 4, :, :])
                K.cp(xmes[:, :, :, 5:8], cvin.v)
                K.memset(fb0["xme"][:, :, 0:8], 0.0)

            def front(ci, h, need_v_T, fb, fbn):
                xme = fb["xme"]; xmb = fb["xmb"]; acc = fb["acc"]; xc = fb["xc"]; qT = fb["qT"]; kT = fb["kT"]; vT = fb["vT"]
                samp = (ci == 16)
                t0 = ci * 128
                ps = K.ps()
                for j in range(4):
                    for k in range(KD):
                        K.mm(ps[:, j * 128:(j + 1) * 128], w_x[:, k, j * 128:(j + 1) * 128], hT[:, k, t0:t0 + 128], start=(k == 0), stop=(k == KD - 1))
                if FRONT_STEPS == 0:
                    K.cp(xmb.v, ps[:, :].rr("p (f t) -> p f t", t=128)); return
                if not samp:
                    K.act(xme[:, :, 8:136], ps[:, :].rr("p (f t) -> p f t", t=128), AF.Copy)
                else:
                    K.act(xmes[:, :, :, 8:16], ps[:, :].rr("p (f s t) -> p f s t", t=8, s=16), AF.Copy)
                if not samp:
                    K.cp(xmb.v, xme[:, :, 8:136])
                else:
                    K.cp(xmb.v.rr("p f (s t) -> p f s t", t=8), xmes[:, :, :, 8:16])
                if FRONT_STEPS < 2: return
                for j in range(4):
                    fc = 4 * h + j
                    for tap in range(4):
                        if not samp:
                            src = xme[:, j, 5 + tap:5 + tap + 128]; a_ = acc[:, j, :]
                        else:
                            src = xmes[:, j, :, 5 + tap:5 + tap + 8]; a_ = acc[:, j, :].rr("p (s t) -> p s t", t=8)
                        if tap == 0:
                            K.ts(a_, src, cw[:, fc, 0:1], ALU.mult)
                        else:
                            K.stt(a_, src, cw[:, fc, tap:tap + 1], a_, ALU.mult, ALU.add)
                    if FRONT_STEPS >= 3:
                        K.act(xc[:, j, :], acc[:, j, :], AF.Silu, bias=cb[:, fc:fc + 1])
                if FRONT_STEPS < 4: return
                for nm, src, dst in (("bdq", xc, qT), ("bdk", xc, kT), ("bdv", xmb, vT)):
                    if nm == "bdv" and not need_v_T: continue
                    ps = K.ps()
                    for j in range(4):
                        K.mm(ps[:, j * 128:(j + 1) * 128], bd[nm][:, j, :], src[:, j, :])
                    K.act(dst.v, ps[:, :].rr("p (f t) -> p f t", t=128), AF.Copy)
                if FRONT_STEPS < 5: return
                if not samp:
                    if ci == 15 and need_v_T:
                        K.cp(cvp.v, xme[:, :, 133:136])
                        K.dma(dout["ml_conv_p"][o_][:, 4 * h:4 * h + 4, :], cvp.v)
                    K.cp(fbn["xme"][:, :, 5:8], xme[:, :, 133:136])
                elif need_v_T:
                    K.cp(cvout.v, xmes[:, :, :, 13:16])
                    K.dma(dout["ml_conv_s"][o_][:, 4 * h:4 * h + 4, :, :], cvout.v)

            if MLSTM_MODE == 10:
                K.barrier(); return
            p1a = ExitStack()
            fb1 = front_set(p1a, "1")
            fbs = [fb0, fb1]
            for h in range(4):
                load_head(h)
                if MLSTM_MODE == 12: continue
                for ci in (range(17) if MLSTM_MODE != 13 else range(16)):
                    fb = fbs[ci % 2]
                    front(ci, h, True, fb, fbs[(ci + 1) % 2])
                    if MLSTM_MODE in (11, 13): continue
                    ps = K.ps()
                    i = 0
                    for part, src in enumerate((fb["qT"], fb["kT"], fb["vT"])):
                        for j in range(4):
                            K.mm(ps[:, 0:8], src[:, j, :], wgt[:, part * 16 + 4 * h + j, :], start=(i == 0), stop=(i == 11)); i += 1
                    K.tt(gacc[:, ci, :], ps[:, 0:8], bg.v if h == 0 else gacc[:, ci, :], ALU.add)
            K.barrier()
            p1a.close()
            if MLSTM_MODE in (1, 11, 12, 13):
                K.barrier(); return
            K.act(gacc[:, :, 4:8], gacc[:, :, 4:8], AF.Exp, scale=-1.0)
            K.act(gacc[:, :, 4:8], gacc[:, :, 4:8], AF.Ln, bias=1.0)
            with ExitStack() as p1:
                R32 = {n: K.sb("ml_r_" + n, [32, T], F32, p1) for n in ("ig", "lf", "m", "F", "wr")}
                for n in R32: K.memset(R32[n].v, 0.0)
                R_ = {n: R32[n][0:4, :] for n in R32}
                mprev = K.sb("ml_mprev", [4, 32], F32, p1); rend = K.sb("ml_rend", [4, 32], F32, p1); fst = K.sb("ml_fst", [4, 16], F32, p1)
                dec = K.sb("ml_dec", [4, 32], F32, p1)
                for ci in range(17):
                    ps = K.ps()
                    K.tr(ps[0:4, 0:128], gacc[:, ci, 0:4], ident)
                    K.tr(ps[0:4, 128:256], gacc[:, ci, 4:8], ident)
                    K.cp(R_["ig"][:, ci * 128:(ci + 1) * 128], ps[0:4, 0:128])
                    K.ts(R_["lf"][:, ci * 128:(ci + 1) * 128], ps[0:4, 128:256], -1.0, ALU.mult)
                K.scan(R_["m"][:, 0:TP], R_["lf"][:, 0:TP], R_["ig"][:, 0:TP], 0.0, ALU.add, ALU.max)
                K.scan(R_["F"][:, 0:TP], R_["lf"][:, 0:TP], R_["lf"][:, 0:TP], 0.0, ALU.add, ALU.min)
                for s in range(16):
                    sl = slice(TP + s * 8, TP + s * 8 + 8)
                    K.scan(R_["m"][:, sl], R_["lf"][:, sl], R_["ig"][:, sl], mst[:, s:s + 1], ALU.add, ALU.max)
                    K.scan(R_["F"][:, sl], R_["lf"][:, sl], R_["lf"][:, sl], 0.0, ALU.add, ALU.min)
                seg = lambda n: (R_[n][:, 0:TP].rr("p (c t) -> p c t", t=128), R_[n][:, TP:T].rr("p (c t) -> p c t", t=8))
                Fp = seg("F")[0]; mp = seg("m")[0]
                K.memset(fst[:, 0:1], 0.0); K.memset(mprev[:, 0:1], 0.0)
                K.cp(fst[:, 1:16], Fp[:, 0:15, 127]); K.cp(mprev[:, 1:16], mp[:, 0:15, 127])
                K.cp(mprev[:, 16:32], mst.v)
                K.tt(Fp, Fp, fst.v.un(2).bc([4, 16, 128]), ALU.subtract)
                K.dma(dout["ml_m_p"][o_], R_["m"][:, TP - 1:TP])
                msout = K.sb("ml_msout", [4, 16], F32, p1)
                K.cp(msout.v, seg("m")[1][:, :, 7])
                K.dma(dout["ml_m_s"][o_], msout.v)
                K.tt(R_["ig"], R_["ig"], R_["F"], ALU.subtract)
                K.tt(R_["F"], R_["F"], R_["m"], ALU.subtract)
                K.act(R_["lf"], R_["m"], AF.Exp, scale=-1.0)
                R_["wi"] = R_["m"]; R32["wi"] = R32["m"]
                K.cp(rend[:, 0:16], seg("F")[0][:, :, 127]); K.cp(rend[:, 16:32], seg("F")[1][:, :, 7])
                for pi, (off, L) in enumerate(((0, 128), (16, 8))):
                    K.tt(seg("wi")[pi], seg("F")[pi], mprev[:, off:off + 16].un(2).bc([4, 16, L]), ALU.add)
                    K.tt(seg("wr")[pi], seg("ig")[pi], rend[:, off:off + 16].un(2).bc([4, 16, L]), ALU.add)
                K.act(R_["wi"], R_["wi"], AF.Exp)
                K.act(R_["wr"], R_["wr"], AF.Exp)
                K.ts(R_["wr"], R_["wr"], 512 ** -0.5, ALU.mult)
                K.cp(dec[:, 0:16], seg("wi")[0][:, :, 127]); K.cp(dec[:, 16:32], seg("wi")[1][:, :, 7])
                for ci in range(17):
                    ps = K.ps()
                    for i, n in enumerate(("ig", "wr", "F", "wi", "lf")):
                        K.tr(ps[:, i * 32:(i + 1) * 32], R32[n][:, ci * 128:(ci + 1) * 128], ident[0:32, 0:32])
                    K.cp(colsAll[:, ci, :].rr("p (i f) -> p i f", f=4), ps[:, 0:160].rr("p (i f) -> p i f", f=32)[:, :, 0:4])
                K.barrier()
            if MLSTM_MODE == 2:
                K.barrier(); return
            with ExitStack() as p2:
                w_z = K.sb("ml_wz", [128, KD, 512], BF16, p2)
                w_o = K.sb("ml_wo", [128, 4, 1024], BF16, p2)
                CT = K.sb("ml_CT", [128, 4, 512], F32, p2); CTb = K.sb("ml_CTb", [128, 4, 512], BF16, p2)
                CT2 = K.sb("ml_CT2", [128, 4, 512], F32, p2)
                nS = K.sb("ml_nS", [128, 16, 4, 4], F32, p2); nSn = K.sb("ml_nSn", [128, 16, 4, 4], F32, p2)
                nP = K.sb("ml_nP", [128, 4, 4], F32, p2)
                nB = K.sb("ml_nB", [128, 4, 128], BF16, p2)
                ktok = K.sb("ml_ktok", [128, 512], BF16, p2); vtok = K.sb("ml_vtok", [128, 512], BF16, p2)
                vw = K.sb("ml_vw", [128, 512], BF16, p2); km = K.sb("ml_km", [128, 512], BF16, p2)
                wrb = K.sb("ml_wrb", [128, 4], BF16, p2)
                diag = K.sb("ml_diag", [128, 128], F32, p2)
                bcs = K.sb("ml_bcs", [128, 3, 128], F32, p2)
                arg = K.sb("ml_arg", [128, 128], F32, p2); sT = K.sb("ml_sT", [128, 128], BF16, p2)
                qw = K.sb("ml_qw", [128, 4, 128], BF16, p2)
                hden = K.sb("ml_hden", [128, 128], F32, p2)
                hh = K.sb("ml_hh", [128, 4, 128], F32, p2); hsq = K.sb("ml_hsq", [128, 4, 128], F32, p2)
                mean = K.sb("ml_mean", [128, 128], F32, p2); var = K.sb("ml_var", [128, 128], F32, p2)
                sz = K.sb("ml_sz", [128, 4, 128], BF16, p2); pre = K.sb("ml_pre", [128, 4, 128], BF16, p2)
                skx = K.sb("ml_skx", [128, 128], F32, p2)
                interN = hsq; interD = mean
                K.dma(nS.v, din["ml_n"][o_])
                K.memset(nP.v, 0.0)
                for h in range(4):
                    load_head(h)
                    K.dma(w_z.v, din["ml_w_in"][o_][:, :, 2048 + h * 512:2048 + (h + 1) * 512], q="pool")
                    K.dma(w_o.v, din["ml_w_out"][o_][:, 4 * h:4 * h + 4, :], q="pool")
                    K.memset(CT.v, 0.0); K.memset(CTb.v, 0.0); K.memset(nB.v, 0.0)
                    for ci in range(17):
                        samp = (ci == 16)
                        ti = ci // 4 if not samp else 4
                        c0 = (ci % 4) * 128 if not samp else 0
                        cols = slice(c0, c0 + 128)
                        tsl = slice(ci * 128, ci * 128 + 128)
                        front(ci, h, False, fb0, fb0)
                        xc = fb0["xc"]; xmb = fb0["xmb"]; qT = fb0["qT"]; kT = fb0["kT"]
                        for nm, src, dst in (("bdk", xc, ktok), ("bdv", xmb, vtok)):
                            ps = K.ps()
                            for j in range(4):
                                K.mm(ps[:, j * 128:(j + 1) * 128], src[:, j, :], bd[nm][:, j, :])
                            K.act(dst.v, ps[:, :], AF.Copy)
                        gcol = colsAll[:, ci, h:h + 1]; wrcol = colsAll[:, ci, 4 + h:5 + h]
                        K.cp(wrb.v, colsAll[:, ci, 4:8])
                        ps = K.ps()
                        for i in range(3):
                            K.ts(diag.v, ident, colsAll[:, ci, 8 + 4 * i + h:9 + 4 * i + h], ALU.mult)
                            K.mm(ps[:, i * 128:(i + 1) * 128], ones_f.v, diag.v)
                        K.act(bcs.v, ps[:, 0:384].rr("p (i t) -> p i t", t=128), AF.Copy)
                        K.stt(arg.v, bcs[:, 0, :], gcol, negm[:, 1 if samp else 0, :], ALU.add, ALU.add)
                        K.act(arg.v, arg.v, AF.Exp)
                        ps = K.ps()
                        for j in range(4):
                            K.mm(ps[:, 0:128], kT[:, j, :], qT[:, j, :], start=(j == 0), stop=(j == 3))
                        K.stt(sT.v, ps[:, 0:128], 512 ** -0.5, arg.v, ALU.mult, ALU.mult)
                        K.tt(qw.v, qT.v, bcs[:, 1, :].un(1).bc([128, 4, 128]), ALU.mult)
                        pn = K.ps_pin(0); pd = K.ps_pin(1)
                        if not samp:
                            for vc in range(4):
                                K.mm(pn[:, vc * 128:(vc + 1) * 128], vtok[:, vc * 128:(vc + 1) * 128], sT.v, start=True, stop=False)
                                for dc in range(4):
                                    K.mm(pn[:, vc * 128:(vc + 1) * 128], CTb[:, dc, vc * 128:(vc + 1) * 128], qw[:, dc, :], start=False, stop=(dc == 3))
                            K.mm(pd[:, 0:128], ones_bf.v, sT.v, start=True, stop=False)
                            for dc in range(4):
                                K.mm(pd[:, 0:128], nB[:, dc, :], qw[:, dc, :], start=False, stop=(dc == 3))
                        else:
                            for vc in range(4):
                                K.mm(pn[:, vc * 128:(vc + 1) * 128], vtok[:, vc * 128:(vc + 1) * 128], sT.v, start=True, stop=True)
                            K.mm(pd[:, 0:128], ones_bf.v, sT.v, start=True, stop=True)
                            K.ts(vw.v, vtok.v, wrcol, ALU.mult)
                            CTs = [CT, CT2]
                            K.dma(CT.v, din["ml_CT"][o_, 0, h])
                            for s in range(16):
                                CTc = CTs[s % 2]
                                if s + 1 < 16:
                                    K.dma(CTs[(s + 1) % 2].v, din["ml_CT"][o_, s + 1, h])
                                K.act(CTb.v, CTc.v, AF.Copy)
                                K.cp(nB.v, nS[:, s, h, :].un(2).bc([128, 4, 128]))
                                ssl = slice(s * 8, s * 8 + 8)
                                pi = K.ps()
                                for vc in range(4):
                                    for dc in range(4):
                                        K.mm(pi[:, vc * 8:vc * 8 + 8], CTb[:, dc, vc * 128:(vc + 1) * 128], qw[:, dc, ssl], start=(dc == 0), stop=(dc == 3))
                                for dc in range(4):
                                    K.mm(pi[:, 32:40], nB[:, dc, :], qw[:, dc, ssl], start=(dc == 0), stop=(dc == 3))
                                K.cp(interN[:, :, ssl], pi[:, 0:32].rr("p (v t) -> p v t", t=8))
                                K.cp(interD[:, ssl], pi[:, 32:40])
                                K.ts(km.v, ktok.v, seqmask[:, s:s + 1], ALU.mult)
                                pnn = K.ps()
                                for dc in range(4):
                                    pk = K.ps()
                                    K.mm(pk[:, 0:512], km[:, dc * 128:(dc + 1) * 128], vw.v)
                                    K.stt(CTc[:, dc, :], CTc[:, dc, :], bcs[:, 1, s * 8 + 7:s * 8 + 8], pk[:, 0:512], ALU.mult, ALU.add)
                                    K.mm(pnn[:, dc * 4:(dc + 1) * 4], km[:, dc * 128:(dc + 1) * 128], wrb.v)
                                K.stt(nSn[:, s, h, :], nS[:, s, h, :], bcs[:, 1, s * 8 + 7:s * 8 + 8], pnn[:, 0:16].rr("p (d f) -> p d f", f=4)[:, :, h], ALU.mult, ALU.add)
                                K.dma(dout["ml_CT_s"][o_, s, h], CTc.v, q="pool")
                        if samp:
                            K.tt(interD.v, interD.v, pd[:, 0:128], ALU.add)
                            K.tt(interN.v, interN.v, pn[:, :].rr("p (v t) -> p v t", t=128), ALU.add)
                            den_v = interD.v; num_v = interN.v
                        else:
                            den_v = pd[:, 0:128]; num_v = pn[:, :].rr("p (v t) -> p v t", t=128)
                        K.ts(hden.v, den_v, -1.0, ALU.mult)
                        K.tt(hden.v, hden.v, den_v, ALU.max)
                        K.tt(hden.v, hden.v, bcs[:, 2, :], ALU.max)
                        K.recip(hden.v, hden.v)
                        K.tt(hh.v, num_v, hden.v.un(1).bc([128, 4, 128]), ALU.mult)
                        K.act(hsq.v, hh.v, AF.Square)
                        ps = K.ps()
                        for vc in range(4):
                            K.mm(ps[:, 0:128], ones_f.v, hh[:, vc, :], start=(vc == 0), stop=(vc == 3))
                        for vc in range(4):
                            K.mm(ps[:, 128:256], ones_f.v, hsq[:, vc, :], start=(vc == 0), stop=(vc == 3))
                        K.act(mean.v, ps[:, 0:128], AF.Copy, scale=1.0 / 512)
                        K.tt(var.v, mean.v, mean.v, ALU.mult)
                        K.stt(var.v, ps[:, 128:256], 1.0 / 512, var.v, ALU.mult, ALU.subtract)
                        K.act(var.v, var.v, AF.Ln, bias=epsb.v)
                        K.act(var.v, var.v, AF.Exp, scale=-0.5)
                        K.tt(hh.v, hh.v, mean.v.un(1).bc([128, 4, 128]), ALU.subtract)
                        K.tt(hh.v, hh.v, var.v.un(1).bc([128, 4, 128]), ALU.mult)
                        ps = K.ps()
                        for j in range(4):
                            for k in range(KD):
                                K.mm(ps[:, j * 128:(j + 1) * 128], w_z[:, k, j * 128:(j + 1) * 128], hT[:, k, tsl], start=(k == 0), stop=(k == KD - 1))
                        K.act(sz.v, ps[:, :].rr("p (f t) -> p f t", t=128), AF.Silu)
                        for j in range(4):
                            fc = 4 * h + j
                            K.ts(skx.v, xc[:, j, :], skp[:, fc:fc + 1], ALU.mult)
                            K.stt(hh[:, j, :], hh[:, j, :], gn[:, fc:fc + 1], skx.v, ALU.mult, ALU.add)
                        K.tt(pre.v, hh.v, sz.v, ALU.mult)
                        for half in range(2):
                            ps = K.ps()
                            for dd in range(4):
                                d = half * 4 + dd
                                for j in range(4):
                                    K.mm(ps[:, dd * 128:(dd + 1) * 128], w_o[:, j, d * 128:(d + 1) * 128], pre[:, j, :], start=(j == 0), stop=(j == 3))
                            for dd in range(4):
                                resid_add(ti, half * 4 + dd, cols, ps[:, dd * 128:(dd + 1) * 128], Gvec)
                        if not samp:
                            K.ts(vw.v, vtok.v, wrcol, ALU.mult)
                            pnn = K.ps()
                            for dc in range(4):
                                pk = K.ps()
                                K.mm(pk[:, 0:512], ktok[:, dc * 128:(dc + 1) * 128], vw.v)
                                K.stt(CT[:, dc, :], CT[:, dc, :], bcs[:, 1, 127:128], pk[:, 0:512], ALU.mult, ALU.add)
                                K.mm(pnn[:, dc * 4:(dc + 1) * 4], ktok[:, dc * 128:(dc + 1) * 128], wrb.v)
                            K.stt(nP[:, h, :], nP[:, h, :], bcs[:, 1, 127:128], pnn[:, 0:16].rr("p (d f) -> p d f", f=4)[:, :, h], ALU.mult, ALU.add)
                            K.act(CTb.v, CT.v, AF.Copy)
                            K.cp(nB.v, nP[:, h, :].un(2).bc([128, 4, 128]))
                            if ci == 15:
                                K.dma(dout["ml_CT_p"][o_, h], CT.v)
                K.dma(dout["ml_n_p"][o_], nP.v)
                K.dma(dout["ml_n_s"][o_], nSn.v)
                K.barrier()
        K.barrier()

    for l in range(nlayers):
        if DBG_ONLY_MLSTM:
            if l == 1: mlstm(l)
            continue
        ada(l)
        ffn(l, 0)
        if l % 2 == 0:
            ab_mixer(l)
        elif MLSTM_MODE:
            mlstm(l)
        ffn(l, 1)
    with ExitStack() as ph:
        ada(NL)
        yT = [K.sb("yT%d" % i, [128, KD, n], F32, ph) for i, (o, n) in enumerate(TILES)]
        K.ts(Avec.v, mod(NL, 1), 1.0, ALU.add)
        K.tt(Avec.v, Avec.v, final_norm.v.un(2).bc([128, KD, NSEQ]), ALU.mult)
        B = mod(NL, 0)
        norm_bufs(ph, 512)
        for ti, (o, n) in enumerate(TILES):
            norm_tile(ti, yT[ti].v, B)
            K.dma(dout["yT"][:, :, o:o + n], yT[ti].v)
        K.barrier()


def _consts():
    c = np.zeros((128, 6, 128), np.float32)
    s = np.arange(128)[:, None]; t = np.arange(128)[None, :]
    c[:, C_TRI, :] = (s <= t)
    c[:, C_TRI8, :] = (s <= t) & (s // 8 == t // 8)
    c[:, C_ID, :] = (s == t)
    c[:, C_MISC, 0:16] = (np.arange(128)[:, None] // 8 == np.arange(16)[None, :])
    return c


def _bd(w):
    out = np.zeros((16, 128, 128), np.float32)
    wr = w.reshape(16, 32, 4, 4)
    for n in range(32):
        out[:, n * 4:(n + 1) * 4, n * 4:(n + 1) * 4] = wr[:, n]
    return np.ascontiguousarray(out.transpose(1, 0, 2))


def _kT(w, kc):
    return np.ascontiguousarray(w.reshape(kc, 128, -1).transpose(1, 0, 2))


def prep_shared(inp):
    f = lambda a: np.ascontiguousarray(np.asarray(a, dtype=np.float32))
    S = {}
    S["ada_w"] = f(inp["ada_w"].reshape(NL, KD, 128, 9, 1024).transpose(0, 3, 2, 1, 4))
    S["ada_b"] = f(inp["ada_b"].reshape(NL, 72, 128).transpose(2, 0, 1))
    S["fada_w"] = f(inp["final_ada_w"].reshape(KD, 128, 2, 1024).transpose(2, 1, 0, 3))
    S["fada_b"] = f(inp["final_ada_b"].reshape(16, 128).T)
    S["ffn_norm"] = f(inp["ffn_norm"].reshape(NL, 2, KD, 128).transpose(3, 0, 1, 2))
    S["mix_norm"] = f(inp["mix_norm"].reshape(NL, KD, 128).transpose(2, 0, 1))
    S["final_norm"] = f(inp["final_norm"].reshape(KD, 128).T)
    S["ffn_wg"] = f(inp["ffn_w_gate"].reshape(NL, 2, KD, 128, NG, 256).transpose(0, 1, 4, 3, 2, 5))
    S["ffn_wu"] = f(inp["ffn_w_up"].reshape(NL, 2, KD, 128, NG, 256).transpose(0, 1, 4, 3, 2, 5))
    S["ffn_wd"] = f(inp["ffn_w_down"].reshape(NL, 2, NG, 2, 128, 1024).transpose(0, 1, 2, 4, 3, 5))
    S["ab_w_in"] = f(inp["ab_w_in"].reshape(2, KD, 128, 2576).transpose(0, 2, 1, 3))
    S["ab_w_out"] = f(inp["ab_w_out"].reshape(2, KD, 128, 1024).transpose(0, 2, 1, 3))
    S["gla_wa2"] = f(np.concatenate([inp["gla_w_a2"], inp["gla_b_a"][:, None, :]], axis=1))
    S["gla_norm"] = f(inp["gla_norm"].reshape(2, 128, 1))
    S["gmlp_norm"] = f(np.broadcast_to(inp["gmlp_norm"][:, None, :], (2, 128, 128)))
    ws = np.asarray(inp["gmlp_ws"])
    S["gmlp_wT_p"] = f(ws.transpose(0, 3, 1, 2))
    w8 = ws[:, :, :8, :8]
    S["gmlp_wT_s"] = f(np.tile(w8.transpose(0, 3, 1, 2), (1, 16, 1, 16)))
    bs = np.asarray(inp["gmlp_bs"])
    S["gmlp_bs_p"] = f(np.broadcast_to(bs[:, None, :, :], (2, 128, 4, 128)))
    S["gmlp_bs_s"] = f(np.broadcast_to(np.tile(bs[:, :, :8], (1, 1, 16))[:, None, :, :], (2, 128, 4, 128)))
    S["consts"] = _consts()
    S["ml_w_in"] = f(inp["ml_w_in"].reshape(2, KD, 128, 4096).transpose(0, 2, 1, 3))
    S["ml_w_out"] = f(inp["ml_w_out"].reshape(2, 16, 128, 1024).transpose(0, 2, 1, 3))
    S["ml_conv_w"] = f(inp["ml_conv_w"].reshape(2, 4, 16, 128).transpose(0, 3, 2, 1))
    S["ml_conv_b"] = f(inp["ml_conv_b"].reshape(2, 16, 128).transpose(0, 2, 1))
    for n, k in (("ml_bdq", "ml_wq"), ("ml_bdk", "ml_wk"), ("ml_bdv", "ml_wv")):
        S[n] = np.stack([_bd(np.asarray(inp[k][o])) for o in range(2)])
    S["ml_w_gates"] = f(inp["ml_w_gates"].reshape(2, 48, 128, 8).transpose(0, 2, 1, 3))
    S["ml_b_gates"] = f(np.broadcast_to(inp["ml_b_gates"][:, None, :], (2, 128, 8)))
    S["ml_norm"] = f(inp["ml_norm"].reshape(2, 16, 128).transpose(0, 2, 1))
    S["ml_skip"] = f(inp["ml_skip"].reshape(2, 16, 128).transpose(0, 2, 1))
    return S


def prep_core(inp, i):
    f = lambda a: np.ascontiguousarray(np.asarray(a, dtype=np.float32))
    P = {}
    sl = slice(16 * i, 16 * i + 16)
    x = np.concatenate([inp["x_prompt"][i], inp["x_sample"][sl].reshape(TS, D)], axis=0)
    P["xT"] = f(x.T.reshape(KD, 128, T).transpose(1, 0, 2))
    c = np.concatenate([inp["c_prompt"][i:i + 1], inp["c_sample"][sl]], axis=0)
    P["cT"] = f(c.T.reshape(KD, 128, NSEQ).transpose(1, 0, 2))
    P["gla_S"] = f(inp["state_gla_S"][:, sl].transpose(0, 3, 1, 2, 4))
    C = inp["state_mlstm_C"][:, sl]
    P["ml_CT"] = f(C.transpose(0, 1, 2, 4, 3).reshape(2, 16, 4, 4, 128, 512).transpose(0, 1, 2, 4, 3, 5))
    P["ml_n"] = f(inp["state_mlstm_n"][:, sl].reshape(2, 16, 4, 4, 128).transpose(0, 4, 1, 2, 3))
    P["ml_m"] = f(inp["state_mlstm_m"][:, sl].transpose(0, 2, 1))
    P["ml_conv"] = f(inp["state_mlstm_conv"][:, sl].reshape(2, 16, 3, 16, 128).transpose(0, 4, 3, 1, 2))
    return P


def post_core(r):
    o = {}
    y = r["yT"].transpose(2, 1, 0).reshape(T, D)
    o["y_p"] = y[0:TP]; o["y_s"] = y[TP:].reshape(16, 8, D)
    o["s_p"] = r["gla_S_p"].transpose(0, 2, 1, 3)
    o["s_s"] = r["gla_S_s"].transpose(0, 2, 3, 1, 4)
    o["v_s"] = r["gmlp_v_s"].reshape(2, 16, 8, 512)
    o["c_p"] = r["ml_CT_p"].transpose(0, 1, 3, 2, 4).reshape(2, 4, 512, 512).transpose(0, 1, 3, 2)
    o["c_s"] = r["ml_CT_s"].transpose(0, 1, 2, 4, 3, 5).reshape(2, 16, 4, 512, 512).transpose(0, 1, 2, 4, 3)
    o["n_p"] = r["ml_n_p"].transpose(0, 2, 3, 1).reshape(2, 4, 512)
    o["n_s"] = r["ml_n_s"].transpose(0, 2, 3, 4, 1).reshape(2, 16, 4, 512)
    o["m_p"] = r["ml_m_p"].reshape(2, 4)
    o["m_s"] = r["ml_m_s"].transpose(0, 2, 1)
    o["cv_p"] = r["ml_conv_p"].transpose(0, 3, 2, 1).reshape(2, 3, 2048)
    o["cv_s"] = r["ml_conv_s"].transpose(0, 3, 4, 2, 1).reshape(2, 16, 3, 2048)
    return o


_NC_CACHE = {}


def kernel(**inputs):
    inp = {k: np.asarray(v) for k, v in inputs.items()}
    if "nc" not in _NC_CACHE:
        _NC_CACHE["nc"] = build()
    nc = _NC_CACHE["nc"]
    S = prep_shared(inp)
    in_maps = []
    for i in range(8):
        m = dict(S); m.update(prep_core(inp, i)); in_maps.append(m)
    res = run_bass_kernel_spmd(nc, in_maps, core_ids=list(range(8)))
    po = [post_core(r) for r in res.results]
    cat0 = lambda k: np.ascontiguousarray(np.stack([p[k] for p in po], axis=0)).astype(np.float32)
    cat1 = lambda k: np.ascontiguousarray(np.stack([p[k] for p in po], axis=1)).astype(np.float32)
    cat1s = lambda k: np.ascontiguousarray(np.concatenate([p[k] for p in po], axis=1)).astype(np.float32)
    y_p = cat0("y_p")
    y_s = np.ascontiguousarray(np.concatenate([p["y_s"] for p in po], axis=0)).astype(np.float32)
    return (y_p, y_s, cat1("s_p"), cat1s("s_s"), cat1s("v_s"), cat1("c_p"), cat1s("c_s"), cat1("n_p"), cat1s("n_s"),
            cat1("m_p"), cat1s("m_s"), cat1("cv_p"), cat1s("cv_s"))
```

```python
import numpy as np
from contextlib import ExitStack
import concourse.bass as bass
import concourse.mybir as mybir
from concourse.bass_utils import run_bass_kernel_spmd

F32 = mybir.dt.float32
BF16 = mybir.dt.bfloat16
AF = mybir.ActivationFunctionType
ALU = mybir.AluOpType

D = 1024; KD = 8; TP = 2048; TS = 128; T = TP + TS; NSEQ = 17
DFF = 2816; NG = 11
NL = 4
EPS = 1e-6
NEG = -1.0e30
MLSTM_MODE = 3
DBG_ONLY_MLSTM = False
FRONT_STEPS = 9
SKIP_INTER = False
SAME_ENG_SYNC = True


class V:
    def __init__(self, buf, ap):
        self.buf = buf; self.ap = ap
    def __getitem__(self, idx):
        return V(self.buf, self.ap[idx])
    def rr(self, pat, **kw):
        return V(self.buf, self.ap.rearrange(pat, **kw))
    def bc(self, shape):
        return V(self.buf, self.ap.broadcast_to(list(shape)))
    def un(self, axis):
        return V(self.buf, self.ap.unsqueeze(axis))


class Buf:
    def __init__(self, t, name):
        self.t = t; self.name = name; self.w = None; self.r = {}
    def __getitem__(self, idx):
        return V(self, self.t[idx])
    @property
    def v(self):
        return V(self, self.t[:])


class Kern:
    def __init__(self, nc, es):
        self.nc = nc; self.es = es
        self.engs = {"pe": nc.tensor, "act": nc.scalar, "dve": nc.vector, "sp": nc.sync, "pool": nc.gpsimd}
        self.sems = {}; self.cnt = {}
        for e in ["pe", "act", "dve"]:
            self.sems[e] = es.enter_context(nc.semaphore("s_" + e)); self.cnt[e] = 0
        self.rings = {}
        for q, n in (("sp", 10), ("pool", 10)):
            keys = []
            for i in range(n):
                k = "%s_d%d" % (q, i)
                self.sems[k] = es.enter_context(nc.semaphore("s_" + k)); self.cnt[k] = 0
                keys.append(k)
            self.rings[q] = [keys, 0]
        self.known = {e: {} for e in self.engs}
        self.psb = []
        for i in range(8):
            t = es.enter_context(nc.psum_tensor("psb%d" % i, [128, 512], F32))
            self.psb.append(Buf(t, "psb%d" % i))
        self.psi = 0
        self.ninstr = 0

    def sb(self, name, shape, dt, es=None):
        self.uid = getattr(self, "uid", 0) + 1
        t = (es or self.es).enter_context(self.nc.sbuf_tensor("sb%d_%s" % (self.uid, name), list(shape), dt))
        return Buf(t, name)

    def ps(self):
        b = self.psb[self.psi % 6]; self.psi += 1
        return b

    def ps_pin(self, i):
        return self.psb[6 + i]

    def _emit(self, eng, fn, reads, writes, dma=False):
        waits = {}
        known = self.known[eng]
        def need(tok):
            if tok is None: return
            k, v = tok
            if k == eng and (eng == "pe" or not SAME_ENG_SYNC): return
            if known.get(k, 0) >= v: return
            if waits.get(k, 0) < v: waits[k] = v
        rb = []; wb = []
        for x in reads:
            if x is None or isinstance(x, (int, float)): continue
            b = x.buf if isinstance(x, V) else x
            if b is not None and b not in rb: rb.append(b)
        for x in writes:
            b = x.buf if isinstance(x, V) else x
            if b is not None and b not in wb: wb.append(b)
        for b in rb: need(b.w)
        for b in wb:
            need(b.w)
            for k, v in b.r.items(): need((k, v))
        if dma:
            keys, pos = self.rings[eng]
            k = keys[pos % len(keys)]; self.rings[eng][1] = pos + 1
            need((k, self.cnt[k]))
            self.cnt[k] += 16; tok = (k, self.cnt[k]); inc = 16
        else:
            self.cnt[eng] += 1; tok = (eng, self.cnt[eng]); inc = 1
        e = self.engs[eng]
        for k, v in waits.items():
            e.wait_ge(self.sems[k], v); known[k] = v
        ins = fn(e)
        ins.then_inc(self.sems[tok[0]], inc)
        self.ninstr += 1
        for b in rb:
            if b in wb: continue
            if b.r.get(tok[0], 0) < tok[1]: b.r[tok[0]] = tok[1]
        for b in wb:
            b.w = tok; b.r = {}
        return tok

    def barrier(self):
        for eng in self.engs:
            e = self.engs[eng]; known = self.known[eng]
            for k, v in self.cnt.items():
                if v > 0 and known.get(k, 0) < v:
                    e.wait_ge(self.sems[k], v); known[k] = v

    @staticmethod
    def _a(x):
        return x.ap if isinstance(x, V) else x

    def mm(self, out, lhsT, rhs, start=True, stop=True):
        a = self._a
        return self._emit("pe", lambda e: e.matmul(a(out), lhsT=a(lhsT), rhs=a(rhs), start=start, stop=stop), [lhsT, rhs], [out])

    def tr(self, out, in_, ident):
        a = self._a
        return self._emit("pe", lambda e: e.transpose(a(out), a(in_), a(ident)), [in_, ident], [out])

    def act(self, out, in_, func, bias=None, scale=None, accum=None):
        a = self._a
        kw = {}
        if bias is not None: kw["bias"] = a(bias)
        if scale is not None: kw["scale"] = a(scale)
        if accum is not None: kw["accum_out"] = a(accum)
        w = [out] + ([accum] if accum is not None else [])
        return self._emit("act", lambda e: e.activation(out=a(out), in_=a(in_), func=func, **kw), [in_, bias, scale], w)

    def tt(self, out, in0, in1, op, eng="dve"):
        a = self._a
        return self._emit(eng, lambda e: e.tensor_tensor(out=a(out), in0=a(in0), in1=a(in1), op=op), [in0, in1], [out])

    def ts(self, out, in0, s1, op0, s2=None, op1=None, eng="dve"):
        a = self._a
        if op1 is None:
            return self._emit(eng, lambda e: e.tensor_scalar(out=a(out), in0=a(in0), scalar1=a(s1), scalar2=None, op0=op0), [in0, s1], [out])
        return self._emit(eng, lambda e: e.tensor_scalar(out=a(out), in0=a(in0), scalar1=a(s1), scalar2=a(s2), op0=op0, op1=op1), [in0, s1, s2], [out])

    def stt(self, out, in0, scalar, in1, op0, op1, eng="dve"):
        a = self._a
        return self._emit(eng, lambda e: e.scalar_tensor_tensor(out=a(out), in0=a(in0), scalar=a(scalar), in1=a(in1), op0=op0, op1=op1), [in0, scalar, in1], [out])

    def cp(self, out, in_, eng="dve"):
        a = self._a
        return self._emit(eng, lambda e: e.tensor_copy(out=a(out), in_=a(in_)), [in_], [out])

    def memset(self, out, val, eng="dve"):
        a = self._a
        return self._emit(eng, lambda e: e.memset(a(out), val), [], [out])

    def scan(self, out, d0, d1, init, op0, op1):
        a = self._a
        return self._emit("dve", lambda e: e.tensor_tensor_scan(out=a(out), data0=a(d0), data1=a(d1), initial=a(init), op0=op0, op1=op1), [d0, d1, init], [out])

    def recip(self, out, in_):
        a = self._a
        return self._emit("dve", lambda e: e.reciprocal(out=a(out), in_=a(in_)), [in_], [out])

    def dma(self, out, in_, q="sp"):
        a = self._a
        r = [in_] if isinstance(in_, V) else []
        w = [out] if isinstance(out, V) else []
        return self._emit(q, lambda e: e.dma_start(out=a(out), in_=a(in_)), r, w, dma=True)

    def finish(self):
        self.barrier()


IN_SPECS = {}
OUT_SPECS = {}


def _specs():
    I = {}
    I["xT"] = [128, KD, T]
    I["cT"] = [128, KD, NSEQ]
    I["ada_w"] = [NL, 9, 128, KD, 1024]
    I["ada_b"] = [128, NL, 72]
    I["fada_w"] = [2, 128, KD, 1024]
    I["fada_b"] = [128, 16]
    I["ffn_norm"] = [128, NL, 2, KD]
    I["mix_norm"] = [128, NL, KD]
    I["final_norm"] = [128, KD]
    I["ffn_wg"] = [NL, 2, NG, 128, KD, 256]
    I["ffn_wu"] = [NL, 2, NG, 128, KD, 256]
    I["ffn_wd"] = [NL, 2, NG, 128, 2, 1024]
    I["ab_w_in"] = [2, 128, KD, 2576]
    I["ab_w_out"] = [2, 128, KD, 1024]
    I["gla_wa2"] = [2, 17, 256]
    I["gla_norm"] = [2, 128, 1]
    I["gmlp_norm"] = [2, 128, 128]
    I["gmlp_wT_p"] = [2, 128, 4, 128]
    I["gmlp_wT_s"] = [2, 128, 4, 128]
    I["gmlp_bs_p"] = [2, 128, 4, 128]
    I["gmlp_bs_s"] = [2, 128, 4, 128]
    I["consts"] = [128, 6, 128]
    I["gla_S"] = [2, 64, 16, 4, 128]
    I["ml_w_in"] = [2, 128, KD, 4096]
    I["ml_w_out"] = [2, 128, 16, 1024]
    I["ml_conv_w"] = [2, 128, 16, 4]
    I["ml_conv_b"] = [2, 128, 16]
    I["ml_bdq"] = [2, 128, 16, 128]
    I["ml_bdk"] = [2, 128, 16, 128]
    I["ml_bdv"] = [2, 128, 16, 128]
    I["ml_w_gates"] = [2, 128, 48, 8]
    I["ml_b_gates"] = [2, 128, 8]
    I["ml_norm"] = [2, 128, 16]
    I["ml_skip"] = [2, 128, 16]
    I["ml_CT"] = [2, 16, 4, 128, 4, 512]
    I["ml_n"] = [2, 128, 16, 4, 4]
    I["ml_m"] = [2, 4, 16]
    I["ml_conv"] = [2, 128, 16, 16, 3]
    O = {}
    O["yT"] = [128, KD, T]
    O["gla_S_p"] = [2, 64, 4, 128]
    O["gla_S_s"] = [2, 64, 16, 4, 128]
    O["gmlp_v_s"] = [2, 128, 512]
    O["ml_CT_p"] = [2, 4, 128, 4, 512]
    O["ml_CT_s"] = [2, 16, 4, 128, 4, 512]
    O["ml_n_p"] = [2, 128, 4, 4]
    O["ml_n_s"] = [2, 128, 16, 4, 4]
    O["ml_m_p"] = [2, 4, 1]
    O["ml_m_s"] = [2, 4, 16]
    O["ml_conv_p"] = [2, 128, 16, 3]
    O["ml_conv_s"] = [2, 128, 16, 16, 3]
    return I, O


IN_SPECS, OUT_SPECS = _specs()
C_TRI, C_TRI8, C_ID, C_NEG, C_NEG8, C_MISC = range(6)


def build(nlayers=NL, dbg=None):
    nc = bass.Bass("TRN2", target_bir_lowering=False)
    din = {n: nc.dram_tensor(n, s, F32, kind="ExternalInput").ap() for n, s in IN_SPECS.items()}
    dout = {n: nc.dram_tensor(n, s, F32, kind="ExternalOutput").ap() for n, s in OUT_SPECS.items()}
    with ExitStack() as es:
        K = Kern(nc, es)
        _program(nc, K, es, din, dout, nlayers)
        K.finish()
    return nc


def _program(nc, K, es, din, dout, nlayers):
    TILES = [(i * 512, 512) for i in range(4)] + [(TP, TS)]
    xT = [K.sb("xT%d" % i, [128, KD, n], F32) for i, (o, n) in enumerate(TILES)]
    consts = K.sb("consts", [128, 6, 128], F32)
    ones_bf = K.sb("ones_bf", [128, 128], BF16)
    ones_f = K.sb("ones_f", [128, 128], F32)
    negm = K.sb("negm", [128, 2, 128], F32)
    modT = K.sb("modT", [128, 72, NSEQ], F32)
    csT = K.sb("csT", [128, KD, NSEQ], BF16)
    ffn_norm = K.sb("ffn_norm", [128, NL, 2, KD], F32)
    mix_norm = K.sb("mix_norm", [128, NL, KD], F32)
    final_norm = K.sb("final_norm", [128, KD], F32)
    epsb = K.sb("epsb", [128, 1], F32)
    Avec = K.sb("Avec", [128, KD, NSEQ], F32)
    Gvec = K.sb("Gvec", [128, KD, NSEQ], F32)

    K.dma(consts.v, din["consts"])
    for i, (o, n) in enumerate(TILES):
        K.dma(xT[i].v, din["xT"][:, :, o:o + n])
    K.dma(ffn_norm.v, din["ffn_norm"]); K.dma(mix_norm.v, din["mix_norm"]); K.dma(final_norm.v, din["final_norm"])
    K.memset(ones_bf.v, 1.0); K.memset(ones_f.v, 1.0); K.memset(epsb.v, EPS)
    tri = consts[:, C_TRI, :]; tri8 = consts[:, C_TRI8, :]; ident = consts[:, C_ID, :]
    seqmask = consts[:, C_MISC, 0:16]
    K.ts(negm[:, 0, :], tri, -1.0, ALU.add, -NEG, ALU.mult)
    K.ts(negm[:, 1, :], tri8, -1.0, ALU.add, -NEG, ALU.mult)

    cTf = K.sb("cTf", [128, KD, NSEQ], F32)
    K.dma(cTf.v, din["cT"])
    K.act(csT.v, cTf.v, AF.Silu)

    def ada(l):
        with ExitStack() as ph:
            adab = K.sb("adab", [128, 72], F32, ph)
            wbuf = [K.sb("adaw%d" % i, [128, KD, 1024], BF16, ph) for i in range(2)]
            if l < NL:
                K.dma(adab.v, din["ada_b"][:, l, :])
                pieces = [(din["ada_w"][l, v], v * 8) for v in range(9)]
            else:
                K.dma(adab[:, 0:16], din["fada_b"])
                pieces = [(din["fada_w"][v], v * 8) for v in range(2)]
            for i, (src, off) in enumerate(pieces):
                wb = wbuf[i % 2]
                K.dma(wb.v, src, q="pool")
                ps = K.ps()
                for j in range(8):
                    for k in range(KD):
                        K.mm(ps[:, j * NSEQ:(j + 1) * NSEQ], wb[:, k, j * 128:(j + 1) * 128], csT[:, k, :], start=(k == 0), stop=(k == KD - 1))
                K.tt(modT[:, off:off + 8, :], ps[:, 0:8 * NSEQ].rr("p (j s) -> p j s", s=NSEQ),
                     adab[:, off:off + 8].un(2).bc([128, 8, NSEQ]), ALU.add)
            K.barrier()

    def mod(l, v):
        return modT[:, v * 8:v * 8 + 8, :]

    def prep_AG(gamma, sc, gate, gmul):
        K.ts(Avec.v, sc, 1.0, ALU.add)
        K.tt(Avec.v, Avec.v, gamma.un(2).bc([128, KD, NSEQ]), ALU.mult)
        if gate is not None:
            K.ts(Gvec.v, gate, gmul, ALU.mult)

    def seq_affine(out, in_, A, B, ti, k):
        if ti < 4:
            K.act(out, in_, AF.Identity, bias=B[:, k, 0:1], scale=A[:, k, 0:1])
        else:
            o3 = out.rr("p (s t) -> p s t", t=8); i3 = in_.rr("p (s t) -> p s t", t=8)
            K.tt(o3, i3, A[:, k, 1:17].un(2).bc([128, 16, 8]), ALU.mult)
            K.tt(o3, o3, B[:, k, 1:17].un(2).bc([128, 16, 8]), ALU.add)

    def resid_add(ti, d, cols, ps_v, G):
        xv = xT[ti][:, d, cols]
        if ti < 4:
            K.stt(xv, ps_v, G[:, d, 0:1], xv, ALU.mult, ALU.add)
        else:
            tmp = rs_tmp.v
            K.tt(tmp.rr("p (s t) -> p s t", t=8), ps_v.rr("p (s t) -> p s t", t=8), G[:, d, 1:17].un(2).bc([128, 16, 8]), ALU.mult)
            K.tt(xv, xv, tmp, ALU.add)

    rs_tmp = K.sb("rs_tmp", [128, 128], F32)
    rs_tmp4 = K.sb("rs_tmp4", [128, 4, 128], F32)

    def resid_add4(ti, d0, cols, ps_v, G):
        xv = xT[ti][:, d0:d0 + 4, cols]
        if ti < 4:
            K.tt(rs_tmp4.v, ps_v, G[:, d0:d0 + 4, 0:1].bc([128, 4, 128]), ALU.mult)
        else:
            K.tt(rs_tmp4.v.rr("p d (s t) -> p d s t", t=8), ps_v.rr("p d (s t) -> p d s t", t=8),
                 G[:, d0:d0 + 4, 1:17].un(3).bc([128, 4, 16, 8]), ALU.mult)
        K.tt(xv, xv, rs_tmp4.v, ALU.add)

    NB = {}

    def norm_bufs(es_, n):
        NB["sq"] = K.sb("nrm_sq", [128, KD, n], BF16, es_)
        NB["rstd"] = K.sb("nrm_rstd", [128, n], F32, es_)
        NB["ntmp"] = K.sb("nrm_tmp", [128, n], F32, es_)

    def rms_rstd(ps_ss, n, dim, out):
        K.act(out[:, 0:n], ps_ss[:, 0:n], AF.Ln, bias=epsb.v, scale=1.0 / dim)
        K.act(out[:, 0:n], out[:, 0:n], AF.Exp, scale=-0.5)

    def norm_tile(ti, hdst, B):
        o, n = TILES[ti]
        sq = NB["sq"]; rstd = NB["rstd"]; ntmp = NB["ntmp"]
        K.act(sq[:, :, 0:n], xT[ti].v, AF.Square)
        ps = K.ps()
        for k in range(KD):
            K.mm(ps[:, 0:n], ones_bf.v, sq[:, k, 0:n], start=(k == 0), stop=(k == KD - 1))
        rms_rstd(ps, n, D, rstd)
        for k in range(KD):
            K.tt(ntmp[:, 0:n], xT[ti][:, k, :], rstd[:, 0:n], ALU.mult)
            seq_affine(hdst[:, k, :], ntmp[:, 0:n], Avec, B, ti, k)

    def ffn(l, w):
        with ExitStack() as ph:
            hT = [K.sb("ffn_h%d" % i, [128, KD, n], BF16, ph) for i, (o, n) in enumerate(TILES)]
            wg = [K.sb("ffn_wg%d" % i, [128, KD, 256], BF16, ph) for i in range(2)]
            wu = [K.sb("ffn_wu%d" % i, [128, KD, 256], BF16, ph) for i in range(2)]
            wd = [K.sb("ffn_wd%d" % i, [128, 2, 1024], BF16, ph) for i in range(2)]
            hid = [K.sb("ffn_hid%d" % i, [128, 2, 512], BF16, ph) for i in range(2)]
            sg = K.sb("ffn_sg", [128, 512], F32, ph)
            prep_AG(ffn_norm[:, l, w, :], mod(l, 1 if w == 0 else 7), mod(l, 2 if w == 0 else 8), 0.5)
            B = mod(l, 0 if w == 0 else 6)
            with ExitStack() as nb:
                norm_bufs(nb, 512)
                for ti in range(5):
                    norm_tile(ti, hT[ti].v, B)
                K.barrier()
            hi = 0
            for g in range(NG):
                b = g % 2
                K.dma(wg[b].v, din["ffn_wg"][l, w, g], q="pool")
                K.dma(wu[b].v, din["ffn_wu"][l, w, g], q="pool")
                K.dma(wd[b].v, din["ffn_wd"][l, w, g], q="pool")
                for ti, (o, n) in enumerate(TILES):
                    hb = hid[hi % 2]; hi += 1
                    for c in range(2):
                        pg = K.ps(); pu = K.ps()
                        for k in range(KD):
                            K.mm(pg[:, 0:n], wg[b][:, k, c * 128:(c + 1) * 128], hT[ti][:, k, :], start=(k == 0), stop=(k == KD - 1))
                        for k in range(KD):
                            K.mm(pu[:, 0:n], wu[b][:, k, c * 128:(c + 1) * 128], hT[ti][:, k, :], start=(k == 0), stop=(k == KD - 1))
                        K.act(sg[:, 0:n], pg[:, 0:n], AF.Silu)
                        K.tt(hb[:, c, 0:n], sg[:, 0:n], pu[:, 0:n], ALU.mult)
                    for d in range(KD):
                        py = K.ps()
                        for c in range(2):
                            K.mm(py[:, 0:n], wd[b][:, c, d * 128:(d + 1) * 128], hb[:, c, 0:n], start=(c == 0), stop=(c == 1))
                        resid_add(ti, d, slice(0, n), py[:, 0:n], Gvec)
            K.barrier()

    def ab_mixer(l):
        e = l // 2
        with ExitStack() as ph:
            w_in = K.sb("ab_win", [128, KD, 2576], BF16, ph)
            w_out = K.sb("ab_wout", [128, KD, 1024], BF16, ph)
            wa2 = K.sb("ab_wa2", [17, 256], F32, ph)
            gnorm = K.sb("ab_gn", [128, 1], F32, ph)
            vnB = K.sb("ab_vn", [128, 128], F32, ph)
            wTp = K.sb("ab_wTp", [128, 4, 128], F32, ph); wTs = K.sb("ab_wTs", [128, 4, 128], F32, ph)
            wTpb = K.sb("ab_wTpb", [128, 4, 128], BF16, ph); wTsb = K.sb("ab_wTsb", [128, 4, 128], BF16, ph)
            bsp = K.sb("ab_bsp", [128, 4, 128], F32, ph); bss = K.sb("ab_bss", [128, 4, 128], F32, ph)
            hT = K.sb("ab_h", [128, KD, 128], BF16, ph)
            qT = K.sb("ab_q", [64, 4, 128], F32, ph); kT = K.sb("ab_k", [64, 4, 128], F32, ph)
            qb = K.sb("ab_qb", [64, 4, 128], BF16, ph); kb = K.sb("ab_kb", [64, 4, 128], BF16, ph)
            kd = K.sb("ab_kd", [64, 4, 128], F32, ph); kdt = K.sb("ab_kdt", [128, 4, 64], BF16, ph)
            a_aug = K.sb("ab_aaug", [17, 128], F32, ph)
            la = K.sb("ab_la", [128, 256], F32, ph)
            eb = K.sb("ab_eb", [64, 4, 128], F32, ph); enb = K.sb("ab_enb", [64, 4, 128], F32, ph)
            vtok = K.sb("ab_vtok", [128, 512], BF16, ph)
            sgT = K.sb("ab_sg", [128, 4, 128], F32, ph)
            uT = K.sb("ab_u", [128, 4, 128], F32, ph)
            vbn = K.sb("ab_vbn", [128, 512], F32, ph); vbnb = K.sb("ab_vbnb", [128, 512], BF16, ph)
            ssq = K.sb("ab_ssq", [128, 4], F32, ph); vjunk = K.sb("ab_vj", [128, 128], F32, ph)
            sT = K.sb("ab_sT", [128, 128], BF16, ph)
            S = K.sb("ab_S", [64, 4, 128], F32, ph); Sb = K.sb("ab_Sb", [64, 4, 128], BF16, ph)
            Ss = K.sb("ab_Ss", [64, 4, 128], F32, ph); Ssn = K.sb("ab_Ssn", [64, 4, 128], F32, ph)
            Ssb = K.sb("ab_Ssb", [64, 4, 128], BF16, ph)
            R = K.sb("ab_R", [128, 4, 128], BF16, ph)
            ebl = K.sb("ab_ebl", [64, 4, 16], F32, ph)
            oT = K.sb("ab_o", [128, 4, 128], F32, ph); osq = K.sb("ab_osq", [128, 4, 128], BF16, ph)
            orstd = K.sb("ab_orstd", [128, 4, 128], F32, ph)
            mix = K.sb("ab_mix", [128, KD, 128], BF16, ph)
            ztmp = K.sb("ab_ztmp", [128, 128], F32, ph)
            K.dma(w_in.v, din["ab_w_in"][e], q="pool"); K.dma(w_out.v, din["ab_w_out"][e], q="pool")
            K.dma(wa2.v, din["gla_wa2"][e]); K.dma(gnorm.v, din["gla_norm"][e]); K.dma(vnB.v, din["gmlp_norm"][e])
            K.dma(wTp.v, din["gmlp_wT_p"][e]); K.dma(wTs.v, din["gmlp_wT_s"][e])
            K.dma(bsp.v, din["gmlp_bs_p"][e]); K.dma(bss.v, din["gmlp_bs_s"][e])
            K.tt(wTpb.v, wTp.v, tri.un(1).bc([128, 4, 128]), ALU.mult)
            K.tt(wTsb.v, wTs.v, tri8.un(1).bc([128, 4, 128]), ALU.mult)
            K.memset(a_aug.v, 1.0)
            K.memset(S.v, 0.0); K.memset(Sb.v, 0.0)
            prep_AG(mix_norm[:, l, :], mod(l, 4), mod(l, 5), 1.0)
            B = mod(l, 3)
            norm_bufs(ph, 128)
            sq = NB["sq"]; rstd = NB["rstd"]; ntmp = NB["ntmp"]
            for ci in range(17):
                samp = (ci == 16)
                ti = ci // 4 if not samp else 4
                c0 = (ci % 4) * 128 if not samp else 0
                cols = slice(c0, c0 + 128)
                mask = tri8 if samp else tri
                K.act(sq[:, :, 0:128], xT[ti][:, :, cols], AF.Square)
                ps = K.ps()
                for k in range(KD):
                    K.mm(ps[:, 0:128], ones_bf.v, sq[:, k, 0:128], start=(k == 0), stop=(k == KD - 1))
                rms_rstd(ps, 128, D, rstd)
                for k in range(KD):
                    K.tt(ntmp[:, 0:128], xT[ti][:, k, cols], rstd[:, 0:128], ALU.mult)
                    seq_affine(hT[:, k, :], ntmp[:, 0:128], Avec, B, ti, k)
                def proj_f(col0, m, dst_ps):
                    for k in range(KD):
                        K.mm(dst_ps, w_in[:, k, col0:col0 + m], hT[:, k, :], start=(k == 0), stop=(k == KD - 1))
                ps = K.ps()
                for h in range(4):
                    proj_f(h * 64, 64, ps[0:64, h * 128:(h + 1) * 128])
                K.act(qT.v, ps[0:64, :].rr("p (h t) -> p h t", t=128), AF.Copy, scale=64 ** -0.5)
                ps = K.ps()
                for h in range(4):
                    proj_f(256 + h * 64, 64, ps[0:64, h * 128:(h + 1) * 128])
                K.cp(kT.v, ps[0:64, :].rr("p (h t) -> p h t", t=128))
                ps = K.ps()
                proj_f(1536, 16, ps[0:16, 0:128])
                K.cp(a_aug[0:16, :], ps[0:16, 0:128])
                ps = K.ps()
                for k in range(KD):
                    K.mm(ps[:, 0:512], hT[:, k, :], w_in[:, k, 512:1024], start=(k == 0), stop=(k == KD - 1))
                K.act(vtok.v, ps[:, 0:512], AF.Copy)
                ps = K.ps()
                for j in range(4):
                    proj_f(1024 + j * 128, 128, ps[:, j * 128:(j + 1) * 128])
                K.act(sgT.v, ps[:, :].rr("p (h t) -> p h t", t=128), AF.Silu)
                ps = K.ps()
                for j in range(4):
                    proj_f(1552 + j * 128, 128, ps[:, j * 128:(j + 1) * 128])
                K.cp(uT.v, ps[:, :].rr("p (h t) -> p h t", t=128))
                ps = K.ps()
                for k in range(KD):
                    K.mm(ps[:, 0:512], hT[:, k, :], w_in[:, k, 2576 - 512:2576], start=(k == 0), stop=(k == KD - 1))
                for g in range(4):
                    K.act(vjunk.v, ps[:, g * 128:(g + 1) * 128], AF.Square, accum=ssq[:, g:g + 1])
                K.act(ssq.v, ssq.v, AF.Ln, bias=epsb.v, scale=1.0 / 128)
                K.act(ssq.v, ssq.v, AF.Exp, scale=-0.5)
                for g in range(4):
                    K.stt(vbn[:, g * 128:(g + 1) * 128], ps[:, g * 128:(g + 1) * 128], ssq[:, g:g + 1], vnB.v, ALU.mult, ALU.mult)
                K.cp(vbnb.v, vbn.v)
                if samp:
                    K.dma(dout["gmlp_v_s"][e], vbn.v)
                ps = K.ps()
                K.mm(ps[:, 0:256], a_aug.v, wa2.v)
                K.act(la.v, ps[:, 0:256], AF.Exp, scale=-1.0)
                K.act(la.v, la.v, AF.Ln, bias=1.0)
                ps = K.ps()
                for h in range(4):
                    K.mm(ps[0:64, h * 128:(h + 1) * 128], la[:, h * 64:(h + 1) * 64], mask)
                bp = ps[0:64, :].rr("p (h t) -> p h t", t=128)
                K.act(eb.v, bp, AF.Exp, scale=-1.0 / 16)
                K.act(enb.v, bp, AF.Exp, scale=1.0 / 16)
                K.tt(qb.v, qT.v, eb.v, ALU.mult)
                K.tt(kb.v, kT.v, enb.v, ALU.mult)
                if not samp:
                    K.tt(kd.v, kT.v, enb.v, ALU.mult)
                    for h in range(4):
                        K.ts(kd[:, h, :], kd[:, h, :], eb[:, h, 127:128], ALU.mult)
                else:
                    K.cp(ebl.v, eb.v.rr("p h (s t) -> p h s t", t=8)[:, :, :, 7])
                    K.tt(kd.v, kT.v, enb.v, ALU.mult)
                    K.tt(kd.v.rr("p h (s t) -> p h s t", t=8), kd.v.rr("p h (s t) -> p h s t", t=8),
                         ebl.v.un(3).bc([64, 4, 16, 8]), ALU.mult)
                ps = K.ps()
                for h in range(4):
                    K.tr(ps[:, h * 64:(h + 1) * 64], kd[:, h, :], ident[0:64, 0:64])
                K.cp(kdt.v, ps[:, 0:256].rr("p (h d) -> p h d", d=64))
                for h in range(4):
                    vh = vtok[:, h * 128:(h + 1) * 128]
                    ps = K.ps()
                    K.mm(ps[:, 0:128], kb[:, h, :], qb[:, h, :])
                    K.tt(sT.v, ps[:, 0:128], mask, ALU.mult)
                    po = K.ps_pin(0)
                    if not samp:
                        K.mm(po[:, 0:128], vh, sT.v, start=True, stop=False)
                        K.mm(po[:, 0:128], Sb[:, h, :], qb[:, h, :], start=False, stop=True)
                        K.cp(oT[:, h, :], po[:, 0:128])
                        pk = K.ps()
                        K.mm(pk[0:64, 0:128], kdt[:, h, :], vh)
                        K.stt(S[:, h, :], S[:, h, :], eb[:, h, 127:128], pk[0:64, 0:128], ALU.mult, ALU.add)
                        K.act(Sb[:, h, :], S[:, h, :], AF.Copy)
                    else:
                        K.mm(po[:, 0:128], vh, sT.v, start=True, stop=False)
                        for q4 in range(4):
                            sl = slice(q4 * 4, q4 * 4 + 4)
                            K.dma(Ss.v, din["gla_S"][e][:, sl, h, :])
                            K.cp(Ssb.v, Ss.v)
                            for s4 in range(4):
                                s_ = q4 * 4 + s4
                                K.mm(po[:, s_ * 8:(s_ + 1) * 8], Ssb[:, s4, :], qb[:, h, s_ * 8:(s_ + 1) * 8], start=False, stop=(s_ == 15))
                            K.tt(R.v, vh.un(1).bc([128, 4, 128]), seqmask[:, sl].un(2).bc([128, 4, 128]), ALU.mult)
                            pk = K.ps()
                            K.mm(pk[0:64, 0:512], kdt[:, h, :], R.v.rr("p s v -> p (s v)"))
                            K.tt(Ssn.v, Ss.v, ebl[:, h, sl].un(2).bc([64, 4, 128]), ALU.mult)
                            K.tt(Ssn.v, Ssn.v, pk[0:64, 0:512].rr("p (s v) -> p s v", v=128), ALU.add)
                            K.dma(dout["gla_S_s"][e][:, sl, h, :], Ssn.v)
                        K.cp(oT[:, h, :], po[:, 0:128])
                if ci == 15:
                    K.dma(dout["gla_S_p"][e], S.v)
                K.act(osq.v, oT.v, AF.Square)
                ps = K.ps()
                for h in range(4):
                    K.mm(ps[:, h * 128:(h + 1) * 128], ones_bf.v, osq[:, h, :])
                K.act(orstd.v, ps[:, :].rr("p (h t) -> p h t", t=128), AF.Ln, bias=epsb.v, scale=1.0 / 128)
                K.act(orstd.v, orstd.v, AF.Exp, scale=-0.5)
                K.tt(oT.v, oT.v, orstd.v, ALU.mult)
                K.stt(mix[:, 0:4, :], oT.v, gnorm.v, sgT.v, ALU.mult, ALU.mult)
                wTb = wTsb if samp else wTpb
                bsB = bss if samp else bsp
                for g in range(4):
                    ps = K.ps()
                    K.mm(ps[:, 0:128], vbnb[:, g * 128:(g + 1) * 128], wTb[:, g, :])
                    K.tt(ztmp.v, ps[:, 0:128], bsB[:, g, :], ALU.add)
                    K.tt(mix[:, 4 + g, :], ztmp.v, uT[:, g, :], ALU.mult)
                for half in range(2):
                    ps = K.ps()
                    for dd in range(4):
                        d = half * 4 + dd
                        for k in range(KD):
                            K.mm(ps[:, dd * 128:(dd + 1) * 128], w_out[:, k, d * 128:(d + 1) * 128], mix[:, k, :], start=(k == 0), stop=(k == KD - 1))
                    resid_add4(ti, half * 4, cols, ps[:, :].rr("p (d t) -> p d t", t=128), Gvec)
            K.barrier()

    def mlstm(l):
        o_ = l // 2
        with ExitStack() as ph:
            hT = K.sb("ml_h", [128, KD, T], BF16, ph)
            prep_AG(mix_norm[:, l, :], mod(l, 4), mod(l, 5), 1.0)
            B = mod(l, 3)
            with ExitStack() as nb:
                norm_bufs(nb, 512)
                for ti in range(5):
                    norm_tile(ti, hT[:, :, TILES[ti][0]:TILES[ti][0] + TILES[ti][1]], B)
                K.barrier()
            cw = K.sb("ml_cw", [128, 16, 4], F32, ph); cb = K.sb("ml_cb", [128, 16], F32, ph)
            gn = K.sb("ml_gn", [128, 16], F32, ph); skp = K.sb("ml_skip", [128, 16], F32, ph)
            bd = {n: K.sb("ml_" + n, [128, 4, 128], BF16, ph) for n in ("bdq", "bdk", "bdv")}
            wgt = K.sb("ml_wgt", [128, 48, 8], BF16, ph); bg = K.sb("ml_bg", [128, 8], F32, ph)
            mst = K.sb("ml_mst", [4, 16], F32, ph)
            gacc = K.sb("ml_gacc", [128, 17, 8], F32, ph)
            colsAll = K.sb("ml_cols", [128, 17, 20], F32, ph)
            decB = K.sb("ml_decB", [128, 4, 32], F32, ph)
            sel = K.sb("ml_sel", [4, 4, 128], F32, ph)
            K.dma(cw.v, din["ml_conv_w"][o_]); K.dma(cb.v, din["ml_conv_b"][o_])
            K.dma(gn.v, din["ml_norm"][o_]); K.dma(skp.v, din["ml_skip"][o_])
            K.dma(wgt.v, din["ml_w_gates"][o_], q="pool"); K.dma(bg.v, din["ml_b_gates"][o_])
            K.dma(mst.v, din["ml_m"][o_])
            K.cp(sel.v, ident[0:4, 0:4].un(2).bc([4, 4, 128]))
            xmes = K.sb("ml_xmes", [128, 4, 16, 16], F32, ph)

            def front_set(es_, tag):
                return {"xme": K.sb("ml_xme" + tag, [128, 4, 136], F32, es_), "xmb": K.sb("ml_xmb" + tag, [128, 4, 128], BF16, es_),
                        "acc": [K.sb("ml_acc%d" % j + tag, [128, 128], F32, es_) for j in range(4)], "xc": K.sb("ml_xc" + tag, [128, 4, 128], BF16, es_),
                        "qT": K.sb("ml_q" + tag, [128, 4, 128], BF16, es_), "kT": K.sb("ml_k" + tag, [128, 4, 128], BF16, es_),
                        "vT": K.sb("ml_v" + tag, [128, 4, 128], BF16, es_)}
            fb0 = front_set(ph, "0")
            w_x = K.sb("ml_wx", [128, KD, 512], BF16, ph)
            cvin = K.sb("ml_cvin", [128, 4, 16, 3], F32, ph); cvout = K.sb("ml_cvout", [128, 4, 16, 3], F32, ph)
            cvp = K.sb("ml_cvp", [128, 4, 3], F32, ph)

            def load_head(h):
                K.dma(w_x.v, din["ml_w_in"][o_][:, :, h * 512:(h + 1) * 512], q="pool")
                for n in bd: K.dma(bd[n].v, din["ml_" + n][o_][:, 4 * h:4 * h + 4, :], q="pool")
                K.dma(cvin.v, din["ml_conv"][o_][:, 4 * h:4 * h + 4, :, :])
                K.cp(xmes[:, :, :, 5:8], cvin.v)
                K.memset(fb0["xme"][:, :, 0:8], 0.0)

            def front(ci, h, need_v_T, fb, fbn):
                xme = fb["xme"]; xmb = fb["xmb"]; acc = fb["acc"]; xc = fb["xc"]; qT = fb["qT"]; kT = fb["kT"]; vT = fb["vT"]
                samp = (ci == 16)
                t0 = ci * 128
                ps = K.ps()
                for j in range(4):
                    for k in range(KD):
                        K.mm(ps[:, j * 128:(j + 1) * 128], w_x[:, k, j * 128:(j + 1) * 128], hT[:, k, t0:t0 + 128], start=(k == 0), stop=(k == KD - 1))
                if FRONT_STEPS == 0:
                    K.cp(xmb.v, ps[:, :].rr("p (f t) -> p f t", t=128)); return
                if not samp:
                    K.act(xme[:, :, 8:136], ps[:, :].rr("p (f t) -> p f t", t=128), AF.Copy)
                else:
                    K.act(xmes[:, :, :, 8:16], ps[:, :].rr("p (f s t) -> p f s t", t=8, s=16), AF.Copy)
                if not samp:
                    K.cp(xmb.v, xme[:, :, 8:136])
                else:
                    K.cp(xmb.v.rr("p f (s t) -> p f s t", t=8), xmes[:, :, :, 8:16])
                if FRONT_STEPS < 2: return
                for tap in range(4):
                    for j in range(4):
                        fc = 4 * h + j
                        if not samp:
                            src = xme[:, j, 5 + tap:5 + tap + 128]; a_ = acc[j].v
                        else:
                            src = xmes[:, j, :, 5 + tap:5 + tap + 8]; a_ = acc[j].v.rr("p (s t) -> p s t", t=8)
                        if tap == 0:
                            K.ts(a_, src, cw[:, fc, 0:1], ALU.mult)
                        else:
                            K.stt(a_, src, cw[:, fc, tap:tap + 1], a_, ALU.mult, ALU.add)
                for j in range(4):
                    fc = 4 * h + j
                    K.act(xc[:, j, :], acc[j].v, AF.Silu, bias=cb[:, fc:fc + 1])
                if FRONT_STEPS < 4: return
                for nm, src, dst in (("bdq", xc, qT), ("bdk", xc, kT), ("bdv", xmb, vT)):
                    if nm == "bdv" and not need_v_T: continue
                    ps = K.ps()
                    for j in range(4):
                        K.mm(ps[:, j * 128:(j + 1) * 128], bd[nm][:, j, :], src[:, j, :])
                    K.act(dst.v, ps[:, :].rr("p (f t) -> p f t", t=128), AF.Copy)
                if FRONT_STEPS < 5: return
                if not samp:
                    if ci == 15 and need_v_T:
                        K.cp(cvp.v, xme[:, :, 133:136])
                        K.dma(dout["ml_conv_p"][o_][:, 4 * h:4 * h + 4, :], cvp.v)
                    K.cp(fbn["xme"][:, :, 5:8], xme[:, :, 133:136])
                elif need_v_T:
                    K.cp(cvout.v, xmes[:, :, :, 13:16])
                    K.dma(dout["ml_conv_s"][o_][:, 4 * h:4 * h + 4, :, :], cvout.v)

            if MLSTM_MODE == 10:
                K.barrier(); return
            p1a = ExitStack()
            fb1 = front_set(p1a, "1")
            fbs = [fb0, fb1]
            for h in range(4):
                load_head(h)
                if MLSTM_MODE == 12: continue
                for ci in (range(17) if MLSTM_MODE != 13 else range(16)):
                    fb = fbs[ci % 2]
                    front(ci, h, True, fb, fbs[(ci + 1) % 2])
                    if MLSTM_MODE in (11, 13): continue
                    ps = K.ps()
                    i = 0
                    for part, src in enumerate((fb["qT"], fb["kT"], fb["vT"])):
                        for j in range(4):
                            K.mm(ps[:, 0:8], src[:, j, :], wgt[:, part * 16 + 4 * h + j, :], start=(i == 0), stop=(i == 11)); i += 1
                    K.tt(gacc[:, ci, :], ps[:, 0:8], bg.v if h == 0 else gacc[:, ci, :], ALU.add)
            K.barrier()
            p1a.close()
            if MLSTM_MODE in (1, 11, 12, 13):
                K.barrier(); return
            K.act(gacc[:, :, 4:8], gacc[:, :, 4:8], AF.Exp, scale=-1.0)
            K.act(gacc[:, :, 4:8], gacc[:, :, 4:8], AF.Ln, bias=1.0)
            with ExitStack() as p1:
                R32 = {n: K.sb("ml_r_" + n, [32, T], F32, p1) for n in ("ig", "lf", "m", "F", "wr")}
                for n in R32: K.memset(R32[n].v, 0.0)
                R_ = {n: R32[n][0:4, :] for n in R32}
                mprev = K.sb("ml_mprev", [4, 32], F32, p1); rend = K.sb("ml_rend", [4, 32], F32, p1); fst = K.sb("ml_fst", [4, 16], F32, p1)
                dec = K.sb("ml_dec", [4, 32], F32, p1)
                for ci in range(17):
                    ps = K.ps()
                    K.tr(ps[0:4, 0:128], gacc[:, ci, 0:4], ident)
                    K.tr(ps[0:4, 128:256], gacc[:, ci, 4:8], ident)
                    K.cp(R_["ig"][:, ci * 128:(ci + 1) * 128], ps[0:4, 0:128])
                    K.ts(R_["lf"][:, ci * 128:(ci + 1) * 128], ps[0:4, 128:256], -1.0, ALU.mult)
                K.scan(R_["m"][:, 0:TP], R_["lf"][:, 0:TP], R_["ig"][:, 0:TP], 0.0, ALU.add, ALU.max)
                K.scan(R_["F"][:, 0:TP], R_["lf"][:, 0:TP], R_["lf"][:, 0:TP], 0.0, ALU.add, ALU.min)
                for s in range(16):
                    sl = slice(TP + s * 8, TP + s * 8 + 8)
                    K.scan(R_["m"][:, sl], R_["lf"][:, sl], R_["ig"][:, sl], mst[:, s:s + 1], ALU.add, ALU.max)
                    K.scan(R_["F"][:, sl], R_["lf"][:, sl], R_["lf"][:, sl], 0.0, ALU.add, ALU.min)
                seg = lambda n: (R_[n][:, 0:TP].rr("p (c t) -> p c t", t=128), R_[n][:, TP:T].rr("p (c t) -> p c t", t=8))
                Fp = seg("F")[0]; mp = seg("m")[0]
                K.memset(fst[:, 0:1], 0.0); K.memset(mprev[:, 0:1], 0.0)
                K.cp(fst[:, 1:16], Fp[:, 0:15, 127]); K.cp(mprev[:, 1:16], mp[:, 0:15, 127])
                K.cp(mprev[:, 16:32], mst.v)
                K.tt(Fp, Fp, fst.v.un(2).bc([4, 16, 128]), ALU.subtract)
                K.dma(dout["ml_m_p"][o_], R_["m"][:, TP - 1:TP])
                msout = K.sb("ml_msout", [4, 16], F32, p1)
                K.cp(msout.v, seg("m")[1][:, :, 7])
                K.dma(dout["ml_m_s"][o_], msout.v)
                K.tt(R_["ig"], R_["ig"], R_["F"], ALU.subtract)
                K.tt(R_["F"], R_["F"], R_["m"], ALU.subtract)
                K.act(R_["lf"], R_["m"], AF.Exp, scale=-1.0)
                R_["wi"] = R_["m"]; R32["wi"] = R32["m"]
                K.cp(rend[:, 0:16], seg("F")[0][:, :, 127]); K.cp(rend[:, 16:32], seg("F")[1][:, :, 7])
                for pi, (off, L) in enumerate(((0, 128), (16, 8))):
                    K.tt(seg("wi")[pi], seg("F")[pi], mprev[:, off:off + 16].un(2).bc([4, 16, L]), ALU.add)
                    K.tt(seg("wr")[pi], seg("ig")[pi], rend[:, off:off + 16].un(2).bc([4, 16, L]), ALU.add)
                K.act(R_["wi"], R_["wi"], AF.Exp)
                K.act(R_["wr"], R_["wr"], AF.Exp)
                K.ts(R_["wr"], R_["wr"], 512 ** -0.5, ALU.mult)
                K.cp(dec[:, 0:16], seg("wi")[0][:, :, 127]); K.cp(dec[:, 16:32], seg("wi")[1][:, :, 7])
                for ci in range(17):
                    ps = K.ps()
                    for i, n in enumerate(("ig", "wr", "F", "wi", "lf")):
                        K.tr(ps[:, i * 32:(i + 1) * 32], R32[n][:, ci * 128:(ci + 1) * 128], ident[0:32, 0:32])
                    K.cp(colsAll[:, ci, :].rr("p (i f) -> p i f", f=4), ps[:, 0:160].rr("p (i f) -> p i f", f=32)[:, :, 0:4])
                K.barrier()
            if MLSTM_MODE == 2:
                K.barrier(); return
            with ExitStack() as p2:
                w_z = K.sb("ml_wz", [128, KD, 512], BF16, p2)
                w_o = K.sb("ml_wo", [128, 4, 1024], BF16, p2)
                CT = K.sb("ml_CT", [128, 4, 512], F32, p2); CTb = K.sb("ml_CTb", [128, 4, 512], BF16, p2)
                CT2 = K.sb("ml_CT2", [128, 4, 512], F32, p2)
                nS = K.sb("ml_nS", [128, 16, 4, 4], F32, p2); nSn = K.sb("ml_nSn", [128, 16, 4, 4], F32, p2)
                nP = K.sb("ml_nP", [128, 4, 4], F32, p2)
                nB = K.sb("ml_nB", [128, 4, 128], BF16, p2)
                ktok = K.sb("ml_ktok", [128, 512], BF16, p2); vtok = K.sb("ml_vtok", [128, 512], BF16, p2)
                vw = K.sb("ml_vw", [128, 512], BF16, p2); km = K.sb("ml_km", [128, 512], BF16, p2)
                wrb = K.sb("ml_wrb", [128, 4], BF16, p2)
                diag = [K.sb("ml_diag%d" % i, [128, 128], F32, p2) for i in range(3)]
                bcs = K.sb("ml_bcs", [128, 3, 128], F32, p2)
                arg = K.sb("ml_arg", [128, 128], F32, p2); sT = K.sb("ml_sT", [128, 128], BF16, p2)
                qw = K.sb("ml_qw", [128, 4, 128], BF16, p2)
                hden = K.sb("ml_hden", [128, 128], F32, p2)
                hh = K.sb("ml_hh", [128, 4, 128], F32, p2); hsq = K.sb("ml_hsq", [128, 4, 128], F32, p2)
                mean = K.sb("ml_mean", [128, 128], F32, p2); var = K.sb("ml_var", [128, 128], F32, p2)
                sz = K.sb("ml_sz", [128, 4, 128], BF16, p2); pre = K.sb("ml_pre", [128, 4, 128], BF16, p2)
                skx = K.sb("ml_skx", [128, 4, 128], F32, p2)
                interN = hsq; interD = mean
                K.dma(nS.v, din["ml_n"][o_])
                K.memset(nP.v, 0.0)
                for h in range(4):
                    load_head(h)
                    K.dma(w_z.v, din["ml_w_in"][o_][:, :, 2048 + h * 512:2048 + (h + 1) * 512], q="pool")
                    K.dma(w_o.v, din["ml_w_out"][o_][:, 4 * h:4 * h + 4, :], q="pool")
                    K.memset(CT.v, 0.0); K.memset(CTb.v, 0.0); K.memset(nB.v, 0.0)
                    for ci in range(17):
                        samp = (ci == 16)
                        ti = ci // 4 if not samp else 4
                        c0 = (ci % 4) * 128 if not samp else 0
                        cols = slice(c0, c0 + 128)
                        tsl = slice(ci * 128, ci * 128 + 128)
                        front(ci, h, False, fb0, fb0)
                        xc = fb0["xc"]; xmb = fb0["xmb"]; qT = fb0["qT"]; kT = fb0["kT"]
                        for nm, src, dst in (("bdk", xc, ktok), ("bdv", xmb, vtok)):
                            ps = K.ps()
                            for j in range(4):
                                K.mm(ps[:, j * 128:(j + 1) * 128], src[:, j, :], bd[nm][:, j, :])
                            K.act(dst.v, ps[:, :], AF.Copy)
                        gcol = colsAll[:, ci, h:h + 1]; wrcol = colsAll[:, ci, 4 + h:5 + h]
                        K.cp(wrb.v, colsAll[:, ci, 4:8])
                        ps = K.ps()
                        for i in range(3):
                            K.ts(diag[i].v, ident, colsAll[:, ci, 8 + 4 * i + h:9 + 4 * i + h], ALU.mult)
                        for i in range(3):
                            K.mm(ps[:, i * 128:(i + 1) * 128], ones_f.v, diag[i].v)
                        K.act(bcs.v, ps[:, 0:384].rr("p (i t) -> p i t", t=128), AF.Copy)
                        K.stt(arg.v, bcs[:, 0, :], gcol, negm[:, 1 if samp else 0, :], ALU.add, ALU.add)
                        K.act(arg.v, arg.v, AF.Exp)
                        ps = K.ps()
                        for j in range(4):
                            K.mm(ps[:, 0:128], kT[:, j, :], qT[:, j, :], start=(j == 0), stop=(j == 3))
                        K.stt(sT.v, ps[:, 0:128], 512 ** -0.5, arg.v, ALU.mult, ALU.mult)
                        K.tt(qw.v, qT.v, bcs[:, 1, :].un(1).bc([128, 4, 128]), ALU.mult)
                        pn = K.ps_pin(0); pd = K.ps_pin(1)
                        if not samp:
                            for vc in range(4):
                                K.mm(pn[:, vc * 128:(vc + 1) * 128], vtok[:, vc * 128:(vc + 1) * 128], sT.v, start=True, stop=False)
                                for dc in range(4):
                                    K.mm(pn[:, vc * 128:(vc + 1) * 128], CTb[:, dc, vc * 128:(vc + 1) * 128], qw[:, dc, :], start=False, stop=(dc == 3))
                            K.mm(pd[:, 0:128], ones_bf.v, sT.v, start=True, stop=False)
                            for dc in range(4):
                                K.mm(pd[:, 0:128], nB[:, dc, :], qw[:, dc, :], start=False, stop=(dc == 3))
                        else:
                            for vc in range(4):
                                K.mm(pn[:, vc * 128:(vc + 1) * 128], vtok[:, vc * 128:(vc + 1) * 128], sT.v, start=True, stop=True)
                            K.mm(pd[:, 0:128], ones_bf.v, sT.v, start=True, stop=True)
                            K.ts(vw.v, vtok.v, wrcol, ALU.mult)
                            CTs = [CT, CT2]
                            K.dma(CT.v, din["ml_CT"][o_, 0, h])
                            for s in range(16):
                                CTc = CTs[s % 2]
                                if s + 1 < 16:
                                    K.dma(CTs[(s + 1) % 2].v, din["ml_CT"][o_, s + 1, h])
                                K.act(CTb.v, CTc.v, AF.Copy)
                                K.cp(nB.v, nS[:, s, h, :].un(2).bc([128, 4, 128]))
                                ssl = slice(s * 8, s * 8 + 8)
                                pi = K.ps()
                                for vc in range(4):
                                    for dc in range(4):
                                        K.mm(pi[:, vc * 8:vc * 8 + 8], CTb[:, dc, vc * 128:(vc + 1) * 128], qw[:, dc, ssl], start=(dc == 0), stop=(dc == 3))
                                for dc in range(4):
                                    K.mm(pi[:, 32:40], nB[:, dc, :], qw[:, dc, ssl], start=(dc == 0), stop=(dc == 3))
                                K.cp(interN[:, :, ssl], pi[:, 0:32].rr("p (v t) -> p v t", t=8))
                                K.cp(interD[:, ssl], pi[:, 32:40])
                                K.ts(km.v, ktok.v, seqmask[:, s:s + 1], ALU.mult)
                                pnn = K.ps()
                                for dc in range(4):
                                    pk = K.ps()
                                    K.mm(pk[:, 0:512], km[:, dc * 128:(dc + 1) * 128], vw.v)
                                    K.stt(CTc[:, dc, :], CTc[:, dc, :], bcs[:, 1, s * 8 + 7:s * 8 + 8], pk[:, 0:512], ALU.mult, ALU.add)
                                    K.mm(pnn[:, dc * 4:(dc + 1) * 4], km[:, dc * 128:(dc + 1) * 128], wrb.v)
                                K.stt(nSn[:, s, h, :], nS[:, s, h, :], bcs[:, 1, s * 8 + 7:s * 8 + 8], pnn[:, 0:16].rr("p (d f) -> p d f", f=4)[:, :, h], ALU.mult, ALU.add)
                                K.dma(dout["ml_CT_s"][o_, s, h], CTc.v, q="pool")
                        if samp:
                            K.tt(interD.v, interD.v, pd[:, 0:128], ALU.add)
                            K.tt(interN.v, interN.v, pn[:, :].rr("p (v t) -> p v t", t=128), ALU.add)
                            den_v = interD.v; num_v = interN.v
                        else:
                            den_v = pd[:, 0:128]; num_v = pn[:, :].rr("p (v t) -> p v t", t=128)
                        K.ts(hden.v, den_v, -1.0, ALU.mult)
                        K.tt(hden.v, hden.v, den_v, ALU.max)
                        K.tt(hden.v, hden.v, bcs[:, 2, :], ALU.max)
                        K.recip(hden.v, hden.v)
                        K.tt(hh.v, num_v, hden.v.un(1).bc([128, 4, 128]), ALU.mult)
                        K.act(hsq.v, hh.v, AF.Square)
                        ps = K.ps()
                        for vc in range(4):
                            K.mm(ps[:, 0:128], ones_f.v, hh[:, vc, :], start=(vc == 0), stop=(vc == 3))
                        for vc in range(4):
                            K.mm(ps[:, 128:256], ones_f.v, hsq[:, vc, :], start=(vc == 0), stop=(vc == 3))
                        K.act(mean.v, ps[:, 0:128], AF.Copy, scale=1.0 / 512)
                        K.tt(var.v, mean.v, mean.v, ALU.mult)
                        K.stt(var.v, ps[:, 128:256], 1.0 / 512, var.v, ALU.mult, ALU.subtract)
                        K.act(var.v, var.v, AF.Ln, bias=epsb.v)
                        K.act(var.v, var.v, AF.Exp, scale=-0.5)
                        K.tt(hh.v, hh.v, mean.v.un(1).bc([128, 4, 128]), ALU.subtract)
                        K.tt(hh.v, hh.v, var.v.un(1).bc([128, 4, 128]), ALU.mult)
                        ps = K.ps()
                        for j in range(4):
                            for k in range(KD):
                                K.mm(ps[:, j * 128:(j + 1) * 128], w_z[:, k, j * 128:(j + 1) * 128], hT[:, k, tsl], start=(k == 0), stop=(k == KD - 1))
                        K.act(sz.v, ps[:, :].rr("p (f t) -> p f t", t=128), AF.Silu)
                        K.tt(hh.v, hh.v, gn[:, 4 * h:4 * h + 4].un(2).bc([128, 4, 128]), ALU.mult)
                        K.tt(skx.v, xc.v, skp[:, 4 * h:4 * h + 4].un(2).bc([128, 4, 128]), ALU.mult)
                        K.tt(hh.v, hh.v, skx.v, ALU.add)
                        K.tt(pre.v, hh.v, sz.v, ALU.mult)
                        for half in range(2):
                            ps = K.ps()
                            for dd in range(4):
                                d = half * 4 + dd
                                for j in range(4):
                                    K.mm(ps[:, dd * 128:(dd + 1) * 128], w_o[:, j, d * 128:(d + 1) * 128], pre[:, j, :], start=(j == 0), stop=(j == 3))
                            resid_add4(ti, half * 4, cols, ps[:, :].rr("p (d t) -> p d t", t=128), Gvec)
                        if not samp:
                            K.ts(vw.v, vtok.v, wrcol, ALU.mult)
                            pnn = K.ps()
                            for dc in range(4):
                                pk = K.ps()
                                K.mm(pk[:, 0:512], ktok[:, dc * 128:(dc + 1) * 128], vw.v)
                                K.stt(CT[:, dc, :], CT[:, dc, :], bcs[:, 1, 127:128], pk[:, 0:512], ALU.mult, ALU.add)
                                K.mm(pnn[:, dc * 4:(dc + 1) * 4], ktok[:, dc * 128:(dc + 1) * 128], wrb.v)
                            K.stt(nP[:, h, :], nP[:, h, :], bcs[:, 1, 127:128], pnn[:, 0:16].rr("p (d f) -> p d f", f=4)[:, :, h], ALU.mult, ALU.add)
                            K.act(CTb.v, CT.v, AF.Copy)
                            K.cp(nB.v, nP[:, h, :].un(2).bc([128, 4, 128]))
                            if ci == 15:
                                K.dma(dout["ml_CT_p"][o_, h], CT.v)
                K.dma(dout["ml_n_p"][o_], nP.v)
                K.dma(dout["ml_n_s"][o_], nSn.v)
                K.barrier()
        K.barrier()

    for l in range(nlayers):
        if DBG_ONLY_MLSTM:
            if l == 1: mlstm(l)
            continue
        ada(l)
        ffn(l, 0)
        if l % 2 == 0:
            ab_mixer(l)
        elif MLSTM_MODE:
            mlstm(l)
        ffn(l, 1)
    with ExitStack() as ph:
        ada(NL)
        yT = [K.sb("yT%d" % i, [128, KD, n], F32, ph) for i, (o, n) in enumerate(TILES)]
        K.ts(Avec.v, mod(NL, 1), 1.0, ALU.add)
        K.tt(Avec.v, Avec.v, final_norm.v.un(2).bc([128, KD, NSEQ]), ALU.mult)
        B = mod(NL, 0)
        norm_bufs(ph, 512)
        for ti, (o, n) in enumerate(TILES):
            norm_tile(ti, yT[ti].v, B)
            K.dma(dout["yT"][:, :, o:o + n], yT[ti].v)
        K.barrier()


def _consts():
    c = np.zeros((128, 6, 128), np.float32)
    s = np.arange(128)[:, None]; t = np.arange(128)[None, :]
    c[:, C_TRI, :] = (s <= t)
    c[:, C_TRI8, :] = (s <= t) & (s // 8 == t // 8)
    c[:, C_ID, :] = (s == t)
    c[:, C_MISC, 0:16] = (np.arange(128)[:, None] // 8 == np.arange(16)[None, :])
    return c


def _bd(w):
    out = np.zeros((16, 128, 128), np.float32)
    wr = w.reshape(16, 32, 4, 4)
    for n in range(32):
        out[:, n * 4:(n + 1) * 4, n * 4:(n + 1) * 4] = wr[:, n]
    return np.ascontiguousarray(out.transpose(1, 0, 2))


def _kT(w, kc):
    return np.ascontiguousarray(w.reshape(kc, 128, -1).transpose(1, 0, 2))


def prep_shared(inp):
    f = lambda a: np.ascontiguousarray(np.asarray(a, dtype=np.float32))
    S = {}
    S["ada_w"] = f(inp["ada_w"].reshape(NL, KD, 128, 9, 1024).transpose(0, 3, 2, 1, 4))
    S["ada_b"] = f(inp["ada_b"].reshape(NL, 72, 128).transpose(2, 0, 1))
    S["fada_w"] = f(inp["final_ada_w"].reshape(KD, 128, 2, 1024).transpose(2, 1, 0, 3))
    S["fada_b"] = f(inp["final_ada_b"].reshape(16, 128).T)
    S["ffn_norm"] = f(inp["ffn_norm"].reshape(NL, 2, KD, 128).transpose(3, 0, 1, 2))
    S["mix_norm"] = f(inp["mix_norm"].reshape(NL, KD, 128).transpose(2, 0, 1))
    S["final_norm"] = f(inp["final_norm"].reshape(KD, 128).T)
    S["ffn_wg"] = f(inp["ffn_w_gate"].reshape(NL, 2, KD, 128, NG, 256).transpose(0, 1, 4, 3, 2, 5))
    S["ffn_wu"] = f(inp["ffn_w_up"].reshape(NL, 2, KD, 128, NG, 256).transpose(0, 1, 4, 3, 2, 5))
    S["ffn_wd"] = f(inp["ffn_w_down"].reshape(NL, 2, NG, 2, 128, 1024).transpose(0, 1, 2, 4, 3, 5))
    S["ab_w_in"] = f(inp["ab_w_in"].reshape(2, KD, 128, 2576).transpose(0, 2, 1, 3))
    S["ab_w_out"] = f(inp["ab_w_out"].reshape(2, KD, 128, 1024).transpose(0, 2, 1, 3))
    S["gla_wa2"] = f(np.concatenate([inp["gla_w_a2"], inp["gla_b_a"][:, None, :]], axis=1))
    S["gla_norm"] = f(inp["gla_norm"].reshape(2, 128, 1))
    S["gmlp_norm"] = f(np.broadcast_to(inp["gmlp_norm"][:, None, :], (2, 128, 128)))
    ws = np.asarray(inp["gmlp_ws"])
    S["gmlp_wT_p"] = f(ws.transpose(0, 3, 1, 2))
    w8 = ws[:, :, :8, :8]
    S["gmlp_wT_s"] = f(np.tile(w8.transpose(0, 3, 1, 2), (1, 16, 1, 16)))
    bs = np.asarray(inp["gmlp_bs"])
    S["gmlp_bs_p"] = f(np.broadcast_to(bs[:, None, :, :], (2, 128, 4, 128)))
    S["gmlp_bs_s"] = f(np.broadcast_to(np.tile(bs[:, :, :8], (1, 1, 16))[:, None, :, :], (2, 128, 4, 128)))
    S["consts"] = _consts()
    S["ml_w_in"] = f(inp["ml_w_in"].reshape(2, KD, 128, 4096).transpose(0, 2, 1, 3))
    S["ml_w_out"] = f(inp["ml_w_out"].reshape(2, 16, 128, 1024).transpose(0, 2, 1, 3))
    S["ml_conv_w"] = f(inp["ml_conv_w"].reshape(2, 4, 16, 128).transpose(0, 3, 2, 1))
    S["ml_conv_b"] = f(inp["ml_conv_b"].reshape(2, 16, 128).transpose(0, 2, 1))
    for n, k in (("ml_bdq", "ml_wq"), ("ml_bdk", "ml_wk"), ("ml_bdv", "ml_wv")):
        S[n] = np.stack([_bd(np.asarray(inp[k][o])) for o in range(2)])
    S["ml_w_gates"] = f(inp["ml_w_gates"].reshape(2, 48, 128, 8).transpose(0, 2, 1, 3))
    S["ml_b_gates"] = f(np.broadcast_to(inp["ml_b_gates"][:, None, :], (2, 128, 8)))
    S["ml_norm"] = f(inp["ml_norm"].reshape(2, 16, 128).transpose(0, 2, 1))
    S["ml_skip"] = f(inp["ml_skip"].reshape(2, 16, 128).transpose(0, 2, 1))
    return S


def prep_core(inp, i):
    f = lambda a: np.ascontiguousarray(np.asarray(a, dtype=np.float32))
    P = {}
    sl = slice(16 * i, 16 * i + 16)
    x = np.concatenate([inp["x_prompt"][i], inp["x_sample"][sl].reshape(TS, D)], axis=0)
    P["xT"] = f(x.T.reshape(KD, 128, T).transpose(1, 0, 2))
    c = np.concatenate([inp["c_prompt"][i:i + 1], inp["c_sample"][sl]], axis=0)
    P["cT"] = f(c.T.reshape(KD, 128, NSEQ).transpose(1, 0, 2))
    P["gla_S"] = f(inp["state_gla_S"][:, sl].transpose(0, 3, 1, 2, 4))
    C = inp["state_mlstm_C"][:, sl]
    P["ml_CT"] = f(C.transpose(0, 1, 2, 4, 3).reshape(2, 16, 4, 4, 128, 512).transpose(0, 1, 2, 4, 3, 5))
    P["ml_n"] = f(inp["state_mlstm_n"][:, sl].reshape(2, 16, 4, 4, 128).transpose(0, 4, 1, 2, 3))
    P["ml_m"] = f(inp["state_mlstm_m"][:, sl].transpose(0, 2, 1))
    P["ml_conv"] = f(inp["state_mlstm_conv"][:, sl].reshape(2, 16, 3, 16, 128).transpose(0, 4, 3, 1, 2))
    return P


def post_core(r):
    o = {}
    y = r["yT"].transpose(2, 1, 0).reshape(T, D)
    o["y_p"] = y[0:TP]; o["y_s"] = y[TP:].reshape(16, 8, D)
    o["s_p"] = r["gla_S_p"].transpose(0, 2, 1, 3)
    o["s_s"] = r["gla_S_s"].transpose(0, 2, 3, 1, 4)
    o["v_s"] = r["gmlp_v_s"].reshape(2, 16, 8, 512)
    o["c_p"] = r["ml_CT_p"].transpose(0, 1, 3, 2, 4).reshape(2, 4, 512, 512).transpose(0, 1, 3, 2)
    o["c_s"] = r["ml_CT_s"].transpose(0, 1, 2, 4, 3, 5).reshape(2, 16, 4, 512, 512).transpose(0, 1, 2, 4, 3)
    o["n_p"] = r["ml_n_p"].transpose(0, 2, 3, 1).reshape(2, 4, 512)
    o["n_s"] = r["ml_n_s"].transpose(0, 2, 3, 4, 1).reshape(2, 16, 4, 512)
    o["m_p"] = r["ml_m_p"].reshape(2, 4)
    o["m_s"] = r["ml_m_s"].transpose(0, 2, 1)
    o["cv_p"] = r["ml_conv_p"].transpose(0, 3, 2, 1).reshape(2, 3, 2048)
    o["cv_s"] = r["ml_conv_s"].transpose(0, 3, 4, 2, 1).reshape(2, 16, 3, 2048)
    return o


_NC_CACHE = {}


def kernel(**inputs):
    inp = {k: np.asarray(v) for k, v in inputs.items()}
    if "nc" not in _NC_CACHE:
        _NC_CACHE["nc"] = build()
    nc = _NC_CACHE["nc"]
    S = prep_shared(inp)
    in_maps = []
    for i in range(8):
        m = dict(S); m.update(prep_core(inp, i)); in_maps.append(m)
    res = run_bass_kernel_spmd(nc, in_maps, core_ids=list(range(8)))
    po = [post_core(r) for r in res.results]
    cat0 = lambda k: np.ascontiguousarray(np.stack([p[k] for p in po], axis=0)).astype(np.float32)
    cat1 = lambda k: np.ascontiguousarray(np.stack([p[k] for p in po], axis=1)).astype(np.float32)
    cat1s = lambda k: np.ascontiguousarray(np.concatenate([p[k] for p in po], axis=1)).astype(np.float32)
    y_p = cat0("y_p")
    y_s = np.ascontiguousarray(np.concatenate([p["y_s"] for p in po], axis=0)).astype(np.float32)
    return (y_p, y_s, cat1("s_p"), cat1s("s_s"), cat1s("v_s"), cat1("c_p"), cat1s("c_s"), cat1("n_p"), cat1s("n_s"),
            cat1("m_p"), cat1s("m_s"), cat1("cv_p"), cat1s("cv_s"))
```

```python
import numpy as np
from contextlib import ExitStack
import concourse.bass as bass
import concourse.mybir as mybir
from concourse.bass_utils import run_bass_kernel_spmd

F32 = mybir.dt.float32
BF16 = mybir.dt.bfloat16
AF = mybir.ActivationFunctionType
ALU = mybir.AluOpType

D = 1024; KD = 8; TP = 2048; TS = 128; T = TP + TS; NSEQ = 17
DFF = 2816; NG = 11
NL = 4
EPS = 1e-6
NEG = -1.0e30
MLSTM_MODE = 3
DBG_ONLY_MLSTM = False
FRONT_STEPS = 9
SKIP_INTER = False
SAME_ENG_SYNC = True


class V:
    def __init__(self, buf, ap):
        self.buf = buf; self.ap = ap
    def __getitem__(self, idx):
        return V(self.buf, self.ap[idx])
    def rr(self, pat, **kw):
        return V(self.buf, self.ap.rearrange(pat, **kw))
    def bc(self, shape):
        return V(self.buf, self.ap.broadcast_to(list(shape)))
    def un(self, axis):
        return V(self.buf, self.ap.unsqueeze(axis))


class Buf:
    def __init__(self, t, name):
        self.t = t; self.name = name; self.w = None; self.r = {}
    def __getitem__(self, idx):
        return V(self, self.t[idx])
    @property
    def v(self):
        return V(self, self.t[:])


class Kern:
    def __init__(self, nc, es):
        self.nc = nc; self.es = es
        self.engs = {"pe": nc.tensor, "act": nc.scalar, "dve": nc.vector, "sp": nc.sync, "pool": nc.gpsimd}
        self.sems = {}; self.cnt = {}
        for e in ["pe", "act", "dve"]:
            self.sems[e] = es.enter_context(nc.semaphore("s_" + e)); self.cnt[e] = 0
        self.rings = {}
        for q, n in (("sp", 10), ("pool", 10)):
            keys = []
            for i in range(n):
                k = "%s_d%d" % (q, i)
                self.sems[k] = es.enter_context(nc.semaphore("s_" + k)); self.cnt[k] = 0
                keys.append(k)
            self.rings[q] = [keys, 0]
        self.known = {e: {} for e in self.engs}
        self.psb = []
        for i in range(8):
            t = es.enter_context(nc.psum_tensor("psb%d" % i, [128, 512], F32))
            self.psb.append(Buf(t, "psb%d" % i))
        self.psi = 0
        self.ninstr = 0

    def sb(self, name, shape, dt, es=None):
        self.uid = getattr(self, "uid", 0) + 1
        t = (es or self.es).enter_context(self.nc.sbuf_tensor("sb%d_%s" % (self.uid, name), list(shape), dt))
        return Buf(t, name)

    def ps(self):
        b = self.psb[self.psi % 6]; self.psi += 1
        return b

    def ps_pin(self, i):
        return self.psb[6 + i]

    def _emit(self, eng, fn, reads, writes, dma=False):
        waits = {}
        known = self.known[eng]
        def need(tok):
            if tok is None: return
            k, v = tok
            if k == eng and (eng == "pe" or not SAME_ENG_SYNC): return
            if known.get(k, 0) >= v: return
            if waits.get(k, 0) < v: waits[k] = v
        rb = []; wb = []
        for x in reads:
            if x is None or isinstance(x, (int, float)): continue
            b = x.buf if isinstance(x, V) else x
            if b is not None and b not in rb: rb.append(b)
        for x in writes:
            b = x.buf if isinstance(x, V) else x
            if b is not None and b not in wb: wb.append(b)
        for b in rb: need(b.w)
        for b in wb:
            need(b.w)
            for k, v in b.r.items(): need((k, v))
        if dma:
            keys, pos = self.rings[eng]
            k = keys[pos % len(keys)]; self.rings[eng][1] = pos + 1
            need((k, self.cnt[k]))
            self.cnt[k] += 16; tok = (k, self.cnt[k]); inc = 16
        else:
            self.cnt[eng] += 1; tok = (eng, self.cnt[eng]); inc = 1
        e = self.engs[eng]
        for k, v in waits.items():
            e.wait_ge(self.sems[k], v); known[k] = v
        ins = fn(e)
        ins.then_inc(self.sems[tok[0]], inc)
        self.ninstr += 1
        for b in rb:
            if b in wb: continue
            if b.r.get(tok[0], 0) < tok[1]: b.r[tok[0]] = tok[1]
        for b in wb:
            b.w = tok; b.r = {}
        return tok

    def barrier(self):
        for eng in self.engs:
            e = self.engs[eng]; known = self.known[eng]
            for k, v in self.cnt.items():
                if v > 0 and known.get(k, 0) < v:
                    e.wait_ge(self.sems[k], v); known[k] = v

    @staticmethod
    def _a(x):
        return x.ap if isinstance(x, V) else x

    def mm(self, out, lhsT, rhs, start=True, stop=True):
        a = self._a
        return self._emit("pe", lambda e: e.matmul(a(out), lhsT=a(lhsT), rhs=a(rhs), start=start, stop=stop), [lhsT, rhs], [out])

    def tr(self, out, in_, ident):
        a = self._a
        return self._emit("pe", lambda e: e.transpose(a(out), a(in_), a(ident)), [in_, ident], [out])

    def act(self, out, in_, func, bias=None, scale=None, accum=None):
        a = self._a
        kw = {}
        if bias is not None: kw["bias"] = a(bias)
        if scale is not None: kw["scale"] = a(scale)
        if accum is not None: kw["accum_out"] = a(accum)
        w = [out] + ([accum] if accum is not None else [])
        return self._emit("act", lambda e: e.activation(out=a(out), in_=a(in_), func=func, **kw), [in_, bias, scale], w)

    def tt(self, out, in0, in1, op, eng="dve"):
        a = self._a
        return self._emit(eng, lambda e: e.tensor_tensor(out=a(out), in0=a(in0), in1=a(in1), op=op), [in0, in1], [out])

    def ts(self, out, in0, s1, op0, s2=None, op1=None, eng="dve"):
        a = self._a
        if op1 is None:
            return self._emit(eng, lambda e: e.tensor_scalar(out=a(out), in0=a(in0), scalar1=a(s1), scalar2=None, op0=op0), [in0, s1], [out])
        return self._emit(eng, lambda e: e.tensor_scalar(out=a(out), in0=a(in0), scalar1=a(s1), scalar2=a(s2), op0=op0, op1=op1), [in0, s1, s2], [out])

    def stt(self, out, in0, scalar, in1, op0, op1, eng="dve"):
        a = self._a
        return self._emit(eng, lambda e: e.scalar_tensor_tensor(out=a(out), in0=a(in0), scalar=a(scalar), in1=a(in1), op0=op0, op1=op1), [in0, scalar, in1], [out])

    def cp(self, out, in_, eng="dve"):
        a = self._a
        return self._emit(eng, lambda e: e.tensor_copy(out=a(out), in_=a(in_)), [in_], [out])

    def memset(self, out, val, eng="dve"):
        a = self._a
        return self._emit(eng, lambda e: e.memset(a(out), val), [], [out])

    def scan(self, out, d0, d1, init, op0, op1):
        a = self._a
        return self._emit("dve", lambda e: e.tensor_tensor_scan(out=a(out), data0=a(d0), data1=a(d1), initial=a(init), op0=op0, op1=op1), [d0, d1, init], [out])

    def recip(self, out, in_):
        a = self._a
        return self._emit("dve", lambda e: e.reciprocal(out=a(out), in_=a(in_)), [in_], [out])

    def dma(self, out, in_, q="sp"):
        a = self._a
        r = [in_] if isinstance(in_, V) else []
        w = [out] if isinstance(out, V) else []
        return self._emit(q, lambda e: e.dma_start(out=a(out), in_=a(in_)), r, w, dma=True)

    def finish(self):
        self.barrier()


IN_SPECS = {}
OUT_SPECS = {}


def _specs():
    I = {}
    I["xT"] = [128, KD, T]
    I["cT"] = [128, KD, NSEQ]
    I["ada_w"] = [NL, 9, 128, KD, 1024]
    I["ada_b"] = [128, NL, 72]
    I["fada_w"] = [2, 128, KD, 1024]
    I["fada_b"] = [128, 16]
    I["ffn_norm"] = [128, NL, 2, KD]
    I["mix_norm"] = [128, NL, KD]
    I["final_norm"] = [128, KD]
    I["ffn_wg"] = [NL, 2, NG, 128, KD, 256]
    I["ffn_wu"] = [NL, 2, NG, 128, KD, 256]
    I["ffn_wd"] = [NL, 2, NG, 128, 2, 1024]
    I["ab_w_in"] = [2, 128, KD, 2576]
    I["ab_w_out"] = [2, 128, KD, 1024]
    I["gla_wa2"] = [2, 17, 256]
    I["gla_norm"] = [2, 128, 1]
    I["gmlp_norm"] = [2, 128, 128]
    I["gmlp_wT_p"] = [2, 128, 4, 128]
    I["gmlp_wT_s"] = [2, 128, 4, 128]
    I["gmlp_bs_p"] = [2, 128, 4, 128]
    I["gmlp_bs_s"] = [2, 128, 4, 128]
    I["consts"] = [128, 6, 128]
    I["gla_S"] = [2, 64, 16, 4, 128]
    I["ml_w_in"] = [2, 128, KD, 4096]
    I["ml_w_out"] = [2, 128, 16, 1024]
    I["ml_conv_w"] = [2, 128, 16, 4]
    I["ml_conv_b"] = [2, 128, 16]
    I["ml_bdq"] = [2, 128, 16, 128]
    I["ml_bdk"] = [2, 128, 16, 128]
    I["ml_bdv"] = [2, 128, 16, 128]
    I["ml_w_gates"] = [2, 128, 48, 8]
    I["ml_b_gates"] = [2, 128, 8]
    I["ml_norm"] = [2, 128, 16]
    I["ml_skip"] = [2, 128, 16]
    I["ml_CT"] = [2, 16, 4, 128, 4, 512]
    I["ml_n"] = [2, 128, 16, 4, 4]
    I["ml_m"] = [2, 4, 16]
    I["ml_conv"] = [2, 128, 16, 16, 3]
    O = {}
    O["yT"] = [128, KD, T]
    O["gla_S_p"] = [2, 64, 4, 128]
    O["gla_S_s"] = [2, 64, 16, 4, 128]
    O["gmlp_v_s"] = [2, 128, 512]
    O["ml_CT_p"] = [2, 4, 128, 4, 512]
    O["ml_CT_s"] = [2, 16, 4, 128, 4, 512]
    O["ml_n_p"] = [2, 128, 4, 4]
    O["ml_n_s"] = [2, 128, 16, 4, 4]
    O["ml_m_p"] = [2, 4, 1]
    O["ml_m_s"] = [2, 4, 16]
    O["ml_conv_p"] = [2, 128, 16, 3]
    O["ml_conv_s"] = [2, 128, 16, 16, 3]
    return I, O


IN_SPECS, OUT_SPECS = _specs()
C_TRI, C_TRI8, C_ID, C_NEG, C_NEG8, C_MISC = range(6)


def build(nlayers=NL, dbg=None):
    nc = bass.Bass("TRN2", target_bir_lowering=False)
    din = {n: nc.dram_tensor(n, s, F32, kind="ExternalInput").ap() for n, s in IN_SPECS.items()}
    dout = {n: nc.dram_tensor(n, s, F32, kind="ExternalOutput").ap() for n, s in OUT_SPECS.items()}
    with ExitStack() as es:
        K = Kern(nc, es)
        _program(nc, K, es, din, dout, nlayers)
        K.finish()
    return nc


def _program(nc, K, es, din, dout, nlayers):
    TILES = [(i * 512, 512) for i in range(4)] + [(TP, TS)]
    xT = [K.sb("xT%d" % i, [128, KD, n], F32) for i, (o, n) in enumerate(TILES)]
    consts = K.sb("consts", [128, 6, 128], F32)
    ones_bf = K.sb("ones_bf", [128, 128], BF16)
    ones_f = K.sb("ones_f", [128, 128], F32)
    negm = K.sb("negm", [128, 2, 128], F32)
    modT = K.sb("modT", [128, 72, NSEQ], F32)
    csT = K.sb("csT", [128, KD, NSEQ], BF16)
    ffn_norm = K.sb("ffn_norm", [128, NL, 2, KD], F32)
    mix_norm = K.sb("mix_norm", [128, NL, KD], F32)
    final_norm = K.sb("final_norm", [128, KD], F32)
    epsb = K.sb("epsb", [128, 1], F32)
    Avec = K.sb("Avec", [128, KD, NSEQ], F32)
    Gvec = K.sb("Gvec", [128, KD, NSEQ], F32)

    K.dma(consts.v, din["consts"])
    for i, (o, n) in enumerate(TILES):
        K.dma(xT[i].v, din["xT"][:, :, o:o + n])
    K.dma(ffn_norm.v, din["ffn_norm"]); K.dma(mix_norm.v, din["mix_norm"]); K.dma(final_norm.v, din["final_norm"])
    K.memset(ones_bf.v, 1.0); K.memset(ones_f.v, 1.0); K.memset(epsb.v, EPS)
    tri = consts[:, C_TRI, :]; tri8 = consts[:, C_TRI8, :]; ident = consts[:, C_ID, :]
    seqmask = consts[:, C_MISC, 0:16]
    K.ts(negm[:, 0, :], tri, -1.0, ALU.add, -NEG, ALU.mult)
    K.ts(negm[:, 1, :], tri8, -1.0, ALU.add, -NEG, ALU.mult)

    cTf = K.sb("cTf", [128, KD, NSEQ], F32)
    K.dma(cTf.v, din["cT"])
    K.act(csT.v, cTf.v, AF.Silu)

    def ada_steps(l, es_):
        adab = K.sb("adab", [128, 72], F32, es_)
        wbuf = [K.sb("adaw%d" % i, [128, KD, 1024], BF16, es_) for i in range(2)]
        if l < NL:
            pieces = [(din["ada_w"][l, v], v * 8) for v in range(9)]
        else:
            pieces = [(din["fada_w"][v], v * 8) for v in range(2)]

        def load(i):
            if i == 0:
                if l < NL: K.dma(adab.v, din["ada_b"][:, l, :])
                else: K.dma(adab[:, 0:16], din["fada_b"])
            K.dma(wbuf[i % 2].v, pieces[i][0], q="pool")

        def comp(i):
            wb = wbuf[i % 2]; off = pieces[i][1]
            ps = K.ps()
            for j in range(8):
                for k in range(KD):
                    K.mm(ps[:, j * NSEQ:(j + 1) * NSEQ], wb[:, k, j * 128:(j + 1) * 128], csT[:, k, :], start=(k == 0), stop=(k == KD - 1))
            K.tt(modT[:, off:off + 8, :], ps[:, 0:8 * NSEQ].rr("p (j s) -> p j s", s=NSEQ),
                 adab[:, off:off + 8].un(2).bc([128, 8, NSEQ]), ALU.add)

        n = len(pieces)
        steps = []
        for i in range(n + 1):
            def step(i=i):
                if i < n: load(i)
                if i >= 1: comp(i - 1)
            steps.append(step)
        return steps

    def ada(l):
        with ExitStack() as ph:
            for st in ada_steps(l, ph): st()
            K.barrier()

    def mod(l, v):
        return modT[:, v * 8:v * 8 + 8, :]

    def prep_AG(gamma, sc, gate, gmul):
        K.ts(Avec.v, sc, 1.0, ALU.add)
        K.tt(Avec.v, Avec.v, gamma.un(2).bc([128, KD, NSEQ]), ALU.mult)
        if gate is not None:
            K.ts(Gvec.v, gate, gmul, ALU.mult)

    def seq_affine(out, in_, A, B, ti, k):
        if ti < 4:
            K.act(out, in_, AF.Identity, bias=B[:, k, 0:1], scale=A[:, k, 0:1])
        else:
            o3 = out.rr("p (s t) -> p s t", t=8); i3 = in_.rr("p (s t) -> p s t", t=8)
            K.tt(o3, i3, A[:, k, 1:17].un(2).bc([128, 16, 8]), ALU.mult)
            K.tt(o3, o3, B[:, k, 1:17].un(2).bc([128, 16, 8]), ALU.add)

    def resid_add(ti, d, cols, ps_v, G):
        xv = xT[ti][:, d, cols]
        if ti < 4:
            K.stt(xv, ps_v, G[:, d, 0:1], xv, ALU.mult, ALU.add)
        else:
            tmp = rs_tmp.v
            K.tt(tmp.rr("p (s t) -> p s t", t=8), ps_v.rr("p (s t) -> p s t", t=8), G[:, d, 1:17].un(2).bc([128, 16, 8]), ALU.mult)
            K.tt(xv, xv, tmp, ALU.add)

    rs_tmp = K.sb("rs_tmp", [128, 128], F32)
    rs_tmp4 = K.sb("rs_tmp4", [128, 4, 128], F32)

    def resid_add4(ti, d0, cols, ps_v, G):
        xv = xT[ti][:, d0:d0 + 4, cols]
        if ti < 4:
            K.tt(rs_tmp4.v, ps_v, G[:, d0:d0 + 4, 0:1].bc([128, 4, 128]), ALU.mult)
        else:
            K.tt(rs_tmp4.v.rr("p d (s t) -> p d s t", t=8), ps_v.rr("p d (s t) -> p d s t", t=8),
                 G[:, d0:d0 + 4, 1:17].un(3).bc([128, 4, 16, 8]), ALU.mult)
        K.tt(xv, xv, rs_tmp4.v, ALU.add)

    NB = {}

    def norm_bufs(es_, n):
        NB["sq"] = K.sb("nrm_sq", [128, KD, n], BF16, es_)
        NB["rstd"] = K.sb("nrm_rstd", [128, n], F32, es_)
        NB["ntmp"] = K.sb("nrm_tmp", [128, n], F32, es_)

    def rms_rstd(ps_ss, n, dim, out):
        K.act(out[:, 0:n], ps_ss[:, 0:n], AF.Ln, bias=epsb.v, scale=1.0 / dim)
        K.act(out[:, 0:n], out[:, 0:n], AF.Exp, scale=-0.5)

    def norm_tile(ti, hdst, B):
        o, n = TILES[ti]
        sq = NB["sq"]; rstd = NB["rstd"]; ntmp = NB["ntmp"]
        K.act(sq[:, :, 0:n], xT[ti].v, AF.Square)
        ps = K.ps()
        for k in range(KD):
            K.mm(ps[:, 0:n], ones_bf.v, sq[:, k, 0:n], start=(k == 0), stop=(k == KD - 1))
        rms_rstd(ps, n, D, rstd)
        for k in range(KD):
            K.tt(ntmp[:, 0:n], xT[ti][:, k, :], rstd[:, 0:n], ALU.mult)
            seq_affine(hdst[:, k, :], ntmp[:, 0:n], Avec, B, ti, k)

    def ffn(l, w):
        with ExitStack() as ph:
            hT = [K.sb("ffn_h%d" % i, [128, KD, n], BF16, ph) for i, (o, n) in enumerate(TILES)]
            wg = [K.sb("ffn_wg%d" % i, [128, KD, 256], BF16, ph) for i in range(2)]
            wu = [K.sb("ffn_wu%d" % i, [128, KD, 256], BF16, ph) for i in range(2)]
            wd = [K.sb("ffn_wd%d" % i, [128, 2, 1024], BF16, ph) for i in range(2)]
            hid = [K.sb("ffn_hid%d" % i, [128, 2, 512], BF16, ph) for i in range(2)]
            sg = K.sb("ffn_sg", [128, 512], F32, ph)
            prep_AG(ffn_norm[:, l, w, :], mod(l, 1 if w == 0 else 7), mod(l, 2 if w == 0 else 8), 0.5)
            B = mod(l, 0 if w == 0 else 6)
            with ExitStack() as nb:
                norm_bufs(nb, 512)
                for ti in range(5):
                    norm_tile(ti, hT[ti].v, B)
                K.barrier()
            hi = 0
            asteps = ada_steps(l + 1, ph) if w == 1 else []
            for g in range(NG):
                b = g % 2
                if g < len(asteps): asteps[g]()
                K.dma(wg[b].v, din["ffn_wg"][l, w, g], q="pool")
                K.dma(wu[b].v, din["ffn_wu"][l, w, g], q="pool")
                K.dma(wd[b].v, din["ffn_wd"][l, w, g], q="pool")
                for ti, (o, n) in enumerate(TILES):
                    hb = hid[hi % 2]; hi += 1
                    for c in range(2):
                        pg = K.ps(); pu = K.ps()
                        for k in range(KD):
                            K.mm(pg[:, 0:n], wg[b][:, k, c * 128:(c + 1) * 128], hT[ti][:, k, :], start=(k == 0), stop=(k == KD - 1))
                        for k in range(KD):
                            K.mm(pu[:, 0:n], wu[b][:, k, c * 128:(c + 1) * 128], hT[ti][:, k, :], start=(k == 0), stop=(k == KD - 1))
                        K.act(sg[:, 0:n], pg[:, 0:n], AF.Silu)
                        K.tt(hb[:, c, 0:n], sg[:, 0:n], pu[:, 0:n], ALU.mult)
                    for d in range(KD):
                        py = K.ps()
                        for c in range(2):
                            K.mm(py[:, 0:n], wd[b][:, c, d * 128:(d + 1) * 128], hb[:, c, 0:n], start=(c == 0), stop=(c == 1))
                        resid_add(ti, d, slice(0, n), py[:, 0:n], Gvec)
            K.barrier()

    def ab_mixer(l):
        e = l // 2
        with ExitStack() as ph:
            w_in = K.sb("ab_win", [128, KD, 2576], BF16, ph)
            w_out = K.sb("ab_wout", [128, KD, 1024], BF16, ph)
            wa2 = K.sb("ab_wa2", [17, 256], F32, ph)
            gnorm = K.sb("ab_gn", [128, 1], F32, ph)
            vnB = K.sb("ab_vn", [128, 128], F32, ph)
            wTp = K.sb("ab_wTp", [128, 4, 128], F32, ph); wTs = K.sb("ab_wTs", [128, 4, 128], F32, ph)
            wTpb = K.sb("ab_wTpb", [128, 4, 128], BF16, ph); wTsb = K.sb("ab_wTsb", [128, 4, 128], BF16, ph)
            bsp = K.sb("ab_bsp", [128, 4, 128], F32, ph); bss = K.sb("ab_bss", [128, 4, 128], F32, ph)
            hT = K.sb("ab_h", [128, KD, 128], BF16, ph)
            qT = K.sb("ab_q", [64, 4, 128], F32, ph); kT = K.sb("ab_k", [64, 4, 128], F32, ph)
            qb = K.sb("ab_qb", [64, 4, 128], BF16, ph); kb = K.sb("ab_kb", [64, 4, 128], BF16, ph)
            kd = K.sb("ab_kd", [64, 4, 128], F32, ph); kdt = K.sb("ab_kdt", [128, 4, 64], BF16, ph)
            a_aug = K.sb("ab_aaug", [17, 128], F32, ph)
            la = K.sb("ab_la", [128, 256], F32, ph)
            eb = K.sb("ab_eb", [64, 4, 128], F32, ph); enb = K.sb("ab_enb", [64, 4, 128], F32, ph)
            vtok = K.sb("ab_vtok", [128, 512], BF16, ph)
            sgT = K.sb("ab_sg", [128, 4, 128], F32, ph)
            uT = K.sb("ab_u", [128, 4, 128], F32, ph)
            vbn = K.sb("ab_vbn", [128, 512], F32, ph); vbnb = K.sb("ab_vbnb", [128, 512], BF16, ph)
            ssq = K.sb("ab_ssq", [128, 4], F32, ph); vjunk = K.sb("ab_vj", [128, 128], F32, ph)
            sT = K.sb("ab_sT", [128, 128], BF16, ph)
            S = K.sb("ab_S", [64, 4, 128], F32, ph); Sb = K.sb("ab_Sb", [64, 4, 128], BF16, ph)
            Ss = K.sb("ab_Ss", [64, 4, 128], F32, ph); Ssn = K.sb("ab_Ssn", [64, 4, 128], F32, ph)
            Ssb = K.sb("ab_Ssb", [64, 4, 128], BF16, ph)
            R = K.sb("ab_R", [128, 4, 128], BF16, ph)
            ebl = K.sb("ab_ebl", [64, 4, 16], F32, ph)
            oT = K.sb("ab_o", [128, 4, 128], F32, ph); osq = K.sb("ab_osq", [128, 4, 128], BF16, ph)
            orstd = K.sb("ab_orstd", [128, 4, 128], F32, ph)
            mix = K.sb("ab_mix", [128, KD, 128], BF16, ph)
            ztmp = K.sb("ab_ztmp", [128, 128], F32, ph)
            K.dma(w_in.v, din["ab_w_in"][e], q="pool"); K.dma(w_out.v, din["ab_w_out"][e], q="pool")
            K.dma(wa2.v, din["gla_wa2"][e]); K.dma(gnorm.v, din["gla_norm"][e]); K.dma(vnB.v, din["gmlp_norm"][e])
            K.dma(wTp.v, din["gmlp_wT_p"][e]); K.dma(wTs.v, din["gmlp_wT_s"][e])
            K.dma(bsp.v, din["gmlp_bs_p"][e]); K.dma(bss.v, din["gmlp_bs_s"][e])
            K.tt(wTpb.v, wTp.v, tri.un(1).bc([128, 4, 128]), ALU.mult)
            K.tt(wTsb.v, wTs.v, tri8.un(1).bc([128, 4, 128]), ALU.mult)
            K.memset(a_aug.v, 1.0)
            K.memset(S.v, 0.0); K.memset(Sb.v, 0.0)
            prep_AG(mix_norm[:, l, :], mod(l, 4), mod(l, 5), 1.0)
            B = mod(l, 3)
            norm_bufs(ph, 128)
            sq = NB["sq"]; rstd = NB["rstd"]; ntmp = NB["ntmp"]
            for ci in range(17):
                samp = (ci == 16)
                ti = ci // 4 if not samp else 4
                c0 = (ci % 4) * 128 if not samp else 0
                cols = slice(c0, c0 + 128)
                mask = tri8 if samp else tri
                K.act(sq[:, :, 0:128], xT[ti][:, :, cols], AF.Square)
                ps = K.ps()
                for k in range(KD):
                    K.mm(ps[:, 0:128], ones_bf.v, sq[:, k, 0:128], start=(k == 0), stop=(k == KD - 1))
                rms_rstd(ps, 128, D, rstd)
                for k in range(KD):
                    K.tt(ntmp[:, 0:128], xT[ti][:, k, cols], rstd[:, 0:128], ALU.mult)
                    seq_affine(hT[:, k, :], ntmp[:, 0:128], Avec, B, ti, k)
                def proj_f(col0, m, dst_ps):
                    for k in range(KD):
                        K.mm(dst_ps, w_in[:, k, col0:col0 + m], hT[:, k, :], start=(k == 0), stop=(k == KD - 1))
                ps = K.ps()
                for h in range(4):
                    proj_f(h * 64, 64, ps[0:64, h * 128:(h + 1) * 128])
                K.act(qT.v, ps[0:64, :].rr("p (h t) -> p h t", t=128), AF.Copy, scale=64 ** -0.5)
                ps = K.ps()
                for h in range(4):
                    proj_f(256 + h * 64, 64, ps[0:64, h * 128:(h + 1) * 128])
                K.cp(kT.v, ps[0:64, :].rr("p (h t) -> p h t", t=128))
                ps = K.ps()
                proj_f(1536, 16, ps[0:16, 0:128])
                K.cp(a_aug[0:16, :], ps[0:16, 0:128])
                ps = K.ps()
                for k in range(KD):
                    K.mm(ps[:, 0:512], hT[:, k, :], w_in[:, k, 512:1024], start=(k == 0), stop=(k == KD - 1))
                K.act(vtok.v, ps[:, 0:512], AF.Copy)
                ps = K.ps()
                for j in range(4):
                    proj_f(1024 + j * 128, 128, ps[:, j * 128:(j + 1) * 128])
                K.act(sgT.v, ps[:, :].rr("p (h t) -> p h t", t=128), AF.Silu)
                ps = K.ps()
                for j in range(4):
                    proj_f(1552 + j * 128, 128, ps[:, j * 128:(j + 1) * 128])
                K.cp(uT.v, ps[:, :].rr("p (h t) -> p h t", t=128))
                ps = K.ps()
                for k in range(KD):
                    K.mm(ps[:, 0:512], hT[:, k, :], w_in[:, k, 2576 - 512:2576], start=(k == 0), stop=(k == KD - 1))
                for g in range(4):
                    K.act(vjunk.v, ps[:, g * 128:(g + 1) * 128], AF.Square, accum=ssq[:, g:g + 1])
                K.act(ssq.v, ssq.v, AF.Ln, bias=epsb.v, scale=1.0 / 128)
                K.act(ssq.v, ssq.v, AF.Exp, scale=-0.5)
                for g in range(4):
                    K.stt(vbn[:, g * 128:(g + 1) * 128], ps[:, g * 128:(g + 1) * 128], ssq[:, g:g + 1], vnB.v, ALU.mult, ALU.mult)
                K.cp(vbnb.v, vbn.v)
                if samp:
                    K.dma(dout["gmlp_v_s"][e], vbn.v)
                ps = K.ps()
                K.mm(ps[:, 0:256], a_aug.v, wa2.v)
                K.act(la.v, ps[:, 0:256], AF.Exp, scale=-1.0)
                K.act(la.v, la.v, AF.Ln, bias=1.0)
                ps = K.ps()
                for h in range(4):
                    K.mm(ps[0:64, h * 128:(h + 1) * 128], la[:, h * 64:(h + 1) * 64], mask)
                bp = ps[0:64, :].rr("p (h t) -> p h t", t=128)
                K.act(eb.v, bp, AF.Exp, scale=-1.0 / 16)
                K.act(enb.v, bp, AF.Exp, scale=1.0 / 16)
                K.tt(qb.v, qT.v, eb.v, ALU.mult)
                K.tt(kb.v, kT.v, enb.v, ALU.mult)
                if not samp:
                    K.tt(kd.v, kT.v, enb.v, ALU.mult)
                    for h in range(4):
                        K.ts(kd[:, h, :], kd[:, h, :], eb[:, h, 127:128], ALU.mult)
                else:
                    K.cp(ebl.v, eb.v.rr("p h (s t) -> p h s t", t=8)[:, :, :, 7])
                    K.tt(kd.v, kT.v, enb.v, ALU.mult)
                    K.tt(kd.v.rr("p h (s t) -> p h s t", t=8), kd.v.rr("p h (s t) -> p h s t", t=8),
                         ebl.v.un(3).bc([64, 4, 16, 8]), ALU.mult)
                ps = K.ps()
                for h in range(4):
                    K.tr(ps[:, h * 64:(h + 1) * 64], kd[:, h, :], ident[0:64, 0:64])
                K.cp(kdt.v, ps[:, 0:256].rr("p (h d) -> p h d", d=64))
                for h in range(4):
                    vh = vtok[:, h * 128:(h + 1) * 128]
                    ps = K.ps()
                    K.mm(ps[:, 0:128], kb[:, h, :], qb[:, h, :])
                    K.tt(sT.v, ps[:, 0:128], mask, ALU.mult)
                    po = K.ps_pin(0)
                    if not samp:
                        K.mm(po[:, 0:128], vh, sT.v, start=True, stop=False)
                        K.mm(po[:, 0:128], Sb[:, h, :], qb[:, h, :], start=False, stop=True)
                        K.cp(oT[:, h, :], po[:, 0:128])
                        pk = K.ps()
                        K.mm(pk[0:64, 0:128], kdt[:, h, :], vh)
                        K.stt(S[:, h, :], S[:, h, :], eb[:, h, 127:128], pk[0:64, 0:128], ALU.mult, ALU.add)
                        K.act(Sb[:, h, :], S[:, h, :], AF.Copy)
                    else:
                        K.mm(po[:, 0:128], vh, sT.v, start=True, stop=False)
                        for q4 in range(4):
                            sl = slice(q4 * 4, q4 * 4 + 4)
                            K.dma(Ss.v, din["gla_S"][e][:, sl, h, :])
                            K.cp(Ssb.v, Ss.v)
                            for s4 in range(4):
                                s_ = q4 * 4 + s4
                                K.mm(po[:, s_ * 8:(s_ + 1) * 8], Ssb[:, s4, :], qb[:, h, s_ * 8:(s_ + 1) * 8], start=False, stop=(s_ == 15))
                            K.tt(R.v, vh.un(1).bc([128, 4, 128]), seqmask[:, sl].un(2).bc([128, 4, 128]), ALU.mult)
                            pk = K.ps()
                            K.mm(pk[0:64, 0:512], kdt[:, h, :], R.v.rr("p s v -> p (s v)"))
                            K.tt(Ssn.v, Ss.v, ebl[:, h, sl].un(2).bc([64, 4, 128]), ALU.mult)
                            K.tt(Ssn.v, Ssn.v, pk[0:64, 0:512].rr("p (s v) -> p s v", v=128), ALU.add)
                            K.dma(dout["gla_S_s"][e][:, sl, h, :], Ssn.v)
                        K.cp(oT[:, h, :], po[:, 0:128])
                if ci == 15:
                    K.dma(dout["gla_S_p"][e], S.v)
                K.act(osq.v, oT.v, AF.Square)
                ps = K.ps()
                for h in range(4):
                    K.mm(ps[:, h * 128:(h + 1) * 128], ones_bf.v, osq[:, h, :])
                K.act(orstd.v, ps[:, :].rr("p (h t) -> p h t", t=128), AF.Ln, bias=epsb.v, scale=1.0 / 128)
                K.act(orstd.v, orstd.v, AF.Exp, scale=-0.5)
                K.tt(oT.v, oT.v, orstd.v, ALU.mult)
                K.stt(mix[:, 0:4, :], oT.v, gnorm.v, sgT.v, ALU.mult, ALU.mult)
                wTb = wTsb if samp else wTpb
                bsB = bss if samp else bsp
                for g in range(4):
                    ps = K.ps()
                    K.mm(ps[:, 0:128], vbnb[:, g * 128:(g + 1) * 128], wTb[:, g, :])
                    K.tt(ztmp.v, ps[:, 0:128], bsB[:, g, :], ALU.add)
                    K.tt(mix[:, 4 + g, :], ztmp.v, uT[:, g, :], ALU.mult)
                for half in range(2):
                    ps = K.ps()
                    for dd in range(4):
                        d = half * 4 + dd
                        for k in range(KD):
                            K.mm(ps[:, dd * 128:(dd + 1) * 128], w_out[:, k, d * 128:(d + 1) * 128], mix[:, k, :], start=(k == 0), stop=(k == KD - 1))
                    resid_add4(ti, half * 4, cols, ps[:, :].rr("p (d t) -> p d t", t=128), Gvec)
            K.barrier()

    def mlstm(l):
        o_ = l // 2
        with ExitStack() as ph:
            hT = K.sb("ml_h", [128, KD, T], BF16, ph)
            prep_AG(mix_norm[:, l, :], mod(l, 4), mod(l, 5), 1.0)
            B = mod(l, 3)
            with ExitStack() as nb:
                norm_bufs(nb, 512)
                for ti in range(5):
                    norm_tile(ti, hT[:, :, TILES[ti][0]:TILES[ti][0] + TILES[ti][1]], B)
                K.barrier()
            cw = K.sb("ml_cw", [128, 16, 4], F32, ph); cb = K.sb("ml_cb", [128, 16], F32, ph)
            gn = K.sb("ml_gn", [128, 16], F32, ph); skp = K.sb("ml_skip", [128, 16], F32, ph)
            bd = {n: K.sb("ml_" + n, [128, 4, 128], BF16, ph) for n in ("bdq", "bdk", "bdv")}
            wgt = K.sb("ml_wgt", [128, 48, 8], BF16, ph); bg = K.sb("ml_bg", [128, 8], F32, ph)
            mst = K.sb("ml_mst", [4, 16], F32, ph)
            gacc = K.sb("ml_gacc", [128, 17, 8], F32, ph)
            colsAll = K.sb("ml_cols", [128, 17, 20], F32, ph)
            decB = K.sb("ml_decB", [128, 4, 32], F32, ph)
            sel = K.sb("ml_sel", [4, 4, 128], F32, ph)
            K.dma(cw.v, din["ml_conv_w"][o_]); K.dma(cb.v, din["ml_conv_b"][o_])
            K.dma(gn.v, din["ml_norm"][o_]); K.dma(skp.v, din["ml_skip"][o_])
            K.dma(wgt.v, din["ml_w_gates"][o_], q="pool"); K.dma(bg.v, din["ml_b_gates"][o_])
            K.dma(mst.v, din["ml_m"][o_])
            K.cp(sel.v, ident[0:4, 0:4].un(2).bc([4, 4, 128]))
            xmes = K.sb("ml_xmes", [128, 4, 16, 16], F32, ph)

            def front_set(es_, tag):
                return {"xme": K.sb("ml_xme" + tag, [128, 4, 136], F32, es_), "xmb": K.sb("ml_xmb" + tag, [128, 4, 128], BF16, es_),
                        "acc": [K.sb("ml_acc%d" % j + tag, [128, 128], F32, es_) for j in range(4)], "xc": K.sb("ml_xc" + tag, [128, 4, 128], BF16, es_),
                        "qT": K.sb("ml_q" + tag, [128, 4, 128], BF16, es_), "kT": K.sb("ml_k" + tag, [128, 4, 128], BF16, es_),
                        "vT": K.sb("ml_v" + tag, [128, 4, 128], BF16, es_)}
            fb0 = front_set(ph, "0")
            w_x = K.sb("ml_wx", [128, KD, 512], BF16, ph)
            cvin = K.sb("ml_cvin", [128, 4, 16, 3], F32, ph); cvout = K.sb("ml_cvout", [128, 4, 16, 3], F32, ph)
            cvp = K.sb("ml_cvp", [128, 4, 3], F32, ph)

            def load_head(h):
                K.dma(w_x.v, din["ml_w_in"][o_][:, :, h * 512:(h + 1) * 512], q="pool")
                for n in bd: K.dma(bd[n].v, din["ml_" + n][o_][:, 4 * h:4 * h + 4, :], q="pool")
                K.dma(cvin.v, din["ml_conv"][o_][:, 4 * h:4 * h + 4, :, :])
                K.cp(xmes[:, :, :, 5:8], cvin.v)
                K.memset(fb0["xme"][:, :, 0:8], 0.0)

            def front(ci, h, need_v_T, fb, fbn):
                xme = fb["xme"]; xmb = fb["xmb"]; acc = fb["acc"]; xc = fb["xc"]; qT = fb["qT"]; kT = fb["kT"]; vT = fb["vT"]
                samp = (ci == 16)
                t0 = ci * 128
                ps = K.ps()
                for j in range(4):
                    for k in range(KD):
                        K.mm(ps[:, j * 128:(j + 1) * 128], w_x[:, k, j * 128:(j + 1) * 128], hT[:, k, t0:t0 + 128], start=(k == 0), stop=(k == KD - 1))
                if FRONT_STEPS == 0:
                    K.cp(xmb.v, ps[:, :].rr("p (f t) -> p f t", t=128)); return
                if not samp:
                    K.act(xme[:, :, 8:136], ps[:, :].rr("p (f t) -> p f t", t=128), AF.Copy)
                else:
                    K.act(xmes[:, :, :, 8:16], ps[:, :].rr("p (f s t) -> p f s t", t=8, s=16), AF.Copy)
                if not samp:
                    K.act(xmb.v, xme[:, :, 8:136], AF.Copy)
                else:
                    K.act(xmb.v.rr("p f (s t) -> p f s t", t=8), xmes[:, :, :, 8:16], AF.Copy)
                if FRONT_STEPS < 2: return
                for tap in range(4):
                    for j in range(4):
                        fc = 4 * h + j
                        if not samp:
                            src = xme[:, j, 5 + tap:5 + tap + 128]; a_ = acc[j].v
                        else:
                            src = xmes[:, j, :, 5 + tap:5 + tap + 8]; a_ = acc[j].v.rr("p (s t) -> p s t", t=8)
                        if tap == 0:
                            K.ts(a_, src, cw[:, fc, 0:1], ALU.mult)
                        else:
                            K.stt(a_, src, cw[:, fc, tap:tap + 1], a_, ALU.mult, ALU.add)
                for j in range(4):
                    fc = 4 * h + j
                    K.act(xc[:, j, :], acc[j].v, AF.Silu, bias=cb[:, fc:fc + 1])
                if FRONT_STEPS < 4: return
                for nm, src, dst in (("bdq", xc, qT), ("bdk", xc, kT), ("bdv", xmb, vT)):
                    if nm == "bdv" and not need_v_T: continue
                    ps = K.ps()
                    for j in range(4):
                        K.mm(ps[:, j * 128:(j + 1) * 128], bd[nm][:, j, :], src[:, j, :])
                    K.act(dst.v, ps[:, :].rr("p (f t) -> p f t", t=128), AF.Copy)
                if FRONT_STEPS < 5: return
                if not samp:
                    if ci == 15 and need_v_T:
                        K.cp(cvp.v, xme[:, :, 133:136])
                        K.dma(dout["ml_conv_p"][o_][:, 4 * h:4 * h + 4, :], cvp.v)
                    K.act(fbn["xme"][:, :, 5:8], xme[:, :, 133:136], AF.Copy)
                elif need_v_T:
                    K.cp(cvout.v, xmes[:, :, :, 13:16])
                    K.dma(dout["ml_conv_s"][o_][:, 4 * h:4 * h + 4, :, :], cvout.v)

            if MLSTM_MODE == 10:
                K.barrier(); return
            p1a = ExitStack()
            fb1 = front_set(p1a, "1")
            fbs = [fb0, fb1]
            for h in range(4):
                load_head(h)
                if MLSTM_MODE == 12: continue
                for ci in (range(17) if MLSTM_MODE != 13 else range(16)):
                    fb = fbs[ci % 2]
                    front(ci, h, True, fb, fbs[(ci + 1) % 2])
                    if MLSTM_MODE in (11, 13): continue
                    ps = K.ps()
                    i = 0
                    for part, src in enumerate((fb["qT"], fb["kT"], fb["vT"])):
                        for j in range(4):
                            K.mm(ps[:, 0:8], src[:, j, :], wgt[:, part * 16 + 4 * h + j, :], start=(i == 0), stop=(i == 11)); i += 1
                    K.tt(gacc[:, ci, :], ps[:, 0:8], bg.v if h == 0 else gacc[:, ci, :], ALU.add)
            K.barrier()
            p1a.close()
            if MLSTM_MODE in (1, 11, 12, 13):
                K.barrier(); return
            K.act(gacc[:, :, 4:8], gacc[:, :, 4:8], AF.Exp, scale=-1.0)
            K.act(gacc[:, :, 4:8], gacc[:, :, 4:8], AF.Ln, bias=1.0)
            with ExitStack() as p1:
                R32 = {n: K.sb("ml_r_" + n, [32, T], F32, p1) for n in ("ig", "lf", "m", "F", "wr")}
                for n in R32: K.memset(R32[n].v, 0.0)
                R_ = {n: R32[n][0:4, :] for n in R32}
                mprev = K.sb("ml_mprev", [4, 32], F32, p1); rend = K.sb("ml_rend", [4, 32], F32, p1); fst = K.sb("ml_fst", [4, 16], F32, p1)
                dec = K.sb("ml_dec", [4, 32], F32, p1)
                for ci in range(17):
                    ps = K.ps()
                    K.tr(ps[0:4, 0:128], gacc[:, ci, 0:4], ident)
                    K.tr(ps[0:4, 128:256], gacc[:, ci, 4:8], ident)
                    K.cp(R_["ig"][:, ci * 128:(ci + 1) * 128], ps[0:4, 0:128])
                    K.ts(R_["lf"][:, ci * 128:(ci + 1) * 128], ps[0:4, 128:256], -1.0, ALU.mult)
                K.scan(R_["m"][:, 0:TP], R_["lf"][:, 0:TP], R_["ig"][:, 0:TP], 0.0, ALU.add, ALU.max)
                K.scan(R_["F"][:, 0:TP], R_["lf"][:, 0:TP], R_["lf"][:, 0:TP], 0.0, ALU.add, ALU.min)
                for s in range(16):
                    sl = slice(TP + s * 8, TP + s * 8 + 8)
                    K.scan(R_["m"][:, sl], R_["lf"][:, sl], R_["ig"][:, sl], mst[:, s:s + 1], ALU.add, ALU.max)
                    K.scan(R_["F"][:, sl], R_["lf"][:, sl], R_["lf"][:, sl], 0.0, ALU.add, ALU.min)
                seg = lambda n: (R_[n][:, 0:TP].rr("p (c t) -> p c t", t=128), R_[n][:, TP:T].rr("p (c t) -> p c t", t=8))
                Fp = seg("F")[0]; mp = seg("m")[0]
                K.memset(fst[:, 0:1], 0.0); K.memset(mprev[:, 0:1], 0.0)
                K.cp(fst[:, 1:16], Fp[:, 0:15, 127]); K.cp(mprev[:, 1:16], mp[:, 0:15, 127])
                K.cp(mprev[:, 16:32], mst.v)
                K.tt(Fp, Fp, fst.v.un(2).bc([4, 16, 128]), ALU.subtract)
                K.dma(dout["ml_m_p"][o_], R_["m"][:, TP - 1:TP])
                msout = K.sb("ml_msout", [4, 16], F32, p1)
                K.cp(msout.v, seg("m")[1][:, :, 7])
                K.dma(dout["ml_m_s"][o_], msout.v)
                K.tt(R_["ig"], R_["ig"], R_["F"], ALU.subtract)
                K.tt(R_["F"], R_["F"], R_["m"], ALU.subtract)
                K.act(R_["lf"], R_["m"], AF.Exp, scale=-1.0)
                R_["wi"] = R_["m"]; R32["wi"] = R32["m"]
                K.cp(rend[:, 0:16], seg("F")[0][:, :, 127]); K.cp(rend[:, 16:32], seg("F")[1][:, :, 7])
                for pi, (off, L) in enumerate(((0, 128), (16, 8))):
                    K.tt(seg("wi")[pi], seg("F")[pi], mprev[:, off:off + 16].un(2).bc([4, 16, L]), ALU.add)
                    K.tt(seg("wr")[pi], seg("ig")[pi], rend[:, off:off + 16].un(2).bc([4, 16, L]), ALU.add)
                K.act(R_["wi"], R_["wi"], AF.Exp)
                K.act(R_["wr"], R_["wr"], AF.Exp)
                K.ts(R_["wr"], R_["wr"], 512 ** -0.5, ALU.mult)
                K.cp(dec[:, 0:16], seg("wi")[0][:, :, 127]); K.cp(dec[:, 16:32], seg("wi")[1][:, :, 7])
                for ci in range(17):
                    ps = K.ps()
                    for i, n in enumerate(("ig", "wr", "F", "wi", "lf")):
                        K.tr(ps[:, i * 32:(i + 1) * 32], R32[n][:, ci * 128:(ci + 1) * 128], ident[0:32, 0:32])
                    K.cp(colsAll[:, ci, :].rr("p (i f) -> p i f", f=4), ps[:, 0:160].rr("p (i f) -> p i f", f=32)[:, :, 0:4])
                K.barrier()
            if MLSTM_MODE == 2:
                K.barrier(); return
            with ExitStack() as p2:
                w_z = K.sb("ml_wz", [128, KD, 512], BF16, p2)
                w_o = K.sb("ml_wo", [128, 4, 1024], BF16, p2)
                CT = K.sb("ml_CT", [128, 4, 512], F32, p2); CTb = K.sb("ml_CTb", [128, 4, 512], BF16, p2)
                CT2 = K.sb("ml_CT2", [128, 4, 512], F32, p2)
                nS = K.sb("ml_nS", [128, 16, 4, 4], F32, p2); nSn = K.sb("ml_nSn", [128, 16, 4, 4], F32, p2)
                nP = K.sb("ml_nP", [128, 4, 4], F32, p2)
                nB = K.sb("ml_nB", [128, 4, 128], BF16, p2)
                ktok = K.sb("ml_ktok", [128, 512], BF16, p2); vtok = K.sb("ml_vtok", [128, 512], BF16, p2)
                vw = K.sb("ml_vw", [128, 512], BF16, p2); km = K.sb("ml_km", [128, 512], BF16, p2)
                wrb = K.sb("ml_wrb", [128, 4], BF16, p2)
                diag = [K.sb("ml_diag%d" % i, [128, 128], F32, p2) for i in range(3)]
                bcs = K.sb("ml_bcs", [128, 3, 128], F32, p2)
                arg = K.sb("ml_arg", [128, 128], F32, p2); sT = K.sb("ml_sT", [128, 128], BF16, p2)
                qw = K.sb("ml_qw", [128, 4, 128], BF16, p2)
                hden = K.sb("ml_hden", [128, 128], F32, p2)
                hh = K.sb("ml_hh", [128, 4, 128], F32, p2); hsq = K.sb("ml_hsq", [128, 4, 128], F32, p2)
                mean = K.sb("ml_mean", [128, 128], F32, p2); var = K.sb("ml_var", [128, 128], F32, p2)
                sz = K.sb("ml_sz", [128, 4, 128], BF16, p2); pre = K.sb("ml_pre", [128, 4, 128], BF16, p2)
                skx = K.sb("ml_skx", [128, 4, 128], F32, p2)
                interN = hsq; interD = mean
                K.dma(nS.v, din["ml_n"][o_])
                K.memset(nP.v, 0.0)
                for h in range(4):
                    load_head(h)
                    K.dma(w_z.v, din["ml_w_in"][o_][:, :, 2048 + h * 512:2048 + (h + 1) * 512], q="pool")
                    K.dma(w_o.v, din["ml_w_out"][o_][:, 4 * h:4 * h + 4, :], q="pool")
                    K.memset(CT.v, 0.0); K.memset(CTb.v, 0.0); K.memset(nB.v, 0.0)
                    for ci in range(17):
                        samp = (ci == 16)
                        ti = ci // 4 if not samp else 4
                        c0 = (ci % 4) * 128 if not samp else 0
                        cols = slice(c0, c0 + 128)
                        tsl = slice(ci * 128, ci * 128 + 128)
                        front(ci, h, False, fb0, fb0)
                        xc = fb0["xc"]; xmb = fb0["xmb"]; qT = fb0["qT"]; kT = fb0["kT"]
                        for nm, src, dst in (("bdk", xc, ktok), ("bdv", xmb, vtok)):
                            ps = K.ps()
                            for j in range(4):
                                K.mm(ps[:, j * 128:(j + 1) * 128], src[:, j, :], bd[nm][:, j, :])
                            K.act(dst.v, ps[:, :], AF.Copy)
                        gcol = colsAll[:, ci, h:h + 1]; wrcol = colsAll[:, ci, 4 + h:5 + h]
                        K.cp(wrb.v, colsAll[:, ci, 4:8])
                        ps = K.ps()
                        for i in range(3):
                            K.ts(diag[i].v, ident, colsAll[:, ci, 8 + 4 * i + h:9 + 4 * i + h], ALU.mult)
                        for i in range(3):
                            K.mm(ps[:, i * 128:(i + 1) * 128], ones_f.v, diag[i].v)
                        K.act(bcs.v, ps[:, 0:384].rr("p (i t) -> p i t", t=128), AF.Copy)
                        K.stt(arg.v, bcs[:, 0, :], gcol, negm[:, 1 if samp else 0, :], ALU.add, ALU.add)
                        K.act(arg.v, arg.v, AF.Exp)
                        ps = K.ps()
                        for j in range(4):
                            K.mm(ps[:, 0:128], kT[:, j, :], qT[:, j, :], start=(j == 0), stop=(j == 3))
                        K.stt(sT.v, ps[:, 0:128], 512 ** -0.5, arg.v, ALU.mult, ALU.mult)
                        K.tt(qw.v, qT.v, bcs[:, 1, :].un(1).bc([128, 4, 128]), ALU.mult)
                        pn = K.ps_pin(0); pd = K.ps_pin(1)
                        if not samp:
                            for vc in range(4):
                                K.mm(pn[:, vc * 128:(vc + 1) * 128], vtok[:, vc * 128:(vc + 1) * 128], sT.v, start=True, stop=False)
                                for dc in range(4):
                                    K.mm(pn[:, vc * 128:(vc + 1) * 128], CTb[:, dc, vc * 128:(vc + 1) * 128], qw[:, dc, :], start=False, stop=(dc == 3))
                            K.mm(pd[:, 0:128], ones_bf.v, sT.v, start=True, stop=False)
                            for dc in range(4):
                                K.mm(pd[:, 0:128], nB[:, dc, :], qw[:, dc, :], start=False, stop=(dc == 3))
                        else:
                            for vc in range(4):
                                K.mm(pn[:, vc * 128:(vc + 1) * 128], vtok[:, vc * 128:(vc + 1) * 128], sT.v, start=True, stop=True)
                            K.mm(pd[:, 0:128], ones_bf.v, sT.v, start=True, stop=True)
                            K.act(vw.v, vtok.v, AF.Copy, scale=wrcol)
                            CTs = [CT, CT2]
                            K.dma(CT.v, din["ml_CT"][o_, 0, h])
                            for s in range(16):
                                CTc = CTs[s % 2]
                                if s + 1 < 16:
                                    K.dma(CTs[(s + 1) % 2].v, din["ml_CT"][o_, s + 1, h])
                                K.act(CTb.v, CTc.v, AF.Copy)
                                K.cp(nB.v, nS[:, s, h, :].un(2).bc([128, 4, 128]))
                                ssl = slice(s * 8, s * 8 + 8)
                                pi = K.ps()
                                for vc in range(4):
                                    for dc in range(4):
                                        K.mm(pi[:, vc * 8:vc * 8 + 8], CTb[:, dc, vc * 128:(vc + 1) * 128], qw[:, dc, ssl], start=(dc == 0), stop=(dc == 3))
                                for dc in range(4):
                                    K.mm(pi[:, 32:40], nB[:, dc, :], qw[:, dc, ssl], start=(dc == 0), stop=(dc == 3))
                                K.cp(interN[:, :, ssl], pi[:, 0:32].rr("p (v t) -> p v t", t=8))
                                K.cp(interD[:, ssl], pi[:, 32:40])
                                K.ts(km.v, ktok.v, seqmask[:, s:s + 1], ALU.mult)
                                pnn = K.ps()
                                for dc in range(4):
                                    pk = K.ps()
                                    K.mm(pk[:, 0:512], km[:, dc * 128:(dc + 1) * 128], vw.v)
                                    K.stt(CTc[:, dc, :], CTc[:, dc, :], bcs[:, 1, s * 8 + 7:s * 8 + 8], pk[:, 0:512], ALU.mult, ALU.add)
                                    K.mm(pnn[:, dc * 4:(dc + 1) * 4], km[:, dc * 128:(dc + 1) * 128], wrb.v)
                                K.stt(nSn[:, s, h, :], nS[:, s, h, :], bcs[:, 1, s * 8 + 7:s * 8 + 8], pnn[:, 0:16].rr("p (d f) -> p d f", f=4)[:, :, h], ALU.mult, ALU.add)
                                K.dma(dout["ml_CT_s"][o_, s, h], CTc.v, q="pool")
                        if samp:
                            K.tt(interD.v, interD.v, pd[:, 0:128], ALU.add)
                            K.tt(interN.v, interN.v, pn[:, :].rr("p (v t) -> p v t", t=128), ALU.add)
                            den_v = interD.v; num_v = interN.v
                        else:
                            den_v = pd[:, 0:128]; num_v = pn[:, :].rr("p (v t) -> p v t", t=128)
                        K.ts(hden.v, den_v, -1.0, ALU.mult)
                        K.tt(hden.v, hden.v, den_v, ALU.max)
                        K.tt(hden.v, hden.v, bcs[:, 2, :], ALU.max)
                        K.recip(hden.v, hden.v)
                        K.tt(hh.v, num_v, hden.v.un(1).bc([128, 4, 128]), ALU.mult)
                        K.act(hsq.v, hh.v, AF.Square)
                        ps = K.ps()
                        for vc in range(4):
                            K.mm(ps[:, 0:128], ones_f.v, hh[:, vc, :], start=(vc == 0), stop=(vc == 3))
                        for vc in range(4):
                            K.mm(ps[:, 128:256], ones_f.v, hsq[:, vc, :], start=(vc == 0), stop=(vc == 3))
                        K.act(mean.v, ps[:, 0:128], AF.Copy, scale=1.0 / 512)
                        K.tt(var.v, mean.v, mean.v, ALU.mult)
                        K.stt(var.v, ps[:, 128:256], 1.0 / 512, var.v, ALU.mult, ALU.subtract)
                        K.act(var.v, var.v, AF.Ln, bias=epsb.v)
                        K.act(var.v, var.v, AF.Exp, scale=-0.5)
                        K.tt(hh.v, hh.v, mean.v.un(1).bc([128, 4, 128]), ALU.subtract)
                        K.tt(hh.v, hh.v, var.v.un(1).bc([128, 4, 128]), ALU.mult)
                        ps = K.ps()
                        for j in range(4):
                            for k in range(KD):
                                K.mm(ps[:, j * 128:(j + 1) * 128], w_z[:, k, j * 128:(j + 1) * 128], hT[:, k, tsl], start=(k == 0), stop=(k == KD - 1))
                        K.act(sz.v, ps[:, :].rr("p (f t) -> p f t", t=128), AF.Silu)
                        K.tt(hh.v, hh.v, gn[:, 4 * h:4 * h + 4].un(2).bc([128, 4, 128]), ALU.mult)
                        K.tt(skx.v, xc.v, skp[:, 4 * h:4 * h + 4].un(2).bc([128, 4, 128]), ALU.mult)
                        K.tt(hh.v, hh.v, skx.v, ALU.add)
                        K.tt(pre.v, hh.v, sz.v, ALU.mult)
                        for half in range(2):
                            ps = K.ps()
                            for dd in range(4):
                                d = half * 4 + dd
                                for j in range(4):
                                    K.mm(ps[:, dd * 128:(dd + 1) * 128], w_o[:, j, d * 128:(d + 1) * 128], pre[:, j, :], start=(j == 0), stop=(j == 3))
                            resid_add4(ti, half * 4, cols, ps[:, :].rr("p (d t) -> p d t", t=128), Gvec)
                        if not samp:
                            K.act(vw.v, vtok.v, AF.Copy, scale=wrcol)
                            pnn = K.ps()
                            for dc in range(4):
                                pk = K.ps()
                                K.mm(pk[:, 0:512], ktok[:, dc * 128:(dc + 1) * 128], vw.v)
                                K.stt(CT[:, dc, :], CT[:, dc, :], bcs[:, 1, 127:128], pk[:, 0:512], ALU.mult, ALU.add)
                                K.mm(pnn[:, dc * 4:(dc + 1) * 4], ktok[:, dc * 128:(dc + 1) * 128], wrb.v)
                            K.stt(nP[:, h, :], nP[:, h, :], bcs[:, 1, 127:128], pnn[:, 0:16].rr("p (d f) -> p d f", f=4)[:, :, h], ALU.mult, ALU.add)
                            K.act(CTb.v, CT.v, AF.Copy)
                            K.cp(nB.v, nP[:, h, :].un(2).bc([128, 4, 128]))
                            if ci == 15:
                                K.dma(dout["ml_CT_p"][o_, h], CT.v)
                K.dma(dout["ml_n_p"][o_], nP.v)
                K.dma(dout["ml_n_s"][o_], nSn.v)
                K.barrier()
        K.barrier()

    for l in range(nlayers):
        if DBG_ONLY_MLSTM:
            if l == 1: mlstm(l)
            continue
        if l == 0: ada(l)
        ffn(l, 0)
        if l % 2 == 0:
            ab_mixer(l)
        elif MLSTM_MODE:
            mlstm(l)
        ffn(l, 1)
    with ExitStack() as ph:
        if nlayers != NL: ada(NL)
        yT = [K.sb("yT%d" % i, [128, KD, n], F32, ph) for i, (o, n) in enumerate(TILES)]
        K.ts(Avec.v, mod(NL, 1), 1.0, ALU.add)
        K.tt(Avec.v, Avec.v, final_norm.v.un(2).bc([128, KD, NSEQ]), ALU.mult)
        B = mod(NL, 0)
        norm_bufs(ph, 512)
        for ti, (o, n) in enumerate(TILES):
            norm_tile(ti, yT[ti].v, B)
            K.dma(dout["yT"][:, :, o:o + n], yT[ti].v)
        K.barrier()


def _consts():
    c = np.zeros((128, 6, 128), np.float32)
    s = np.arange(128)[:, None]; t = np.arange(128)[None, :]
    c[:, C_TRI, :] = (s <= t)
    c[:, C_TRI8, :] = (s <= t) & (s // 8 == t // 8)
    c[:, C_ID, :] = (s == t)
    c[:, C_MISC, 0:16] = (np.arange(128)[:, None] // 8 == np.arange(16)[None, :])
    return c


def _bd(w):
    out = np.zeros((16, 128, 128), np.float32)
    wr = w.reshape(16, 32, 4, 4)
    for n in range(32):
        out[:, n * 4:(n + 1) * 4, n * 4:(n + 1) * 4] = wr[:, n]
    return np.ascontiguousarray(out.transpose(1, 0, 2))


def _kT(w, kc):
    return np.ascontiguousarray(w.reshape(kc, 128, -1).transpose(1, 0, 2))


def prep_shared(inp):
    f = lambda a: np.ascontiguousarray(np.asarray(a, dtype=np.float32))
    S = {}
    S["ada_w"] = f(inp["ada_w"].reshape(NL, KD, 128, 9, 1024).transpose(0, 3, 2, 1, 4))
    S["ada_b"] = f(inp["ada_b"].reshape(NL, 72, 128).transpose(2, 0, 1))
    S["fada_w"] = f(inp["final_ada_w"].reshape(KD, 128, 2, 1024).transpose(2, 1, 0, 3))
    S["fada_b"] = f(inp["final_ada_b"].reshape(16, 128).T)
    S["ffn_norm"] = f(inp["ffn_norm"].reshape(NL, 2, KD, 128).transpose(3, 0, 1, 2))
    S["mix_norm"] = f(inp["mix_norm"].reshape(NL, KD, 128).transpose(2, 0, 1))
    S["final_norm"] = f(inp["final_norm"].reshape(KD, 128).T)
    S["ffn_wg"] = f(inp["ffn_w_gate"].reshape(NL, 2, KD, 128, NG, 256).transpose(0, 1, 4, 3, 2, 5))
    S["ffn_wu"] = f(inp["ffn_w_up"].reshape(NL, 2, KD, 128, NG, 256).transpose(0, 1, 4, 3, 2, 5))
    S["ffn_wd"] = f(inp["ffn_w_down"].reshape(NL, 2, NG, 2, 128, 1024).transpose(0, 1, 2, 4, 3, 5))
    S["ab_w_in"] = f(inp["ab_w_in"].reshape(2, KD, 128, 2576).transpose(0, 2, 1, 3))
    S["ab_w_out"] = f(inp["ab_w_out"].reshape(2, KD, 128, 1024).transpose(0, 2, 1, 3))
    S["gla_wa2"] = f(np.concatenate([inp["gla_w_a2"], inp["gla_b_a"][:, None, :]], axis=1))
    S["gla_norm"] = f(inp["gla_norm"].reshape(2, 128, 1))
    S["gmlp_norm"] = f(np.broadcast_to(inp["gmlp_norm"][:, None, :], (2, 128, 128)))
    ws = np.asarray(inp["gmlp_ws"])
    S["gmlp_wT_p"] = f(ws.transpose(0, 3, 1, 2))
    w8 = ws[:, :, :8, :8]
    S["gmlp_wT_s"] = f(np.tile(w8.transpose(0, 3, 1, 2), (1, 16, 1, 16)))
    bs = np.asarray(inp["gmlp_bs"])
    S["gmlp_bs_p"] = f(np.broadcast_to(bs[:, None, :, :], (2, 128, 4, 128)))
    S["gmlp_bs_s"] = f(np.broadcast_to(np.tile(bs[:, :, :8], (1, 1, 16))[:, None, :, :], (2, 128, 4, 128)))
    S["consts"] = _consts()
    S["ml_w_in"] = f(inp["ml_w_in"].reshape(2, KD, 128, 4096).transpose(0, 2, 1, 3))
    S["ml_w_out"] = f(inp["ml_w_out"].reshape(2, 16, 128, 1024).transpose(0, 2, 1, 3))
    S["ml_conv_w"] = f(inp["ml_conv_w"].reshape(2, 4, 16, 128).transpose(0, 3, 2, 1))
    S["ml_conv_b"] = f(inp["ml_conv_b"].reshape(2, 16, 128).transpose(0, 2, 1))
    for n, k in (("ml_bdq", "ml_wq"), ("ml_bdk", "ml_wk"), ("ml_bdv", "ml_wv")):
        S[n] = np.stack([_bd(np.asarray(inp[k][o])) for o in range(2)])
    S["ml_w_gates"] = f(inp["ml_w_gates"].reshape(2, 48, 128, 8).transpose(0, 2, 1, 3))
    S["ml_b_gates"] = f(np.broadcast_to(inp["ml_b_gates"][:, None, :], (2, 128, 8)))
    S["ml_norm"] = f(inp["ml_norm"].reshape(2, 16, 128).transpose(0, 2, 1))
    S["ml_skip"] = f(inp["ml_skip"].reshape(2, 16, 128).transpose(0, 2, 1))
    return S


def prep_core(inp, i):
    f = lambda a: np.ascontiguousarray(np.asarray(a, dtype=np.float32))
    P = {}
    sl = slice(16 * i, 16 * i + 16)
    x = np.concatenate([inp["x_prompt"][i], inp["x_sample"][sl].reshape(TS, D)], axis=0)
    P["xT"] = f(x.T.reshape(KD, 128, T).transpose(1, 0, 2))
    c = np.concatenate([inp["c_prompt"][i:i + 1], inp["c_sample"][sl]], axis=0)
    P["cT"] = f(c.T.reshape(KD, 128, NSEQ).transpose(1, 0, 2))
    P["gla_S"] = f(inp["state_gla_S"][:, sl].transpose(0, 3, 1, 2, 4))
    C = inp["state_mlstm_C"][:, sl]
    P["ml_CT"] = f(C.transpose(0, 1, 2, 4, 3).reshape(2, 16, 4, 4, 128, 512).transpose(0, 1, 2, 4, 3, 5))
    P["ml_n"] = f(inp["state_mlstm_n"][:, sl].reshape(2, 16, 4, 4, 128).transpose(0, 4, 1, 2, 3))
    P["ml_m"] = f(inp["state_mlstm_m"][:, sl].transpose(0, 2, 1))
    P["ml_conv"] = f(inp["state_mlstm_conv"][:, sl].reshape(2, 16, 3, 16, 128).transpose(0, 4, 3, 1, 2))
    return P


def post_core(r):
    o = {}
    y = r["yT"].transpose(2, 1, 0).reshape(T, D)
    o["y_p"] = y[0:TP]; o["y_s"] = y[TP:].reshape(16, 8, D)
    o["s_p"] = r["gla_S_p"].transpose(0, 2, 1, 3)
    o["s_s"] = r["gla_S_s"].transpose(0, 2, 3, 1, 4)
    o["v_s"] = r["gmlp_v_s"].reshape(2, 16, 8, 512)
    o["c_p"] = r["ml_CT_p"].transpose(0, 1, 3, 2, 4).reshape(2, 4, 512, 512).transpose(0, 1, 3, 2)
    o["c_s"] = r["ml_CT_s"].transpose(0, 1, 2, 4, 3, 5).reshape(2, 16, 4, 512, 512).transpose(0, 1, 2, 4, 3)
    o["n_p"] = r["ml_n_p"].transpose(0, 2, 3, 1).reshape(2, 4, 512)
    o["n_s"] = r["ml_n_s"].transpose(0, 2, 3, 4, 1).reshape(2, 16, 4, 512)
    o["m_p"] = r["ml_m_p"].reshape(2, 4)
    o["m_s"] = r["ml_m_s"].transpose(0, 2, 1)
    o["cv_p"] = r["ml_conv_p"].transpose(0, 3, 2, 1).reshape(2, 3, 2048)
    o["cv_s"] = r["ml_conv_s"].transpose(0, 3, 4, 2, 1).reshape(2, 16, 3, 2048)
    return o


_NC_CACHE = {}


def kernel(**inputs):
    inp = {k: np.asarray(v) for k, v in inputs.items()}
    if "nc" not in _NC_CACHE:
        _NC_CACHE["nc"] = build()
    nc = _NC_CACHE["nc"]
    S = prep_shared(inp)
    in_maps = []
    for i in range(8):
        m = dict(S); m.update(prep_core(inp, i)); in_maps.append(m)
    res = run_bass_kernel_spmd(nc, in_maps, core_ids=list(range(8)))
    po = [post_core(r) for r in res.results]
    cat0 = lambda k: np.ascontiguousarray(np.stack([p[k] for p in po], axis=0)).astype(np.float32)
    cat1 = lambda k: np.ascontiguousarray(np.stack([p[k] for p in po], axis=1)).astype(np.float32)
    cat1s = lambda k: np.ascontiguousarray(np.concatenate([p[k] for p in po], axis=1)).astype(np.float32)
    y_p = cat0("y_p")
    y_s = np.ascontiguousarray(np.concatenate([p["y_s"] for p in po], axis=0)).astype(np.float32)
    return (y_p, y_s, cat1("s_p"), cat1s("s_s"), cat1s("v_s"), cat1("c_p"), cat1s("c_s"), cat1("n_p"), cat1s("n_s"),
            cat1("m_p"), cat1s("m_s"), cat1("cv_p"), cat1s("cv_s"))
```

```python
import numpy as np
from contextlib import ExitStack
import concourse.bass as bass
import concourse.mybir as mybir
from concourse.bass_utils import run_bass_kernel_spmd

F32 = mybir.dt.float32
BF16 = mybir.dt.bfloat16
AF = mybir.ActivationFunctionType
ALU = mybir.AluOpType

D = 1024; KD = 8; TP = 2048; TS = 128; T = TP + TS; NSEQ = 17
DFF = 2816; NG = 11
NL = 4
EPS = 1e-6
NEG = -1.0e30
MLSTM_MODE = 3
DBG_ONLY_MLSTM = False
FRONT_STEPS = 9
SKIP_INTER = False
SAME_ENG_SYNC = True


class V:
    def __init__(self, buf, ap):
        self.buf = buf; self.ap = ap
    def __getitem__(self, idx):
        return V(self.buf, self.ap[idx])
    def rr(self, pat, **kw):
        return V(self.buf, self.ap.rearrange(pat, **kw))
    def bc(self, shape):
        return V(self.buf, self.ap.broadcast_to(list(shape)))
    def un(self, axis):
        return V(self.buf, self.ap.unsqueeze(axis))


class Buf:
    def __init__(self, t, name):
        self.t = t; self.name = name; self.w = None; self.r = {}
    def __getitem__(self, idx):
        return V(self, self.t[idx])
    @property
    def v(self):
        return V(self, self.t[:])


class Kern:
    def __init__(self, nc, es):
        self.nc = nc; self.es = es
        self.engs = {"pe": nc.tensor, "act": nc.scalar, "dve": nc.vector, "sp": nc.sync, "pool": nc.gpsimd}
        self.sems = {}; self.cnt = {}
        for e in ["pe", "act", "dve"]:
            self.sems[e] = es.enter_context(nc.semaphore("s_" + e)); self.cnt[e] = 0
        self.rings = {}
        for q, n in (("sp", 10), ("pool", 10)):
            keys = []
            for i in range(n):
                k = "%s_d%d" % (q, i)
                self.sems[k] = es.enter_context(nc.semaphore("s_" + k)); self.cnt[k] = 0
                keys.append(k)
            self.rings[q] = [keys, 0]
        self.known = {e: {} for e in self.engs}
        self.psb = []
        for i in range(8):
            t = es.enter_context(nc.psum_tensor("psb%d" % i, [128, 512], F32))
            self.psb.append(Buf(t, "psb%d" % i))
        self.psi = 0
        self.nrot = 6
        self.ninstr = 0

    def sb(self, name, shape, dt, es=None):
        self.uid = getattr(self, "uid", 0) + 1
        t = (es or self.es).enter_context(self.nc.sbuf_tensor("sb%d_%s" % (self.uid, name), list(shape), dt))
        return Buf(t, name)

    def ps(self):
        b = self.psb[self.psi % self.nrot]; self.psi += 1
        return b

    def ps_pin(self, i):
        return self.psb[6 + i]

    def _emit(self, eng, fn, reads, writes, dma=False):
        waits = {}
        known = self.known[eng]
        def need(tok):
            if tok is None: return
            k, v = tok
            if k == eng and (eng == "pe" or not SAME_ENG_SYNC): return
            if known.get(k, 0) >= v: return
            if waits.get(k, 0) < v: waits[k] = v
        rb = []; wb = []
        for x in reads:
            if x is None or isinstance(x, (int, float)): continue
            b = x.buf if isinstance(x, V) else x
            if b is not None and b not in rb: rb.append(b)
        for x in writes:
            b = x.buf if isinstance(x, V) else x
            if b is not None and b not in wb: wb.append(b)
        for b in rb: need(b.w)
        for b in wb:
            need(b.w)
            for k, v in b.r.items(): need((k, v))
        if dma:
            keys, pos = self.rings[eng]
            k = keys[pos % len(keys)]; self.rings[eng][1] = pos + 1
            need((k, self.cnt[k]))
            self.cnt[k] += 16; tok = (k, self.cnt[k]); inc = 16
        else:
            self.cnt[eng] += 1; tok = (eng, self.cnt[eng]); inc = 1
        e = self.engs[eng]
        for k, v in waits.items():
            e.wait_ge(self.sems[k], v); known[k] = v
        ins = fn(e)
        ins.then_inc(self.sems[tok[0]], inc)
        self.ninstr += 1
        for b in rb:
            if b in wb: continue
            if b.r.get(tok[0], 0) < tok[1]: b.r[tok[0]] = tok[1]
        for b in wb:
            b.w = tok; b.r = {}
        return tok

    def barrier(self):
        for eng in self.engs:
            e = self.engs[eng]; known = self.known[eng]
            for k, v in self.cnt.items():
                if v > 0 and known.get(k, 0) < v:
                    e.wait_ge(self.sems[k], v); known[k] = v

    @staticmethod
    def _a(x):
        return x.ap if isinstance(x, V) else x

    def mm(self, out, lhsT, rhs, start=True, stop=True):
        a = self._a
        return self._emit("pe", lambda e: e.matmul(a(out), lhsT=a(lhsT), rhs=a(rhs), start=start, stop=stop), [lhsT, rhs], [out])

    def tr(self, out, in_, ident):
        a = self._a
        return self._emit("pe", lambda e: e.transpose(a(out), a(in_), a(ident)), [in_, ident], [out])

    def act(self, out, in_, func, bias=None, scale=None, accum=None):
        a = self._a
        kw = {}
        if bias is not None: kw["bias"] = a(bias)
        if scale is not None: kw["scale"] = a(scale)
        if accum is not None: kw["accum_out"] = a(accum)
        w = [out] + ([accum] if accum is not None else [])
        return self._emit("act", lambda e: e.activation(out=a(out), in_=a(in_), func=func, **kw), [in_, bias, scale], w)

    def tt(self, out, in0, in1, op, eng="dve"):
        a = self._a
        return self._emit(eng, lambda e: e.tensor_tensor(out=a(out), in0=a(in0), in1=a(in1), op=op), [in0, in1], [out])

    def ts(self, out, in0, s1, op0, s2=None, op1=None, eng="dve"):
        a = self._a
        if op1 is None:
            return self._emit(eng, lambda e: e.tensor_scalar(out=a(out), in0=a(in0), scalar1=a(s1), scalar2=None, op0=op0), [in0, s1], [out])
        return self._emit(eng, lambda e: e.tensor_scalar(out=a(out), in0=a(in0), scalar1=a(s1), scalar2=a(s2), op0=op0, op1=op1), [in0, s1, s2], [out])

    def stt(self, out, in0, scalar, in1, op0, op1, eng="dve"):
        a = self._a
        return self._emit(eng, lambda e: e.scalar_tensor_tensor(out=a(out), in0=a(in0), scalar=a(scalar), in1=a(in1), op0=op0, op1=op1), [in0, scalar, in1], [out])

    def cp(self, out, in_, eng="dve"):
        a = self._a
        return self._emit(eng, lambda e: e.tensor_copy(out=a(out), in_=a(in_)), [in_], [out])

    def memset(self, out, val, eng="dve"):
        a = self._a
        return self._emit(eng, lambda e: e.memset(a(out), val), [], [out])

    def scan(self, out, d0, d1, init, op0, op1):
        a = self._a
        return self._emit("dve", lambda e: e.tensor_tensor_scan(out=a(out), data0=a(d0), data1=a(d1), initial=a(init), op0=op0, op1=op1), [d0, d1, init], [out])

    def recip(self, out, in_):
        a = self._a
        return self._emit("dve", lambda e: e.reciprocal(out=a(out), in_=a(in_)), [in_], [out])

    def dma(self, out, in_, q="sp"):
        a = self._a
        r = [in_] if isinstance(in_, V) else []
        w = [out] if isinstance(out, V) else []
        return self._emit(q, lambda e: e.dma_start(out=a(out), in_=a(in_)), r, w, dma=True)

    def finish(self):
        self.barrier()


IN_SPECS = {}
OUT_SPECS = {}


def _specs():
    I = {}
    I["xT"] = [128, KD, T]
    I["cT"] = [128, KD, NSEQ]
    I["ada_w"] = [NL, 9, 128, KD, 1024]
    I["ada_b"] = [128, NL, 72]
    I["fada_w"] = [2, 128, KD, 1024]
    I["fada_b"] = [128, 16]
    I["ffn_norm"] = [128, NL, 2, KD]
    I["mix_norm"] = [128, NL, KD]
    I["final_norm"] = [128, KD]
    I["ffn_wg"] = [NL, 2, NG, 128, KD, 256]
    I["ffn_wu"] = [NL, 2, NG, 128, KD, 256]
    I["ffn_wd"] = [NL, 2, NG, 128, 2, 1024]
    I["ab_w_in"] = [2, 128, KD, 2576]
    I["ab_w_out"] = [2, 128, KD, 1024]
    I["gla_wa2"] = [2, 17, 256]
    I["gla_norm"] = [2, 128, 1]
    I["gmlp_norm"] = [2, 128, 128]
    I["gmlp_wT_p"] = [2, 128, 4, 128]
    I["gmlp_wT_s"] = [2, 128, 4, 128]
    I["gmlp_bs_p"] = [2, 128, 4, 128]
    I["gmlp_bs_s"] = [2, 128, 4, 128]
    I["consts"] = [128, 6, 128]
    I["gla_S"] = [2, 64, 16, 4, 128]
    I["ml_w_in"] = [2, 128, KD, 4096]
    I["ml_w_out"] = [2, 128, 16, 1024]
    I["ml_conv_w"] = [2, 128, 16, 4]
    I["ml_conv_b"] = [2, 128, 16]
    I["ml_bdq"] = [2, 128, 16, 128]
    I["ml_bdk"] = [2, 128, 16, 128]
    I["ml_bdv"] = [2, 128, 16, 128]
    I["ml_w_gates"] = [2, 128, 48, 8]
    I["ml_b_gates"] = [2, 128, 8]
    I["ml_norm"] = [2, 128, 16]
    I["ml_skip"] = [2, 128, 16]
    I["ml_CT"] = [2, 16, 4, 128, 4, 512]
    I["ml_n"] = [2, 128, 16, 4, 4]
    I["ml_m"] = [2, 4, 16]
    I["ml_conv"] = [2, 128, 16, 16, 3]
    O = {}
    O["yT"] = [128, KD, T]
    O["gla_S_p"] = [2, 64, 4, 128]
    O["gla_S_s"] = [2, 64, 16, 4, 128]
    O["gmlp_v_s"] = [2, 128, 512]
    O["ml_CT_p"] = [2, 4, 128, 4, 512]
    O["ml_CT_s"] = [2, 16, 4, 128, 4, 512]
    O["ml_n_p"] = [2, 128, 4, 4]
    O["ml_n_s"] = [2, 128, 16, 4, 4]
    O["ml_m_p"] = [2, 4, 1]
    O["ml_m_s"] = [2, 4, 16]
    O["ml_conv_p"] = [2, 128, 16, 3]
    O["ml_conv_s"] = [2, 128, 16, 16, 3]
    return I, O


IN_SPECS, OUT_SPECS = _specs()
C_TRI, C_TRI8, C_ID, C_NEG, C_NEG8, C_MISC = range(6)


def build(nlayers=NL, dbg=None):
    nc = bass.Bass("TRN2", target_bir_lowering=False)
    din = {n: nc.dram_tensor(n, s, F32, kind="ExternalInput").ap() for n, s in IN_SPECS.items()}
    dout = {n: nc.dram_tensor(n, s, F32, kind="ExternalOutput").ap() for n, s in OUT_SPECS.items()}
    with ExitStack() as es:
        K = Kern(nc, es)
        _program(nc, K, es, din, dout, nlayers)
        K.finish()
    return nc


def _program(nc, K, es, din, dout, nlayers):
    TILES = [(i * 512, 512) for i in range(4)] + [(TP, TS)]
    xT = [K.sb("xT%d" % i, [128, KD, n], F32) for i, (o, n) in enumerate(TILES)]
    consts = K.sb("consts", [128, 6, 128], F32)
    ones_bf = K.sb("ones_bf", [128, 128], BF16)
    ones_f = K.sb("ones_f", [128, 128], F32)
    negm = K.sb("negm", [128, 2, 128], F32)
    modT = K.sb("modT", [128, 72, NSEQ], F32)
    csT = K.sb("csT", [128, KD, NSEQ], BF16)
    ffn_norm = K.sb("ffn_norm", [128, NL, 2, KD], F32)
    mix_norm = K.sb("mix_norm", [128, NL, KD], F32)
    final_norm = K.sb("final_norm", [128, KD], F32)
    epsb = K.sb("epsb", [128, 1], F32)
    Avec = K.sb("Avec", [128, KD, NSEQ], F32)
    Gvec = K.sb("Gvec", [128, KD, NSEQ], F32)

    K.dma(consts.v, din["consts"])
    for i, (o, n) in enumerate(TILES):
        K.dma(xT[i].v, din["xT"][:, :, o:o + n])
    K.dma(ffn_norm.v, din["ffn_norm"]); K.dma(mix_norm.v, din["mix_norm"]); K.dma(final_norm.v, din["final_norm"])
    K.memset(ones_bf.v, 1.0); K.memset(ones_f.v, 1.0); K.memset(epsb.v, EPS)
    tri = consts[:, C_TRI, :]; tri8 = consts[:, C_TRI8, :]; ident = consts[:, C_ID, :]
    seqmask = consts[:, C_MISC, 0:16]
    K.ts(negm[:, 0, :], tri, -1.0, ALU.add, -NEG, ALU.mult)
    K.ts(negm[:, 1, :], tri8, -1.0, ALU.add, -NEG, ALU.mult)

    cTf = K.sb("cTf", [128, KD, NSEQ], F32)
    K.dma(cTf.v, din["cT"])
    K.act(csT.v, cTf.v, AF.Silu)

    def ada_steps(l, es_):
        adab = K.sb("adab", [128, 72], F32, es_)
        wbuf = [K.sb("adaw%d" % i, [128, KD, 1024], BF16, es_) for i in range(2)]
        if l < NL:
            pieces = [(din["ada_w"][l, v], v * 8) for v in range(9)]
        else:
            pieces = [(din["fada_w"][v], v * 8) for v in range(2)]

        def load(i):
            if i == 0:
                if l < NL: K.dma(adab.v, din["ada_b"][:, l, :])
                else: K.dma(adab[:, 0:16], din["fada_b"])
            K.dma(wbuf[i % 2].v, pieces[i][0], q="pool")

        def comp(i):
            wb = wbuf[i % 2]; off = pieces[i][1]
            ps = K.ps()
            for j in range(8):
                for k in range(KD):
                    K.mm(ps[:, j * NSEQ:(j + 1) * NSEQ], wb[:, k, j * 128:(j + 1) * 128], csT[:, k, :], start=(k == 0), stop=(k == KD - 1))
            K.tt(modT[:, off:off + 8, :], ps[:, 0:8 * NSEQ].rr("p (j s) -> p j s", s=NSEQ),
                 adab[:, off:off + 8].un(2).bc([128, 8, NSEQ]), ALU.add)

        n = len(pieces)
        steps = []
        for i in range(n + 1):
            def step(i=i):
                if i < n: load(i)
                if i >= 1: comp(i - 1)
            steps.append(step)
        return steps

    def ada(l):
        with ExitStack() as ph:
            for st in ada_steps(l, ph): st()
            K.barrier()

    def mod(l, v):
        return modT[:, v * 8:v * 8 + 8, :]

    def prep_AG(gamma, sc, gate, gmul):
        K.ts(Avec.v, sc, 1.0, ALU.add)
        K.tt(Avec.v, Avec.v, gamma.un(2).bc([128, KD, NSEQ]), ALU.mult)
        if gate is not None:
            K.ts(Gvec.v, gate, gmul, ALU.mult)

    def seq_affine(out, in_, A, B, ti, k):
        if ti < 4:
            K.act(out, in_, AF.Identity, bias=B[:, k, 0:1], scale=A[:, k, 0:1])
        else:
            o3 = out.rr("p (s t) -> p s t", t=8); i3 = in_.rr("p (s t) -> p s t", t=8)
            K.tt(o3, i3, A[:, k, 1:17].un(2).bc([128, 16, 8]), ALU.mult)
            K.tt(o3, o3, B[:, k, 1:17].un(2).bc([128, 16, 8]), ALU.add)

    def resid_add(ti, d, cols, ps_v, G):
        xv = xT[ti][:, d, cols]
        if ti < 4:
            K.stt(xv, ps_v, G[:, d, 0:1], xv, ALU.mult, ALU.add)
        else:
            tmp = rs_tmp.v
            K.tt(tmp.rr("p (s t) -> p s t", t=8), ps_v.rr("p (s t) -> p s t", t=8), G[:, d, 1:17].un(2).bc([128, 16, 8]), ALU.mult)
            K.tt(xv, xv, tmp, ALU.add)

    rs_tmp = K.sb("rs_tmp", [128, 128], F32)
    rs_tmp4 = K.sb("rs_tmp4", [128, 4, 128], F32)

    def resid_add4(ti, d0, cols, ps_v, G):
        xv = xT[ti][:, d0:d0 + 4, cols]
        if ti < 4:
            K.tt(rs_tmp4.v, ps_v, G[:, d0:d0 + 4, 0:1].bc([128, 4, 128]), ALU.mult)
        else:
            K.tt(rs_tmp4.v.rr("p d (s t) -> p d s t", t=8), ps_v.rr("p d (s t) -> p d s t", t=8),
                 G[:, d0:d0 + 4, 1:17].un(3).bc([128, 4, 16, 8]), ALU.mult)
        K.tt(xv, xv, rs_tmp4.v, ALU.add)

    NB = {}

    def norm_bufs(es_, n):
        NB["sq"] = K.sb("nrm_sq", [128, KD, n], BF16, es_)
        NB["rstd"] = K.sb("nrm_rstd", [128, n], F32, es_)
        NB["ntmp"] = K.sb("nrm_tmp", [128, n], F32, es_)

    def rms_rstd(ps_ss, n, dim, out):
        K.act(out[:, 0:n], ps_ss[:, 0:n], AF.Ln, bias=epsb.v, scale=1.0 / dim)
        K.act(out[:, 0:n], out[:, 0:n], AF.Exp, scale=-0.5)

    def norm_tile(ti, hdst, B):
        o, n = TILES[ti]
        sq = NB["sq"]; rstd = NB["rstd"]; ntmp = NB["ntmp"]
        K.act(sq[:, :, 0:n], xT[ti].v, AF.Square)
        ps = K.ps()
        for k in range(KD):
            K.mm(ps[:, 0:n], ones_bf.v, sq[:, k, 0:n], start=(k == 0), stop=(k == KD - 1))
        rms_rstd(ps, n, D, rstd)
        for k in range(KD):
            K.tt(ntmp[:, 0:n], xT[ti][:, k, :], rstd[:, 0:n], ALU.mult)
            seq_affine(hdst[:, k, :], ntmp[:, 0:n], Avec, B, ti, k)

    def ffn(l, w):
        with ExitStack() as ph:
            hT = [K.sb("ffn_h%d" % i, [128, KD, n], BF16, ph) for i, (o, n) in enumerate(TILES)]
            wg = [K.sb("ffn_wg%d" % i, [128, KD, 256], BF16, ph) for i in range(2)]
            wu = [K.sb("ffn_wu%d" % i, [128, KD, 256], BF16, ph) for i in range(2)]
            wd = [K.sb("ffn_wd%d" % i, [128, 2, 1024], BF16, ph) for i in range(2)]
            hid = [K.sb("ffn_hid%d" % i, [128, 2, 512], BF16, ph) for i in range(2)]
            sg = K.sb("ffn_sg", [128, 512], F32, ph)
            prep_AG(ffn_norm[:, l, w, :], mod(l, 1 if w == 0 else 7), mod(l, 2 if w == 0 else 8), 0.5)
            B = mod(l, 0 if w == 0 else 6)
            with ExitStack() as nb:
                norm_bufs(nb, 512)
                for ti in range(5):
                    norm_tile(ti, hT[ti].v, B)
                K.barrier()
            hi = 0
            asteps = ada_steps(l + 1, ph) if w == 1 else []
            for g in range(NG):
                b = g % 2
                if g < len(asteps): asteps[g]()
                K.dma(wg[b].v, din["ffn_wg"][l, w, g], q="pool")
                K.dma(wu[b].v, din["ffn_wu"][l, w, g], q="pool")
                K.dma(wd[b].v, din["ffn_wd"][l, w, g], q="pool")
                for ti, (o, n) in enumerate(TILES):
                    hb = hid[hi % 2]; hi += 1
                    for c in range(2):
                        pg = K.ps(); pu = K.ps()
                        for k in range(KD):
                            K.mm(pg[:, 0:n], wg[b][:, k, c * 128:(c + 1) * 128], hT[ti][:, k, :], start=(k == 0), stop=(k == KD - 1))
                        for k in range(KD):
                            K.mm(pu[:, 0:n], wu[b][:, k, c * 128:(c + 1) * 128], hT[ti][:, k, :], start=(k == 0), stop=(k == KD - 1))
                        K.act(sg[:, 0:n], pg[:, 0:n], AF.Silu)
                        K.tt(hb[:, c, 0:n], sg[:, 0:n], pu[:, 0:n], ALU.mult)
                    for d in range(KD):
                        py = K.ps()
                        for c in range(2):
                            K.mm(py[:, 0:n], wd[b][:, c, d * 128:(d + 1) * 128], hb[:, c, 0:n], start=(c == 0), stop=(c == 1))
                        resid_add(ti, d, slice(0, n), py[:, 0:n], Gvec)
            K.barrier()

    def ab_mixer(l):
        e = l // 2
        with ExitStack() as ph:
            w_in = K.sb("ab_win", [128, KD, 2576], BF16, ph)
            w_out = K.sb("ab_wout", [128, KD, 1024], BF16, ph)
            wa2 = K.sb("ab_wa2", [17, 256], F32, ph)
            gnorm = K.sb("ab_gn", [128, 1], F32, ph)
            vnB = K.sb("ab_vn", [128, 128], F32, ph)
            wTp = K.sb("ab_wTp", [128, 4, 128], F32, ph); wTs = K.sb("ab_wTs", [128, 4, 128], F32, ph)
            wTpb = K.sb("ab_wTpb", [128, 4, 128], BF16, ph); wTsb = K.sb("ab_wTsb", [128, 4, 128], BF16, ph)
            bsp = K.sb("ab_bsp", [128, 4, 128], F32, ph); bss = K.sb("ab_bss", [128, 4, 128], F32, ph)
            hT = K.sb("ab_h", [128, KD, 128], BF16, ph)
            qT = K.sb("ab_q", [64, 4, 128], F32, ph); kT = K.sb("ab_k", [64, 4, 128], F32, ph)
            qb = K.sb("ab_qb", [64, 4, 128], BF16, ph); kb = K.sb("ab_kb", [64, 4, 128], BF16, ph)
            kd = K.sb("ab_kd", [64, 4, 128], F32, ph); kdt = K.sb("ab_kdt", [128, 4, 64], BF16, ph)
            a_aug = K.sb("ab_aaug", [17, 128], F32, ph)
            la = K.sb("ab_la", [128, 256], F32, ph)
            eb = K.sb("ab_eb", [64, 4, 128], F32, ph); enb = K.sb("ab_enb", [64, 4, 128], F32, ph)
            vtok = K.sb("ab_vtok", [128, 512], BF16, ph)
            sgT = K.sb("ab_sg", [128, 4, 128], F32, ph)
            uT = K.sb("ab_u", [128, 4, 128], F32, ph)
            vbn = K.sb("ab_vbn", [128, 512], F32, ph); vbnb = K.sb("ab_vbnb", [128, 512], BF16, ph)
            ssq = K.sb("ab_ssq", [128, 4], F32, ph); vjunk = K.sb("ab_vj", [128, 128], F32, ph)
            sT = K.sb("ab_sT", [128, 128], BF16, ph)
            S = K.sb("ab_S", [64, 4, 128], F32, ph); Sb = K.sb("ab_Sb", [64, 4, 128], BF16, ph)
            Ss = K.sb("ab_Ss", [64, 4, 128], F32, ph); Ssn = K.sb("ab_Ssn", [64, 4, 128], F32, ph)
            Ssb = K.sb("ab_Ssb", [64, 4, 128], BF16, ph)
            R = K.sb("ab_R", [128, 4, 128], BF16, ph)
            ebl = K.sb("ab_ebl", [64, 4, 16], F32, ph)
            oT = K.sb("ab_o", [128, 4, 128], F32, ph); osq = K.sb("ab_osq", [128, 4, 128], BF16, ph)
            orstd = K.sb("ab_orstd", [128, 4, 128], F32, ph)
            mix = K.sb("ab_mix", [128, KD, 128], BF16, ph)
            ztmp = K.sb("ab_ztmp", [128, 128], F32, ph)
            K.dma(w_in.v, din["ab_w_in"][e], q="pool"); K.dma(w_out.v, din["ab_w_out"][e], q="pool")
            K.dma(wa2.v, din["gla_wa2"][e]); K.dma(gnorm.v, din["gla_norm"][e]); K.dma(vnB.v, din["gmlp_norm"][e])
            K.dma(wTp.v, din["gmlp_wT_p"][e]); K.dma(wTs.v, din["gmlp_wT_s"][e])
            K.dma(bsp.v, din["gmlp_bs_p"][e]); K.dma(bss.v, din["gmlp_bs_s"][e])
            K.tt(wTpb.v, wTp.v, tri.un(1).bc([128, 4, 128]), ALU.mult)
            K.tt(wTsb.v, wTs.v, tri8.un(1).bc([128, 4, 128]), ALU.mult)
            K.memset(a_aug.v, 1.0)
            K.memset(S.v, 0.0); K.memset(Sb.v, 0.0)
            prep_AG(mix_norm[:, l, :], mod(l, 4), mod(l, 5), 1.0)
            B = mod(l, 3)
            norm_bufs(ph, 128)
            sq = NB["sq"]; rstd = NB["rstd"]; ntmp = NB["ntmp"]
            for ci in range(17):
                samp = (ci == 16)
                ti = ci // 4 if not samp else 4
                c0 = (ci % 4) * 128 if not samp else 0
                cols = slice(c0, c0 + 128)
                mask = tri8 if samp else tri
                K.act(sq[:, :, 0:128], xT[ti][:, :, cols], AF.Square)
                ps = K.ps()
                for k in range(KD):
                    K.mm(ps[:, 0:128], ones_bf.v, sq[:, k, 0:128], start=(k == 0), stop=(k == KD - 1))
                rms_rstd(ps, 128, D, rstd)
                for k in range(KD):
                    K.tt(ntmp[:, 0:128], xT[ti][:, k, cols], rstd[:, 0:128], ALU.mult)
                    seq_affine(hT[:, k, :], ntmp[:, 0:128], Avec, B, ti, k)
                def proj_f(col0, m, dst_ps):
                    for k in range(KD):
                        K.mm(dst_ps, w_in[:, k, col0:col0 + m], hT[:, k, :], start=(k == 0), stop=(k == KD - 1))
                ps = K.ps()
                for h in range(4):
                    proj_f(h * 64, 64, ps[0:64, h * 128:(h + 1) * 128])
                K.act(qT.v, ps[0:64, :].rr("p (h t) -> p h t", t=128), AF.Copy, scale=64 ** -0.5)
                ps = K.ps()
                for h in range(4):
                    proj_f(256 + h * 64, 64, ps[0:64, h * 128:(h + 1) * 128])
                K.cp(kT.v, ps[0:64, :].rr("p (h t) -> p h t", t=128))
                ps = K.ps()
                proj_f(1536, 16, ps[0:16, 0:128])
                K.cp(a_aug[0:16, :], ps[0:16, 0:128])
                ps = K.ps()
                for k in range(KD):
                    K.mm(ps[:, 0:512], hT[:, k, :], w_in[:, k, 512:1024], start=(k == 0), stop=(k == KD - 1))
                K.act(vtok.v, ps[:, 0:512], AF.Copy)
                ps = K.ps()
                for j in range(4):
                    proj_f(1024 + j * 128, 128, ps[:, j * 128:(j + 1) * 128])
                K.act(sgT.v, ps[:, :].rr("p (h t) -> p h t", t=128), AF.Silu)
                ps = K.ps()
                for j in range(4):
                    proj_f(1552 + j * 128, 128, ps[:, j * 128:(j + 1) * 128])
                K.cp(uT.v, ps[:, :].rr("p (h t) -> p h t", t=128))
                ps = K.ps()
                for k in range(KD):
                    K.mm(ps[:, 0:512], hT[:, k, :], w_in[:, k, 2576 - 512:2576], start=(k == 0), stop=(k == KD - 1))
                for g in range(4):
                    K.act(vjunk.v, ps[:, g * 128:(g + 1) * 128], AF.Square, accum=ssq[:, g:g + 1])
                K.act(ssq.v, ssq.v, AF.Ln, bias=epsb.v, scale=1.0 / 128)
                K.act(ssq.v, ssq.v, AF.Exp, scale=-0.5)
                for g in range(4):
                    K.stt(vbn[:, g * 128:(g + 1) * 128], ps[:, g * 128:(g + 1) * 128], ssq[:, g:g + 1], vnB.v, ALU.mult, ALU.mult)
                K.cp(vbnb.v, vbn.v)
                if samp:
                    K.dma(dout["gmlp_v_s"][e], vbn.v)
                ps = K.ps()
                K.mm(ps[:, 0:256], a_aug.v, wa2.v)
                K.act(la.v, ps[:, 0:256], AF.Exp, scale=-1.0)
                K.act(la.v, la.v, AF.Ln, bias=1.0)
                ps = K.ps()
                for h in range(4):
                    K.mm(ps[0:64, h * 128:(h + 1) * 128], la[:, h * 64:(h + 1) * 64], mask)
                bp = ps[0:64, :].rr("p (h t) -> p h t", t=128)
                K.act(eb.v, bp, AF.Exp, scale=-1.0 / 16)
                K.act(enb.v, bp, AF.Exp, scale=1.0 / 16)
                K.tt(qb.v, qT.v, eb.v, ALU.mult)
                K.tt(kb.v, kT.v, enb.v, ALU.mult)
                if not samp:
                    K.tt(kd.v, kT.v, enb.v, ALU.mult)
                    for h in range(4):
                        K.ts(kd[:, h, :], kd[:, h, :], eb[:, h, 127:128], ALU.mult)
                else:
                    K.cp(ebl.v, eb.v.rr("p h (s t) -> p h s t", t=8)[:, :, :, 7])
                    K.tt(kd.v, kT.v, enb.v, ALU.mult)
                    K.tt(kd.v.rr("p h (s t) -> p h s t", t=8), kd.v.rr("p h (s t) -> p h s t", t=8),
                         ebl.v.un(3).bc([64, 4, 16, 8]), ALU.mult)
                ps = K.ps()
                for h in range(4):
                    K.tr(ps[:, h * 64:(h + 1) * 64], kd[:, h, :], ident[0:64, 0:64])
                K.cp(kdt.v, ps[:, 0:256].rr("p (h d) -> p h d", d=64))
                for h in range(4):
                    vh = vtok[:, h * 128:(h + 1) * 128]
                    ps = K.ps()
                    K.mm(ps[:, 0:128], kb[:, h, :], qb[:, h, :])
                    K.tt(sT.v, ps[:, 0:128], mask, ALU.mult)
                    po = K.ps_pin(0)
                    if not samp:
                        K.mm(po[:, 0:128], vh, sT.v, start=True, stop=False)
                        K.mm(po[:, 0:128], Sb[:, h, :], qb[:, h, :], start=False, stop=True)
                        K.cp(oT[:, h, :], po[:, 0:128])
                        pk = K.ps()
                        K.mm(pk[0:64, 0:128], kdt[:, h, :], vh)
                        K.stt(S[:, h, :], S[:, h, :], eb[:, h, 127:128], pk[0:64, 0:128], ALU.mult, ALU.add)
                        K.act(Sb[:, h, :], S[:, h, :], AF.Copy)
                    else:
                        K.mm(po[:, 0:128], vh, sT.v, start=True, stop=False)
                        for q4 in range(4):
                            sl = slice(q4 * 4, q4 * 4 + 4)
                            K.dma(Ss.v, din["gla_S"][e][:, sl, h, :])
                            K.cp(Ssb.v, Ss.v)
                            for s4 in range(4):
                                s_ = q4 * 4 + s4
                                K.mm(po[:, s_ * 8:(s_ + 1) * 8], Ssb[:, s4, :], qb[:, h, s_ * 8:(s_ + 1) * 8], start=False, stop=(s_ == 15))
                            K.tt(R.v, vh.un(1).bc([128, 4, 128]), seqmask[:, sl].un(2).bc([128, 4, 128]), ALU.mult)
                            pk = K.ps()
                            K.mm(pk[0:64, 0:512], kdt[:, h, :], R.v.rr("p s v -> p (s v)"))
                            K.tt(Ssn.v, Ss.v, ebl[:, h, sl].un(2).bc([64, 4, 128]), ALU.mult)
                            K.tt(Ssn.v, Ssn.v, pk[0:64, 0:512].rr("p (s v) -> p s v", v=128), ALU.add)
                            K.dma(dout["gla_S_s"][e][:, sl, h, :], Ssn.v)
                        K.cp(oT[:, h, :], po[:, 0:128])
                if ci == 15:
                    K.dma(dout["gla_S_p"][e], S.v)
                K.act(osq.v, oT.v, AF.Square)
                ps = K.ps()
                for h in range(4):
                    K.mm(ps[:, h * 128:(h + 1) * 128], ones_bf.v, osq[:, h, :])
                K.act(orstd.v, ps[:, :].rr("p (h t) -> p h t", t=128), AF.Ln, bias=epsb.v, scale=1.0 / 128)
                K.act(orstd.v, orstd.v, AF.Exp, scale=-0.5)
                K.tt(oT.v, oT.v, orstd.v, ALU.mult)
                K.stt(mix[:, 0:4, :], oT.v, gnorm.v, sgT.v, ALU.mult, ALU.mult)
                wTb = wTsb if samp else wTpb
                bsB = bss if samp else bsp
                for g in range(4):
                    ps = K.ps()
                    K.mm(ps[:, 0:128], vbnb[:, g * 128:(g + 1) * 128], wTb[:, g, :])
                    K.tt(ztmp.v, ps[:, 0:128], bsB[:, g, :], ALU.add)
                    K.tt(mix[:, 4 + g, :], ztmp.v, uT[:, g, :], ALU.mult)
                for half in range(2):
                    ps = K.ps()
                    for dd in range(4):
                        d = half * 4 + dd
                        for k in range(KD):
                            K.mm(ps[:, dd * 128:(dd + 1) * 128], w_out[:, k, d * 128:(d + 1) * 128], mix[:, k, :], start=(k == 0), stop=(k == KD - 1))
                    resid_add4(ti, half * 4, cols, ps[:, :].rr("p (d t) -> p d t", t=128), Gvec)
            K.barrier()

    def mlstm(l):
        o_ = l // 2
        with ExitStack() as ph:
            hT = K.sb("ml_h", [128, KD, T], BF16, ph)
            prep_AG(mix_norm[:, l, :], mod(l, 4), mod(l, 5), 1.0)
            B = mod(l, 3)
            with ExitStack() as nb:
                norm_bufs(nb, 512)
                for ti in range(5):
                    norm_tile(ti, hT[:, :, TILES[ti][0]:TILES[ti][0] + TILES[ti][1]], B)
                K.barrier()
            cw = K.sb("ml_cw", [128, 16, 4], F32, ph); cb = K.sb("ml_cb", [128, 16], F32, ph)
            gn = K.sb("ml_gn", [128, 16], F32, ph); skp = K.sb("ml_skip", [128, 16], F32, ph)
            bd = {n: K.sb("ml_" + n, [128, 4, 128], BF16, ph) for n in ("bdq", "bdk", "bdv")}
            wgt = K.sb("ml_wgt", [128, 48, 8], BF16, ph); bg = K.sb("ml_bg", [128, 8], F32, ph)
            mst = K.sb("ml_mst", [4, 16], F32, ph)
            gacc = K.sb("ml_gacc", [128, 17, 8], F32, ph)
            colsAll = K.sb("ml_cols", [128, 17, 20], F32, ph)
            decB = K.sb("ml_decB", [128, 4, 32], F32, ph)
            sel = K.sb("ml_sel", [4, 4, 128], F32, ph)
            K.dma(cw.v, din["ml_conv_w"][o_]); K.dma(cb.v, din["ml_conv_b"][o_])
            K.dma(gn.v, din["ml_norm"][o_]); K.dma(skp.v, din["ml_skip"][o_])
            K.dma(wgt.v, din["ml_w_gates"][o_], q="pool"); K.dma(bg.v, din["ml_b_gates"][o_])
            K.dma(mst.v, din["ml_m"][o_])
            K.cp(sel.v, ident[0:4, 0:4].un(2).bc([4, 4, 128]))
            xmes = K.sb("ml_xmes", [128, 4, 16, 16], F32, ph)

            def front_set(es_, tag):
                return {"xme": K.sb("ml_xme" + tag, [128, 4, 136], F32, es_), "xmb": K.sb("ml_xmb" + tag, [128, 4, 128], BF16, es_),
                        "acc": [K.sb("ml_acc%d" % j + tag, [128, 128], F32, es_) for j in range(4)], "xc": K.sb("ml_xc" + tag, [128, 4, 128], BF16, es_),
                        "qT": K.sb("ml_q" + tag, [128, 4, 128], BF16, es_), "kT": K.sb("ml_k" + tag, [128, 4, 128], BF16, es_),
                        "vT": K.sb("ml_v" + tag, [128, 4, 128], BF16, es_)}
            fb0 = front_set(ph, "0")
            w_x = K.sb("ml_wx", [128, KD, 512], BF16, ph)
            cvin = K.sb("ml_cvin", [128, 4, 16, 3], F32, ph); cvout = K.sb("ml_cvout", [128, 4, 16, 3], F32, ph)
            cvp = K.sb("ml_cvp", [128, 4, 3], F32, ph)

            WS = {"w_x": w_x, "bd": bd}

            def load_w(h, ws):
                K.dma(ws["w_x"].v, din["ml_w_in"][o_][:, :, h * 512:(h + 1) * 512], q="pool")
                for n in ws["bd"]: K.dma(ws["bd"][n].v, din["ml_" + n][o_][:, 4 * h:4 * h + 4, :], q="pool")

            def load_head(h):
                K.dma(cvin.v, din["ml_conv"][o_][:, 4 * h:4 * h + 4, :, :])
                K.cp(xmes[:, :, :, 5:8], cvin.v)
                K.memset(fb0["xme"][:, :, 0:8], 0.0)

            def front(ci, h, need_v_T, fb, fbn):
                xme = fb["xme"]; xmb = fb["xmb"]; acc = fb["acc"]; xc = fb["xc"]; qT = fb["qT"]; kT = fb["kT"]; vT = fb["vT"]
                samp = (ci == 16)
                t0 = ci * 128
                ps = K.ps()
                for j in range(4):
                    for k in range(KD):
                        K.mm(ps[:, j * 128:(j + 1) * 128], WS["w_x"][:, k, j * 128:(j + 1) * 128], hT[:, k, t0:t0 + 128], start=(k == 0), stop=(k == KD - 1))
                if FRONT_STEPS == 0:
                    K.cp(xmb.v, ps[:, :].rr("p (f t) -> p f t", t=128)); return
                if not samp:
                    K.act(xme[:, :, 8:136], ps[:, :].rr("p (f t) -> p f t", t=128), AF.Copy)
                else:
                    K.act(xmes[:, :, :, 8:16], ps[:, :].rr("p (f s t) -> p f s t", t=8, s=16), AF.Copy)
                if not samp:
                    K.act(xmb.v, xme[:, :, 8:136], AF.Copy)
                else:
                    K.act(xmb.v.rr("p f (s t) -> p f s t", t=8), xmes[:, :, :, 8:16], AF.Copy)
                if FRONT_STEPS < 2: return
                for tap in range(4):
                    for j in range(4):
                        fc = 4 * h + j
                        if not samp:
                            src = xme[:, j, 5 + tap:5 + tap + 128]; a_ = acc[j].v
                        else:
                            src = xmes[:, j, :, 5 + tap:5 + tap + 8]; a_ = acc[j].v.rr("p (s t) -> p s t", t=8)
                        if tap == 0:
                            K.ts(a_, src, cw[:, fc, 0:1], ALU.mult)
                        else:
                            K.stt(a_, src, cw[:, fc, tap:tap + 1], a_, ALU.mult, ALU.add)
                for j in range(4):
                    fc = 4 * h + j
                    K.act(xc[:, j, :], acc[j].v, AF.Silu, bias=cb[:, fc:fc + 1])
                if FRONT_STEPS < 4: return
                for nm, src, dst in (("bdq", xc, qT), ("bdk", xc, kT), ("bdv", xmb, vT)):
                    if nm == "bdv" and not need_v_T: continue
                    ps = K.ps()
                    for j in range(4):
                        K.mm(ps[:, j * 128:(j + 1) * 128], WS["bd"][nm][:, j, :], src[:, j, :])
                    K.act(dst.v, ps[:, :].rr("p (f t) -> p f t", t=128), AF.Copy)
                if FRONT_STEPS < 5: return
                if not samp:
                    if ci == 15 and need_v_T:
                        K.cp(cvp.v, xme[:, :, 133:136])
                        K.dma(dout["ml_conv_p"][o_][:, 4 * h:4 * h + 4, :], cvp.v)
                    K.act(fbn["xme"][:, :, 5:8], xme[:, :, 133:136], AF.Copy)
                elif need_v_T:
                    K.cp(cvout.v, xmes[:, :, :, 13:16])
                    K.dma(dout["ml_conv_s"][o_][:, 4 * h:4 * h + 4, :, :], cvout.v)

            if MLSTM_MODE == 10:
                K.barrier(); return
            p1a = ExitStack()
            fb1 = front_set(p1a, "1")
            fbs = [fb0, fb1]
            w_x2 = K.sb("ml_wx2", [128, KD, 512], BF16, p1a)
            bd2 = {n: K.sb("ml2_" + n, [128, 4, 128], BF16, p1a) for n in ("bdq", "bdk", "bdv")}
            wsets = [{"w_x": w_x, "bd": bd}, {"w_x": w_x2, "bd": bd2}]
            K.nrot = 8
            load_w(0, wsets[0])
            for h in range(4):
                if h + 1 < 4: load_w(h + 1, wsets[(h + 1) % 2])
                WS.update(wsets[h % 2])
                load_head(h)
                if MLSTM_MODE == 12: continue
                for ci in (range(17) if MLSTM_MODE != 13 else range(16)):
                    fb = fbs[ci % 2]
                    front(ci, h, True, fb, fbs[(ci + 1) % 2])
                    if MLSTM_MODE in (11, 13): continue
                    ps = K.ps()
                    i = 0
                    for part, src in enumerate((fb["qT"], fb["kT"], fb["vT"])):
                        for j in range(4):
                            K.mm(ps[:, 0:8], src[:, j, :], wgt[:, part * 16 + 4 * h + j, :], start=(i == 0), stop=(i == 11)); i += 1
                    K.tt(gacc[:, ci, :], ps[:, 0:8], bg.v if h == 0 else gacc[:, ci, :], ALU.add)
            K.barrier()
            p1a.close()
            K.nrot = 6
            WS.update(wsets[0])
            if MLSTM_MODE in (1, 11, 12, 13):
                K.barrier(); return
            K.act(gacc[:, :, 4:8], gacc[:, :, 4:8], AF.Exp, scale=-1.0)
            K.act(gacc[:, :, 4:8], gacc[:, :, 4:8], AF.Ln, bias=1.0)
            with ExitStack() as p1:
                R32 = {n: K.sb("ml_r_" + n, [32, T], F32, p1) for n in ("ig", "lf", "m", "F", "wr")}
                for n in R32: K.memset(R32[n].v, 0.0)
                R_ = {n: R32[n][0:4, :] for n in R32}
                mprev = K.sb("ml_mprev", [4, 32], F32, p1); rend = K.sb("ml_rend", [4, 32], F32, p1); fst = K.sb("ml_fst", [4, 16], F32, p1)
                dec = K.sb("ml_dec", [4, 32], F32, p1)
                for ci in range(17):
                    ps = K.ps()
                    K.tr(ps[0:4, 0:128], gacc[:, ci, 0:4], ident)
                    K.tr(ps[0:4, 128:256], gacc[:, ci, 4:8], ident)
                    K.cp(R_["ig"][:, ci * 128:(ci + 1) * 128], ps[0:4, 0:128])
                    K.ts(R_["lf"][:, ci * 128:(ci + 1) * 128], ps[0:4, 128:256], -1.0, ALU.mult)
                K.scan(R_["m"][:, 0:TP], R_["lf"][:, 0:TP], R_["ig"][:, 0:TP], 0.0, ALU.add, ALU.max)
                K.scan(R_["F"][:, 0:TP], R_["lf"][:, 0:TP], R_["lf"][:, 0:TP], 0.0, ALU.add, ALU.min)
                for s in range(16):
                    sl = slice(TP + s * 8, TP + s * 8 + 8)
                    K.scan(R_["m"][:, sl], R_["lf"][:, sl], R_["ig"][:, sl], mst[:, s:s + 1], ALU.add, ALU.max)
                    K.scan(R_["F"][:, sl], R_["lf"][:, sl], R_["lf"][:, sl], 0.0, ALU.add, ALU.min)
                seg = lambda n: (R_[n][:, 0:TP].rr("p (c t) -> p c t", t=128), R_[n][:, TP:T].rr("p (c t) -> p c t", t=8))
                Fp = seg("F")[0]; mp = seg("m")[0]
                K.memset(fst[:, 0:1], 0.0); K.memset(mprev[:, 0:1], 0.0)
                K.cp(fst[:, 1:16], Fp[:, 0:15, 127]); K.cp(mprev[:, 1:16], mp[:, 0:15, 127])
                K.cp(mprev[:, 16:32], mst.v)
                K.tt(Fp, Fp, fst.v.un(2).bc([4, 16, 128]), ALU.subtract)
                K.dma(dout["ml_m_p"][o_], R_["m"][:, TP - 1:TP])
                msout = K.sb("ml_msout", [4, 16], F32, p1)
                K.cp(msout.v, seg("m")[1][:, :, 7])
                K.dma(dout["ml_m_s"][o_], msout.v)
                K.tt(R_["ig"], R_["ig"], R_["F"], ALU.subtract)
                K.tt(R_["F"], R_["F"], R_["m"], ALU.subtract)
                K.act(R_["lf"], R_["m"], AF.Exp, scale=-1.0)
                R_["wi"] = R_["m"]; R32["wi"] = R32["m"]
                K.cp(rend[:, 0:16], seg("F")[0][:, :, 127]); K.cp(rend[:, 16:32], seg("F")[1][:, :, 7])
                for pi, (off, L) in enumerate(((0, 128), (16, 8))):
                    K.tt(seg("wi")[pi], seg("F")[pi], mprev[:, off:off + 16].un(2).bc([4, 16, L]), ALU.add)
                    K.tt(seg("wr")[pi], seg("ig")[pi], rend[:, off:off + 16].un(2).bc([4, 16, L]), ALU.add)
                K.act(R_["wi"], R_["wi"], AF.Exp)
                K.act(R_["wr"], R_["wr"], AF.Exp)
                K.ts(R_["wr"], R_["wr"], 512 ** -0.5, ALU.mult)
                K.cp(dec[:, 0:16], seg("wi")[0][:, :, 127]); K.cp(dec[:, 16:32], seg("wi")[1][:, :, 7])
                for ci in range(17):
                    ps = K.ps()
                    for i, n in enumerate(("ig", "wr", "F", "wi", "lf")):
                        K.tr(ps[:, i * 32:(i + 1) * 32], R32[n][:, ci * 128:(ci + 1) * 128], ident[0:32, 0:32])
                    K.cp(colsAll[:, ci, :].rr("p (i f) -> p i f", f=4), ps[:, 0:160].rr("p (i f) -> p i f", f=32)[:, :, 0:4])
                K.barrier()
            if MLSTM_MODE == 2:
                K.barrier(); return
            with ExitStack() as p2:
                w_z = K.sb("ml_wz", [128, KD, 512], BF16, p2)
                w_o = K.sb("ml_wo", [128, 4, 1024], BF16, p2)
                CT = K.sb("ml_CT", [128, 4, 512], F32, p2); CTb = K.sb("ml_CTb", [128, 4, 512], BF16, p2)
                CT2 = K.sb("ml_CT2", [128, 4, 512], F32, p2)
                nS = K.sb("ml_nS", [128, 16, 4, 4], F32, p2); nSn = K.sb("ml_nSn", [128, 16, 4, 4], F32, p2)
                nP = K.sb("ml_nP", [128, 4, 4], F32, p2)
                nB = K.sb("ml_nB", [128, 4, 128], BF16, p2)
                ktok = K.sb("ml_ktok", [128, 512], BF16, p2); vtok = K.sb("ml_vtok", [128, 512], BF16, p2)
                vw = K.sb("ml_vw", [128, 512], BF16, p2); km = K.sb("ml_km", [128, 512], BF16, p2)
                wrb = K.sb("ml_wrb", [128, 4], BF16, p2)
                diag = [K.sb("ml_diag%d" % i, [128, 128], F32, p2) for i in range(3)]
                bcs = K.sb("ml_bcs", [128, 3, 128], F32, p2)
                arg = K.sb("ml_arg", [128, 128], F32, p2); sT = K.sb("ml_sT", [128, 128], BF16, p2)
                qw = K.sb("ml_qw", [128, 4, 128], BF16, p2)
                hden = K.sb("ml_hden", [128, 128], F32, p2)
                hh = K.sb("ml_hh", [128, 4, 128], F32, p2); hsq = K.sb("ml_hsq", [128, 4, 128], F32, p2)
                mean = K.sb("ml_mean", [128, 128], F32, p2); var = K.sb("ml_var", [128, 128], F32, p2)
                sz = K.sb("ml_sz", [128, 4, 128], BF16, p2); pre = K.sb("ml_pre", [128, 4, 128], BF16, p2)
                skx = K.sb("ml_skx", [128, 4, 128], F32, p2)
                interN = hsq; interD = mean
                K.dma(nS.v, din["ml_n"][o_])
                K.memset(nP.v, 0.0)
                for h in range(4):
                    load_w(h, wsets[0])
                    load_head(h)
                    K.dma(w_z.v, din["ml_w_in"][o_][:, :, 2048 + h * 512:2048 + (h + 1) * 512], q="pool")
                    K.dma(w_o.v, din["ml_w_out"][o_][:, 4 * h:4 * h + 4, :], q="pool")
                    K.memset(CT.v, 0.0); K.memset(CTb.v, 0.0); K.memset(nB.v, 0.0)
                    for ci in range(17):
                        samp = (ci == 16)
                        ti = ci // 4 if not samp else 4
                        c0 = (ci % 4) * 128 if not samp else 0
                        cols = slice(c0, c0 + 128)
                        tsl = slice(ci * 128, ci * 128 + 128)
                        front(ci, h, False, fb0, fb0)
                        xc = fb0["xc"]; xmb = fb0["xmb"]; qT = fb0["qT"]; kT = fb0["kT"]
                        for nm, src, dst in (("bdk", xc, ktok), ("bdv", xmb, vtok)):
                            ps = K.ps()
                            for j in range(4):
                                K.mm(ps[:, j * 128:(j + 1) * 128], src[:, j, :], bd[nm][:, j, :])
                            K.act(dst.v, ps[:, :], AF.Copy)
                        gcol = colsAll[:, ci, h:h + 1]; wrcol = colsAll[:, ci, 4 + h:5 + h]
                        K.cp(wrb.v, colsAll[:, ci, 4:8])
                        ps = K.ps()
                        for i in range(3):
                            K.ts(diag[i].v, ident, colsAll[:, ci, 8 + 4 * i + h:9 + 4 * i + h], ALU.mult)
                        for i in range(3):
                            K.mm(ps[:, i * 128:(i + 1) * 128], ones_f.v, diag[i].v)
                        K.act(bcs.v, ps[:, 0:384].rr("p (i t) -> p i t", t=128), AF.Copy)
                        K.stt(arg.v, bcs[:, 0, :], gcol, negm[:, 1 if samp else 0, :], ALU.add, ALU.add)
                        K.act(arg.v, arg.v, AF.Exp)
                        ps = K.ps()
                        for j in range(4):
                            K.mm(ps[:, 0:128], kT[:, j, :], qT[:, j, :], start=(j == 0), stop=(j == 3))
                        K.stt(sT.v, ps[:, 0:128], 512 ** -0.5, arg.v, ALU.mult, ALU.mult)
                        K.tt(qw.v, qT.v, bcs[:, 1, :].un(1).bc([128, 4, 128]), ALU.mult)
                        pn = K.ps_pin(0); pd = K.ps_pin(1)
                        if not samp:
                            for vc in range(4):
                                K.mm(pn[:, vc * 128:(vc + 1) * 128], vtok[:, vc * 128:(vc + 1) * 128], sT.v, start=True, stop=False)
                                for dc in range(4):
                                    K.mm(pn[:, vc * 128:(vc + 1) * 128], CTb[:, dc, vc * 128:(vc + 1) * 128], qw[:, dc, :], start=False, stop=(dc == 3))
                            K.mm(pd[:, 0:128], ones_bf.v, sT.v, start=True, stop=False)
                            for dc in range(4):
                                K.mm(pd[:, 0:128], nB[:, dc, :], qw[:, dc, :], start=False, stop=(dc == 3))
                        else:
                            for vc in range(4):
                                K.mm(pn[:, vc * 128:(vc + 1) * 128], vtok[:, vc * 128:(vc + 1) * 128], sT.v, start=True, stop=True)
                            K.mm(pd[:, 0:128], ones_bf.v, sT.v, start=True, stop=True)
                            K.act(vw.v, vtok.v, AF.Copy, scale=wrcol)
                            CTs = [CT, CT2]
                            K.dma(CT.v, din["ml_CT"][o_, 0, h])
                            for s in range(16):
                                CTc = CTs[s % 2]
                                if s + 1 < 16:
                                    K.dma(CTs[(s + 1) % 2].v, din["ml_CT"][o_, s + 1, h])
                                K.act(CTb.v, CTc.v, AF.Copy)
                                K.cp(nB.v, nS[:, s, h, :].un(2).bc([128, 4, 128]))
                                ssl = slice(s * 8, s * 8 + 8)
                                pi = K.ps()
                                for vc in range(4):
                                    for dc in range(4):
                                        K.mm(pi[:, vc * 8:vc * 8 + 8], CTb[:, dc, vc * 128:(vc + 1) * 128], qw[:, dc, ssl], start=(dc == 0), stop=(dc == 3))
                                for dc in range(4):
                                    K.mm(pi[:, 32:40], nB[:, dc, :], qw[:, dc, ssl], start=(dc == 0), stop=(dc == 3))
                                K.cp(interN[:, :, ssl], pi[:, 0:32].rr("p (v t) -> p v t", t=8))
                                K.cp(interD[:, ssl], pi[:, 32:40])
                                K.ts(km.v, ktok.v, seqmask[:, s:s + 1], ALU.mult)
                                pnn = K.ps()
                                for dc in range(4):
                                    pk = K.ps()
                                    K.mm(pk[:, 0:512], km[:, dc * 128:(dc + 1) * 128], vw.v)
                                    K.stt(CTc[:, dc, :], CTc[:, dc, :], bcs[:, 1, s * 8 + 7:s * 8 + 8], pk[:, 0:512], ALU.mult, ALU.add)
                                    K.mm(pnn[:, dc * 4:(dc + 1) * 4], km[:, dc * 128:(dc + 1) * 128], wrb.v)
                                K.stt(nSn[:, s, h, :], nS[:, s, h, :], bcs[:, 1, s * 8 + 7:s * 8 + 8], pnn[:, 0:16].rr("p (d f) -> p d f", f=4)[:, :, h], ALU.mult, ALU.add)
                                K.dma(dout["ml_CT_s"][o_, s, h], CTc.v, q="pool")
                        if samp:
                            K.tt(interD.v, interD.v, pd[:, 0:128], ALU.add)
                            K.tt(interN.v, interN.v, pn[:, :].rr("p (v t) -> p v t", t=128), ALU.add)
                            den_v = interD.v; num_v = interN.v
                        else:
                            den_v = pd[:, 0:128]; num_v = pn[:, :].rr("p (v t) -> p v t", t=128)
                        K.ts(hden.v, den_v, -1.0, ALU.mult)
                        K.tt(hden.v, hden.v, den_v, ALU.max)
                        K.tt(hden.v, hden.v, bcs[:, 2, :], ALU.max)
                        K.recip(hden.v, hden.v)
                        K.tt(hh.v, num_v, hden.v.un(1).bc([128, 4, 128]), ALU.mult)
                        K.act(hsq.v, hh.v, AF.Square)
                        ps = K.ps()
                        for vc in range(4):
                            K.mm(ps[:, 0:128], ones_f.v, hh[:, vc, :], start=(vc == 0), stop=(vc == 3))
                        for vc in range(4):
                            K.mm(ps[:, 128:256], ones_f.v, hsq[:, vc, :], start=(vc == 0), stop=(vc == 3))
                        K.act(mean.v, ps[:, 0:128], AF.Copy, scale=1.0 / 512)
                        K.tt(var.v, mean.v, mean.v, ALU.mult)
                        K.stt(var.v, ps[:, 128:256], 1.0 / 512, var.v, ALU.mult, ALU.subtract)
                        K.act(var.v, var.v, AF.Ln, bias=epsb.v)
                        K.act(var.v, var.v, AF.Exp, scale=-0.5)
                        K.tt(hh.v, hh.v, mean.v.un(1).bc([128, 4, 128]), ALU.subtract)
                        K.tt(hh.v, hh.v, var.v.un(1).bc([128, 4, 128]), ALU.mult)
                        ps = K.ps()
                        for j in range(4):
                            for k in range(KD):
                                K.mm(ps[:, j * 128:(j + 1) * 128], w_z[:, k, j * 128:(j + 1) * 128], hT[:, k, tsl], start=(k == 0), stop=(k == KD - 1))
                        K.act(sz.v, ps[:, :].rr("p (f t) -> p f t", t=128), AF.Silu)
                        K.tt(hh.v, hh.v, gn[:, 4 * h:4 * h + 4].un(2).bc([128, 4, 128]), ALU.mult)
                        K.tt(skx.v, xc.v, skp[:, 4 * h:4 * h + 4].un(2).bc([128, 4, 128]), ALU.mult)
                        K.tt(hh.v, hh.v, skx.v, ALU.add)
                        K.tt(pre.v, hh.v, sz.v, ALU.mult)
                        for half in range(2):
                            ps = K.ps()
                            for dd in range(4):
                                d = half * 4 + dd
                                for j in range(4):
                                    K.mm(ps[:, dd * 128:(dd + 1) * 128], w_o[:, j, d * 128:(d + 1) * 128], pre[:, j, :], start=(j == 0), stop=(j == 3))
                            resid_add4(ti, half * 4, cols, ps[:, :].rr("p (d t) -> p d t", t=128), Gvec)
                        if not samp:
                            K.act(vw.v, vtok.v, AF.Copy, scale=wrcol)
                            pnn = K.ps()
                            for dc in range(4):
                                pk = K.ps()
                                K.mm(pk[:, 0:512], ktok[:, dc * 128:(dc + 1) * 128], vw.v)
                                K.stt(CT[:, dc, :], CT[:, dc, :], bcs[:, 1, 127:128], pk[:, 0:512], ALU.mult, ALU.add)
                                K.mm(pnn[:, dc * 4:(dc + 1) * 4], ktok[:, dc * 128:(dc + 1) * 128], wrb.v)
                            K.stt(nP[:, h, :], nP[:, h, :], bcs[:, 1, 127:128], pnn[:, 0:16].rr("p (d f) -> p d f", f=4)[:, :, h], ALU.mult, ALU.add)
                            K.act(CTb.v, CT.v, AF.Copy)
                            K.cp(nB.v, nP[:, h, :].un(2).bc([128, 4, 128]))
                            if ci == 15:
                                K.dma(dout["ml_CT_p"][o_, h], CT.v)
                K.dma(dout["ml_n_p"][o_], nP.v)
                K.dma(dout["ml_n_s"][o_], nSn.v)
                K.barrier()
        K.barrier()

    for l in range(nlayers):
        if DBG_ONLY_MLSTM:
            if l == 1: mlstm(l)
            continue
        if l == 0: ada(l)
        ffn(l, 0)
        if l % 2 == 0:
            ab_mixer(l)
        elif MLSTM_MODE:
            mlstm(l)
        ffn(l, 1)
    with ExitStack() as ph:
        if nlayers != NL: ada(NL)
        yT = [K.sb("yT%d" % i, [128, KD, n], F32, ph) for i, (o, n) in enumerate(TILES)]
        K.ts(Avec.v, mod(NL, 1), 1.0, ALU.add)
        K.tt(Avec.v, Avec.v, final_norm.v.un(2).bc([128, KD, NSEQ]), ALU.mult)
        B = mod(NL, 0)
        norm_bufs(ph, 512)
        for ti, (o, n) in enumerate(TILES):
            norm_tile(ti, yT[ti].v, B)
            K.dma(dout["yT"][:, :, o:o + n], yT[ti].v)
        K.barrier()


def _consts():
    c = np.zeros((128, 6, 128), np.float32)
    s = np.arange(128)[:, None]; t = np.arange(128)[None, :]
    c[:, C_TRI, :] = (s <= t)
    c[:, C_TRI8, :] = (s <= t) & (s // 8 == t // 8)
    c[:, C_ID, :] = (s == t)
    c[:, C_MISC, 0:16] = (np.arange(128)[:, None] // 8 == np.arange(16)[None, :])
    return c


def _bd(w):
    out = np.zeros((16, 128, 128), np.float32)
    wr = w.reshape(16, 32, 4, 4)
    for n in range(32):
        out[:, n * 4:(n + 1) * 4, n * 4:(n + 1) * 4] = wr[:, n]
    return np.ascontiguousarray(out.transpose(1, 0, 2))


def _kT(w, kc):
    return np.ascontiguousarray(w.reshape(kc, 128, -1).transpose(1, 0, 2))


def prep_shared(inp):
    f = lambda a: np.ascontiguousarray(np.asarray(a, dtype=np.float32))
    S = {}
    S["ada_w"] = f(inp["ada_w"].reshape(NL, KD, 128, 9, 1024).transpose(0, 3, 2, 1, 4))
    S["ada_b"] = f(inp["ada_b"].reshape(NL, 72, 128).transpose(2, 0, 1))
    S["fada_w"] = f(inp["final_ada_w"].reshape(KD, 128, 2, 1024).transpose(2, 1, 0, 3))
    S["fada_b"] = f(inp["final_ada_b"].reshape(16, 128).T)
    S["ffn_norm"] = f(inp["ffn_norm"].reshape(NL, 2, KD, 128).transpose(3, 0, 1, 2))
    S["mix_norm"] = f(inp["mix_norm"].reshape(NL, KD, 128).transpose(2, 0, 1))
    S["final_norm"] = f(inp["final_norm"].reshape(KD, 128).T)
    S["ffn_wg"] = f(inp["ffn_w_gate"].reshape(NL, 2, KD, 128, NG, 256).transpose(0, 1, 4, 3, 2, 5))
    S["ffn_wu"] = f(inp["ffn_w_up"].reshape(NL, 2, KD, 128, NG, 256).transpose(0, 1, 4, 3, 2, 5))
    S["ffn_wd"] = f(inp["ffn_w_down"].reshape(NL, 2, NG, 2, 128, 1024).transpose(0, 1, 2, 4, 3, 5))
    S["ab_w_in"] = f(inp["ab_w_in"].reshape(2, KD, 128, 2576).transpose(0, 2, 1, 3))
    S["ab_w_out"] = f(inp["ab_w_out"].reshape(2, KD, 128, 1024).transpose(0, 2, 1, 3))
    S["gla_wa2"] = f(np.concatenate([inp["gla_w_a2"], inp["gla_b_a"][:, None, :]], axis=1))
    S["gla_norm"] = f(inp["gla_norm"].reshape(2, 128, 1))
    S["gmlp_norm"] = f(np.broadcast_to(inp["gmlp_norm"][:, None, :], (2, 128, 128)))
    ws = np.asarray(inp["gmlp_ws"])
    S["gmlp_wT_p"] = f(ws.transpose(0, 3, 1, 2))
    w8 = ws[:, :, :8, :8]
    S["gmlp_wT_s"] = f(np.tile(w8.transpose(0, 3, 1, 2), (1, 16, 1, 16)))
    bs = np.asarray(inp["gmlp_bs"])
    S["gmlp_bs_p"] = f(np.broadcast_to(bs[:, None, :, :], (2, 128, 4, 128)))
    S["gmlp_bs_s"] = f(np.broadcast_to(np.tile(bs[:, :, :8], (1, 1, 16))[:, None, :, :], (2, 128, 4, 128)))
    S["consts"] = _consts()
    S["ml_w_in"] = f(inp["ml_w_in"].reshape(2, KD, 128, 4096).transpose(0, 2, 1, 3))
    S["ml_w_out"] = f(inp["ml_w_out"].reshape(2, 16, 128, 1024).transpose(0, 2, 1, 3))
    S["ml_conv_w"] = f(inp["ml_conv_w"].reshape(2, 4, 16, 128).transpose(0, 3, 2, 1))
    S["ml_conv_b"] = f(inp["ml_conv_b"].reshape(2, 16, 128).transpose(0, 2, 1))
    for n, k in (("ml_bdq", "ml_wq"), ("ml_bdk", "ml_wk"), ("ml_bdv", "ml_wv")):
        S[n] = np.stack([_bd(np.asarray(inp[k][o])) for o in range(2)])
    S["ml_w_gates"] = f(inp["ml_w_gates"].reshape(2, 48, 128, 8).transpose(0, 2, 1, 3))
    S["ml_b_gates"] = f(np.broadcast_to(inp["ml_b_gates"][:, None, :], (2, 128, 8)))
    S["ml_norm"] = f(inp["ml_norm"].reshape(2, 16, 128).transpose(0, 2, 1))
    S["ml_skip"] = f(inp["ml_skip"].reshape(2, 16, 128).transpose(0, 2, 1))
    return S


def prep_core(inp, i):
    f = lambda a: np.ascontiguousarray(np.asarray(a, dtype=np.float32))
    P = {}
    sl = slice(16 * i, 16 * i + 16)
    x = np.concatenate([inp["x_prompt"][i], inp["x_sample"][sl].reshape(TS, D)], axis=0)
    P["xT"] = f(x.T.reshape(KD, 128, T).transpose(1, 0, 2))
    c = np.concatenate([inp["c_prompt"][i:i + 1], inp["c_sample"][sl]], axis=0)
    P["cT"] = f(c.T.reshape(KD, 128, NSEQ).transpose(1, 0, 2))
    P["gla_S"] = f(inp["state_gla_S"][:, sl].transpose(0, 3, 1, 2, 4))
    C = inp["state_mlstm_C"][:, sl]
    P["ml_CT"] = f(C.transpose(0, 1, 2, 4, 3).reshape(2, 16, 4, 4, 128, 512).transpose(0, 1, 2, 4, 3, 5))
    P["ml_n"] = f(inp["state_mlstm_n"][:, sl].reshape(2, 16, 4, 4, 128).transpose(0, 4, 1, 2, 3))
    P["ml_m"] = f(inp["state_mlstm_m"][:, sl].transpose(0, 2, 1))
    P["ml_conv"] = f(inp["state_mlstm_conv"][:, sl].reshape(2, 16, 3, 16, 128).transpose(0, 4, 3, 1, 2))
    return P


def post_core(r):
    o = {}
    y = r["yT"].transpose(2, 1, 0).reshape(T, D)
    o["y_p"] = y[0:TP]; o["y_s"] = y[TP:].reshape(16, 8, D)
    o["s_p"] = r["gla_S_p"].transpose(0, 2, 1, 3)
    o["s_s"] = r["gla_S_s"].transpose(0, 2, 3, 1, 4)
    o["v_s"] = r["gmlp_v_s"].reshape(2, 16, 8, 512)
    o["c_p"] = r["ml_CT_p"].transpose(0, 1, 3, 2, 4).reshape(2, 4, 512, 512).transpose(0, 1, 3, 2)
    o["c_s"] = r["ml_CT_s"].transpose(0, 1, 2, 4, 3, 5).reshape(2, 16, 4, 512, 512).transpose(0, 1, 2, 4, 3)
    o["n_p"] = r["ml_n_p"].transpose(0, 2, 3, 1).reshape(2, 4, 512)
    o["n_s"] = r["ml_n_s"].transpose(0, 2, 3, 4, 1).reshape(2, 16, 4, 512)
    o["m_p"] = r["ml_m_p"].reshape(2, 4)
    o["m_s"] = r["ml_m_s"].transpose(0, 2, 1)
    o["cv_p"] = r["ml_conv_p"].transpose(0, 3, 2, 1).reshape(2, 3, 2048)
    o["cv_s"] = r["ml_conv_s"].transpose(0, 3, 4, 2, 1).reshape(2, 16, 3, 2048)
    return o


_NC_CACHE = {}


def kernel(**inputs):
    inp = {k: np.asarray(v) for k, v in inputs.items()}
    if "nc" not in _NC_CACHE:
        _NC_CACHE["nc"] = build()
    nc = _NC_CACHE["nc"]
    S = prep_shared(inp)
    in_maps = []
    for i in range(8):
        m = dict(S); m.update(prep_core(inp, i)); in_maps.append(m)
    res = run_bass_kernel_spmd(nc, in_maps, core_ids=list(range(8)))
    po = [post_core(r) for r in res.results]
    cat0 = lambda k: np.ascontiguousarray(np.stack([p[k] for p in po], axis=0)).astype(np.float32)
    cat1 = lambda k: np.ascontiguousarray(np.stack([p[k] for p in po], axis=1)).astype(np.float32)
    cat1s = lambda k: np.ascontiguousarray(np.concatenate([p[k] for p in po], axis=1)).astype(np.float32)
    y_p = cat0("y_p")
    y_s = np.ascontiguousarray(np.concatenate([p["y_s"] for p in po], axis=0)).astype(np.float32)
    return (y_p, y_s, cat1("s_p"), cat1s("s_s"), cat1s("v_s"), cat1("c_p"), cat1s("c_s"), cat1("n_p"), cat1s("n_s"),
            cat1("m_p"), cat1s("m_s"), cat1("cv_p"), cat1s("cv_s"))
```

```python
import numpy as np
from contextlib import ExitStack
import concourse.bass as bass
import concourse.mybir as mybir
from concourse.bass_utils import run_bass_kernel_spmd

F32 = mybir.dt.float32
BF16 = mybir.dt.bfloat16
AF = mybir.ActivationFunctionType
ALU = mybir.AluOpType

D = 1024; KD = 8; TP = 2048; TS = 128; T = TP + TS; NSEQ = 17
DFF = 2816; NG = 11
NL = 4
EPS = 1e-6
NEG = -1.0e30
MLSTM_MODE = 3
DBG_ONLY_MLSTM = False
FRONT_STEPS = 9
SKIP_INTER = False
SAME_ENG_SYNC = True


class V:
    def __init__(self, buf, ap):
        self.buf = buf; self.ap = ap
    def __getitem__(self, idx):
        return V(self.buf, self.ap[idx])
    def rr(self, pat, **kw):
        return V(self.buf, self.ap.rearrange(pat, **kw))
    def bc(self, shape):
        return V(self.buf, self.ap.broadcast_to(list(shape)))
    def un(self, axis):
        return V(self.buf, self.ap.unsqueeze(axis))


class Buf:
    def __init__(self, t, name):
        self.t = t; self.name = name; self.w = None; self.r = {}
    def __getitem__(self, idx):
        return V(self, self.t[idx])
    @property
    def v(self):
        return V(self, self.t[:])


class Kern:
    def __init__(self, nc, es):
        self.nc = nc; self.es = es
        self.engs = {"pe": nc.tensor, "act": nc.scalar, "dve": nc.vector, "sp": nc.sync, "pool": nc.gpsimd}
        self.sems = {}; self.cnt = {}
        for e in ["pe", "act", "dve"]:
            self.sems[e] = es.enter_context(nc.semaphore("s_" + e)); self.cnt[e] = 0
        self.rings = {}
        for q, n in (("sp", 10), ("pool", 10)):
            keys = []
            for i in range(n):
                k = "%s_d%d" % (q, i)
                self.sems[k] = es.enter_context(nc.semaphore("s_" + k)); self.cnt[k] = 0
                keys.append(k)
            self.rings[q] = [keys, 0]
        self.known = {e: {} for e in self.engs}
        self.psb = []
        for i in range(8):
            t = es.enter_context(nc.psum_tensor("psb%d" % i, [128, 512], F32))
            self.psb.append(Buf(t, "psb%d" % i))
        self.psi = 0
        self.nrot = 6
        self.ninstr = 0

    def sb(self, name, shape, dt, es=None):
        self.uid = getattr(self, "uid", 0) + 1
        t = (es or self.es).enter_context(self.nc.sbuf_tensor("sb%d_%s" % (self.uid, name), list(shape), dt))
        return Buf(t, name)

    def ps(self):
        b = self.psb[self.psi % self.nrot]; self.psi += 1
        return b

    def ps_pin(self, i):
        return self.psb[6 + i]

    def _emit(self, eng, fn, reads, writes, dma=False):
        waits = {}
        known = self.known[eng]
        def need(tok):
            if tok is None: return
            k, v = tok
            if k == eng and (eng == "pe" or not SAME_ENG_SYNC): return
            if known.get(k, 0) >= v: return
            if waits.get(k, 0) < v: waits[k] = v
        rb = []; wb = []
        for x in reads:
            if x is None or isinstance(x, (int, float)): continue
            b = x.buf if isinstance(x, V) else x
            if b is not None and b not in rb: rb.append(b)
        for x in writes:
            b = x.buf if isinstance(x, V) else x
            if b is not None and b not in wb: wb.append(b)
        for b in rb: need(b.w)
        for b in wb:
            need(b.w)
            for k, v in b.r.items(): need((k, v))
        if dma:
            keys, pos = self.rings[eng]
            k = keys[pos % len(keys)]; self.rings[eng][1] = pos + 1
            need((k, self.cnt[k]))
            self.cnt[k] += 16; tok = (k, self.cnt[k]); inc = 16
        else:
            self.cnt[eng] += 1; tok = (eng, self.cnt[eng]); inc = 1
        e = self.engs[eng]
        for k, v in waits.items():
            e.wait_ge(self.sems[k], v); known[k] = v
        ins = fn(e)
        ins.then_inc(self.sems[tok[0]], inc)
        self.ninstr += 1
        for b in rb:
            if b in wb: continue
            if b.r.get(tok[0], 0) < tok[1]: b.r[tok[0]] = tok[1]
        for b in wb:
            b.w = tok; b.r = {}
        return tok

    def barrier(self):
        for eng in self.engs:
            e = self.engs[eng]; known = self.known[eng]
            for k, v in self.cnt.items():
                if v > 0 and known.get(k, 0) < v:
                    e.wait_ge(self.sems[k], v); known[k] = v

    @staticmethod
    def _a(x):
        return x.ap if isinstance(x, V) else x

    def mm(self, out, lhsT, rhs, start=True, stop=True):
        a = self._a
        return self._emit("pe", lambda e: e.matmul(a(out), lhsT=a(lhsT), rhs=a(rhs), start=start, stop=stop), [lhsT, rhs], [out])

    def tr(self, out, in_, ident):
        a = self._a
        return self._emit("pe", lambda e: e.transpose(a(out), a(in_), a(ident)), [in_, ident], [out])

    def act(self, out, in_, func, bias=None, scale=None, accum=None):
        a = self._a
        kw = {}
        if bias is not None: kw["bias"] = a(bias)
        if scale is not None: kw["scale"] = a(scale)
        if accum is not None: kw["accum_out"] = a(accum)
        w = [out] + ([accum] if accum is not None else [])
        return self._emit("act", lambda e: e.activation(out=a(out), in_=a(in_), func=func, **kw), [in_, bias, scale], w)

    def tt(self, out, in0, in1, op, eng="dve"):
        a = self._a
        return self._emit(eng, lambda e: e.tensor_tensor(out=a(out), in0=a(in0), in1=a(in1), op=op), [in0, in1], [out])

    def ts(self, out, in0, s1, op0, s2=None, op1=None, eng="dve"):
        a = self._a
        if op1 is None:
            return self._emit(eng, lambda e: e.tensor_scalar(out=a(out), in0=a(in0), scalar1=a(s1), scalar2=None, op0=op0), [in0, s1], [out])
        return self._emit(eng, lambda e: e.tensor_scalar(out=a(out), in0=a(in0), scalar1=a(s1), scalar2=a(s2), op0=op0, op1=op1), [in0, s1, s2], [out])

    def stt(self, out, in0, scalar, in1, op0, op1, eng="dve"):
        a = self._a
        return self._emit(eng, lambda e: e.scalar_tensor_tensor(out=a(out), in0=a(in0), scalar=a(scalar), in1=a(in1), op0=op0, op1=op1), [in0, scalar, in1], [out])

    def cp(self, out, in_, eng="dve"):
        a = self._a
        return self._emit(eng, lambda e: e.tensor_copy(out=a(out), in_=a(in_)), [in_], [out])

    def memset(self, out, val, eng="dve"):
        a = self._a
        return self._emit(eng, lambda e: e.memset(a(out), val), [], [out])

    def scan(self, out, d0, d1, init, op0, op1):
        a = self._a
        return self._emit("dve", lambda e: e.tensor_tensor_scan(out=a(out), data0=a(d0), data1=a(d1), initial=a(init), op0=op0, op1=op1), [d0, d1, init], [out])

    def recip(self, out, in_):
        a = self._a
        return self._emit("dve", lambda e: e.reciprocal(out=a(out), in_=a(in_)), [in_], [out])

    def dma(self, out, in_, q="sp"):
        a = self._a
        r = [in_] if isinstance(in_, V) else []
        w = [out] if isinstance(out, V) else []
        return self._emit(q, lambda e: e.dma_start(out=a(out), in_=a(in_)), r, w, dma=True)

    def finish(self):
        self.barrier()


IN_SPECS = {}
OUT_SPECS = {}


def _specs():
    I = {}
    I["xT"] = [128, KD, T]
    I["cT"] = [128, KD, NSEQ]
    I["ada_w"] = [NL, 9, 128, KD, 1024]
    I["ada_b"] = [128, NL, 72]
    I["fada_w"] = [2, 128, KD, 1024]
    I["fada_b"] = [128, 16]
    I["ffn_norm"] = [128, NL, 2, KD]
    I["mix_norm"] = [128, NL, KD]
    I["final_norm"] = [128, KD]
    I["ffn_wg"] = [NL, 2, NG, 128, KD, 256]
    I["ffn_wu"] = [NL, 2, NG, 128, KD, 256]
    I["ffn_wd"] = [NL, 2, NG, 128, 2, 1024]
    I["ab_w_in"] = [2, 128, KD, 2576]
    I["ab_w_out"] = [2, 128, KD, 1024]
    I["gla_wa2"] = [2, 17, 256]
    I["gla_norm"] = [2, 128, 1]
    I["gmlp_norm"] = [2, 128, 128]
    I["gmlp_wT_p"] = [2, 128, 4, 128]
    I["gmlp_wT_s"] = [2, 128, 4, 128]
    I["gmlp_bs_p"] = [2, 128, 4, 128]
    I["gmlp_bs_s"] = [2, 128, 4, 128]
    I["consts"] = [128, 6, 128]
    I["gla_S"] = [2, 64, 16, 4, 128]
    I["ml_w_in"] = [2, 128, KD, 4096]
    I["ml_w_out"] = [2, 128, 16, 1024]
    I["ml_conv_w"] = [2, 128, 16, 4]
    I["ml_conv_b"] = [2, 128, 16]
    I["ml_bdq"] = [2, 128, 16, 128]
    I["ml_bdk"] = [2, 128, 16, 128]
    I["ml_bdv"] = [2, 128, 16, 128]
    I["ml_w_gates"] = [2, 128, 48, 8]
    I["ml_b_gates"] = [2, 128, 8]
    I["ml_norm"] = [2, 128, 16]
    I["ml_skip"] = [2, 128, 16]
    I["ml_CT"] = [2, 16, 4, 128, 4, 512]
    I["ml_n"] = [2, 128, 16, 4, 4]
    I["ml_m"] = [2, 4, 16]
    I["ml_conv"] = [2, 128, 16, 16, 3]
    O = {}
    O["yT"] = [128, KD, T]
    O["gla_S_p"] = [2, 64, 4, 128]
    O["gla_S_s"] = [2, 64, 16, 4, 128]
    O["gmlp_v_s"] = [2, 128, 512]
    O["ml_CT_p"] = [2, 4, 128, 4, 512]
    O["ml_CT_s"] = [2, 16, 4, 128, 4, 512]
    O["ml_n_p"] = [2, 128, 4, 4]
    O["ml_n_s"] = [2, 128, 16, 4, 4]
    O["ml_m_p"] = [2, 4, 1]
    O["ml_m_s"] = [2, 4, 16]
    O["ml_conv_p"] = [2, 128, 16, 3]
    O["ml_conv_s"] = [2, 128, 16, 16, 3]
    return I, O


IN_SPECS, OUT_SPECS = _specs()
C_TRI, C_TRI8, C_ID, C_NEG, C_NEG8, C_MISC = range(6)


def build(nlayers=NL, dbg=None):
    nc = bass.Bass("TRN2", target_bir_lowering=False)
    din = {n: nc.dram_tensor(n, s, F32, kind="ExternalInput").ap() for n, s in IN_SPECS.items()}
    dout = {n: nc.dram_tensor(n, s, F32, kind="ExternalOutput").ap() for n, s in OUT_SPECS.items()}
    with ExitStack() as es:
        K = Kern(nc, es)
        _program(nc, K, es, din, dout, nlayers)
        K.finish()
    return nc


def _program(nc, K, es, din, dout, nlayers):
    TILES = [(i * 512, 512) for i in range(4)] + [(TP, TS)]
    xT = [K.sb("xT%d" % i, [128, KD, n], F32) for i, (o, n) in enumerate(TILES)]
    consts = K.sb("consts", [128, 6, 128], F32)
    ones_bf = K.sb("ones_bf", [128, 128], BF16)
    ones_f = K.sb("ones_f", [128, 128], F32)
    negm = K.sb("negm", [128, 2, 128], F32)
    modT = K.sb("modT", [128, 72, NSEQ], F32)
    csT = K.sb("csT", [128, KD, NSEQ], BF16)
    ffn_norm = K.sb("ffn_norm", [128, NL, 2, KD], F32)
    mix_norm = K.sb("mix_norm", [128, NL, KD], F32)
    final_norm = K.sb("final_norm", [128, KD], F32)
    epsb = K.sb("epsb", [128, 1], F32)
    Avec = K.sb("Avec", [128, KD, NSEQ], F32)
    Gvec = K.sb("Gvec", [128, KD, NSEQ], F32)

    K.dma(consts.v, din["consts"])
    for i, (o, n) in enumerate(TILES):
        K.dma(xT[i].v, din["xT"][:, :, o:o + n])
    K.dma(ffn_norm.v, din["ffn_norm"]); K.dma(mix_norm.v, din["mix_norm"]); K.dma(final_norm.v, din["final_norm"])
    K.memset(ones_bf.v, 1.0); K.memset(ones_f.v, 1.0); K.memset(epsb.v, EPS)
    tri = consts[:, C_TRI, :]; tri8 = consts[:, C_TRI8, :]; ident = consts[:, C_ID, :]
    seqmask = consts[:, C_MISC, 0:16]
    K.ts(negm[:, 0, :], tri, -1.0, ALU.add, -NEG, ALU.mult)
    K.ts(negm[:, 1, :], tri8, -1.0, ALU.add, -NEG, ALU.mult)

    cTf = K.sb("cTf", [128, KD, NSEQ], F32)
    K.dma(cTf.v, din["cT"])
    K.act(csT.v, cTf.v, AF.Silu)

    def ada_steps(l, es_):
        adab = K.sb("adab", [128, 72], F32, es_)
        wbuf = [K.sb("adaw%d" % i, [128, KD, 1024], BF16, es_) for i in range(2)]
        if l < NL:
            pieces = [(din["ada_w"][l, v], v * 8) for v in range(9)]
        else:
            pieces = [(din["fada_w"][v], v * 8) for v in range(2)]

        def load(i):
            if i == 0:
                if l < NL: K.dma(adab.v, din["ada_b"][:, l, :])
                else: K.dma(adab[:, 0:16], din["fada_b"])
            K.dma(wbuf[i % 2].v, pieces[i][0], q="pool")

        def comp(i):
            wb = wbuf[i % 2]; off = pieces[i][1]
            ps = K.ps()
            for j in range(8):
                for k in range(KD):
                    K.mm(ps[:, j * NSEQ:(j + 1) * NSEQ], wb[:, k, j * 128:(j + 1) * 128], csT[:, k, :], start=(k == 0), stop=(k == KD - 1))
            K.tt(modT[:, off:off + 8, :], ps[:, 0:8 * NSEQ].rr("p (j s) -> p j s", s=NSEQ),
                 adab[:, off:off + 8].un(2).bc([128, 8, NSEQ]), ALU.add)

        n = len(pieces)
        steps = []
        for i in range(n + 1):
            def step(i=i):
                if i < n: load(i)
                if i >= 1: comp(i - 1)
            steps.append(step)
        return steps

    def ada(l):
        with ExitStack() as ph:
            for st in ada_steps(l, ph): st()
            K.barrier()

    def mod(l, v):
        return modT[:, v * 8:v * 8 + 8, :]

    def prep_AG(gamma, sc, gate, gmul):
        K.ts(Avec.v, sc, 1.0, ALU.add)
        K.tt(Avec.v, Avec.v, gamma.un(2).bc([128, KD, NSEQ]), ALU.mult)
        if gate is not None:
            K.ts(Gvec.v, gate, gmul, ALU.mult)

    def seq_affine(out, in_, A, B, ti, k):
        if ti < 4:
            K.act(out, in_, AF.Identity, bias=B[:, k, 0:1], scale=A[:, k, 0:1])
        else:
            o3 = out.rr("p (s t) -> p s t", t=8); i3 = in_.rr("p (s t) -> p s t", t=8)
            K.tt(o3, i3, A[:, k, 1:17].un(2).bc([128, 16, 8]), ALU.mult)
            K.tt(o3, o3, B[:, k, 1:17].un(2).bc([128, 16, 8]), ALU.add)

    def resid_add(ti, d, cols, ps_v, G):
        xv = xT[ti][:, d, cols]
        if ti < 4:
            K.stt(xv, ps_v, G[:, d, 0:1], xv, ALU.mult, ALU.add)
        else:
            tmp = rs_tmp.v
            K.tt(tmp.rr("p (s t) -> p s t", t=8), ps_v.rr("p (s t) -> p s t", t=8), G[:, d, 1:17].un(2).bc([128, 16, 8]), ALU.mult)
            K.tt(xv, xv, tmp, ALU.add)

    rs_tmp = K.sb("rs_tmp", [128, 128], F32)
    rs_tmp4 = K.sb("rs_tmp4", [128, 4, 128], F32)

    def resid_add4(ti, d0, cols, ps_v, G):
        xv = xT[ti][:, d0:d0 + 4, cols]
        if ti < 4:
            K.tt(rs_tmp4.v, ps_v, G[:, d0:d0 + 4, 0:1].bc([128, 4, 128]), ALU.mult)
        else:
            K.tt(rs_tmp4.v.rr("p d (s t) -> p d s t", t=8), ps_v.rr("p d (s t) -> p d s t", t=8),
                 G[:, d0:d0 + 4, 1:17].un(3).bc([128, 4, 16, 8]), ALU.mult)
        K.tt(xv, xv, rs_tmp4.v, ALU.add)

    NB = {}

    def norm_bufs(es_, n):
        NB["sq"] = K.sb("nrm_sq", [128, KD, n], BF16, es_)
        NB["rstd"] = K.sb("nrm_rstd", [128, n], F32, es_)
        NB["ntmp"] = K.sb("nrm_tmp", [128, n], F32, es_)

    def rms_rstd(ps_ss, n, dim, out):
        K.act(out[:, 0:n], ps_ss[:, 0:n], AF.Ln, bias=epsb.v, scale=1.0 / dim)
        K.act(out[:, 0:n], out[:, 0:n], AF.Exp, scale=-0.5)

    def norm_tile(ti, hdst, B):
        o, n = TILES[ti]
        sq = NB["sq"]; rstd = NB["rstd"]; ntmp = NB["ntmp"]
        K.act(sq[:, :, 0:n], xT[ti].v, AF.Square)
        ps = K.ps()
        for k in range(KD):
            K.mm(ps[:, 0:n], ones_bf.v, sq[:, k, 0:n], start=(k == 0), stop=(k == KD - 1))
        rms_rstd(ps, n, D, rstd)
        for k in range(KD):
            K.tt(ntmp[:, 0:n], xT[ti][:, k, :], rstd[:, 0:n], ALU.mult)
            seq_affine(hdst[:, k, :], ntmp[:, 0:n], Avec, B, ti, k)

    def ffn(l, w):
        with ExitStack() as ph:
            hT = [K.sb("ffn_h%d" % i, [128, KD, n], BF16, ph) for i, (o, n) in enumerate(TILES)]
            wg = [K.sb("ffn_wg%d" % i, [128, KD, 256], BF16, ph) for i in range(2)]
            wu = [K.sb("ffn_wu%d" % i, [128, KD, 256], BF16, ph) for i in range(2)]
            wd = [K.sb("ffn_wd%d" % i, [128, 2, 1024], BF16, ph) for i in range(2)]
            hid = [K.sb("ffn_hid%d" % i, [128, 2, 512], BF16, ph) for i in range(2)]
            sg = K.sb("ffn_sg", [128, 512], F32, ph)
            prep_AG(ffn_norm[:, l, w, :], mod(l, 1 if w == 0 else 7), mod(l, 2 if w == 0 else 8), 0.5)
            B = mod(l, 0 if w == 0 else 6)
            with ExitStack() as nb:
                norm_bufs(nb, 512)
                for ti in range(5):
                    norm_tile(ti, hT[ti].v, B)
                K.barrier()
            hi = 0
            asteps = ada_steps(l + 1, ph) if w == 1 else []
            for g in range(NG):
                b = g % 2
                if g < len(asteps): asteps[g]()
                K.dma(wg[b].v, din["ffn_wg"][l, w, g], q="pool")
                K.dma(wu[b].v, din["ffn_wu"][l, w, g], q="pool")
                K.dma(wd[b].v, din["ffn_wd"][l, w, g], q="pool")
                for ti, (o, n) in enumerate(TILES):
                    hb = hid[hi % 2]; hi += 1
                    for c in range(2):
                        pg = K.ps(); pu = K.ps()
                        for k in range(KD):
                            K.mm(pg[:, 0:n], wg[b][:, k, c * 128:(c + 1) * 128], hT[ti][:, k, :], start=(k == 0), stop=(k == KD - 1))
                        for k in range(KD):
                            K.mm(pu[:, 0:n], wu[b][:, k, c * 128:(c + 1) * 128], hT[ti][:, k, :], start=(k == 0), stop=(k == KD - 1))
                        K.act(sg[:, 0:n], pg[:, 0:n], AF.Silu)
                        K.tt(hb[:, c, 0:n], sg[:, 0:n], pu[:, 0:n], ALU.mult)
                    for d in range(KD):
                        py = K.ps()
                        for c in range(2):
                            K.mm(py[:, 0:n], wd[b][:, c, d * 128:(d + 1) * 128], hb[:, c, 0:n], start=(c == 0), stop=(c == 1))
                        resid_add(ti, d, slice(0, n), py[:, 0:n], Gvec)
            K.barrier()

    def ab_mixer(l):
        e = l // 2
        with ExitStack() as ph:
            w_in = K.sb("ab_win", [128, KD, 2576], BF16, ph)
            w_out = K.sb("ab_wout", [128, KD, 1024], BF16, ph)
            wa2 = K.sb("ab_wa2", [17, 256], F32, ph)
            gnorm = K.sb("ab_gn", [128, 1], F32, ph)
            vnB = K.sb("ab_vn", [128, 128], F32, ph)
            wTp = K.sb("ab_wTp", [128, 4, 128], F32, ph); wTs = K.sb("ab_wTs", [128, 4, 128], F32, ph)
            wTpb = K.sb("ab_wTpb", [128, 4, 128], BF16, ph); wTsb = K.sb("ab_wTsb", [128, 4, 128], BF16, ph)
            bsp = K.sb("ab_bsp", [128, 4, 128], F32, ph); bss = K.sb("ab_bss", [128, 4, 128], F32, ph)
            hT = K.sb("ab_h", [128, KD, 128], BF16, ph)
            qT = K.sb("ab_q", [64, 4, 128], F32, ph); kT = K.sb("ab_k", [64, 4, 128], F32, ph)
            qb = K.sb("ab_qb", [64, 4, 128], BF16, ph); kb = K.sb("ab_kb", [64, 4, 128], BF16, ph)
            kd = K.sb("ab_kd", [64, 4, 128], F32, ph); kdt = K.sb("ab_kdt", [128, 4, 64], BF16, ph)
            a_aug = K.sb("ab_aaug", [17, 128], F32, ph)
            la = K.sb("ab_la", [128, 256], F32, ph)
            eb = K.sb("ab_eb", [64, 4, 128], F32, ph); enb = K.sb("ab_enb", [64, 4, 128], F32, ph)
            vtok = K.sb("ab_vtok", [128, 512], BF16, ph)
            sgT = K.sb("ab_sg", [128, 4, 128], F32, ph)
            uT = K.sb("ab_u", [128, 4, 128], F32, ph)
            vbn = K.sb("ab_vbn", [128, 512], F32, ph); vbnb = K.sb("ab_vbnb", [128, 512], BF16, ph)
            ssq = K.sb("ab_ssq", [128, 4], F32, ph); vjunk = K.sb("ab_vj", [128, 128], F32, ph)
            sT = K.sb("ab_sT", [128, 128], BF16, ph)
            S = K.sb("ab_S", [64, 4, 128], F32, ph); Sb = K.sb("ab_Sb", [64, 4, 128], BF16, ph)
            Ss = K.sb("ab_Ss", [64, 4, 128], F32, ph); Ssn = K.sb("ab_Ssn", [64, 4, 128], F32, ph)
            Ssb = K.sb("ab_Ssb", [64, 4, 128], BF16, ph)
            R = K.sb("ab_R", [128, 4, 128], BF16, ph)
            ebl = K.sb("ab_ebl", [64, 4, 16], F32, ph)
            oT = K.sb("ab_o", [128, 4, 128], F32, ph); osq = K.sb("ab_osq", [128, 4, 128], BF16, ph)
            orstd = K.sb("ab_orstd", [128, 4, 128], F32, ph)
            mix = K.sb("ab_mix", [128, KD, 128], BF16, ph)
            ztmp = K.sb("ab_ztmp", [128, 128], F32, ph)
            K.dma(w_in.v, din["ab_w_in"][e], q="pool"); K.dma(w_out.v, din["ab_w_out"][e], q="pool")
            K.dma(wa2.v, din["gla_wa2"][e]); K.dma(gnorm.v, din["gla_norm"][e]); K.dma(vnB.v, din["gmlp_norm"][e])
            K.dma(wTp.v, din["gmlp_wT_p"][e]); K.dma(wTs.v, din["gmlp_wT_s"][e])
            K.dma(bsp.v, din["gmlp_bs_p"][e]); K.dma(bss.v, din["gmlp_bs_s"][e])
            K.tt(wTpb.v, wTp.v, tri.un(1).bc([128, 4, 128]), ALU.mult)
            K.tt(wTsb.v, wTs.v, tri8.un(1).bc([128, 4, 128]), ALU.mult)
            K.memset(a_aug.v, 1.0)
            K.memset(S.v, 0.0); K.memset(Sb.v, 0.0)
            prep_AG(mix_norm[:, l, :], mod(l, 4), mod(l, 5), 1.0)
            B = mod(l, 3)
            norm_bufs(ph, 128)
            sq = NB["sq"]; rstd = NB["rstd"]; ntmp = NB["ntmp"]
            for ci in range(17):
                samp = (ci == 16)
                ti = ci // 4 if not samp else 4
                c0 = (ci % 4) * 128 if not samp else 0
                cols = slice(c0, c0 + 128)
                mask = tri8 if samp else tri
                K.act(sq[:, :, 0:128], xT[ti][:, :, cols], AF.Square)
                ps = K.ps()
                for k in range(KD):
                    K.mm(ps[:, 0:128], ones_bf.v, sq[:, k, 0:128], start=(k == 0), stop=(k == KD - 1))
                rms_rstd(ps, 128, D, rstd)
                for k in range(KD):
                    K.tt(ntmp[:, 0:128], xT[ti][:, k, cols], rstd[:, 0:128], ALU.mult)
                    seq_affine(hT[:, k, :], ntmp[:, 0:128], Avec, B, ti, k)
                def proj_f(col0, m, dst_ps):
                    for k in range(KD):
                        K.mm(dst_ps, w_in[:, k, col0:col0 + m], hT[:, k, :], start=(k == 0), stop=(k == KD - 1))
                ps = K.ps()
                for h in range(4):
                    proj_f(h * 64, 64, ps[0:64, h * 128:(h + 1) * 128])
                K.act(qT.v, ps[0:64, :].rr("p (h t) -> p h t", t=128), AF.Copy, scale=64 ** -0.5)
                ps = K.ps()
                for h in range(4):
                    proj_f(256 + h * 64, 64, ps[0:64, h * 128:(h + 1) * 128])
                K.cp(kT.v, ps[0:64, :].rr("p (h t) -> p h t", t=128))
                ps = K.ps()
                proj_f(1536, 16, ps[0:16, 0:128])
                K.cp(a_aug[0:16, :], ps[0:16, 0:128])
                ps = K.ps()
                for k in range(KD):
                    K.mm(ps[:, 0:512], hT[:, k, :], w_in[:, k, 512:1024], start=(k == 0), stop=(k == KD - 1))
                K.act(vtok.v, ps[:, 0:512], AF.Copy)
                ps = K.ps()
                for j in range(4):
                    proj_f(1024 + j * 128, 128, ps[:, j * 128:(j + 1) * 128])
                K.act(sgT.v, ps[:, :].rr("p (h t) -> p h t", t=128), AF.Silu)
                ps = K.ps()
                for j in range(4):
                    proj_f(1552 + j * 128, 128, ps[:, j * 128:(j + 1) * 128])
                K.cp(uT.v, ps[:, :].rr("p (h t) -> p h t", t=128))
                ps = K.ps()
                for k in range(KD):
                    K.mm(ps[:, 0:512], hT[:, k, :], w_in[:, k, 2576 - 512:2576], start=(k == 0), stop=(k == KD - 1))
                for g in range(4):
                    K.act(vjunk.v, ps[:, g * 128:(g + 1) * 128], AF.Square, accum=ssq[:, g:g + 1])
                K.act(ssq.v, ssq.v, AF.Ln, bias=epsb.v, scale=1.0 / 128)
                K.act(ssq.v, ssq.v, AF.Exp, scale=-0.5)
                for g in range(4):
                    K.stt(vbn[:, g * 128:(g + 1) * 128], ps[:, g * 128:(g + 1) * 128], ssq[:, g:g + 1], vnB.v, ALU.mult, ALU.mult)
                K.cp(vbnb.v, vbn.v)
                if samp:
                    K.dma(dout["gmlp_v_s"][e], vbn.v)
                ps = K.ps()
                K.mm(ps[:, 0:256], a_aug.v, wa2.v)
                K.act(la.v, ps[:, 0:256], AF.Exp, scale=-1.0)
                K.act(la.v, la.v, AF.Ln, bias=1.0)
                ps = K.ps()
                for h in range(4):
                    K.mm(ps[0:64, h * 128:(h + 1) * 128], la[:, h * 64:(h + 1) * 64], mask)
                bp = ps[0:64, :].rr("p (h t) -> p h t", t=128)
                K.act(eb.v, bp, AF.Exp, scale=-1.0 / 16)
                K.act(enb.v, bp, AF.Exp, scale=1.0 / 16)
                K.tt(qb.v, qT.v, eb.v, ALU.mult)
                K.tt(kb.v, kT.v, enb.v, ALU.mult)
                if not samp:
                    K.tt(kd.v, kT.v, enb.v, ALU.mult)
                    for h in range(4):
                        K.ts(kd[:, h, :], kd[:, h, :], eb[:, h, 127:128], ALU.mult)
                else:
                    K.cp(ebl.v, eb.v.rr("p h (s t) -> p h s t", t=8)[:, :, :, 7])
                    K.tt(kd.v, kT.v, enb.v, ALU.mult)
                    K.tt(kd.v.rr("p h (s t) -> p h s t", t=8), kd.v.rr("p h (s t) -> p h s t", t=8),
                         ebl.v.un(3).bc([64, 4, 16, 8]), ALU.mult)
                ps = K.ps()
                for h in range(4):
                    K.tr(ps[:, h * 64:(h + 1) * 64], kd[:, h, :], ident[0:64, 0:64])
                K.cp(kdt.v, ps[:, 0:256].rr("p (h d) -> p h d", d=64))
                for h in range(4):
                    vh = vtok[:, h * 128:(h + 1) * 128]
                    ps = K.ps()
                    K.mm(ps[:, 0:128], kb[:, h, :], qb[:, h, :])
                    K.tt(sT.v, ps[:, 0:128], mask, ALU.mult)
                    po = K.ps_pin(0)
                    if not samp:
                        K.mm(po[:, 0:128], vh, sT.v, start=True, stop=False)
                        K.mm(po[:, 0:128], Sb[:, h, :], qb[:, h, :], start=False, stop=True)
                        K.cp(oT[:, h, :], po[:, 0:128])
                        pk = K.ps()
                        K.mm(pk[0:64, 0:128], kdt[:, h, :], vh)
                        K.stt(S[:, h, :], S[:, h, :], eb[:, h, 127:128], pk[0:64, 0:128], ALU.mult, ALU.add)
                        K.act(Sb[:, h, :], S[:, h, :], AF.Copy)
                    else:
                        K.mm(po[:, 0:128], vh, sT.v, start=True, stop=False)
                        for q4 in range(4):
                            sl = slice(q4 * 4, q4 * 4 + 4)
                            K.dma(Ss.v, din["gla_S"][e][:, sl, h, :])
                            K.cp(Ssb.v, Ss.v)
                            for s4 in range(4):
                                s_ = q4 * 4 + s4
                                K.mm(po[:, s_ * 8:(s_ + 1) * 8], Ssb[:, s4, :], qb[:, h, s_ * 8:(s_ + 1) * 8], start=False, stop=(s_ == 15))
                            K.tt(R.v, vh.un(1).bc([128, 4, 128]), seqmask[:, sl].un(2).bc([128, 4, 128]), ALU.mult)
                            pk = K.ps()
                            K.mm(pk[0:64, 0:512], kdt[:, h, :], R.v.rr("p s v -> p (s v)"))
                            K.tt(Ssn.v, Ss.v, ebl[:, h, sl].un(2).bc([64, 4, 128]), ALU.mult)
                            K.tt(Ssn.v, Ssn.v, pk[0:64, 0:512].rr("p (s v) -> p s v", v=128), ALU.add)
                            K.dma(dout["gla_S_s"][e][:, sl, h, :], Ssn.v)
                        K.cp(oT[:, h, :], po[:, 0:128])
                if ci == 15:
                    K.dma(dout["gla_S_p"][e], S.v)
                K.act(osq.v, oT.v, AF.Square)
                ps = K.ps()
                for h in range(4):
                    K.mm(ps[:, h * 128:(h + 1) * 128], ones_bf.v, osq[:, h, :])
                K.act(orstd.v, ps[:, :].rr("p (h t) -> p h t", t=128), AF.Ln, bias=epsb.v, scale=1.0 / 128)
                K.act(orstd.v, orstd.v, AF.Exp, scale=-0.5)
                K.tt(oT.v, oT.v, orstd.v, ALU.mult)
                K.stt(mix[:, 0:4, :], oT.v, gnorm.v, sgT.v, ALU.mult, ALU.mult)
                wTb = wTsb if samp else wTpb
                bsB = bss if samp else bsp
                for g in range(4):
                    ps = K.ps()
                    K.mm(ps[:, 0:128], vbnb[:, g * 128:(g + 1) * 128], wTb[:, g, :])
                    K.tt(ztmp.v, ps[:, 0:128], bsB[:, g, :], ALU.add)
                    K.tt(mix[:, 4 + g, :], ztmp.v, uT[:, g, :], ALU.mult)
                for half in range(2):
                    ps = K.ps()
                    for dd in range(4):
                        d = half * 4 + dd
                        for k in range(KD):
                            K.mm(ps[:, dd * 128:(dd + 1) * 128], w_out[:, k, d * 128:(d + 1) * 128], mix[:, k, :], start=(k == 0), stop=(k == KD - 1))
                    resid_add4(ti, half * 4, cols, ps[:, :].rr("p (d t) -> p d t", t=128), Gvec)
            K.barrier()

    def mlstm(l):
        o_ = l // 2
        with ExitStack() as ph:
            hT = K.sb("ml_h", [128, KD, T], BF16, ph)
            prep_AG(mix_norm[:, l, :], mod(l, 4), mod(l, 5), 1.0)
            B = mod(l, 3)
            with ExitStack() as nb:
                norm_bufs(nb, 512)
                for ti in range(5):
                    norm_tile(ti, hT[:, :, TILES[ti][0]:TILES[ti][0] + TILES[ti][1]], B)
                K.barrier()
            cw = K.sb("ml_cw", [128, 16, 4], F32, ph); cb = K.sb("ml_cb", [128, 16], F32, ph)
            gn = K.sb("ml_gn", [128, 16], F32, ph); skp = K.sb("ml_skip", [128, 16], F32, ph)
            bd = {n: K.sb("ml_" + n, [128, 4, 128], BF16, ph) for n in ("bdq", "bdk", "bdv")}
            wgt = K.sb("ml_wgt", [128, 48, 8], BF16, ph); bg = K.sb("ml_bg", [128, 8], F32, ph)
            mst = K.sb("ml_mst", [4, 16], F32, ph)
            gacc = K.sb("ml_gacc", [128, 17, 8], F32, ph)
            colsAll = K.sb("ml_cols", [128, 17, 20], F32, ph)
            decB = K.sb("ml_decB", [128, 4, 32], F32, ph)
            sel = K.sb("ml_sel", [4, 4, 128], F32, ph)
            K.dma(cw.v, din["ml_conv_w"][o_]); K.dma(cb.v, din["ml_conv_b"][o_])
            K.dma(gn.v, din["ml_norm"][o_]); K.dma(skp.v, din["ml_skip"][o_])
            K.dma(wgt.v, din["ml_w_gates"][o_], q="pool"); K.dma(bg.v, din["ml_b_gates"][o_])
            K.dma(mst.v, din["ml_m"][o_])
            K.cp(sel.v, ident[0:4, 0:4].un(2).bc([4, 4, 128]))
            xmes = K.sb("ml_xmes", [128, 4, 16, 16], F32, ph)

            def front_set(es_, tag):
                return {"xme": K.sb("ml_xme" + tag, [128, 4, 136], F32, es_), "xmb": K.sb("ml_xmb" + tag, [128, 4, 128], BF16, es_),
                        "acc": [K.sb("ml_acc%d" % j + tag, [128, 128], F32, es_) for j in range(4)], "xc": K.sb("ml_xc" + tag, [128, 4, 128], BF16, es_),
                        "qT": K.sb("ml_q" + tag, [128, 4, 128], BF16, es_), "kT": K.sb("ml_k" + tag, [128, 4, 128], BF16, es_),
                        "vT": K.sb("ml_v" + tag, [128, 4, 128], BF16, es_)}
            fb0 = front_set(ph, "0")
            w_x = K.sb("ml_wx", [128, KD, 512], BF16, ph)
            cvin = K.sb("ml_cvin", [128, 4, 16, 3], F32, ph); cvout = K.sb("ml_cvout", [128, 4, 16, 3], F32, ph)
            cvp = K.sb("ml_cvp", [128, 4, 3], F32, ph)

            WS = {"w_x": w_x, "bd": bd}

            def load_w(h, ws):
                K.dma(ws["w_x"].v, din["ml_w_in"][o_][:, :, h * 512:(h + 1) * 512], q="pool")
                for n in ws["bd"]: K.dma(ws["bd"][n].v, din["ml_" + n][o_][:, 4 * h:4 * h + 4, :], q="pool")

            def load_head(h):
                K.dma(cvin.v, din["ml_conv"][o_][:, 4 * h:4 * h + 4, :, :])
                K.cp(xmes[:, :, :, 5:8], cvin.v)
                K.memset(fb0["xme"][:, :, 0:8], 0.0)

            def front(ci, h, need_v_T, fb, fbn):
                frontA(ci, h, fb, fbn)
                frontB(ci, h, need_v_T, fb)

            def frontA(ci, h, fb, fbn):
                xme = fb["xme"]; xmb = fb["xmb"]; acc = fb["acc"]; xc = fb["xc"]
                samp = (ci == 16)
                t0 = ci * 128
                ps = K.ps()
                for j in range(4):
                    for k in range(KD):
                        K.mm(ps[:, j * 128:(j + 1) * 128], WS["w_x"][:, k, j * 128:(j + 1) * 128], hT[:, k, t0:t0 + 128], start=(k == 0), stop=(k == KD - 1))
                if FRONT_STEPS == 0:
                    K.cp(xmb.v, ps[:, :].rr("p (f t) -> p f t", t=128)); return
                if not samp:
                    K.act(xme[:, :, 8:136], ps[:, :].rr("p (f t) -> p f t", t=128), AF.Copy)
                else:
                    K.act(xmes[:, :, :, 8:16], ps[:, :].rr("p (f s t) -> p f s t", t=8, s=16), AF.Copy)
                if not samp:
                    K.act(xmb.v, xme[:, :, 8:136], AF.Copy)
                else:
                    K.act(xmb.v.rr("p f (s t) -> p f s t", t=8), xmes[:, :, :, 8:16], AF.Copy)
                if FRONT_STEPS < 2: return
                for tap in range(4):
                    for j in range(4):
                        fc = 4 * h + j
                        if not samp:
                            src = xme[:, j, 5 + tap:5 + tap + 128]; a_ = acc[j].v
                        else:
                            src = xmes[:, j, :, 5 + tap:5 + tap + 8]; a_ = acc[j].v.rr("p (s t) -> p s t", t=8)
                        if tap == 0:
                            K.ts(a_, src, cw[:, fc, 0:1], ALU.mult)
                        else:
                            K.stt(a_, src, cw[:, fc, tap:tap + 1], a_, ALU.mult, ALU.add)
                for j in range(4):
                    fc = 4 * h + j
                    K.act(xc[:, j, :], acc[j].v, AF.Silu, bias=cb[:, fc:fc + 1])
                if not samp:
                    K.act(fbn["xme"][:, :, 5:8], xme[:, :, 133:136], AF.Copy)

            def frontB(ci, h, need_v_T, fb):
                xme = fb["xme"]; xmb = fb["xmb"]; xc = fb["xc"]; qT = fb["qT"]; kT = fb["kT"]; vT = fb["vT"]
                samp = (ci == 16)
                for nm, src, dst in (("bdq", xc, qT), ("bdk", xc, kT), ("bdv", xmb, vT)):
                    if nm == "bdv" and not need_v_T: continue
                    ps = K.ps()
                    for j in range(4):
                        K.mm(ps[:, j * 128:(j + 1) * 128], WS["bd"][nm][:, j, :], src[:, j, :])
                    K.act(dst.v, ps[:, :].rr("p (f t) -> p f t", t=128), AF.Copy)
                if FRONT_STEPS < 5: return
                if not samp:
                    if ci == 15 and need_v_T:
                        K.cp(cvp.v, xme[:, :, 133:136])
                        K.dma(dout["ml_conv_p"][o_][:, 4 * h:4 * h + 4, :], cvp.v)
                elif need_v_T:
                    K.cp(cvout.v, xmes[:, :, :, 13:16])
                    K.dma(dout["ml_conv_s"][o_][:, 4 * h:4 * h + 4, :, :], cvout.v)

            if MLSTM_MODE == 10:
                K.barrier(); return
            p1a = ExitStack()
            fb1 = front_set(p1a, "1")
            fbs = [fb0, fb1]
            w_x2 = K.sb("ml_wx2", [128, KD, 512], BF16, p1a)
            bd2 = {n: K.sb("ml2_" + n, [128, 4, 128], BF16, p1a) for n in ("bdq", "bdk", "bdv")}
            wsets = [{"w_x": w_x, "bd": bd}, {"w_x": w_x2, "bd": bd2}]
            K.nrot = 8
            load_w(0, wsets[0])
            for h in range(4):
                if h + 1 < 4: load_w(h + 1, wsets[(h + 1) % 2])
                WS.update(wsets[h % 2])
                load_head(h)
                frontA(0, h, fbs[0], fbs[1])
                for ci in range(17):
                    fb = fbs[ci % 2]
                    if ci + 1 < 17:
                        frontA(ci + 1, h, fbs[(ci + 1) % 2], fbs[ci % 2])
                    frontB(ci, h, True, fb)
                    ps = K.ps()
                    i = 0
                    for part, src in enumerate((fb["qT"], fb["kT"], fb["vT"])):
                        for j in range(4):
                            K.mm(ps[:, 0:8], src[:, j, :], wgt[:, part * 16 + 4 * h + j, :], start=(i == 0), stop=(i == 11)); i += 1
                    K.tt(gacc[:, ci, :], ps[:, 0:8], bg.v if h == 0 else gacc[:, ci, :], ALU.add)
            K.barrier()
            p1a.close()
            K.nrot = 6
            WS.update(wsets[0])
            if MLSTM_MODE in (1, 11, 12, 13):
                K.barrier(); return
            K.act(gacc[:, :, 4:8], gacc[:, :, 4:8], AF.Exp, scale=-1.0)
            K.act(gacc[:, :, 4:8], gacc[:, :, 4:8], AF.Ln, bias=1.0)
            with ExitStack() as p1:
                R32 = {n: K.sb("ml_r_" + n, [32, T], F32, p1) for n in ("ig", "lf", "m", "F", "wr")}
                for n in R32: K.memset(R32[n].v, 0.0)
                R_ = {n: R32[n][0:4, :] for n in R32}
                mprev = K.sb("ml_mprev", [4, 32], F32, p1); rend = K.sb("ml_rend", [4, 32], F32, p1); fst = K.sb("ml_fst", [4, 16], F32, p1)
                dec = K.sb("ml_dec", [4, 32], F32, p1)
                for ci in range(17):
                    ps = K.ps()
                    K.tr(ps[0:4, 0:128], gacc[:, ci, 0:4], ident)
                    K.tr(ps[0:4, 128:256], gacc[:, ci, 4:8], ident)
                    K.cp(R_["ig"][:, ci * 128:(ci + 1) * 128], ps[0:4, 0:128])
                    K.ts(R_["lf"][:, ci * 128:(ci + 1) * 128], ps[0:4, 128:256], -1.0, ALU.mult)
                K.scan(R_["m"][:, 0:TP], R_["lf"][:, 0:TP], R_["ig"][:, 0:TP], 0.0, ALU.add, ALU.max)
                K.scan(R_["F"][:, 0:TP], R_["lf"][:, 0:TP], R_["lf"][:, 0:TP], 0.0, ALU.add, ALU.min)
                for s in range(16):
                    sl = slice(TP + s * 8, TP + s * 8 + 8)
                    K.scan(R_["m"][:, sl], R_["lf"][:, sl], R_["ig"][:, sl], mst[:, s:s + 1], ALU.add, ALU.max)
                    K.scan(R_["F"][:, sl], R_["lf"][:, sl], R_["lf"][:, sl], 0.0, ALU.add, ALU.min)
                seg = lambda n: (R_[n][:, 0:TP].rr("p (c t) -> p c t", t=128), R_[n][:, TP:T].rr("p (c t) -> p c t", t=8))
                Fp = seg("F")[0]; mp = seg("m")[0]
                K.memset(fst[:, 0:1], 0.0); K.memset(mprev[:, 0:1], 0.0)
                K.cp(fst[:, 1:16], Fp[:, 0:15, 127]); K.cp(mprev[:, 1:16], mp[:, 0:15, 127])
                K.cp(mprev[:, 16:32], mst.v)
                K.tt(Fp, Fp, fst.v.un(2).bc([4, 16, 128]), ALU.subtract)
                K.dma(dout["ml_m_p"][o_], R_["m"][:, TP - 1:TP])
                msout = K.sb("ml_msout", [4, 16], F32, p1)
                K.cp(msout.v, seg("m")[1][:, :, 7])
                K.dma(dout["ml_m_s"][o_], msout.v)
                K.tt(R_["ig"], R_["ig"], R_["F"], ALU.subtract)
                K.tt(R_["F"], R_["F"], R_["m"], ALU.subtract)
                K.act(R_["lf"], R_["m"], AF.Exp, scale=-1.0)
                R_["wi"] = R_["m"]; R32["wi"] = R32["m"]
                K.cp(rend[:, 0:16], seg("F")[0][:, :, 127]); K.cp(rend[:, 16:32], seg("F")[1][:, :, 7])
                for pi, (off, L) in enumerate(((0, 128), (16, 8))):
                    K.tt(seg("wi")[pi], seg("F")[pi], mprev[:, off:off + 16].un(2).bc([4, 16, L]), ALU.add)
                    K.tt(seg("wr")[pi], seg("ig")[pi], rend[:, off:off + 16].un(2).bc([4, 16, L]), ALU.add)
                K.act(R_["wi"], R_["wi"], AF.Exp)
                K.act(R_["wr"], R_["wr"], AF.Exp)
                K.ts(R_["wr"], R_["wr"], 512 ** -0.5, ALU.mult)
                K.cp(dec[:, 0:16], seg("wi")[0][:, :, 127]); K.cp(dec[:, 16:32], seg("wi")[1][:, :, 7])
                for ci in range(17):
                    ps = K.ps()
                    for i, n in enumerate(("ig", "wr", "F", "wi", "lf")):
                        K.tr(ps[:, i * 32:(i + 1) * 32], R32[n][:, ci * 128:(ci + 1) * 128], ident[0:32, 0:32])
                    K.cp(colsAll[:, ci, :].rr("p (i f) -> p i f", f=4), ps[:, 0:160].rr("p (i f) -> p i f", f=32)[:, :, 0:4])
                K.barrier()
            if MLSTM_MODE == 2:
                K.barrier(); return
            with ExitStack() as p2:
                w_z = K.sb("ml_wz", [128, KD, 512], BF16, p2)
                w_o = K.sb("ml_wo", [128, 4, 1024], BF16, p2)
                CT = K.sb("ml_CT", [128, 4, 512], F32, p2); CTb = K.sb("ml_CTb", [128, 4, 512], BF16, p2)
                CT2 = K.sb("ml_CT2", [128, 4, 512], F32, p2)
                nS = K.sb("ml_nS", [128, 16, 4, 4], F32, p2); nSn = K.sb("ml_nSn", [128, 16, 4, 4], F32, p2)
                nP = K.sb("ml_nP", [128, 4, 4], F32, p2)
                nB = K.sb("ml_nB", [128, 4, 128], BF16, p2)
                ktok = K.sb("ml_ktok", [128, 512], BF16, p2); vtok = K.sb("ml_vtok", [128, 512], BF16, p2)
                vw = K.sb("ml_vw", [128, 512], BF16, p2); km = K.sb("ml_km", [128, 512], BF16, p2)
                wrb = K.sb("ml_wrb", [128, 4], BF16, p2)
                diag = [K.sb("ml_diag%d" % i, [128, 128], F32, p2) for i in range(3)]
                bcs = K.sb("ml_bcs", [128, 3, 128], F32, p2)
                arg = K.sb("ml_arg", [128, 128], F32, p2); sT = K.sb("ml_sT", [128, 128], BF16, p2)
                qw = K.sb("ml_qw", [128, 4, 128], BF16, p2)
                hden = K.sb("ml_hden", [128, 128], F32, p2)
                hh = K.sb("ml_hh", [128, 4, 128], F32, p2); hsq = K.sb("ml_hsq", [128, 4, 128], F32, p2)
                mean = K.sb("ml_mean", [128, 128], F32, p2); var = K.sb("ml_var", [128, 128], F32, p2)
                sz = K.sb("ml_sz", [128, 4, 128], BF16, p2); pre = K.sb("ml_pre", [128, 4, 128], BF16, p2)
                skx = K.sb("ml_skx", [128, 4, 128], F32, p2)
                interN = hsq; interD = mean
                K.dma(nS.v, din["ml_n"][o_])
                K.memset(nP.v, 0.0)
                for h in range(4):
                    load_w(h, wsets[0])
                    load_head(h)
                    K.dma(w_z.v, din["ml_w_in"][o_][:, :, 2048 + h * 512:2048 + (h + 1) * 512], q="pool")
                    K.dma(w_o.v, din["ml_w_out"][o_][:, 4 * h:4 * h + 4, :], q="pool")
                    K.memset(CT.v, 0.0); K.memset(CTb.v, 0.0); K.memset(nB.v, 0.0)
                    for ci in range(17):
                        samp = (ci == 16)
                        ti = ci // 4 if not samp else 4
                        c0 = (ci % 4) * 128 if not samp else 0
                        cols = slice(c0, c0 + 128)
                        tsl = slice(ci * 128, ci * 128 + 128)
                        front(ci, h, False, fb0, fb0)
                        xc = fb0["xc"]; xmb = fb0["xmb"]; qT = fb0["qT"]; kT = fb0["kT"]
                        for nm, src, dst in (("bdk", xc, ktok), ("bdv", xmb, vtok)):
                            ps = K.ps()
                            for j in range(4):
                                K.mm(ps[:, j * 128:(j + 1) * 128], src[:, j, :], bd[nm][:, j, :])
                            K.act(dst.v, ps[:, :], AF.Copy)
                        gcol = colsAll[:, ci, h:h + 1]; wrcol = colsAll[:, ci, 4 + h:5 + h]
                        K.cp(wrb.v, colsAll[:, ci, 4:8])
                        ps = K.ps()
                        for i in range(3):
                            K.ts(diag[i].v, ident, colsAll[:, ci, 8 + 4 * i + h:9 + 4 * i + h], ALU.mult)
                        for i in range(3):
                            K.mm(ps[:, i * 128:(i + 1) * 128], ones_f.v, diag[i].v)
                        K.act(bcs.v, ps[:, 0:384].rr("p (i t) -> p i t", t=128), AF.Copy)
                        K.stt(arg.v, bcs[:, 0, :], gcol, negm[:, 1 if samp else 0, :], ALU.add, ALU.add)
                        K.act(arg.v, arg.v, AF.Exp)
                        ps = K.ps()
                        for j in range(4):
                            K.mm(ps[:, 0:128], kT[:, j, :], qT[:, j, :], start=(j == 0), stop=(j == 3))
                        K.stt(sT.v, ps[:, 0:128], 512 ** -0.5, arg.v, ALU.mult, ALU.mult)
                        K.tt(qw.v, qT.v, bcs[:, 1, :].un(1).bc([128, 4, 128]), ALU.mult)
                        pn = K.ps_pin(0); pd = K.ps_pin(1)
                        if not samp:
                            for vc in range(4):
                                K.mm(pn[:, vc * 128:(vc + 1) * 128], vtok[:, vc * 128:(vc + 1) * 128], sT.v, start=True, stop=False)
                                for dc in range(4):
                                    K.mm(pn[:, vc * 128:(vc + 1) * 128], CTb[:, dc, vc * 128:(vc + 1) * 128], qw[:, dc, :], start=False, stop=(dc == 3))
                            K.mm(pd[:, 0:128], ones_bf.v, sT.v, start=True, stop=False)
                            for dc in range(4):
                                K.mm(pd[:, 0:128], nB[:, dc, :], qw[:, dc, :], start=False, stop=(dc == 3))
                        else:
                            for vc in range(4):
                                K.mm(pn[:, vc * 128:(vc + 1) * 128], vtok[:, vc * 128:(vc + 1) * 128], sT.v, start=True, stop=True)
                            K.mm(pd[:, 0:128], ones_bf.v, sT.v, start=True, stop=True)
                            K.act(vw.v, vtok.v, AF.Copy, scale=wrcol)
                            CTs = [CT, CT2]
                            K.dma(CT.v, din["ml_CT"][o_, 0, h])
                            for s in range(16):
                                CTc = CTs[s % 2]
                                if s + 1 < 16:
                                    K.dma(CTs[(s + 1) % 2].v, din["ml_CT"][o_, s + 1, h])
                                K.act(CTb.v, CTc.v, AF.Copy)
                                K.cp(nB.v, nS[:, s, h, :].un(2).bc([128, 4, 128]))
                                ssl = slice(s * 8, s * 8 + 8)
                                pi = K.ps()
                                for vc in range(4):
                                    for dc in range(4):
                                        K.mm(pi[:, vc * 8:vc * 8 + 8], CTb[:, dc, vc * 128:(vc + 1) * 128], qw[:, dc, ssl], start=(dc == 0), stop=(dc == 3))
                                for dc in range(4):
                                    K.mm(pi[:, 32:40], nB[:, dc, :], qw[:, dc, ssl], start=(dc == 0), stop=(dc == 3))
                                K.cp(interN[:, :, ssl], pi[:, 0:32].rr("p (v t) -> p v t", t=8))
                                K.cp(interD[:, ssl], pi[:, 32:40])
                                K.ts(km.v, ktok.v, seqmask[:, s:s + 1], ALU.mult)
                                pnn = K.ps()
                                for dc in range(4):
                                    pk = K.ps()
                                    K.mm(pk[:, 0:512], km[:, dc * 128:(dc + 1) * 128], vw.v)
                                    K.stt(CTc[:, dc, :], CTc[:, dc, :], bcs[:, 1, s * 8 + 7:s * 8 + 8], pk[:, 0:512], ALU.mult, ALU.add)
                                    K.mm(pnn[:, dc * 4:(dc + 1) * 4], km[:, dc * 128:(dc + 1) * 128], wrb.v)
                                K.stt(nSn[:, s, h, :], nS[:, s, h, :], bcs[:, 1, s * 8 + 7:s * 8 + 8], pnn[:, 0:16].rr("p (d f) -> p d f", f=4)[:, :, h], ALU.mult, ALU.add)
                                K.dma(dout["ml_CT_s"][o_, s, h], CTc.v, q="pool")
                        if samp:
                            K.tt(interD.v, interD.v, pd[:, 0:128], ALU.add)
                            K.tt(interN.v, interN.v, pn[:, :].rr("p (v t) -> p v t", t=128), ALU.add)
                            den_v = interD.v; num_v = interN.v
                        else:
                            den_v = pd[:, 0:128]; num_v = pn[:, :].rr("p (v t) -> p v t", t=128)
                        K.ts(hden.v, den_v, -1.0, ALU.mult)
                        K.tt(hden.v, hden.v, den_v, ALU.max)
                        K.tt(hden.v, hden.v, bcs[:, 2, :], ALU.max)
                        K.recip(hden.v, hden.v)
                        K.tt(hh.v, num_v, hden.v.un(1).bc([128, 4, 128]), ALU.mult)
                        K.act(hsq.v, hh.v, AF.Square)
                        ps = K.ps()
                        for vc in range(4):
                            K.mm(ps[:, 0:128], ones_f.v, hh[:, vc, :], start=(vc == 0), stop=(vc == 3))
                        for vc in range(4):
                            K.mm(ps[:, 128:256], ones_f.v, hsq[:, vc, :], start=(vc == 0), stop=(vc == 3))
                        K.act(mean.v, ps[:, 0:128], AF.Copy, scale=1.0 / 512)
                        K.tt(var.v, mean.v, mean.v, ALU.mult)
                        K.stt(var.v, ps[:, 128:256], 1.0 / 512, var.v, ALU.mult, ALU.subtract)
                        K.act(var.v, var.v, AF.Ln, bias=epsb.v)
                        K.act(var.v, var.v, AF.Exp, scale=-0.5)
                        K.tt(hh.v, hh.v, mean.v.un(1).bc([128, 4, 128]), ALU.subtract)
                        K.tt(hh.v, hh.v, var.v.un(1).bc([128, 4, 128]), ALU.mult)
                        ps = K.ps()
                        for j in range(4):
                            for k in range(KD):
                                K.mm(ps[:, j * 128:(j + 1) * 128], w_z[:, k, j * 128:(j + 1) * 128], hT[:, k, tsl], start=(k == 0), stop=(k == KD - 1))
                        K.act(sz.v, ps[:, :].rr("p (f t) -> p f t", t=128), AF.Silu)
                        K.tt(hh.v, hh.v, gn[:, 4 * h:4 * h + 4].un(2).bc([128, 4, 128]), ALU.mult)
                        K.tt(skx.v, xc.v, skp[:, 4 * h:4 * h + 4].un(2).bc([128, 4, 128]), ALU.mult)
                        K.tt(hh.v, hh.v, skx.v, ALU.add)
                        K.tt(pre.v, hh.v, sz.v, ALU.mult)
                        for half in range(2):
                            ps = K.ps()
                            for dd in range(4):
                                d = half * 4 + dd
                                for j in range(4):
                                    K.mm(ps[:, dd * 128:(dd + 1) * 128], w_o[:, j, d * 128:(d + 1) * 128], pre[:, j, :], start=(j == 0), stop=(j == 3))
                            resid_add4(ti, half * 4, cols, ps[:, :].rr("p (d t) -> p d t", t=128), Gvec)
                        if not samp:
                            K.act(vw.v, vtok.v, AF.Copy, scale=wrcol)
                            pnn = K.ps()
                            for dc in range(4):
                                pk = K.ps()
                                K.mm(pk[:, 0:512], ktok[:, dc * 128:(dc + 1) * 128], vw.v)
                                K.stt(CT[:, dc, :], CT[:, dc, :], bcs[:, 1, 127:128], pk[:, 0:512], ALU.mult, ALU.add)
                                K.mm(pnn[:, dc * 4:(dc + 1) * 4], ktok[:, dc * 128:(dc + 1) * 128], wrb.v)
                            K.stt(nP[:, h, :], nP[:, h, :], bcs[:, 1, 127:128], pnn[:, 0:16].rr("p (d f) -> p d f", f=4)[:, :, h], ALU.mult, ALU.add)
                            K.act(CTb.v, CT.v, AF.Copy)
                            K.cp(nB.v, nP[:, h, :].un(2).bc([128, 4, 128]))
                            if ci == 15:
                                K.dma(dout["ml_CT_p"][o_, h], CT.v)
                K.dma(dout["ml_n_p"][o_], nP.v)
                K.dma(dout["ml_n_s"][o_], nSn.v)
                K.barrier()
        K.barrier()

    for l in range(nlayers):
        if DBG_ONLY_MLSTM:
            if l == 1: mlstm(l)
            continue
        if l == 0: ada(l)
        ffn(l, 0)
        if l % 2 == 0:
            ab_mixer(l)
        elif MLSTM_MODE:
            mlstm(l)
        ffn(l, 1)
    with ExitStack() as ph:
        if nlayers != NL: ada(NL)
        yT = [K.sb("yT%d" % i, [128, KD, n], F32, ph) for i, (o, n) in enumerate(TILES)]
        K.ts(Avec.v, mod(NL, 1), 1.0, ALU.add)
        K.tt(Avec.v, Avec.v, final_norm.v.un(2).bc([128, KD, NSEQ]), ALU.mult)
        B = mod(NL, 0)
        norm_bufs(ph, 512)
        for ti, (o, n) in enumerate(TILES):
            norm_tile(ti, yT[ti].v, B)
            K.dma(dout["yT"][:, :, o:o + n], yT[ti].v)
        K.barrier()


def _consts():
    c = np.zeros((128, 6, 128), np.float32)
    s = np.arange(128)[:, None]; t = np.arange(128)[None, :]
    c[:, C_TRI, :] = (s <= t)
    c[:, C_TRI8, :] = (s <= t) & (s // 8 == t // 8)
    c[:, C_ID, :] = (s == t)
    c[:, C_MISC, 0:16] = (np.arange(128)[:, None] // 8 == np.arange(16)[None, :])
    return c


def _bd(w):
    out = np.zeros((16, 128, 128), np.float32)
    wr = w.reshape(16, 32, 4, 4)
    for n in range(32):
        out[:, n * 4:(n + 1) * 4, n * 4:(n + 1) * 4] = wr[:, n]
    return np.ascontiguousarray(out.transpose(1, 0, 2))


def _kT(w, kc):
    return np.ascontiguousarray(w.reshape(kc, 128, -1).transpose(1, 0, 2))


def prep_shared(inp):
    f = lambda a: np.ascontiguousarray(np.asarray(a, dtype=np.float32))
    S = {}
    S["ada_w"] = f(inp["ada_w"].reshape(NL, KD, 128, 9, 1024).transpose(0, 3, 2, 1, 4))
    S["ada_b"] = f(inp["ada_b"].reshape(NL, 72, 128).transpose(2, 0, 1))
    S["fada_w"] = f(inp["final_ada_w"].reshape(KD, 128, 2, 1024).transpose(2, 1, 0, 3))
    S["fada_b"] = f(inp["final_ada_b"].reshape(16, 128).T)
    S["ffn_norm"] = f(inp["ffn_norm"].reshape(NL, 2, KD, 128).transpose(3, 0, 1, 2))
    S["mix_norm"] = f(inp["mix_norm"].reshape(NL, KD, 128).transpose(2, 0, 1))
    S["final_norm"] = f(inp["final_norm"].reshape(KD, 128).T)
    S["ffn_wg"] = f(inp["ffn_w_gate"].reshape(NL, 2, KD, 128, NG, 256).transpose(0, 1, 4, 3, 2, 5))
    S["ffn_wu"] = f(inp["ffn_w_up"].reshape(NL, 2, KD, 128, NG, 256).transpose(0, 1, 4, 3, 2, 5))
    S["ffn_wd"] = f(inp["ffn_w_down"].reshape(NL, 2, NG, 2, 128, 1024).transpose(0, 1, 2, 4, 3, 5))
    S["ab_w_in"] = f(inp["ab_w_in"].reshape(2, KD, 128, 2576).transpose(0, 2, 1, 3))
    S["ab_w_out"] = f(inp["ab_w_out"].reshape(2, KD, 128, 1024).transpose(0, 2, 1, 3))
    S["gla_wa2"] = f(np.concatenate([inp["gla_w_a2"], inp["gla_b_a"][:, None, :]], axis=1))
    S["gla_norm"] = f(inp["gla_norm"].reshape(2, 128, 1))
    S["gmlp_norm"] = f(np.broadcast_to(inp["gmlp_norm"][:, None, :], (2, 128, 128)))
    ws = np.asarray(inp["gmlp_ws"])
    S["gmlp_wT_p"] = f(ws.transpose(0, 3, 1, 2))
    w8 = ws[:, :, :8, :8]
    S["gmlp_wT_s"] = f(np.tile(w8.transpose(0, 3, 1, 2), (1, 16, 1, 16)))
    bs = np.asarray(inp["gmlp_bs"])
    S["gmlp_bs_p"] = f(np.broadcast_to(bs[:, None, :, :], (2, 128, 4, 128)))
    S["gmlp_bs_s"] = f(np.broadcast_to(np.tile(bs[:, :, :8], (1, 1, 16))[:, None, :, :], (2, 128, 4, 128)))
    S["consts"] = _consts()
    S["ml_w_in"] = f(inp["ml_w_in"].reshape(2, KD, 128, 4096).transpose(0, 2, 1, 3))
    S["ml_w_out"] = f(inp["ml_w_out"].reshape(2, 16, 128, 1024).transpose(0, 2, 1, 3))
    S["ml_conv_w"] = f(inp["ml_conv_w"].reshape(2, 4, 16, 128).transpose(0, 3, 2, 1))
    S["ml_conv_b"] = f(inp["ml_conv_b"].reshape(2, 16, 128).transpose(0, 2, 1))
    for n, k in (("ml_bdq", "ml_wq"), ("ml_bdk", "ml_wk"), ("ml_bdv", "ml_wv")):
        S[n] = np.stack([_bd(np.asarray(inp[k][o])) for o in range(2)])
    S["ml_w_gates"] = f(inp["ml_w_gates"].reshape(2, 48, 128, 8).transpose(0, 2, 1, 3))
    S["ml_b_gates"] = f(np.broadcast_to(inp["ml_b_gates"][:, None, :], (2, 128, 8)))
    S["ml_norm"] = f(inp["ml_norm"].reshape(2, 16, 128).transpose(0, 2, 1))
    S["ml_skip"] = f(inp["ml_skip"].reshape(2, 16, 128).transpose(0, 2, 1))
    return S


def prep_core(inp, i):
    f = lambda a: np.ascontiguousarray(np.asarray(a, dtype=np.float32))
    P = {}
    sl = slice(16 * i, 16 * i + 16)
    x = np.concatenate([inp["x_prompt"][i], inp["x_sample"][sl].reshape(TS, D)], axis=0)
    P["xT"] = f(x.T.reshape(KD, 128, T).transpose(1, 0, 2))
    c = np.concatenate([inp["c_prompt"][i:i + 1], inp["c_sample"][sl]], axis=0)
    P["cT"] = f(c.T.reshape(KD, 128, NSEQ).transpose(1, 0, 2))
    P["gla_S"] = f(inp["state_gla_S"][:, sl].transpose(0, 3, 1, 2, 4))
    C = inp["state_mlstm_C"][:, sl]
    P["ml_CT"] = f(C.transpose(0, 1, 2, 4, 3).reshape(2, 16, 4, 4, 128, 512).transpose(0, 1, 2, 4, 3, 5))
    P["ml_n"] = f(inp["state_mlstm_n"][:, sl].reshape(2, 16, 4, 4, 128).transpose(0, 4, 1, 2, 3))
    P["ml_m"] = f(inp["state_mlstm_m"][:, sl].transpose(0, 2, 1))
    P["ml_conv"] = f(inp["state_mlstm_conv"][:, sl].reshape(2, 16, 3, 16, 128).transpose(0, 4, 3, 1, 2))
    return P


def post_core(r):
    o = {}
    y = r["yT"].transpose(2, 1, 0).reshape(T, D)
    o["y_p"] = y[0:TP]; o["y_s"] = y[TP:].reshape(16, 8, D)
    o["s_p"] = r["gla_S_p"].transpose(0, 2, 1, 3)
    o["s_s"] = r["gla_S_s"].transpose(0, 2, 3, 1, 4)
    o["v_s"] = r["gmlp_v_s"].reshape(2, 16, 8, 512)
    o["c_p"] = r["ml_CT_p"].transpose(0, 1, 3, 2, 4).reshape(2, 4, 512, 512).transpose(0, 1, 3, 2)
    o["c_s"] = r["ml_CT_s"].transpose(0, 1, 2, 4, 3, 5).reshape(2, 16, 4, 512, 512).transpose(0, 1, 2, 4, 3)
    o["n_p"] = r["ml_n_p"].transpose(0, 2, 3, 1).reshape(2, 4, 512)
    o["n_s"] = r["ml_n_s"].transpose(0, 2, 3, 4, 1).reshape(2, 16, 4, 512)
    o["m_p"] = r["ml_m_p"].reshape(2, 4)
    o["m_s"] = r["ml_m_s"].transpose(0, 2, 1)
    o["cv_p"] = r["ml_conv_p"].transpose(0, 3, 2, 1).reshape(2, 3, 2048)
    o["cv_s"] = r["ml_conv_s"].transpose(0, 3, 4, 2, 1).reshape(2, 16, 3, 2048)
    return o


_NC_CACHE = {}


def kernel(**inputs):
    inp = {k: np.asarray(v) for k, v in inputs.items()}
    if "nc" not in _NC_CACHE:
        _NC_CACHE["nc"] = build()
    nc = _NC_CACHE["nc"]
    S = prep_shared(inp)
    in_maps = []
    for i in range(8):
        m = dict(S); m.update(prep_core(inp, i)); in_maps.append(m)
    res = run_bass_kernel_spmd(nc, in_maps, core_ids=list(range(8)))
    po = [post_core(r) for r in res.results]
    cat0 = lambda k: np.ascontiguousarray(np.stack([p[k] for p in po], axis=0)).astype(np.float32)
    cat1 = lambda k: np.ascontiguousarray(np.stack([p[k] for p in po], axis=1)).astype(np.float32)
    cat1s = lambda k: np.ascontiguousarray(np.concatenate([p[k] for p in po], axis=1)).astype(np.float32)
    y_p = cat0("y_p")
    y_s = np.ascontiguousarray(np.concatenate([p["y_s"] for p in po], axis=0)).astype(np.float32)
    return (y_p, y_s, cat1("s_p"), cat1s("s_s"), cat1s("v_s"), cat1("c_p"), cat1s("c_s"), cat1("n_p"), cat1s("n_s"),
            cat1("m_p"), cat1s("m_s"), cat1("cv_p"), cat1s("cv_s"))
```

```python
import numpy as np
from contextlib import ExitStack
import concourse.bass as bass
import concourse.mybir as mybir
from concourse.bass_utils import run_bass_kernel_spmd

F32 = mybir.dt.float32
BF16 = mybir.dt.bfloat16
AF = mybir.ActivationFunctionType
ALU = mybir.AluOpType

D = 1024; KD = 8; TP = 2048; TS = 128; T = TP + TS; NSEQ = 17
DFF = 2816; NG = 11
NL = 4
EPS = 1e-6
NEG = -1.0e30
MLSTM_MODE = 3
DBG_ONLY_MLSTM = False
FRONT_STEPS = 9
SKIP_INTER = False
SAME_ENG_SYNC = True


class V:
    def __init__(self, buf, ap):
        self.buf = buf; self.ap = ap
    def __getitem__(self, idx):
        return V(self.buf, self.ap[idx])
    def rr(self, pat, **kw):
        return V(self.buf, self.ap.rearrange(pat, **kw))
    def bc(self, shape):
        return V(self.buf, self.ap.broadcast_to(list(shape)))
    def un(self, axis):
        return V(self.buf, self.ap.unsqueeze(axis))


class Buf:
    def __init__(self, t, name):
        self.t = t; self.name = name; self.w = None; self.r = {}
    def __getitem__(self, idx):
        return V(self, self.t[idx])
    @property
    def v(self):
        return V(self, self.t[:])


class Kern:
    def __init__(self, nc, es):
        self.nc = nc; self.es = es
        self.engs = {"pe": nc.tensor, "act": nc.scalar, "dve": nc.vector, "sp": nc.sync, "pool": nc.gpsimd}
        self.sems = {}; self.cnt = {}
        for e in ["pe", "act", "dve"]:
            self.sems[e] = es.enter_context(nc.semaphore("s_" + e)); self.cnt[e] = 0
        self.rings = {}
        for q, n in (("sp", 10), ("pool", 10)):
            keys = []
            for i in range(n):
                k = "%s_d%d" % (q, i)
                self.sems[k] = es.enter_context(nc.semaphore("s_" + k)); self.cnt[k] = 0
                keys.append(k)
            self.rings[q] = [keys, 0]
        self.known = {e: {} for e in self.engs}
        self.psb = []
        for i in range(8):
            t = es.enter_context(nc.psum_tensor("psb%d" % i, [128, 512], F32))
            self.psb.append(Buf(t, "psb%d" % i))
        self.psi = 0
        self.nrot = 6
        self.ninstr = 0

    def sb(self, name, shape, dt, es=None):
        self.uid = getattr(self, "uid", 0) + 1
        t = (es or self.es).enter_context(self.nc.sbuf_tensor("sb%d_%s" % (self.uid, name), list(shape), dt))
        return Buf(t, name)

    def ps(self):
        b = self.psb[self.psi % self.nrot]; self.psi += 1
        return b

    def ps_pin(self, i):
        return self.psb[6 + i]

    def _emit(self, eng, fn, reads, writes, dma=False):
        waits = {}
        known = self.known[eng]
        def need(tok):
            if tok is None: return
            k, v = tok
            if k == eng and (eng == "pe" or not SAME_ENG_SYNC): return
            if known.get(k, 0) >= v: return
            if waits.get(k, 0) < v: waits[k] = v
        rb = []; wb = []
        for x in reads:
            if x is None or isinstance(x, (int, float)): continue
            b = x.buf if isinstance(x, V) else x
            if b is not None and b not in rb: rb.append(b)
        for x in writes:
            b = x.buf if isinstance(x, V) else x
            if b is not None and b not in wb: wb.append(b)
        for b in rb: need(b.w)
        for b in wb:
            need(b.w)
            for k, v in b.r.items(): need((k, v))
        if dma:
            keys, pos = self.rings[eng]
            k = keys[pos % len(keys)]; self.rings[eng][1] = pos + 1
            need((k, self.cnt[k]))
            self.cnt[k] += 16; tok = (k, self.cnt[k]); inc = 16
        else:
            self.cnt[eng] += 1; tok = (eng, self.cnt[eng]); inc = 1
        e = self.engs[eng]
        for k, v in waits.items():
            e.wait_ge(self.sems[k], v); known[k] = v
        ins = fn(e)
        ins.then_inc(self.sems[tok[0]], inc)
        self.ninstr += 1
        for b in rb:
            if b in wb: continue
            if b.r.get(tok[0], 0) < tok[1]: b.r[tok[0]] = tok[1]
        for b in wb:
            b.w = tok; b.r = {}
        return tok

    def barrier(self):
        for eng in self.engs:
            e = self.engs[eng]; known = self.known[eng]
            for k, v in self.cnt.items():
                if v > 0 and known.get(k, 0) < v:
                    e.wait_ge(self.sems[k], v); known[k] = v

    @staticmethod
    def _a(x):
        return x.ap if isinstance(x, V) else x

    def mm(self, out, lhsT, rhs, start=True, stop=True):
        a = self._a
        return self._emit("pe", lambda e: e.matmul(a(out), lhsT=a(lhsT), rhs=a(rhs), start=start, stop=stop), [lhsT, rhs], [out])

    def tr(self, out, in_, ident):
        a = self._a
        return self._emit("pe", lambda e: e.transpose(a(out), a(in_), a(ident)), [in_, ident], [out])

    def act(self, out, in_, func, bias=None, scale=None, accum=None):
        a = self._a
        kw = {}
        if bias is not None: kw["bias"] = a(bias)
        if scale is not None: kw["scale"] = a(scale)
        if accum is not None: kw["accum_out"] = a(accum)
        w = [out] + ([accum] if accum is not None else [])
        return self._emit("act", lambda e: e.activation(out=a(out), in_=a(in_), func=func, **kw), [in_, bias, scale], w)

    def tt(self, out, in0, in1, op, eng="dve"):
        a = self._a
        return self._emit(eng, lambda e: e.tensor_tensor(out=a(out), in0=a(in0), in1=a(in1), op=op), [in0, in1], [out])

    def ts(self, out, in0, s1, op0, s2=None, op1=None, eng="dve"):
        a = self._a
        if op1 is None:
            return self._emit(eng, lambda e: e.tensor_scalar(out=a(out), in0=a(in0), scalar1=a(s1), scalar2=None, op0=op0), [in0, s1], [out])
        return self._emit(eng, lambda e: e.tensor_scalar(out=a(out), in0=a(in0), scalar1=a(s1), scalar2=a(s2), op0=op0, op1=op1), [in0, s1, s2], [out])

    def stt(self, out, in0, scalar, in1, op0, op1, eng="dve"):
        a = self._a
        return self._emit(eng, lambda e: e.scalar_tensor_tensor(out=a(out), in0=a(in0), scalar=a(scalar), in1=a(in1), op0=op0, op1=op1), [in0, scalar, in1], [out])

    def cp(self, out, in_, eng="dve"):
        a = self._a
        return self._emit(eng, lambda e: e.tensor_copy(out=a(out), in_=a(in_)), [in_], [out])

    def memset(self, out, val, eng="dve"):
        a = self._a
        return self._emit(eng, lambda e: e.memset(a(out), val), [], [out])

    def scan(self, out, d0, d1, init, op0, op1):
        a = self._a
        return self._emit("dve", lambda e: e.tensor_tensor_scan(out=a(out), data0=a(d0), data1=a(d1), initial=a(init), op0=op0, op1=op1), [d0, d1, init], [out])

    def recip(self, out, in_):
        a = self._a
        return self._emit("dve", lambda e: e.reciprocal(out=a(out), in_=a(in_)), [in_], [out])

    def dma(self, out, in_, q="sp"):
        a = self._a
        r = [in_] if isinstance(in_, V) else []
        w = [out] if isinstance(out, V) else []
        return self._emit(q, lambda e: e.dma_start(out=a(out), in_=a(in_)), r, w, dma=True)

    def finish(self):
        self.barrier()


IN_SPECS = {}
OUT_SPECS = {}


def _specs():
    I = {}
    I["xT"] = [128, KD, T]
    I["cT"] = [128, KD, NSEQ]
    I["ada_w"] = [NL, 9, 128, KD, 1024]
    I["ada_b"] = [128, NL, 72]
    I["fada_w"] = [2, 128, KD, 1024]
    I["fada_b"] = [128, 16]
    I["ffn_norm"] = [128, NL, 2, KD]
    I["mix_norm"] = [128, NL, KD]
    I["final_norm"] = [128, KD]
    I["ffn_wg"] = [NL, 2, NG, 128, KD, 256]
    I["ffn_wu"] = [NL, 2, NG, 128, KD, 256]
    I["ffn_wd"] = [NL, 2, NG, 128, 2, 1024]
    I["ab_w_in"] = [2, 128, KD, 2576]
    I["ab_w_out"] = [2, 128, KD, 1024]
    I["gla_wa2"] = [2, 17, 256]
    I["gla_norm"] = [2, 128, 1]
    I["gmlp_norm"] = [2, 128, 128]
    I["gmlp_wT_p"] = [2, 128, 4, 128]
    I["gmlp_wT_s"] = [2, 128, 4, 128]
    I["gmlp_bs_p"] = [2, 128, 4, 128]
    I["gmlp_bs_s"] = [2, 128, 4, 128]
    I["consts"] = [128, 6, 128]
    I["gla_S"] = [2, 64, 16, 4, 128]
    I["ml_w_in"] = [2, 128, KD, 4096]
    I["ml_w_out"] = [2, 128, 16, 1024]
    I["ml_conv_w"] = [2, 128, 16, 4]
    I["ml_conv_b"] = [2, 128, 16]
    I["ml_bdq"] = [2, 128, 16, 128]
    I["ml_bdk"] = [2, 128, 16, 128]
    I["ml_bdv"] = [2, 128, 16, 128]
    I["ml_w_gates"] = [2, 128, 48, 8]
    I["ml_b_gates"] = [2, 128, 8]
    I["ml_norm"] = [2, 128, 16]
    I["ml_skip"] = [2, 128, 16]
    I["ml_CT"] = [2, 16, 4, 128, 4, 512]
    I["ml_n"] = [2, 128, 16, 4, 4]
    I["ml_m"] = [2, 4, 16]
    I["ml_conv"] = [2, 128, 16, 16, 3]
    O = {}
    O["yT"] = [128, KD, T]
    O["gla_S_p"] = [2, 64, 4, 128]
    O["gla_S_s"] = [2, 64, 16, 4, 128]
    O["gmlp_v_s"] = [2, 128, 512]
    O["ml_CT_p"] = [2, 4, 128, 4, 512]
    O["ml_CT_s"] = [2, 16, 4, 128, 4, 512]
    O["ml_n_p"] = [2, 128, 4, 4]
    O["ml_n_s"] = [2, 128, 16, 4, 4]
    O["ml_m_p"] = [2, 4, 1]
    O["ml_m_s"] = [2, 4, 16]
    O["ml_conv_p"] = [2, 128, 16, 3]
    O["ml_conv_s"] = [2, 128, 16, 16, 3]
    return I, O


IN_SPECS, OUT_SPECS = _specs()
C_TRI, C_TRI8, C_ID, C_NEG, C_NEG8, C_MISC = range(6)


def build(nlayers=NL, dbg=None):
    nc = bass.Bass("TRN2", target_bir_lowering=False)
    din = {n: nc.dram_tensor(n, s, F32, kind="ExternalInput").ap() for n, s in IN_SPECS.items()}
    dout = {n: nc.dram_tensor(n, s, F32, kind="ExternalOutput").ap() for n, s in OUT_SPECS.items()}
    with ExitStack() as es:
        K = Kern(nc, es)
        _program(nc, K, es, din, dout, nlayers)
        K.finish()
    return nc


def _program(nc, K, es, din, dout, nlayers):
    TILES = [(i * 512, 512) for i in range(4)] + [(TP, TS)]
    xT = [K.sb("xT%d" % i, [128, KD, n], F32) for i, (o, n) in enumerate(TILES)]
    consts = K.sb("consts", [128, 6, 128], F32)
    ones_bf = K.sb("ones_bf", [128, 128], BF16)
    ones_f = K.sb("ones_f", [128, 128], F32)
    negm = K.sb("negm", [128, 2, 128], F32)
    modT = K.sb("modT", [128, 72, NSEQ], F32)
    csT = K.sb("csT", [128, KD, NSEQ], BF16)
    ffn_norm = K.sb("ffn_norm", [128, NL, 2, KD], F32)
    mix_norm = K.sb("mix_norm", [128, NL, KD], F32)
    final_norm = K.sb("final_norm", [128, KD], F32)
    epsb = K.sb("epsb", [128, 1], F32)
    Avec = K.sb("Avec", [128, KD, NSEQ], F32)
    Gvec = K.sb("Gvec", [128, KD, NSEQ], F32)

    K.dma(consts.v, din["consts"])
    for i, (o, n) in enumerate(TILES):
        K.dma(xT[i].v, din["xT"][:, :, o:o + n])
    K.dma(ffn_norm.v, din["ffn_norm"]); K.dma(mix_norm.v, din["mix_norm"]); K.dma(final_norm.v, din["final_norm"])
    K.memset(ones_bf.v, 1.0); K.memset(ones_f.v, 1.0); K.memset(epsb.v, EPS)
    tri = consts[:, C_TRI, :]; tri8 = consts[:, C_TRI8, :]; ident = consts[:, C_ID, :]
    seqmask = consts[:, C_MISC, 0:16]
    K.ts(negm[:, 0, :], tri, -1.0, ALU.add, -NEG, ALU.mult)
    K.ts(negm[:, 1, :], tri8, -1.0, ALU.add, -NEG, ALU.mult)

    cTf = K.sb("cTf", [128, KD, NSEQ], F32)
    K.dma(cTf.v, din["cT"])
    K.act(csT.v, cTf.v, AF.Silu)

    def ada_steps(l, es_):
        adab = K.sb("adab", [128, 72], F32, es_)
        wbuf = [K.sb("adaw%d" % i, [128, KD, 1024], BF16, es_) for i in range(2)]
        if l < NL:
            pieces = [(din["ada_w"][l, v], v * 8) for v in range(9)]
        else:
            pieces = [(din["fada_w"][v], v * 8) for v in range(2)]

        def load(i):
            if i == 0:
                if l < NL: K.dma(adab.v, din["ada_b"][:, l, :])
                else: K.dma(adab[:, 0:16], din["fada_b"])
            K.dma(wbuf[i % 2].v, pieces[i][0], q="pool")

        def comp(i):
            wb = wbuf[i % 2]; off = pieces[i][1]
            ps = K.ps()
            for j in range(8):
                for k in range(KD):
                    K.mm(ps[:, j * NSEQ:(j + 1) * NSEQ], wb[:, k, j * 128:(j + 1) * 128], csT[:, k, :], start=(k == 0), stop=(k == KD - 1))
            K.tt(modT[:, off:off + 8, :], ps[:, 0:8 * NSEQ].rr("p (j s) -> p j s", s=NSEQ),
                 adab[:, off:off + 8].un(2).bc([128, 8, NSEQ]), ALU.add)

        n = len(pieces)
        steps = []
        for i in range(n + 1):
            def step(i=i):
                if i < n: load(i)
                if i >= 1: comp(i - 1)
            steps.append(step)
        return steps

    def ada(l):
        with ExitStack() as ph:
            for st in ada_steps(l, ph): st()
            K.barrier()

    def mod(l, v):
        return modT[:, v * 8:v * 8 + 8, :]

    def prep_AG(gamma, sc, gate, gmul):
        K.ts(Avec.v, sc, 1.0, ALU.add)
        K.tt(Avec.v, Avec.v, gamma.un(2).bc([128, KD, NSEQ]), ALU.mult)
        if gate is not None:
            K.ts(Gvec.v, gate, gmul, ALU.mult)

    def seq_affine(out, in_, A, B, ti, k):
        if ti < 4:
            K.act(out, in_, AF.Identity, bias=B[:, k, 0:1], scale=A[:, k, 0:1])
        else:
            o3 = out.rr("p (s t) -> p s t", t=8); i3 = in_.rr("p (s t) -> p s t", t=8)
            K.tt(o3, i3, A[:, k, 1:17].un(2).bc([128, 16, 8]), ALU.mult)
            K.tt(o3, o3, B[:, k, 1:17].un(2).bc([128, 16, 8]), ALU.add)

    def resid_add(ti, d, cols, ps_v, G):
        xv = xT[ti][:, d, cols]
        if ti < 4:
            K.stt(xv, ps_v, G[:, d, 0:1], xv, ALU.mult, ALU.add)
        else:
            tmp = rs_tmp.v
            K.tt(tmp.rr("p (s t) -> p s t", t=8), ps_v.rr("p (s t) -> p s t", t=8), G[:, d, 1:17].un(2).bc([128, 16, 8]), ALU.mult)
            K.tt(xv, xv, tmp, ALU.add)

    rs_tmp = K.sb("rs_tmp", [128, 128], F32)
    rs_tmp4 = K.sb("rs_tmp4", [128, 4, 128], F32)

    def resid_add4(ti, d0, cols, ps_v, G):
        xv = xT[ti][:, d0:d0 + 4, cols]
        if ti < 4:
            K.tt(rs_tmp4.v, ps_v, G[:, d0:d0 + 4, 0:1].bc([128, 4, 128]), ALU.mult)
        else:
            K.tt(rs_tmp4.v.rr("p d (s t) -> p d s t", t=8), ps_v.rr("p d (s t) -> p d s t", t=8),
                 G[:, d0:d0 + 4, 1:17].un(3).bc([128, 4, 16, 8]), ALU.mult)
        K.tt(xv, xv, rs_tmp4.v, ALU.add)

    NB = {}

    def norm_bufs(es_, n):
        NB["sq"] = K.sb("nrm_sq", [128, KD, n], BF16, es_)
        NB["rstd"] = K.sb("nrm_rstd", [128, n], F32, es_)
        NB["ntmp"] = K.sb("nrm_tmp", [128, n], F32, es_)

    def rms_rstd(ps_ss, n, dim, out):
        K.act(out[:, 0:n], ps_ss[:, 0:n], AF.Ln, bias=epsb.v, scale=1.0 / dim)
        K.act(out[:, 0:n], out[:, 0:n], AF.Exp, scale=-0.5)

    def norm_tile(ti, hdst, B):
        o, n = TILES[ti]
        sq = NB["sq"]; rstd = NB["rstd"]; ntmp = NB["ntmp"]
        K.act(sq[:, :, 0:n], xT[ti].v, AF.Square)
        ps = K.ps()
        for k in range(KD):
            K.mm(ps[:, 0:n], ones_bf.v, sq[:, k, 0:n], start=(k == 0), stop=(k == KD - 1))
        rms_rstd(ps, n, D, rstd)
        for k in range(KD):
            K.tt(ntmp[:, 0:n], xT[ti][:, k, :], rstd[:, 0:n], ALU.mult)
            seq_affine(hdst[:, k, :], ntmp[:, 0:n], Avec, B, ti, k)

    def ffn(l, w):
        with ExitStack() as ph:
            hT = [K.sb("ffn_h%d" % i, [128, KD, n], BF16, ph) for i, (o, n) in enumerate(TILES)]
            wg = [K.sb("ffn_wg%d" % i, [128, KD, 256], BF16, ph) for i in range(2)]
            wu = [K.sb("ffn_wu%d" % i, [128, KD, 256], BF16, ph) for i in range(2)]
            wd = [K.sb("ffn_wd%d" % i, [128, 2, 1024], BF16, ph) for i in range(2)]
            hid = [K.sb("ffn_hid%d" % i, [128, 2, 512], BF16, ph) for i in range(2)]
            sg = K.sb("ffn_sg", [128, 512], F32, ph)
            prep_AG(ffn_norm[:, l, w, :], mod(l, 1 if w == 0 else 7), mod(l, 2 if w == 0 else 8), 0.5)
            B = mod(l, 0 if w == 0 else 6)
            with ExitStack() as nb:
                norm_bufs(nb, 512)
                for ti in range(5):
                    norm_tile(ti, hT[ti].v, B)
                K.barrier()
            hi = 0
            asteps = ada_steps(l + 1, ph) if w == 1 else []
            for g in range(NG):
                b = g % 2
                if g < len(asteps): asteps[g]()
                K.dma(wg[b].v, din["ffn_wg"][l, w, g], q="pool")
                K.dma(wu[b].v, din["ffn_wu"][l, w, g], q="pool")
                K.dma(wd[b].v, din["ffn_wd"][l, w, g], q="pool")
                for ti, (o, n) in enumerate(TILES):
                    hb = hid[hi % 2]; hi += 1
                    for c in range(2):
                        pg = K.ps(); pu = K.ps()
                        for k in range(KD):
                            K.mm(pg[:, 0:n], wg[b][:, k, c * 128:(c + 1) * 128], hT[ti][:, k, :], start=(k == 0), stop=(k == KD - 1))
                        for k in range(KD):
                            K.mm(pu[:, 0:n], wu[b][:, k, c * 128:(c + 1) * 128], hT[ti][:, k, :], start=(k == 0), stop=(k == KD - 1))
                        K.act(sg[:, 0:n], pg[:, 0:n], AF.Silu)
                        K.tt(hb[:, c, 0:n], sg[:, 0:n], pu[:, 0:n], ALU.mult)
                    for d in range(KD):
                        py = K.ps()
                        for c in range(2):
                            K.mm(py[:, 0:n], wd[b][:, c, d * 128:(d + 1) * 128], hb[:, c, 0:n], start=(c == 0), stop=(c == 1))
                        resid_add(ti, d, slice(0, n), py[:, 0:n], Gvec)
            K.barrier()

    def ab_mixer(l):
        e = l // 2
        with ExitStack() as ph:
            w_in = K.sb("ab_win", [128, KD, 2576], BF16, ph)
            w_out = K.sb("ab_wout", [128, KD, 1024], BF16, ph)
            wa2 = K.sb("ab_wa2", [17, 256], F32, ph)
            gnorm = K.sb("ab_gn", [128, 1], F32, ph)
            vnB = K.sb("ab_vn", [128, 128], F32, ph)
            wTp = K.sb("ab_wTp", [128, 4, 128], F32, ph); wTs = K.sb("ab_wTs", [128, 4, 128], F32, ph)
            wTpb = K.sb("ab_wTpb", [128, 4, 128], BF16, ph); wTsb = K.sb("ab_wTsb", [128, 4, 128], BF16, ph)
            bsp = K.sb("ab_bsp", [128, 4, 128], F32, ph); bss = K.sb("ab_bss", [128, 4, 128], F32, ph)
            hT = K.sb("ab_h", [128, KD, 128], BF16, ph)
            qT = K.sb("ab_q", [64, 4, 128], F32, ph); kT = K.sb("ab_k", [64, 4, 128], F32, ph)
            qb = K.sb("ab_qb", [64, 4, 128], BF16, ph); kb = K.sb("ab_kb", [64, 4, 128], BF16, ph)
            kd = K.sb("ab_kd", [64, 4, 128], F32, ph); kdt = K.sb("ab_kdt", [128, 4, 64], BF16, ph)
            a_aug = K.sb("ab_aaug", [17, 128], F32, ph)
            la = K.sb("ab_la", [128, 256], F32, ph)
            eb = K.sb("ab_eb", [64, 4, 128], F32, ph); enb = K.sb("ab_enb", [64, 4, 128], F32, ph)
            vtok = K.sb("ab_vtok", [128, 512], BF16, ph)
            sgT = K.sb("ab_sg", [128, 4, 128], F32, ph)
            uT = K.sb("ab_u", [128, 4, 128], F32, ph)
            vbn = K.sb("ab_vbn", [128, 512], F32, ph); vbnb = K.sb("ab_vbnb", [128, 512], BF16, ph)
            ssq = K.sb("ab_ssq", [128, 4], F32, ph); vjunk = K.sb("ab_vj", [128, 128], F32, ph)
            sT = K.sb("ab_sT", [128, 128], BF16, ph)
            S = K.sb("ab_S", [64, 4, 128], F32, ph); Sb = K.sb("ab_Sb", [64, 4, 128], BF16, ph)
            Ss = K.sb("ab_Ss", [64, 4, 128], F32, ph); Ssn = K.sb("ab_Ssn", [64, 4, 128], F32, ph)
            Ssb = K.sb("ab_Ssb", [64, 4, 128], BF16, ph)
            R = K.sb("ab_R", [128, 4, 128], BF16, ph)
            ebl = K.sb("ab_ebl", [64, 4, 16], F32, ph)
            oT = K.sb("ab_o", [128, 4, 128], F32, ph); osq = K.sb("ab_osq", [128, 4, 128], BF16, ph)
            orstd = K.sb("ab_orstd", [128, 4, 128], F32, ph)
            mix = K.sb("ab_mix", [128, KD, 128], BF16, ph)
            ztmp = K.sb("ab_ztmp", [128, 128], F32, ph)
            K.dma(w_in.v, din["ab_w_in"][e], q="pool"); K.dma(w_out.v, din["ab_w_out"][e], q="pool")
            K.dma(wa2.v, din["gla_wa2"][e]); K.dma(gnorm.v, din["gla_norm"][e]); K.dma(vnB.v, din["gmlp_norm"][e])
            K.dma(wTp.v, din["gmlp_wT_p"][e]); K.dma(wTs.v, din["gmlp_wT_s"][e])
            K.dma(bsp.v, din["gmlp_bs_p"][e]); K.dma(bss.v, din["gmlp_bs_s"][e])
            K.tt(wTpb.v, wTp.v, tri.un(1).bc([128, 4, 128]), ALU.mult)
            K.tt(wTsb.v, wTs.v, tri8.un(1).bc([128, 4, 128]), ALU.mult)
            K.memset(a_aug.v, 1.0)
            K.memset(S.v, 0.0); K.memset(Sb.v, 0.0)
            prep_AG(mix_norm[:, l, :], mod(l, 4), mod(l, 5), 1.0)
            B = mod(l, 3)
            norm_bufs(ph, 128)
            sq = NB["sq"]; rstd = NB["rstd"]; ntmp = NB["ntmp"]
            for ci in range(17):
                samp = (ci == 16)
                ti = ci // 4 if not samp else 4
                c0 = (ci % 4) * 128 if not samp else 0
                cols = slice(c0, c0 + 128)
                mask = tri8 if samp else tri
                K.act(sq[:, :, 0:128], xT[ti][:, :, cols], AF.Square)
                ps = K.ps()
                for k in range(KD):
                    K.mm(ps[:, 0:128], ones_bf.v, sq[:, k, 0:128], start=(k == 0), stop=(k == KD - 1))
                rms_rstd(ps, 128, D, rstd)
                for k in range(KD):
                    K.tt(ntmp[:, 0:128], xT[ti][:, k, cols], rstd[:, 0:128], ALU.mult)
                    seq_affine(hT[:, k, :], ntmp[:, 0:128], Avec, B, ti, k)
                def proj_f(col0, m, dst_ps):
                    for k in range(KD):
                        K.mm(dst_ps, w_in[:, k, col0:col0 + m], hT[:, k, :], start=(k == 0), stop=(k == KD - 1))
                ps = K.ps()
                for h in range(4):
                    proj_f(h * 64, 64, ps[0:64, h * 128:(h + 1) * 128])
                K.act(qT.v, ps[0:64, :].rr("p (h t) -> p h t", t=128), AF.Copy, scale=64 ** -0.5)
                ps = K.ps()
                for h in range(4):
                    proj_f(256 + h * 64, 64, ps[0:64, h * 128:(h + 1) * 128])
                K.cp(kT.v, ps[0:64, :].rr("p (h t) -> p h t", t=128))
                ps = K.ps()
                proj_f(1536, 16, ps[0:16, 0:128])
                K.cp(a_aug[0:16, :], ps[0:16, 0:128])
                ps = K.ps()
                for k in range(KD):
                    K.mm(ps[:, 0:512], hT[:, k, :], w_in[:, k, 512:1024], start=(k == 0), stop=(k == KD - 1))
                K.act(vtok.v, ps[:, 0:512], AF.Copy)
                ps = K.ps()
                for j in range(4):
                    proj_f(1024 + j * 128, 128, ps[:, j * 128:(j + 1) * 128])
                K.act(sgT.v, ps[:, :].rr("p (h t) -> p h t", t=128), AF.Silu)
                ps = K.ps()
                for j in range(4):
                    proj_f(1552 + j * 128, 128, ps[:, j * 128:(j + 1) * 128])
                K.cp(uT.v, ps[:, :].rr("p (h t) -> p h t", t=128))
                ps = K.ps()
                for k in range(KD):
                    K.mm(ps[:, 0:512], hT[:, k, :], w_in[:, k, 2576 - 512:2576], start=(k == 0), stop=(k == KD - 1))
                for g in range(4):
                    K.act(vjunk.v, ps[:, g * 128:(g + 1) * 128], AF.Square, accum=ssq[:, g:g + 1])
                K.act(ssq.v, ssq.v, AF.Ln, bias=epsb.v, scale=1.0 / 128)
                K.act(ssq.v, ssq.v, AF.Exp, scale=-0.5)
                for g in range(4):
                    K.stt(vbn[:, g * 128:(g + 1) * 128], ps[:, g * 128:(g + 1) * 128], ssq[:, g:g + 1], vnB.v, ALU.mult, ALU.mult)
                K.cp(vbnb.v, vbn.v)
                if samp:
                    K.dma(dout["gmlp_v_s"][e], vbn.v)
                ps = K.ps()
                K.mm(ps[:, 0:256], a_aug.v, wa2.v)
                K.act(la.v, ps[:, 0:256], AF.Exp, scale=-1.0)
                K.act(la.v, la.v, AF.Ln, bias=1.0)
                ps = K.ps()
                for h in range(4):
                    K.mm(ps[0:64, h * 128:(h + 1) * 128], la[:, h * 64:(h + 1) * 64], mask)
                bp = ps[0:64, :].rr("p (h t) -> p h t", t=128)
                K.act(eb.v, bp, AF.Exp, scale=-1.0 / 16)
                K.act(enb.v, bp, AF.Exp, scale=1.0 / 16)
                K.tt(qb.v, qT.v, eb.v, ALU.mult)
                K.tt(kb.v, kT.v, enb.v, ALU.mult)
                if not samp:
                    K.tt(kd.v, kT.v, enb.v, ALU.mult)
                    for h in range(4):
                        K.ts(kd[:, h, :], kd[:, h, :], eb[:, h, 127:128], ALU.mult)
                else:
                    K.cp(ebl.v, eb.v.rr("p h (s t) -> p h s t", t=8)[:, :, :, 7])
                    K.tt(kd.v, kT.v, enb.v, ALU.mult)
                    K.tt(kd.v.rr("p h (s t) -> p h s t", t=8), kd.v.rr("p h (s t) -> p h s t", t=8),
                         ebl.v.un(3).bc([64, 4, 16, 8]), ALU.mult)
                ps = K.ps()
                for h in range(4):
                    K.tr(ps[:, h * 64:(h + 1) * 64], kd[:, h, :], ident[0:64, 0:64])
                K.cp(kdt.v, ps[:, 0:256].rr("p (h d) -> p h d", d=64))
                for h in range(4):
                    vh = vtok[:, h * 128:(h + 1) * 128]
                    ps = K.ps()
                    K.mm(ps[:, 0:128], kb[:, h, :], qb[:, h, :])
                    K.tt(sT.v, ps[:, 0:128], mask, ALU.mult)
                    po = K.ps_pin(0)
                    if not samp:
                        K.mm(po[:, 0:128], vh, sT.v, start=True, stop=False)
                        K.mm(po[:, 0:128], Sb[:, h, :], qb[:, h, :], start=False, stop=True)
                        K.cp(oT[:, h, :], po[:, 0:128])
                        pk = K.ps()
                        K.mm(pk[0:64, 0:128], kdt[:, h, :], vh)
                        K.stt(S[:, h, :], S[:, h, :], eb[:, h, 127:128], pk[0:64, 0:128], ALU.mult, ALU.add)
                        K.act(Sb[:, h, :], S[:, h, :], AF.Copy)
                    else:
                        K.mm(po[:, 0:128], vh, sT.v, start=True, stop=False)
                        for q4 in range(4):
                            sl = slice(q4 * 4, q4 * 4 + 4)
                            K.dma(Ss.v, din["gla_S"][e][:, sl, h, :])
                            K.cp(Ssb.v, Ss.v)
                            for s4 in range(4):
                                s_ = q4 * 4 + s4
                                K.mm(po[:, s_ * 8:(s_ + 1) * 8], Ssb[:, s4, :], qb[:, h, s_ * 8:(s_ + 1) * 8], start=False, stop=(s_ == 15))
                            K.tt(R.v, vh.un(1).bc([128, 4, 128]), seqmask[:, sl].un(2).bc([128, 4, 128]), ALU.mult)
                            pk = K.ps()
                            K.mm(pk[0:64, 0:512], kdt[:, h, :], R.v.rr("p s v -> p (s v)"))
                            K.tt(Ssn.v, Ss.v, ebl[:, h, sl].un(2).bc([64, 4, 128]), ALU.mult)
                            K.tt(Ssn.v, Ssn.v, pk[0:64, 0:512].rr("p (s v) -> p s v", v=128), ALU.add)
                            K.dma(dout["gla_S_s"][e][:, sl, h, :], Ssn.v)
                        K.cp(oT[:, h, :], po[:, 0:128])
                if ci == 15:
                    K.dma(dout["gla_S_p"][e], S.v)
                K.act(osq.v, oT.v, AF.Square)
                ps = K.ps()
                for h in range(4):
                    K.mm(ps[:, h * 128:(h + 1) * 128], ones_bf.v, osq[:, h, :])
                K.act(orstd.v, ps[:, :].rr("p (h t) -> p h t", t=128), AF.Ln, bias=epsb.v, scale=1.0 / 128)
                K.act(orstd.v, orstd.v, AF.Exp, scale=-0.5)
                K.tt(oT.v, oT.v, orstd.v, ALU.mult)
                K.stt(mix[:, 0:4, :], oT.v, gnorm.v, sgT.v, ALU.mult, ALU.mult)
                wTb = wTsb if samp else wTpb
                bsB = bss if samp else bsp
                for g in range(4):
                    ps = K.ps()
                    K.mm(ps[:, 0:128], vbnb[:, g * 128:(g + 1) * 128], wTb[:, g, :])
                    K.tt(ztmp.v, ps[:, 0:128], bsB[:, g, :], ALU.add)
                    K.tt(mix[:, 4 + g, :], ztmp.v, uT[:, g, :], ALU.mult)
                for half in range(2):
                    ps = K.ps()
                    for dd in range(4):
                        d = half * 4 + dd
                        for k in range(KD):
                            K.mm(ps[:, dd * 128:(dd + 1) * 128], w_out[:, k, d * 128:(d + 1) * 128], mix[:, k, :], start=(k == 0), stop=(k == KD - 1))
                    resid_add4(ti, half * 4, cols, ps[:, :].rr("p (d t) -> p d t", t=128), Gvec)
            K.barrier()

    def mlstm(l):
        o_ = l // 2
        with ExitStack() as ph:
            hT = K.sb("ml_h", [128, KD, T], BF16, ph)
            prep_AG(mix_norm[:, l, :], mod(l, 4), mod(l, 5), 1.0)
            B = mod(l, 3)
            with ExitStack() as nb:
                norm_bufs(nb, 512)
                for ti in range(5):
                    norm_tile(ti, hT[:, :, TILES[ti][0]:TILES[ti][0] + TILES[ti][1]], B)
                K.barrier()
            cw = K.sb("ml_cw", [128, 16, 4], F32, ph); cb = K.sb("ml_cb", [128, 16], F32, ph)
            gn = K.sb("ml_gn", [128, 16], F32, ph); skp = K.sb("ml_skip", [128, 16], F32, ph)
            bd = {n: K.sb("ml_" + n, [128, 4, 128], BF16, ph) for n in ("bdq", "bdk", "bdv")}
            wgt = K.sb("ml_wgt", [128, 48, 8], BF16, ph); bg = K.sb("ml_bg", [128, 8], F32, ph)
            mst = K.sb("ml_mst", [4, 16], F32, ph)
            gacc = K.sb("ml_gacc", [128, 17, 8], F32, ph)
            colsAll = K.sb("ml_cols", [128, 17, 20], F32, ph)
            decB = K.sb("ml_decB", [128, 4, 32], F32, ph)
            sel = K.sb("ml_sel", [4, 4, 128], F32, ph)
            K.dma(cw.v, din["ml_conv_w"][o_]); K.dma(cb.v, din["ml_conv_b"][o_])
            K.dma(gn.v, din["ml_norm"][o_]); K.dma(skp.v, din["ml_skip"][o_])
            K.dma(wgt.v, din["ml_w_gates"][o_], q="pool"); K.dma(bg.v, din["ml_b_gates"][o_])
            K.dma(mst.v, din["ml_m"][o_])
            K.cp(sel.v, ident[0:4, 0:4].un(2).bc([4, 4, 128]))
            xmes = K.sb("ml_xmes", [128, 4, 16, 16], F32, ph)

            def front_set(es_, tag):
                return {"xme": K.sb("ml_xme" + tag, [128, 4, 136], F32, es_), "xmb": K.sb("ml_xmb" + tag, [128, 4, 128], BF16, es_),
                        "acc": [K.sb("ml_acc%d" % j + tag, [128, 128], F32, es_) for j in range(4)], "xc": K.sb("ml_xc" + tag, [128, 4, 128], BF16, es_),
                        "qT": K.sb("ml_q" + tag, [128, 4, 128], BF16, es_), "kT": K.sb("ml_k" + tag, [128, 4, 128], BF16, es_),
                        "vT": K.sb("ml_v" + tag, [128, 4, 128], BF16, es_)}
            fb0 = front_set(ph, "0")
            w_x = K.sb("ml_wx", [128, KD, 512], BF16, ph)
            cvin = K.sb("ml_cvin", [128, 4, 16, 3], F32, ph); cvout = K.sb("ml_cvout", [128, 4, 16, 3], F32, ph)
            cvp = K.sb("ml_cvp", [128, 4, 3], F32, ph)

            WS = {"w_x": w_x, "bd": bd}

            def load_w(h, ws):
                K.dma(ws["w_x"].v, din["ml_w_in"][o_][:, :, h * 512:(h + 1) * 512], q="pool")
                for n in ws["bd"]: K.dma(ws["bd"][n].v, din["ml_" + n][o_][:, 4 * h:4 * h + 4, :], q="pool")

            def load_head(h):
                K.dma(cvin.v, din["ml_conv"][o_][:, 4 * h:4 * h + 4, :, :])
                K.cp(xmes[:, :, :, 5:8], cvin.v)
                K.memset(fb0["xme"][:, :, 0:8], 0.0)

            def front(ci, h, need_v_T, fb, fbn):
                frontA(ci, h, fb, fbn)
                frontB(ci, h, need_v_T, fb)

            def frontA(ci, h, fb, fbn):
                xme = fb["xme"]; xmb = fb["xmb"]; acc = fb["acc"]; xc = fb["xc"]
                samp = (ci == 16)
                t0 = ci * 128
                ps = K.ps()
                for j in range(4):
                    for k in range(KD):
                        K.mm(ps[:, j * 128:(j + 1) * 128], WS["w_x"][:, k, j * 128:(j + 1) * 128], hT[:, k, t0:t0 + 128], start=(k == 0), stop=(k == KD - 1))
                if FRONT_STEPS == 0:
                    K.cp(xmb.v, ps[:, :].rr("p (f t) -> p f t", t=128)); return
                if not samp:
                    K.act(xme[:, :, 8:136], ps[:, :].rr("p (f t) -> p f t", t=128), AF.Copy)
                else:
                    K.act(xmes[:, :, :, 8:16], ps[:, :].rr("p (f s t) -> p f s t", t=8, s=16), AF.Copy)
                if not samp:
                    K.act(xmb.v, xme[:, :, 8:136], AF.Copy)
                else:
                    K.act(xmb.v.rr("p f (s t) -> p f s t", t=8), xmes[:, :, :, 8:16], AF.Copy)
                if FRONT_STEPS < 2: return
                for tap in range(4):
                    for j in range(4):
                        fc = 4 * h + j
                        if not samp:
                            src = xme[:, j, 5 + tap:5 + tap + 128]; a_ = acc[j].v
                        else:
                            src = xmes[:, j, :, 5 + tap:5 + tap + 8]; a_ = acc[j].v.rr("p (s t) -> p s t", t=8)
                        if tap == 0:
                            K.ts(a_, src, cw[:, fc, 0:1], ALU.mult)
                        else:
                            K.stt(a_, src, cw[:, fc, tap:tap + 1], a_, ALU.mult, ALU.add)
                for j in range(4):
                    fc = 4 * h + j
                    K.act(xc[:, j, :], acc[j].v, AF.Silu, bias=cb[:, fc:fc + 1])
                if not samp:
                    K.act(fbn["xme"][:, :, 5:8], xme[:, :, 133:136], AF.Copy)

            def frontB(ci, h, need_v_T, fb):
                xme = fb["xme"]; xmb = fb["xmb"]; xc = fb["xc"]; qT = fb["qT"]; kT = fb["kT"]; vT = fb["vT"]
                samp = (ci == 16)
                for nm, src, dst in (("bdq", xc, qT), ("bdk", xc, kT), ("bdv", xmb, vT)):
                    if nm == "bdv" and not need_v_T: continue
                    ps = K.ps()
                    for j in range(4):
                        K.mm(ps[:, j * 128:(j + 1) * 128], WS["bd"][nm][:, j, :], src[:, j, :])
                    K.act(dst.v, ps[:, :].rr("p (f t) -> p f t", t=128), AF.Copy)
                if FRONT_STEPS < 5: return
                if not samp:
                    if ci == 15 and need_v_T:
                        K.cp(cvp.v, xme[:, :, 133:136])
                        K.dma(dout["ml_conv_p"][o_][:, 4 * h:4 * h + 4, :], cvp.v)
                elif need_v_T:
                    K.cp(cvout.v, xmes[:, :, :, 13:16])
                    K.dma(dout["ml_conv_s"][o_][:, 4 * h:4 * h + 4, :, :], cvout.v)

            if MLSTM_MODE == 10:
                K.barrier(); return
            p1a = ExitStack()
            fb1 = front_set(p1a, "1")
            fbs = [fb0, fb1]
            w_x2 = K.sb("ml_wx2", [128, KD, 512], BF16, p1a)
            bd2 = {n: K.sb("ml2_" + n, [128, 4, 128], BF16, p1a) for n in ("bdq", "bdk", "bdv")}
            wsets = [{"w_x": w_x, "bd": bd}, {"w_x": w_x2, "bd": bd2}]
            K.nrot = 8
            load_w(0, wsets[0])
            for h in range(4):
                if h + 1 < 4: load_w(h + 1, wsets[(h + 1) % 2])
                WS.update(wsets[h % 2])
                load_head(h)
                frontA(0, h, fbs[0], fbs[1])
                for ci in range(17):
                    fb = fbs[ci % 2]
                    if ci + 1 < 17:
                        frontA(ci + 1, h, fbs[(ci + 1) % 2], fbs[ci % 2])
                    frontB(ci, h, True, fb)
                    ps = K.ps()
                    i = 0
                    for part, src in enumerate((fb["qT"], fb["kT"], fb["vT"])):
                        for j in range(4):
                            K.mm(ps[:, 0:8], src[:, j, :], wgt[:, part * 16 + 4 * h + j, :], start=(i == 0), stop=(i == 11)); i += 1
                    K.tt(gacc[:, ci, :], ps[:, 0:8], bg.v if h == 0 else gacc[:, ci, :], ALU.add)
            K.barrier()
            p1a.close()
            K.nrot = 6
            WS.update(wsets[0])
            if MLSTM_MODE in (1, 11, 12, 13):
                K.barrier(); return
            K.act(gacc[:, :, 4:8], gacc[:, :, 4:8], AF.Exp, scale=-1.0)
            K.act(gacc[:, :, 4:8], gacc[:, :, 4:8], AF.Ln, bias=1.0)
            with ExitStack() as p1:
                R32 = {n: K.sb("ml_r_" + n, [32, T], F32, p1) for n in ("ig", "lf", "m", "F", "wr")}
                for n in R32: K.memset(R32[n].v, 0.0)
                R_ = {n: R32[n][0:4, :] for n in R32}
                mprev = K.sb("ml_mprev", [4, 32], F32, p1); rend = K.sb("ml_rend", [4, 32], F32, p1); fst = K.sb("ml_fst", [4, 16], F32, p1)
                dec = K.sb("ml_dec", [4, 32], F32, p1)
                for ci in range(17):
                    ps = K.ps()
                    K.tr(ps[0:4, 0:128], gacc[:, ci, 0:4], ident)
                    K.tr(ps[0:4, 128:256], gacc[:, ci, 4:8], ident)
                    K.cp(R_["ig"][:, ci * 128:(ci + 1) * 128], ps[0:4, 0:128])
                    K.ts(R_["lf"][:, ci * 128:(ci + 1) * 128], ps[0:4, 128:256], -1.0, ALU.mult)
                K.scan(R_["m"][:, 0:TP], R_["lf"][:, 0:TP], R_["ig"][:, 0:TP], 0.0, ALU.add, ALU.max)
                K.scan(R_["F"][:, 0:TP], R_["lf"][:, 0:TP], R_["lf"][:, 0:TP], 0.0, ALU.add, ALU.min)
                for s in range(16):
                    sl = slice(TP + s * 8, TP + s * 8 + 8)
                    K.scan(R_["m"][:, sl], R_["lf"][:, sl], R_["ig"][:, sl], mst[:, s:s + 1], ALU.add, ALU.max)
                    K.scan(R_["F"][:, sl], R_["lf"][:, sl], R_["lf"][:, sl], 0.0, ALU.add, ALU.min)
                seg = lambda n: (R_[n][:, 0:TP].rr("p (c t) -> p c t", t=128), R_[n][:, TP:T].rr("p (c t) -> p c t", t=8))
                Fp = seg("F")[0]; mp = seg("m")[0]
                K.memset(fst[:, 0:1], 0.0); K.memset(mprev[:, 0:1], 0.0)
                K.cp(fst[:, 1:16], Fp[:, 0:15, 127]); K.cp(mprev[:, 1:16], mp[:, 0:15, 127])
                K.cp(mprev[:, 16:32], mst.v)
                K.tt(Fp, Fp, fst.v.un(2).bc([4, 16, 128]), ALU.subtract)
                K.dma(dout["ml_m_p"][o_], R_["m"][:, TP - 1:TP])
                msout = K.sb("ml_msout", [4, 16], F32, p1)
                K.cp(msout.v, seg("m")[1][:, :, 7])
                K.dma(dout["ml_m_s"][o_], msout.v)
                K.tt(R_["ig"], R_["ig"], R_["F"], ALU.subtract)
                K.tt(R_["F"], R_["F"], R_["m"], ALU.subtract)
                K.act(R_["lf"], R_["m"], AF.Exp, scale=-1.0)
                R_["wi"] = R_["m"]; R32["wi"] = R32["m"]
                K.cp(rend[:, 0:16], seg("F")[0][:, :, 127]); K.cp(rend[:, 16:32], seg("F")[1][:, :, 7])
                for pi, (off, L) in enumerate(((0, 128), (16, 8))):
                    K.tt(seg("wi")[pi], seg("F")[pi], mprev[:, off:off + 16].un(2).bc([4, 16, L]), ALU.add)
                    K.tt(seg("wr")[pi], seg("ig")[pi], rend[:, off:off + 16].un(2).bc([4, 16, L]), ALU.add)
                K.act(R_["wi"], R_["wi"], AF.Exp)
                K.act(R_["wr"], R_["wr"], AF.Exp)
                K.ts(R_["wr"], R_["wr"], 512 ** -0.5, ALU.mult)
                K.cp(dec[:, 0:16], seg("wi")[0][:, :, 127]); K.cp(dec[:, 16:32], seg("wi")[1][:, :, 7])
                for ci in range(17):
                    ps = K.ps()
                    for i, n in enumerate(("ig", "wr", "F", "wi", "lf")):
                        K.tr(ps[:, i * 32:(i + 1) * 32], R32[n][:, ci * 128:(ci + 1) * 128], ident[0:32, 0:32])
                    K.cp(colsAll[:, ci, :].rr("p (i f) -> p i f", f=4), ps[:, 0:160].rr("p (i f) -> p i f", f=32)[:, :, 0:4])
                K.barrier()
            if MLSTM_MODE == 2:
                K.barrier(); return
            with ExitStack() as p2:
                w_z = K.sb("ml_wz", [128, KD, 512], BF16, p2)
                w_o = K.sb("ml_wo", [128, 4, 1024], BF16, p2)
                CT = K.sb("ml_CT", [128, 4, 512], F32, p2); CTb = K.sb("ml_CTb", [128, 4, 512], BF16, p2)
                CT2 = K.sb("ml_CT2", [128, 4, 512], F32, p2)
                nS = K.sb("ml_nS", [128, 16, 4, 4], F32, p2); nSn = K.sb("ml_nSn", [128, 16, 4, 4], F32, p2)
                nP = K.sb("ml_nP", [128, 4, 4], F32, p2)
                nB = K.sb("ml_nB", [128, 4, 128], BF16, p2)
                ktok = K.sb("ml_ktok", [128, 512], BF16, p2); vtok = K.sb("ml_vtok", [128, 512], BF16, p2)
                vw = K.sb("ml_vw", [128, 512], BF16, p2); km = K.sb("ml_km", [128, 512], BF16, p2)
                wrb = K.sb("ml_wrb", [128, 4], BF16, p2)
                diag = [K.sb("ml_diag%d" % i, [128, 128], F32, p2) for i in range(3)]
                bcs = K.sb("ml_bcs", [128, 3, 128], F32, p2)
                arg = K.sb("ml_arg", [128, 128], F32, p2); sT = K.sb("ml_sT", [128, 128], BF16, p2)
                qw = K.sb("ml_qw", [128, 4, 128], BF16, p2)
                hden = K.sb("ml_hden", [128, 128], F32, p2)
                hh = K.sb("ml_hh", [128, 4, 128], F32, p2); hsq = K.sb("ml_hsq", [128, 4, 128], F32, p2)
                mean = K.sb("ml_mean", [128, 128], F32, p2); var = K.sb("ml_var", [128, 128], F32, p2)
                sz = K.sb("ml_sz", [128, 4, 128], BF16, p2); pre = K.sb("ml_pre", [128, 4, 128], BF16, p2)
                skx = rs_tmp4
                fb2 = dict(fb0)
                fb2["xme"] = K.sb("ml_xme2", [128, 4, 136], F32, p2)
                fb2["xmb"] = K.sb("ml_xmb2", [128, 4, 128], BF16, p2)
                fb2["xc"] = K.sb("ml_xc2", [128, 4, 128], BF16, p2)
                fbs2 = [fb0, fb2]
                interN = hsq; interD = mean
                K.dma(nS.v, din["ml_n"][o_])
                K.memset(nP.v, 0.0)
                for h in range(4):
                    load_w(h, wsets[0])
                    load_head(h)
                    K.dma(w_z.v, din["ml_w_in"][o_][:, :, 2048 + h * 512:2048 + (h + 1) * 512], q="pool")
                    K.dma(w_o.v, din["ml_w_out"][o_][:, 4 * h:4 * h + 4, :], q="pool")
                    K.memset(CT.v, 0.0); K.memset(CTb.v, 0.0); K.memset(nB.v, 0.0)
                    frontA(0, h, fbs2[0], fbs2[1])
                    for ci in range(17):
                        samp = (ci == 16)
                        ti = ci // 4 if not samp else 4
                        c0 = (ci % 4) * 128 if not samp else 0
                        cols = slice(c0, c0 + 128)
                        tsl = slice(ci * 128, ci * 128 + 128)
                        fbc = fbs2[ci % 2]
                        if ci + 1 < 17:
                            frontA(ci + 1, h, fbs2[(ci + 1) % 2], fbs2[ci % 2])
                        frontB(ci, h, False, fbc)
                        xc = fbc["xc"]; xmb = fbc["xmb"]; qT = fbc["qT"]; kT = fbc["kT"]
                        for nm, src, dst in (("bdk", xc, ktok), ("bdv", xmb, vtok)):
                            ps = K.ps()
                            for j in range(4):
                                K.mm(ps[:, j * 128:(j + 1) * 128], src[:, j, :], bd[nm][:, j, :])
                            K.act(dst.v, ps[:, :], AF.Copy)
                        gcol = colsAll[:, ci, h:h + 1]; wrcol = colsAll[:, ci, 4 + h:5 + h]
                        K.cp(wrb.v, colsAll[:, ci, 4:8])
                        ps = K.ps()
                        for i in range(3):
                            K.ts(diag[i].v, ident, colsAll[:, ci, 8 + 4 * i + h:9 + 4 * i + h], ALU.mult)
                        for i in range(3):
                            K.mm(ps[:, i * 128:(i + 1) * 128], ones_f.v, diag[i].v)
                        K.act(bcs.v, ps[:, 0:384].rr("p (i t) -> p i t", t=128), AF.Copy)
                        K.stt(arg.v, bcs[:, 0, :], gcol, negm[:, 1 if samp else 0, :], ALU.add, ALU.add)
                        K.act(arg.v, arg.v, AF.Exp)
                        ps = K.ps()
                        for j in range(4):
                            K.mm(ps[:, 0:128], kT[:, j, :], qT[:, j, :], start=(j == 0), stop=(j == 3))
                        K.stt(sT.v, ps[:, 0:128], 512 ** -0.5, arg.v, ALU.mult, ALU.mult)
                        K.tt(qw.v, qT.v, bcs[:, 1, :].un(1).bc([128, 4, 128]), ALU.mult)
                        pn = K.ps_pin(0); pd = K.ps_pin(1)
                        if not samp:
                            for vc in range(4):
                                K.mm(pn[:, vc * 128:(vc + 1) * 128], vtok[:, vc * 128:(vc + 1) * 128], sT.v, start=True, stop=False)
                                for dc in range(4):
                                    K.mm(pn[:, vc * 128:(vc + 1) * 128], CTb[:, dc, vc * 128:(vc + 1) * 128], qw[:, dc, :], start=False, stop=(dc == 3))
                            K.mm(pd[:, 0:128], ones_bf.v, sT.v, start=True, stop=False)
                            for dc in range(4):
                                K.mm(pd[:, 0:128], nB[:, dc, :], qw[:, dc, :], start=False, stop=(dc == 3))
                        else:
                            for vc in range(4):
                                K.mm(pn[:, vc * 128:(vc + 1) * 128], vtok[:, vc * 128:(vc + 1) * 128], sT.v, start=True, stop=True)
                            K.mm(pd[:, 0:128], ones_bf.v, sT.v, start=True, stop=True)
                            K.act(vw.v, vtok.v, AF.Copy, scale=wrcol)
                            CTs = [CT, CT2]
                            K.dma(CT.v, din["ml_CT"][o_, 0, h])
                            for s in range(16):
                                CTc = CTs[s % 2]
                                if s + 1 < 16:
                                    K.dma(CTs[(s + 1) % 2].v, din["ml_CT"][o_, s + 1, h])
                                K.act(CTb.v, CTc.v, AF.Copy)
                                K.cp(nB.v, nS[:, s, h, :].un(2).bc([128, 4, 128]))
                                ssl = slice(s * 8, s * 8 + 8)
                                pi = K.ps()
                                for vc in range(4):
                                    for dc in range(4):
                                        K.mm(pi[:, vc * 8:vc * 8 + 8], CTb[:, dc, vc * 128:(vc + 1) * 128], qw[:, dc, ssl], start=(dc == 0), stop=(dc == 3))
                                for dc in range(4):
                                    K.mm(pi[:, 32:40], nB[:, dc, :], qw[:, dc, ssl], start=(dc == 0), stop=(dc == 3))
                                K.cp(interN[:, :, ssl], pi[:, 0:32].rr("p (v t) -> p v t", t=8))
                                K.cp(interD[:, ssl], pi[:, 32:40])
                                K.ts(km.v, ktok.v, seqmask[:, s:s + 1], ALU.mult)
                                pnn = K.ps()
                                for dc in range(4):
                                    pk = K.ps()
                                    K.mm(pk[:, 0:512], km[:, dc * 128:(dc + 1) * 128], vw.v)
                                    K.stt(CTc[:, dc, :], CTc[:, dc, :], bcs[:, 1, s * 8 + 7:s * 8 + 8], pk[:, 0:512], ALU.mult, ALU.add)
                                    K.mm(pnn[:, dc * 4:(dc + 1) * 4], km[:, dc * 128:(dc + 1) * 128], wrb.v)
                                K.stt(nSn[:, s, h, :], nS[:, s, h, :], bcs[:, 1, s * 8 + 7:s * 8 + 8], pnn[:, 0:16].rr("p (d f) -> p d f", f=4)[:, :, h], ALU.mult, ALU.add)
                                K.dma(dout["ml_CT_s"][o_, s, h], CTc.v, q="pool")
                        if samp:
                            K.tt(interD.v, interD.v, pd[:, 0:128], ALU.add)
                            K.tt(interN.v, interN.v, pn[:, :].rr("p (v t) -> p v t", t=128), ALU.add)
                            den_v = interD.v; num_v = interN.v
                        else:
                            den_v = pd[:, 0:128]; num_v = pn[:, :].rr("p (v t) -> p v t", t=128)
                        K.ts(hden.v, den_v, -1.0, ALU.mult)
                        K.tt(hden.v, hden.v, den_v, ALU.max)
                        K.tt(hden.v, hden.v, bcs[:, 2, :], ALU.max)
                        K.recip(hden.v, hden.v)
                        K.tt(hh.v, num_v, hden.v.un(1).bc([128, 4, 128]), ALU.mult)
                        K.act(hsq.v, hh.v, AF.Square)
                        ps = K.ps()
                        for vc in range(4):
                            K.mm(ps[:, 0:128], ones_f.v, hh[:, vc, :], start=(vc == 0), stop=(vc == 3))
                        for vc in range(4):
                            K.mm(ps[:, 128:256], ones_f.v, hsq[:, vc, :], start=(vc == 0), stop=(vc == 3))
                        K.act(mean.v, ps[:, 0:128], AF.Copy, scale=1.0 / 512)
                        K.tt(var.v, mean.v, mean.v, ALU.mult)
                        K.stt(var.v, ps[:, 128:256], 1.0 / 512, var.v, ALU.mult, ALU.subtract)
                        K.act(var.v, var.v, AF.Ln, bias=epsb.v)
                        K.act(var.v, var.v, AF.Exp, scale=-0.5)
                        K.tt(hh.v, hh.v, mean.v.un(1).bc([128, 4, 128]), ALU.subtract)
                        K.tt(hh.v, hh.v, var.v.un(1).bc([128, 4, 128]), ALU.mult)
                        ps = K.ps()
                        for j in range(4):
                            for k in range(KD):
                                K.mm(ps[:, j * 128:(j + 1) * 128], w_z[:, k, j * 128:(j + 1) * 128], hT[:, k, tsl], start=(k == 0), stop=(k == KD - 1))
                        K.act(sz.v, ps[:, :].rr("p (f t) -> p f t", t=128), AF.Silu)
                        K.tt(hh.v, hh.v, gn[:, 4 * h:4 * h + 4].un(2).bc([128, 4, 128]), ALU.mult)
                        K.tt(skx.v, xc.v, skp[:, 4 * h:4 * h + 4].un(2).bc([128, 4, 128]), ALU.mult)
                        K.tt(hh.v, hh.v, skx.v, ALU.add)
                        K.tt(pre.v, hh.v, sz.v, ALU.mult)
                        for half in range(2):
                            ps = K.ps()
                            for dd in range(4):
                                d = half * 4 + dd
                                for j in range(4):
                                    K.mm(ps[:, dd * 128:(dd + 1) * 128], w_o[:, j, d * 128:(d + 1) * 128], pre[:, j, :], start=(j == 0), stop=(j == 3))
                            resid_add4(ti, half * 4, cols, ps[:, :].rr("p (d t) -> p d t", t=128), Gvec)
                        if not samp:
                            K.act(vw.v, vtok.v, AF.Copy, scale=wrcol)
                            pnn = K.ps()
                            for dc in range(4):
                                pk = K.ps()
                                K.mm(pk[:, 0:512], ktok[:, dc * 128:(dc + 1) * 128], vw.v)
                                K.stt(CT[:, dc, :], CT[:, dc, :], bcs[:, 1, 127:128], pk[:, 0:512], ALU.mult, ALU.add)
                                K.mm(pnn[:, dc * 4:(dc + 1) * 4], ktok[:, dc * 128:(dc + 1) * 128], wrb.v)
                            K.stt(nP[:, h, :], nP[:, h, :], bcs[:, 1, 127:128], pnn[:, 0:16].rr("p (d f) -> p d f", f=4)[:, :, h], ALU.mult, ALU.add)
                            K.act(CTb.v, CT.v, AF.Copy)
                            K.cp(nB.v, nP[:, h, :].un(2).bc([128, 4, 128]))
                            if ci == 15:
                                K.dma(dout["ml_CT_p"][o_, h], CT.v)
                K.dma(dout["ml_n_p"][o_], nP.v)
                K.dma(dout["ml_n_s"][o_], nSn.v)
                K.barrier()
        K.barrier()

    for l in range(nlayers):
        if DBG_ONLY_MLSTM:
            if l == 1: mlstm(l)
            continue
        if l == 0: ada(l)
        ffn(l, 0)
        if l % 2 == 0:
            ab_mixer(l)
        elif MLSTM_MODE:
            mlstm(l)
        ffn(l, 1)
    with ExitStack() as ph:
        if nlayers != NL: ada(NL)
        yT = [K.sb("yT%d" % i, [128, KD, n], F32, ph) for i, (o, n) in enumerate(TILES)]
        K.ts(Avec.v, mod(NL, 1), 1.0, ALU.add)
        K.tt(Avec.v, Avec.v, final_norm.v.un(2).bc([128, KD, NSEQ]), ALU.mult)
        B = mod(NL, 0)
        norm_bufs(ph, 512)
        for ti, (o, n) in enumerate(TILES):
            norm_tile(ti, yT[ti].v, B)
            K.dma(dout["yT"][:, :, o:o + n], yT[ti].v)
        K.barrier()


def _consts():
    c = np.zeros((128, 6, 128), np.float32)
    s = np.arange(128)[:, None]; t = np.arange(128)[None, :]
    c[:, C_TRI, :] = (s <= t)
    c[:, C_TRI8, :] = (s <= t) & (s // 8 == t // 8)
    c[:, C_ID, :] = (s == t)
    c[:, C_MISC, 0:16] = (np.arange(128)[:, None] // 8 == np.arange(16)[None, :])
    return c


def _bd(w):
    out = np.zeros((16, 128, 128), np.float32)
    wr = w.reshape(16, 32, 4, 4)
    for n in range(32):
        out[:, n * 4:(n + 1) * 4, n * 4:(n + 1) * 4] = wr[:, n]
    return np.ascontiguousarray(out.transpose(1, 0, 2))


def _kT(w, kc):
    return np.ascontiguousarray(w.reshape(kc, 128, -1).transpose(1, 0, 2))


def prep_shared(inp):
    f = lambda a: np.ascontiguousarray(np.asarray(a, dtype=np.float32))
    S = {}
    S["ada_w"] = f(inp["ada_w"].reshape(NL, KD, 128, 9, 1024).transpose(0, 3, 2, 1, 4))
    S["ada_b"] = f(inp["ada_b"].reshape(NL, 72, 128).transpose(2, 0, 1))
    S["fada_w"] = f(inp["final_ada_w"].reshape(KD, 128, 2, 1024).transpose(2, 1, 0, 3))
    S["fada_b"] = f(inp["final_ada_b"].reshape(16, 128).T)
    S["ffn_norm"] = f(inp["ffn_norm"].reshape(NL, 2, KD, 128).transpose(3, 0, 1, 2))
    S["mix_norm"] = f(inp["mix_norm"].reshape(NL, KD, 128).transpose(2, 0, 1))
    S["final_norm"] = f(inp["final_norm"].reshape(KD, 128).T)
    S["ffn_wg"] = f(inp["ffn_w_gate"].reshape(NL, 2, KD, 128, NG, 256).transpose(0, 1, 4, 3, 2, 5))
    S["ffn_wu"] = f(inp["ffn_w_up"].reshape(NL, 2, KD, 128, NG, 256).transpose(0, 1, 4, 3, 2, 5))
    S["ffn_wd"] = f(inp["ffn_w_down"].reshape(NL, 2, NG, 2, 128, 1024).transpose(0, 1, 2, 4, 3, 5))
    S["ab_w_in"] = f(inp["ab_w_in"].reshape(2, KD, 128, 2576).transpose(0, 2, 1, 3))
    S["ab_w_out"] = f(inp["ab_w_out"].reshape(2, KD, 128, 1024).transpose(0, 2, 1, 3))
    S["gla_wa2"] = f(np.concatenate([inp["gla_w_a2"], inp["gla_b_a"][:, None, :]], axis=1))
    S["gla_norm"] = f(inp["gla_norm"].reshape(2, 128, 1))
    S["gmlp_norm"] = f(np.broadcast_to(inp["gmlp_norm"][:, None, :], (2, 128, 128)))
    ws = np.asarray(inp["gmlp_ws"])
    S["gmlp_wT_p"] = f(ws.transpose(0, 3, 1, 2))
    w8 = ws[:, :, :8, :8]
    S["gmlp_wT_s"] = f(np.tile(w8.transpose(0, 3, 1, 2), (1, 16, 1, 16)))
    bs = np.asarray(inp["gmlp_bs"])
    S["gmlp_bs_p"] = f(np.broadcast_to(bs[:, None, :, :], (2, 128, 4, 128)))
    S["gmlp_bs_s"] = f(np.broadcast_to(np.tile(bs[:, :, :8], (1, 1, 16))[:, None, :, :], (2, 128, 4, 128)))
    S["consts"] = _consts()
    S["ml_w_in"] = f(inp["ml_w_in"].reshape(2, KD, 128, 4096).transpose(0, 2, 1, 3))
    S["ml_w_out"] = f(inp["ml_w_out"].reshape(2, 16, 128, 1024).transpose(0, 2, 1, 3))
    S["ml_conv_w"] = f(inp["ml_conv_w"].reshape(2, 4, 16, 128).transpose(0, 3, 2, 1))
    S["ml_conv_b"] = f(inp["ml_conv_b"].reshape(2, 16, 128).transpose(0, 2, 1))
    for n, k in (("ml_bdq", "ml_wq"), ("ml_bdk", "ml_wk"), ("ml_bdv", "ml_wv")):
        S[n] = np.stack([_bd(np.asarray(inp[k][o])) for o in range(2)])
    S["ml_w_gates"] = f(inp["ml_w_gates"].reshape(2, 48, 128, 8).transpose(0, 2, 1, 3))
    S["ml_b_gates"] = f(np.broadcast_to(inp["ml_b_gates"][:, None, :], (2, 128, 8)))
    S["ml_norm"] = f(inp["ml_norm"].reshape(2, 16, 128).transpose(0, 2, 1))
    S["ml_skip"] = f(inp["ml_skip"].reshape(2, 16, 128).transpose(0, 2, 1))
    return S


def prep_core(inp, i):
    f = lambda a: np.ascontiguousarray(np.asarray(a, dtype=np.float32))
    P = {}
    sl = slice(16 * i, 16 * i + 16)
    x = np.concatenate([inp["x_prompt"][i], inp["x_sample"][sl].reshape(TS, D)], axis=0)
    P["xT"] = f(x.T.reshape(KD, 128, T).transpose(1, 0, 2))
    c = np.concatenate([inp["c_prompt"][i:i + 1], inp["c_sample"][sl]], axis=0)
    P["cT"] = f(c.T.reshape(KD, 128, NSEQ).transpose(1, 0, 2))
    P["gla_S"] = f(inp["state_gla_S"][:, sl].transpose(0, 3, 1, 2, 4))
    C = inp["state_mlstm_C"][:, sl]
    P["ml_CT"] = f(C.transpose(0, 1, 2, 4, 3).reshape(2, 16, 4, 4, 128, 512).transpose(0, 1, 2, 4, 3, 5))
    P["ml_n"] = f(inp["state_mlstm_n"][:, sl].reshape(2, 16, 4, 4, 128).transpose(0, 4, 1, 2, 3))
    P["ml_m"] = f(inp["state_mlstm_m"][:, sl].transpose(0, 2, 1))
    P["ml_conv"] = f(inp["state_mlstm_conv"][:, sl].reshape(2, 16, 3, 16, 128).transpose(0, 4, 3, 1, 2))
    return P


def post_core(r):
    o = {}
    y = r["yT"].transpose(2, 1, 0).reshape(T, D)
    o["y_p"] = y[0:TP]; o["y_s"] = y[TP:].reshape(16, 8, D)
    o["s_p"] = r["gla_S_p"].transpose(0, 2, 1, 3)
    o["s_s"] = r["gla_S_s"].transpose(0, 2, 3, 1, 4)
    o["v_s"] = r["gmlp_v_s"].reshape(2, 16, 8, 512)
    o["c_p"] = r["ml_CT_p"].transpose(0, 1, 3, 2, 4).reshape(2, 4, 512, 512).transpose(0, 1, 3, 2)
    o["c_s"] = r["ml_CT_s"].transpose(0, 1, 2, 4, 3, 5).reshape(2, 16, 4, 512, 512).transpose(0, 1, 2, 4, 3)
    o["n_p"] = r["ml_n_p"].transpose(0, 2, 3, 1).reshape(2, 4, 512)
    o["n_s"] = r["ml_n_s"].transpose(0, 2, 3, 4, 1).reshape(2, 16, 4, 512)
    o["m_p"] = r["ml_m_p"].reshape(2, 4)
    o["m_s"] = r["ml_m_s"].transpose(0, 2, 1)
    o["cv_p"] = r["ml_conv_p"].transpose(0, 3, 2, 1).reshape(2, 3, 2048)
    o["cv_s"] = r["ml_conv_s"].transpose(0, 3, 4, 2, 1).reshape(2, 16, 3, 2048)
    return o


_NC_CACHE = {}


def kernel(**inputs):
    inp = {k: np.asarray(v) for k, v in inputs.items()}
    if "nc" not in _NC_CACHE:
        _NC_CACHE["nc"] = build()
    nc = _NC_CACHE["nc"]
    S = prep_shared(inp)
    in_maps = []
    for i in range(8):
        m = dict(S); m.update(prep_core(inp, i)); in_maps.append(m)
    res = run_bass_kernel_spmd(nc, in_maps, core_ids=list(range(8)))
    po = [post_core(r) for r in res.results]
    cat0 = lambda k: np.ascontiguousarray(np.stack([p[k] for p in po], axis=0)).astype(np.float32)
    cat1 = lambda k: np.ascontiguousarray(np.stack([p[k] for p in po], axis=1)).astype(np.float32)
    cat1s = lambda k: np.ascontiguousarray(np.concatenate([p[k] for p in po], axis=1)).astype(np.float32)
    y_p = cat0("y_p")
    y_s = np.ascontiguousarray(np.concatenate([p["y_s"] for p in po], axis=0)).astype(np.float32)
    return (y_p, y_s, cat1("s_p"), cat1s("s_s"), cat1s("v_s"), cat1("c_p"), cat1s("c_s"), cat1("n_p"), cat1s("n_s"),
            cat1("m_p"), cat1s("m_s"), cat1("cv_p"), cat1s("cv_s"))
```
